# Optimizing a Trainium2 kernel written in Bass

```python
import jax
import jax.numpy as jnp
from jax import lax
import numpy as np

D_MODEL = 2048
BATCH = 8
SEQ = 2048
DEPTH = 2

GRID_W = 64
CTX_LEN = 256
EPS = 1e-6
NEG_INF = -1e30
ATT_HEADS = 16
ATT_KV_HEADS = 4
ATT_HEAD_DIM = 64
ATT_GROUP = ATT_HEADS // ATT_KV_HEADS
WINDOW = 128
ATT_BLOCK = 128
ROPE_THETA = 10000.0
ML_HEADS = 4
ML_DQK = 128
ML_DV = 256
ML_CHUNK = 128
GATE_SOFTCAP = 15.0
N_EXPERTS = 32
TOP_K = 4
D_FF = 1024
SWIGLU_LIMIT = 7.0
SWIGLU_ALPHA = 1.702
ATT_Q_W = ATT_HEADS * ATT_HEAD_DIM
ATT_KV_W = ATT_KV_HEADS * ATT_HEAD_DIM
ML_QK_W = ML_HEADS * ML_DQK
ML_V_W = ML_HEADS * ML_DV
N_GATES = 4 * ML_HEADS
PROJ_SIZES = (ATT_Q_W, ATT_KV_W, ATT_KV_W, ML_QK_W, ML_QK_W, ML_V_W, ML_V_W, N_GATES)
D_IN = sum(PROJ_SIZES)
D_CAT = ATT_Q_W + ML_V_W

kernel_name = 'hymba_swa_mlstm_moe_dit_prefix'


def _rmsnorm(x, w):
    xf = x.astype(jnp.float32)
    y = xf * lax.rsqrt(jnp.mean(xf * xf, axis=-1, keepdims=True) + EPS)
    return (y * w.astype(jnp.float32)).astype(x.dtype)


def _heads(t, n, d):
    return t.reshape(t.shape[:2] + (n, d))


def _split_proj(p):
    idx = np.cumsum(PROJ_SIZES)[:-1].tolist()
    return jnp.split(p, idx, axis=-1)


def _rope_tables(rows):
    row = jnp.repeat(jnp.arange(rows, dtype=jnp.float32), GRID_W)
    col = jnp.tile(jnp.arange(GRID_W, dtype=jnp.float32), rows)
    half = ATT_HEAD_DIM // 2
    inv_freq = ROPE_THETA ** (-jnp.arange(0, half, 2, dtype=jnp.float32) / half)

    def tab(p):
        ang = p[:, None] * inv_freq[None, :]
        ang = jnp.concatenate([ang, ang], axis=-1)
        return jnp.cos(ang)[:, None, :], jnp.sin(ang)[:, None, :]

    return tab(row), tab(col)


def _rot_half(x, cos, sin):
    x1, x2 = jnp.split(x, 2, axis=-1)
    return x * cos + jnp.concatenate([-x2, x1], axis=-1) * sin


def _rope_2d(x, tables):
    (cr, sr), (cc, sc) = tables
    xr, xc = jnp.split(x.astype(jnp.float32), 2, axis=-1)
    return jnp.concatenate([_rot_half(xr, cr, sr), _rot_half(xc, cc, sc)], axis=-1).astype(x.dtype)


def _latent_attention(q, k, v, kc, vc, sink):
    B, S = q.shape[:2]
    C = kc.shape[1]
    nb = S // ATT_BLOCK
    w3 = 3 * ATT_BLOCK
    qb = q.reshape(B, nb, ATT_BLOCK, ATT_KV_HEADS, ATT_GROUP, ATT_HEAD_DIM)

    def windows(t):
        tp = jnp.pad(t, ((0, 0), (WINDOW, WINDOW), (0, 0), (0, 0)))
        tp = tp.reshape(B, nb + 2, ATT_BLOCK, ATT_KV_HEADS, ATT_HEAD_DIM)
        return jnp.concatenate([tp[:, :-2], tp[:, 1:-1], tp[:, 2:]], axis=2)

    kw, vw = windows(k), windows(v)
    scale = ATT_HEAD_DIM ** -0.5
    s_loc = jnp.einsum('bnqkgd,bnwkd->bnkgqw', qb, kw).astype(jnp.float32) * scale
    r = jnp.arange(ATT_BLOCK)[:, None]
    w = jnp.arange(w3)[None, :]
    rel = w - r
    j = jnp.arange(nb)[:, None, None] * ATT_BLOCK - WINDOW + w[None]
    mask = (rel >= 0) & (rel <= 2 * WINDOW) & (j >= 0) & (j < S)
    s_loc = jnp.where(mask[None, :, None, None], s_loc, NEG_INF)
    s_ctx = jnp.einsum('bnqkgd,bckd->bnkgqc', qb, kc).astype(jnp.float32) * scale
    sink_b = jnp.broadcast_to(sink.astype(jnp.float32).reshape(1, 1, ATT_KV_HEADS, ATT_GROUP, 1, 1),
                              s_loc.shape[:-1] + (1,))
    p = jax.nn.softmax(jnp.concatenate([s_loc, s_ctx, sink_b], axis=-1), axis=-1)
    p_loc = p[..., :w3].astype(v.dtype)
    p_ctx = p[..., w3:w3 + C].astype(v.dtype)
    o = (jnp.einsum('bnkgqw,bnwkd->bnqkgd', p_loc, vw)
         + jnp.einsum('bnkgqc,bckd->bnqkgd', p_ctx, vc))
    return o.reshape(B, S, ATT_Q_W)


def _context_attention(qc, kc, vc, sink):
    B, C = qc.shape[:2]
    qg = qc.reshape(B, C, ATT_KV_HEADS, ATT_GROUP, ATT_HEAD_DIM)
    s = jnp.einsum('bqkgd,bckd->bkgqc', qg, kc).astype(jnp.float32) * ATT_HEAD_DIM ** -0.5
    sink_b = jnp.broadcast_to(sink.astype(jnp.float32).reshape(1, ATT_KV_HEADS, ATT_GROUP, 1, 1),
                              s.shape[:-1] + (1,))
    p = jax.nn.softmax(jnp.concatenate([s, sink_b], axis=-1), axis=-1)[..., :C].astype(vc.dtype)
    return jnp.einsum('bkgqc,bckd->bqkgd', p, vc).reshape(B, C, ATT_Q_W)


def _mlstm_scan(q, k, v, i_pre, f_pre, state):
    B, T, H = q.shape[:3]
    nc = T // ML_CHUNK

    def chunks(t):
        t = t.reshape((B, nc, ML_CHUNK) + t.shape[2:])
        return jnp.moveaxis(jnp.swapaxes(t, 2, 3), 1, 0)

    tril = jnp.tril(jnp.ones((ML_CHUNK, ML_CHUNK), dtype=bool))

    def step(carry, xs):
        Cm, n, m = carry
        qc, kc, vc, ic, fc = xs
        b = jnp.cumsum(jax.nn.log_sigmoid(fc), axis=-1)
        dmat = jnp.where(tril, b[..., :, None] - b[..., None, :] + ic[..., None, :], NEG_INF)
        inter = b + m[..., None]
        m_t = jnp.maximum(inter, jnp.max(dmat, axis=-1))
        w_intra = jnp.einsum('bhtd,bhsd->bhts', qc, kc) * jnp.exp(dmat - m_t[..., None])
        w_inter = jnp.exp(inter - m_t)
        num = (jnp.einsum('bhts,bhsv->bhtv', w_intra, vc)
               + w_inter[..., None] * jnp.einsum('bhvd,bhtd->bhtv', Cm, qc))
        den = jnp.sum(w_intra, axis=-1) + w_inter * jnp.einsum('bhd,bhtd->bht', n, qc)
        h = num / jnp.maximum(jnp.abs(den), jnp.exp(-m_t))[..., None]
        b_last = b[..., -1]
        g = b_last[..., None] - b + ic
        m_new = jnp.maximum(b_last + m, jnp.max(g, axis=-1))
        w_s = jnp.exp(g - m_new[..., None])
        w_c = jnp.exp(b_last + m - m_new)
        C_new = w_c[..., None, None] * Cm + jnp.einsum('bhs,bhsv,bhsd->bhvd', w_s, vc, kc)
        n_new = w_c[..., None] * n + jnp.einsum('bhs,bhsd->bhd', w_s, kc)
        return (C_new, n_new, m_new), h

    state, h = lax.scan(step, state, (chunks(q), chunks(k), chunks(v), chunks(i_pre), chunks(f_pre)))
    h = jnp.swapaxes(jnp.moveaxis(h, 0, 1), 2, 3).reshape(B, T, H, ML_DV)
    return h, state


def _flip(t):
    return jnp.flip(t, axis=1)


def _mlstm_bwd(q, k, v, i_pre, f_pre, state):
    h, st = _mlstm_scan(_flip(q), _flip(k), _flip(v), _flip(i_pre), _flip(f_pre), state)
    return _flip(h), st


def _mlstm_inputs(mq, mk, mv, g, b_gates):
    q = _heads(mq, ML_HEADS, ML_DQK).astype(jnp.float32)
    k = _heads(mk, ML_HEADS, ML_DQK).astype(jnp.float32) * ML_DQK ** -0.5
    v = _heads(mv, ML_HEADS, ML_DV).astype(jnp.float32)
    g = g.astype(jnp.float32) + b_gates.astype(jnp.float32)
    g = GATE_SOFTCAP * jnp.tanh(g / GATE_SOFTCAP)
    i_f, f_f, i_b, f_b = jnp.split(g, 4, axis=-1)
    return q, k, v, i_f, f_f, i_b, f_b


def _mlstm_out(h_f, h_b, o_pre, norm_w, dtype):
    B, T = h_f.shape[:2]
    h = _rmsnorm(h_f + h_b, norm_w.reshape(ML_HEADS, ML_DV)).reshape(B, T, ML_V_W).astype(dtype)
    return h * jax.nn.sigmoid(o_pre)


def _moe(h, w_router, b_router, w_gate_up, b_gate_up, w_down, b_down):
    logits = (h @ w_router).astype(jnp.float32) + b_router.astype(jnp.float32)
    top_v, top_i = lax.top_k(logits, TOP_K)
    top_w = jax.nn.softmax(top_v, axis=-1)
    combine = jnp.sum(jax.nn.one_hot(top_i, N_EXPERTS, dtype=jnp.float32) * top_w[..., None], axis=1)
    out = jnp.zeros(h.shape, jnp.float32)
    for e in range(N_EXPERTS):
        gate, up = jnp.split(h @ w_gate_up[e] + b_gate_up[e], 2, axis=-1)
        gate = jnp.minimum(gate, SWIGLU_LIMIT)
        up = jnp.clip(up, -SWIGLU_LIMIT, SWIGLU_LIMIT)
        y = ((up + 1) * gate * jax.nn.sigmoid(SWIGLU_ALPHA * gate)) @ w_down[e] + b_down[e]
        out = out + combine[:, e:e + 1] * y
    return out.astype(h.dtype)


def _layer(x, ctx, mod_x, mod_c, norm1_w, norm2_w, w_in, b_gates, q_norm_w, k_norm_w, attn_sink,
           mlstm_norm_w, w_out, w_router, b_router, w_gate_up, b_gate_up, w_down, b_down, rope, update_ctx):
    B, S, D = x.shape
    C = ctx.shape[1]
    sh1, sc1, g1, sh2, sc2, g2 = jnp.split(mod_x, 6, axis=-1)
    csh1, csc1, cg1, csh2, csc2, cg2 = jnp.split(mod_c, 6, axis=-1)

    hx = _rmsnorm(x, norm1_w) * (1 + sc1[:, None]) + sh1[:, None]
    hc = _rmsnorm(ctx, norm1_w) * (1 + csc1) + csh1
    qx, kx, vx, mqx, mkx, mvx, mox, gx = _split_proj(hx @ w_in)
    qc, kc, vc, mqc, mkc, mvc, moc, gc = _split_proj(hc @ w_in)

    qx = _rope_2d(_rmsnorm(_heads(qx, ATT_HEADS, ATT_HEAD_DIM), q_norm_w), rope)
    kx = _rope_2d(_rmsnorm(_heads(kx, ATT_KV_HEADS, ATT_HEAD_DIM), k_norm_w), rope)
    vx = _heads(vx, ATT_KV_HEADS, ATT_HEAD_DIM)
    qc = _rmsnorm(_heads(qc, ATT_HEADS, ATT_HEAD_DIM), q_norm_w)
    kc = _rmsnorm(_heads(kc, ATT_KV_HEADS, ATT_HEAD_DIM), k_norm_w)
    vc = _heads(vc, ATT_KV_HEADS, ATT_HEAD_DIM)
    att_x = _latent_attention(qx, kx, vx, kc, vc, attn_sink)

    cq, ck, cv, ci_f, cf_f, ci_b, cf_b = _mlstm_inputs(mqc, mkc, mvc, gc, b_gates)
    lq, lk, lv, li_f, lf_f, li_b, lf_b = _mlstm_inputs(mqx, mkx, mvx, gx, b_gates)
    zero_state = (jnp.zeros((B, ML_HEADS, ML_DV, ML_DQK), jnp.float32),
                  jnp.zeros((B, ML_HEADS, ML_DQK), jnp.float32),
                  jnp.zeros((B, ML_HEADS), jnp.float32))
    hc_f, st_f = _mlstm_scan(cq, ck, cv, ci_f, cf_f, zero_state)
    hc_b, st_b = _mlstm_bwd(cq, ck, cv, ci_b, cf_b, zero_state)
    hx_f, _ = _mlstm_scan(lq, lk, lv, li_f, lf_f, st_f)
    hx_b, _ = _mlstm_bwd(lq, lk, lv, li_b, lf_b, st_b)
    ml_x = _mlstm_out(hx_f, hx_b, mox, mlstm_norm_w, x.dtype)

    x = x + g1[:, None] * (jnp.concatenate([att_x, ml_x], axis=-1) @ w_out)
    if update_ctx:
        att_c = _context_attention(qc, kc, vc, attn_sink)
        ml_c = _mlstm_out(hc_f, hc_b, moc, mlstm_norm_w, ctx.dtype)
        ctx = ctx + cg1 * (jnp.concatenate([att_c, ml_c], axis=-1) @ w_out)

    fx = (_rmsnorm(x, norm2_w) * (1 + sc2[:, None]) + sh2[:, None]).reshape(B * S, D)
    if update_ctx:
        fc = (_rmsnorm(ctx, norm2_w) * (1 + csc2) + csh2).reshape(B * C, D)
        y = _moe(jnp.concatenate([fx, fc], axis=0), w_router, b_router, w_gate_up, b_gate_up, w_down, b_down)
        x = x + g2[:, None] * y[:B * S].reshape(B, S, D)
        ctx = ctx + cg2 * y[B * S:].reshape(B, C, D)
    else:
        y = _moe(fx, w_router, b_router, w_gate_up, b_gate_up, w_down, b_down)
        x = x + g2[:, None] * y.reshape(B, S, D)
    return x, ctx


def setup_inputs(seed: int = 0) -> dict:
    key = jax.random.key(seed)
    ks = jax.random.split(key, 24)
    L, D, E, F = DEPTH, D_MODEL, N_EXPERTS, D_FF
    nrm = jax.random.normal
    f_bias = jnp.broadcast_to(jnp.linspace(3.0, 6.0, ML_HEADS, dtype=jnp.float32), (L, ML_HEADS))
    gate_noise = 0.1 * nrm(ks[4], (L, N_GATES), jnp.float32)
    b_gates = gate_noise + jnp.concatenate([jnp.zeros((L, ML_HEADS)), f_bias, jnp.zeros((L, ML_HEADS)), f_bias], axis=-1)
    return {
        'x': nrm(ks[0], (BATCH, SEQ, D), jnp.float32),
        'c': nrm(ks[1], (BATCH, D), jnp.float32),
        'ctx': nrm(ks[2], (BATCH, CTX_LEN, D), jnp.float32),
        'c_ctx': nrm(ks[3], (D,), jnp.float32),
        'w_ada': 0.5 * D ** -0.5 * nrm(ks[5], (L, D, 6 * D), jnp.float32),
        'b_ada': 0.01 * nrm(ks[6], (L, 6 * D), jnp.float32),
        'norm1_w': 1.0 + 0.1 * nrm(ks[7], (L, D), jnp.float32),
        'norm2_w': 1.0 + 0.1 * nrm(ks[8], (L, D), jnp.float32),
        'w_in': D ** -0.5 * nrm(ks[9], (L, D, D_IN), jnp.float32),
        'b_gates': b_gates,
        'q_norm_w': 1.0 + 0.1 * nrm(ks[10], (L, ATT_HEAD_DIM), jnp.float32),
        'k_norm_w': 1.0 + 0.1 * nrm(ks[11], (L, ATT_HEAD_DIM), jnp.float32),
        'attn_sink': 0.5 * nrm(ks[12], (L, ATT_HEADS), jnp.float32),
        'mlstm_norm_w': 1.0 + 0.1 * nrm(ks[13], (L, ML_V_W), jnp.float32),
        'w_out': D_CAT ** -0.5 * nrm(ks[14], (L, D_CAT, D), jnp.float32),
        'w_router': D ** -0.5 * nrm(ks[15], (L, D, E), jnp.float32),
        'b_router': 0.01 * nrm(ks[16], (L, E), jnp.float32),
        'w_gate_up': D ** -0.5 * nrm(ks[17], (L, E, D, 2 * F), jnp.float32),
        'b_gate_up': 0.01 * nrm(ks[18], (L, E, 2 * F), jnp.float32),
        'w_down': F ** -0.5 * nrm(ks[19], (L, E, F, D), jnp.float32),
        'b_down': 0.01 * nrm(ks[20], (L, E, D), jnp.float32),
    }


def reference(x, c, ctx, c_ctx, w_ada, b_ada, norm1_w, norm2_w, w_in, b_gates, q_norm_w, k_norm_w,
              attn_sink, mlstm_norm_w, w_out, w_router, b_router, w_gate_up, b_gate_up, w_down, b_down):
    ROWS = x.shape[1] // GRID_W
    rope = _rope_tables(ROWS)
    silu_c = jax.nn.silu(c)
    silu_cc = jax.nn.silu(c_ctx)
    for l in range(DEPTH):
        mod_x = silu_c @ w_ada[l] + b_ada[l]
        mod_c = silu_cc @ w_ada[l] + b_ada[l]
        x, ctx = _layer(x, ctx, mod_x, mod_c, norm1_w[l], norm2_w[l], w_in[l], b_gates[l], q_norm_w[l],
                        k_norm_w[l], attn_sink[l], mlstm_norm_w[l], w_out[l], w_router[l], b_router[l],
                        w_gate_up[l], b_gate_up[l], w_down[l], b_down[l], rope, l < DEPTH - 1)
    return x
```

```python
import numpy as np
import ml_dtypes
import concourse.bass as bass
import concourse.mybir as mybir
from concourse.bass_utils import run_bass_kernel_spmd

F32 = mybir.dt.float32
BF16 = mybir.dt.bfloat16
AF = mybir.ActivationFunctionType
ALU = mybir.AluOpType
AX = mybir.AxisListType

D = 2048
NT = 18
TT = NT * 128
DIN = 4624
EPS = 1e-6
NE = 32
DFF = 1024


class Buf:
    __slots__ = ("name", "last_w", "readers")

    def __init__(self, name=""):
        self.name = name
        self.last_w = None
        self.readers = []


class DSem:
    __slots__ = ("sem", "count")

    def __init__(self, sem):
        self.sem = sem
        self.count = 0


class Op:
    __slots__ = ("eng", "emit", "deps", "signal", "signum", "dsem", "dval", "waits", "idx")


ENGS = ("pe", "act", "dve", "pool", "sp")


class Prog:
    def __init__(self):
        self.streams = {e: [] for e in ENGS}
        self.nops = 0
        self.dsems = []

    def new_dsem(self, sem):
        d = DSem(sem)
        self.dsems.append(d)
        return d

    def op(self, eng, emit, reads=(), writes=(), dsem=None, extra_dsem_waits=()):
        o = Op()
        o.eng = eng
        o.emit = emit
        o.signal = False
        o.signum = None
        o.dsem = dsem
        o.dval = None
        o.idx = self.nops
        self.nops += 1
        deps = []
        for b in reads:
            if b.last_w is not None:
                deps.append(b.last_w)
        for b in writes:
            if b.last_w is not None:
                deps.append(b.last_w)
            deps.extend(b.readers)
        seen = set()
        o.deps = []
        for d in deps:
            if id(d) in seen or d is o:
                continue
            seen.add(id(d))
            if d.dsem is not None:
                o.deps.append(("d", d.dsem, d.dsem.count))
            else:
                if d.eng == "pe" and eng == "pe" and dsem is None:
                    continue
                d.signal = True
                o.deps.append(("e", d))
        for ds in extra_dsem_waits:
            if ds.count > 0:
                o.deps.append(("d", ds, ds.count))
        if dsem is not None:
            dsem.count += 16
            o.dval = dsem.count
        for b in reads:
            b.readers.append(o)
        for b in writes:
            b.last_w = o
            b.readers = []
        self.streams[eng].append(o)
        return o

    def finalize(self):
        for e in ENGS:
            n = 0
            for o in self.streams[e]:
                if o.dsem is None and o.signal:
                    n += 1
                    o.signum = n
        for e in ENGS:
            known = {}
            for o in self.streams[e]:
                w = {}
                for d in o.deps:
                    if d[0] == "d":
                        key = ("d", id(d[1]))
                        val = d[2]
                        sem = d[1].sem
                    else:
                        key = ("e", d[1].eng)
                        val = d[1].signum
                        sem = d[1].eng
                    if known.get(key, 0) >= val:
                        continue
                    if key not in w or w[key][1] < val:
                        w[key] = (sem, val)
                for key, (sem, val) in w.items():
                    known[key] = val
                o.waits = list(w.values())

    def emit(self, block, esems):
        def run(engname):
            def f(eng):
                for o in self.streams[engname]:
                    for sem, val in o.waits:
                        s = esems[sem] if isinstance(sem, str) else sem
                        eng.wait_ge(s, val)
                    ins = o.emit(eng)
                    if ins is None:
                        continue
                    if o.dsem is not None:
                        ins.then_inc(o.dsem.sem, 16)
                    elif o.signal:
                        ins.then_inc(esems[engname], 1)
            return f
        block.tensor(run("pe"))
        block.scalar(run("act"))
        block.vector(run("dve"))
        block.gpsimd(run("pool"))
        block.sync(run("sp"))


def _consts():
    c = {}
    c["ident_f"] = np.eye(128, dtype=np.float32)
    c["ident_b"] = np.eye(128, dtype=np.float32).astype(ml_dtypes.bfloat16)
    a = np.arange(128)
    c["mask_prev"] = (a[None, :] <= a[:, None]).astype(np.float32).astype(ml_dtypes.bfloat16)
    c["mask_next"] = (a[:, None] <= a[None, :]).astype(np.float32).astype(ml_dtypes.bfloat16)
    c["ones_f"] = np.ones((128, 128), np.float32)
    sel = np.zeros((32, 32, 128), np.float32)
    for e in range(32):
        sel[e, e, :] = 1.0
    c["sel32"] = sel.transpose(1, 0, 2).copy()
    e0 = np.zeros((2, 2, 128), np.float32)
    e0[0, 0, :] = 1.0
    e0[1, 1, :] = 1.0
    c["sel2"] = e0
    rows = 2048 // 64
    row = np.repeat(np.arange(rows, dtype=np.float32), 64)
    col = np.tile(np.arange(64, dtype=np.float32), rows)
    half = 32
    inv = (10000.0 ** (-np.arange(0, half, 2, dtype=np.float32) / half)).astype(np.float32)

    def tab(p):
        ang = p[:, None] * inv[None, :]
        ang = np.concatenate([ang, ang], -1)
        return np.cos(ang).astype(np.float32), np.sin(ang).astype(np.float32)
    cr, sr = tab(row)
    cc, sc = tab(col)
    cos = np.concatenate([cr, cc], -1)
    sgn = np.concatenate([-np.ones(16), np.ones(16)]).astype(np.float32)
    sins = np.concatenate([sr * sgn, sc * sgn], -1)
    c["rope_cos"] = cos.reshape(16, 128, 64).transpose(1, 0, 2).copy()
    c["rope_sin"] = sins.reshape(16, 128, 64).transpose(1, 0, 2).copy()
    c["ml_mask_f"] = (a[:, None] <= a[None, :]).astype(np.float32).astype(ml_dtypes.bfloat16)
    c["ml_mask_b"] = (a[:, None] >= a[None, :]).astype(np.float32).astype(ml_dtypes.bfloat16)
    c["tri_f"] = (a[:, None] <= a[None, :]).astype(np.float32)
    c["tri_b"] = (a[:, None] >= a[None, :]).astype(np.float32)
    sl = np.zeros((128, 128), np.float32)
    sl[127, :] = 1.0
    c["sel_last"] = sl
    sf = np.zeros((128, 128), np.float32)
    sf[0, :] = 1.0
    c["sel_first"] = sf
    return c


from contextlib import ExitStack


class Builder:
    def __init__(self, nc, stack, upto="all", debug=(), n_exp=NE, layers=2, dev=False):
        self.nc = nc
        self.stack = stack
        self.P = Prog()
        self.upto = upto
        self.debug = set(debug)
        self.n_exp = n_exp
        self.layers = layers
        self.dev = dev
        self.L = 1 if dev else 2
        self.sb_off = 16512
        self.nsem = 0
        self.dpool = []
        self.dnext = 0
        self.dbg_names = []
        self.need_moe = upto == "all" or upto.startswith("G")

    def sb(self, name, shape, dtype):
        esz = 4 if dtype == F32 else 2
        n = 1
        for s in shape[1:]:
            n *= s
        nbytes = (n * esz + 31) // 32 * 32
        t = self.nc.alloc_sbuf_tensor_at(f"{name}_{self.sb_off}", list(shape), dtype, offset=self.sb_off)
        self.sb_off += nbytes
        assert self.sb_off <= 16512 + 212000, (name, self.sb_off)
        return t

    def sem(self, name):
        self.nsem += 1
        return self.stack.enter_context(self.nc.semaphore(name))

    def ds(self):
        if self.dnext >= len(self.dpool):
            self.dpool.append(self.P.new_dsem(self.sem(f"d{len(self.dpool)}")))
        d = self.dpool[self.dnext]
        self.dnext += 1
        return d

    def dram(self, name, shape, dtype, kind="Internal"):
        return self.nc.dram_tensor(name, list(shape), dtype, kind=kind).ap()

    def op(self, eng, fn, r=(), w=(), dsem=None):
        return self.P.op(eng, fn, reads=r, writes=w, dsem=dsem)

    def dma(self, q, out, in_, r=(), w=(), dsem=None, **kw):
        assert dsem is not None
        return self.P.op(q, lambda e: e.dma_start(out=out, in_=in_, **kw), reads=r, writes=w, dsem=dsem)

    def barrier(self):
        P = self.P
        toks = []
        for e in ("act", "dve", "pool"):
            b = Buf("tok_" + e)
            scr = self.scr[e]
            if e == "act":
                P.op(e, lambda en, s=scr: en.activation(out=s[0:1, 0:2], in_=s[0:1, 0:2], func=AF.Identity), writes=[b])
            else:
                P.op(e, lambda en, s=scr: en.memset(s[0:1, 0:2], 0.0), writes=[b])
            toks.append(b)
        for e in ENGS:
            P.op(e, lambda en: None, reads=toks, extra_dsem_waits=list(P.dsems))
        self.dnext = 0

    def dbg(self, name, ap, shape, dtype, r=()):
        if name not in self.debug:
            return
        o = self.dram("dbg_" + name, shape, dtype, kind="ExternalOutput")
        self.dbg_names.append(name)
        d = self.P.new_dsem(self.sem("dbg_" + name))
        bo = Buf("dbg_" + name)
        self.dma("sp", o, ap, r=r, w=[bo], dsem=d)
        self.final_reads.append(bo)

    def setup(self):
        nc = self.nc
        self.esems = {e: self.sem("s_" + e) for e in ("pe", "act", "dve", "pool")}
        self.final_reads = []
        self.in_names = []

        def di(n, s, dt=F32):
            self.in_names.append(n)
            return self.dram(n, s, dt, kind="ExternalInput")
        self.x_in = di("x_in", [TT, D])
        self.c2 = di("c2", [32, 128])
        if not self.dev:
            self.w_ada = di("w_ada", [self.L, D, 6 * D])
        else:
            self.dev_mod = di("dev_mod", [2, 6 * D])
        self.b_ada = di("b_ada", [self.L, 6 * D])
        self.norm1_w = di("norm1_w", [self.L, D])
        self.norm2_w = di("norm2_w", [self.L, D])
        self.w_in = di("w_in", [self.L, D, DIN])
        self.b_gates = di("b_gates", [self.L, 16])
        self.q_norm_w = di("q_norm_w", [self.L, 64])
        self.k_norm_w = di("k_norm_w", [self.L, 64])
        self.attn_sink = di("attn_sink", [self.L, 16])
        self.mlstm_norm_w = di("mlstm_norm_w", [self.L, 1024])
        self.w_out = di("w_out", [self.L, D, D])
        self.w_router = di("w_router", [self.L, D, NE])
        self.b_router = di("b_router", [self.L, NE])
        if self.need_moe:
            self.w_gate_up = di("w_gate_up", [self.L, self.n_exp, D, 2 * DFF])
            self.b_gate_up = di("b_gate_up", [self.L, self.n_exp, 2 * DFF])
            self.w_down = di("w_down", [self.L, self.n_exp, DFF, D])
            self.b_down = di("b_down", [self.L, self.n_exp, D])
        self.cst = {}
        for k, v in _consts().items():
            self.cst[k] = di("cst_" + k, list(v.shape), BF16 if v.dtype != np.float32 else F32)
        self.y_out = self.dram("y", [2048, D], F32, kind="ExternalOutput")
        self.XRES = self.dram("xres", [TT, D], F32)
        self.PX = self.dram("px", [TT, 4608], BF16)
        self.GD = self.dram("gd", [TT, 16], F32)
        self.MQT = self.dram("mqt", [4, 128, TT], BF16)
        self.MKT = self.dram("mkt", [4, 128, TT], BF16)
        self.MODD = self.dram("modd", [2, 6 * D], F32)
        self.CATD = self.dram("catd", [TT, D], BF16)
        self.b_xres = [Buf(f"xres{t}") for t in range(NT)]
        self.b_px = [Buf(f"px{t}") for t in range(NT)]
        self.b_gd = [Buf(f"gd{t}") for t in range(NT)]
        self.b_mqt = Buf("mqt")
        self.b_mkt = Buf("mkt")
        self.b_modd = Buf("modd")
        self.b_catd = [Buf(f"catd{t}") for t in range(NT)]
        self.ps = [nc.alloc_psum_tensor(f"psb{i}", [128, 512], F32) for i in range(8)]
        self.b_ps = [Buf(f"ps{i}") for i in range(8)]
        self.identf = self.sb("identf", [128, 128], F32)
        self.identb = self.sb("identb", [128, 128], BF16)
        self.onesf = self.sb("onesf", [128, 128], F32)
        self.scr = {e: self.sb("scr_" + e, [128, 8], F32) for e in ("act", "dve", "pool")}
        self.modT = self.sb("modT", [128, 96, 2], F32)
        self.S1 = self.sb("S1", [128, 16, 2], F32)
        self.S2 = self.sb("S2", [128, 16, 2], F32)
        self.b_modT = Buf("modT")
        self.b_S = Buf("S12")
        self.b_const = Buf("const")
        dc = self.P.new_dsem(self.sem("dconst"))
        self.dconst = dc
        self.dma("sp", self.identf[:], self.cst["ident_f"], w=[self.b_const], dsem=dc)
        self.dma("sp", self.identb[:], self.cst["ident_b"], w=[self.b_const], dsem=dc)
        self.dma("sp", self.onesf[:], self.cst["ones_f"], w=[self.b_const], dsem=dc)
        self.persist_off = self.sb_off

    def phase_reset(self):
        self.sb_off = self.persist_off

    def phase_A(self, l):
        nc, P = self.nc, self.P
        self.phase_reset()
        vec = self.sb("vec", [64, 128], F32)
        silu2 = self.sb("silu2", [128, 16, 2], F32)
        nwT = self.sb("nwT", [128, 2, 16], F32)
        bada = self.sb("bada", [2, 6 * D], F32)
        modsb = self.sb("modsb", [2, 6 * D], F32)
        sel2 = self.sb("sel2", [2, 2, 128], F32)
        stage = [self.sb(f"wst{i}", [128, 16, 512], F32) for i in range(2)]
        b_vec, b_silu, b_nwT, b_bada, b_modsb = Buf(), Buf(), Buf(), Buf(), Buf()
        b_stage = [Buf(), Buf()]
        d_stage = [self.ds(), self.ds()]
        d0 = self.ds()
        d1 = self.ds()
        self.dma("sp", vec[0:32, :], self.c2, w=[b_vec], dsem=d0)
        self.dma("sp", vec[32:48, :], self.norm1_w[l].rearrange("(k p) -> k p", p=128), w=[b_vec], dsem=d0)
        self.dma("sp", vec[48:64, :], self.norm2_w[l].rearrange("(k p) -> k p", p=128), w=[b_vec], dsem=d0)
        self.dma("sp", bada[0:1, :], self.b_ada[l:l + 1, :], w=[b_bada], dsem=d0)
        self.dma("sp", bada[1:2, :], self.b_ada[l:l + 1, :], w=[b_bada], dsem=d0)
        self.dma("sp", sel2[:], self.cst["sel2"], w=[b_bada], dsem=d0)
        pv = self.ps[7]
        bpv = self.b_ps[7]
        self.op("pe", lambda e: e.transpose(pv[:, 0:64], vec[0:64, :], self.identf[0:64, 0:64]),
                r=[b_vec, self.b_const], w=[bpv])
        self.op("act", lambda e: e.activation(out=silu2[:, :, 0], in_=pv[:, 0:16], func=AF.Silu), r=[bpv], w=[b_silu])
        self.op("act", lambda e: e.activation(out=silu2[:, :, 1], in_=pv[:, 16:32], func=AF.Silu), r=[bpv], w=[b_silu])
        self.op("dve", lambda e: e.tensor_copy(nwT[:].rearrange("p a b -> p (a b)"), pv[:, 32:64]), r=[bpv], w=[b_nwT])
        if self.dev:
            self.dma("sp", modsb[0:2, :], self.dev_mod, w=[b_modsb], dsem=d0)
        wv = None if self.dev else self.w_ada[l].rearrange("(k p) n -> p k n", p=128)
        for n in range(0 if self.dev else 24):
            s = n % 2
            self.dma("sp", stage[s][:], wv[:, :, n * 512:(n + 1) * 512], w=[b_stage[s]], dsem=d_stage[s])
            pm = self.ps[n % 2]
            bpm = self.b_ps[n % 2]
            for k in range(16):
                self.op("pe", lambda e, k=k, s=s, pm=pm: e.matmul(pm[0:2, :], lhsT=silu2[:, k, :], rhs=stage[s][:, k, :],
                                                                  start=(k == 0), stop=(k == 15)),
                        r=[b_silu, b_stage[s]], w=[bpm])
            self.op("dve", lambda e, n=n, pm=pm: e.tensor_tensor(modsb[0:2, n * 512:(n + 1) * 512], pm[0:2, :],
                                                                 bada[0:2, n * 512:(n + 1) * 512], ALU.add),
                    r=[bpm, b_bada], w=[b_modsb])
        self.dma("sp", self.MODD, modsb[0:2, :], r=[b_modsb], w=[self.b_modd], dsem=d1)
        pt = self.ps[2]
        for j in range(96):
            self.op("pe", lambda e, j=j: e.transpose(pt[:, 2 * j:2 * j + 2], modsb[0:2, j * 128:(j + 1) * 128],
                                                      self.identf[0:2, 0:2]),
                    r=[b_modsb, self.b_const], w=[self.b_ps[2]])
        self.op("act", lambda e: e.activation(out=self.modT[:].rearrange("p a b -> p (a b)"), in_=pt[:, 0:192],
                                              func=AF.Identity), r=[self.b_ps[2]], w=[self.b_modT])
        for (S, sc0, wi) in ((self.S1, 16, 0), (self.S2, 64, 1)):
            self.op("dve", lambda e, S=S, sc0=sc0: e.tensor_scalar(S[:], self.modT[:, sc0:sc0 + 16, :], 1.0, None, ALU.add),
                    r=[self.b_modT], w=[self.b_S])
            self.op("dve", lambda e, S=S, wi=wi: e.tensor_tensor(S[:], S[:], nwT[:, wi, :].unsqueeze(2).to_broadcast([128, 16, 2]),
                                                                 ALU.mult),
                    r=[b_nwT], w=[self.b_S])
        self.dbg(f"modT{l}", self.modT[:], [128, 96, 2], F32, r=[self.b_modT])
        self.dbg(f"S1_{l}", self.S1[:], [128, 16, 2], F32, r=[self.b_S])
        self.barrier()

    def norm_tile(self, xt, b_xt, which, S, sh0, f32T, b_f32T, dstT, b_dst, col0, banks, junk, ss, xn, b_tmp):
        P = self
        b_ss, b_xn = b_tmp
        self.op("act", lambda e: e.activation(out=junk[:], in_=xt[:], func=AF.Square, accum_out=ss[:, 0:1]),
                r=[b_xt], w=[b_ss])
        self.op("dve", lambda e: e.tensor_scalar(ss[:, 1:2], ss[:, 0:1], 1.0 / D, EPS, ALU.mult, ALU.add), r=[b_ss], w=[b_ss])
        self.op("act", lambda e: e.activation(out=ss[:, 3:4], in_=ss[:, 1:2], func=AF.Sqrt), r=[b_ss], w=[b_ss])
        self.op("dve", lambda e: e.reciprocal(ss[:, 2:3], ss[:, 3:4]), r=[b_ss], w=[b_ss])
        self.op("dve", lambda e: e.tensor_scalar(xn[:], xt[:], ss[:, 2:3], None, ALU.mult), r=[b_xt, b_ss], w=[b_xn])
        for c in range(16):
            bk = banks[c // 4]
            self.op("pe", lambda e, c=c, bk=bk: e.transpose(self.ps[bk][:, (c % 4) * 128:(c % 4 + 1) * 128],
                                                            xn[:, c * 128:(c + 1) * 128], self.identf[:]),
                    r=[b_xn, self.b_const], w=[self.b_ps[bk]])
        for c in range(16):
            bk = banks[c // 4]
            src = self.ps[bk][:, (c % 4) * 128:(c % 4 + 1) * 128]
            if c % 2 == 0:
                self.op("act", lambda e, c=c, src=src: e.activation(out=f32T[:, c, :], in_=src, func=AF.Identity,
                                                                    scale=S[:, c, which:which + 1],
                                                                    bias=self.modT[:, sh0 + c, which:which + 1]),
                        r=[self.b_ps[bk], self.b_S, self.b_modT], w=[b_f32T])
            else:
                self.op("dve", lambda e, c=c, src=src: e.tensor_scalar(f32T[:, c, :], src, S[:, c, which:which + 1],
                                                                       self.modT[:, sh0 + c, which:which + 1],
                                                                       ALU.mult, ALU.add),
                        r=[self.b_ps[bk], self.b_S, self.b_modT], w=[b_f32T])
        self.op("pool", lambda e: e.tensor_copy(dstT[:, :, col0:col0 + 128], f32T[:]), r=[b_f32T], w=[b_dst])

    def phase_BC(self, l):
        self.phase_reset()
        src = self.x_in if l == 0 else self.XRES
        hT = self.sb("hT", [128, 16, TT], BF16)
        b_hT = Buf("hT")
        xt = [self.sb(f"xt{i}", [128, D], F32) for i in range(2)]
        xn = [self.sb(f"xn{i}", [128, D], F32) for i in range(2)]
        f32T = [self.sb(f"f32T{i}", [128, 16, 128], F32) for i in range(2)]
        junk = self.sb("junk", [128, D], BF16)
        ss = [self.sb(f"ss{i}", [128, 4], F32) for i in range(2)]
        b_xt = [Buf(), Buf()]
        b_f = [Buf(), Buf()]
        b_tmp = [(Buf(), Buf()), (Buf(), Buf())]
        d_xt = [self.ds(), self.ds()]
        mark = self.sb_off
        for t in range(NT):
            i = t % 2
            which = 1 if t < 2 else 0
            rd = [self.b_xres[t]] if l > 0 else []
            self.dma("sp", xt[i][:], src[t * 128:(t + 1) * 128, :], r=rd, w=[b_xt[i]], dsem=d_xt[i])
            self.norm_tile(xt[i], b_xt[i], which, self.S1, 0, f32T[i], b_f[i], hT, b_hT, t * 128,
                           [4 * i + j for j in range(4)], junk, ss[i], xn[i], b_tmp[i])
        self.dbg(f"hT{l}", hT[:], [128, 16, TT], BF16, r=[b_hT])
        if self.upto == f"B{l}":
            return
        wv = self.w_in[l].rearrange("(k p) n -> p k n", p=128)
        CW = 256
        stg = [self.sb(f"pst{i}", [128, 16, CW], F32) for i in range(2)]
        wbf = [self.sb(f"pwb{i}", [128, 16, CW], BF16) for i in range(2)]
        osb = [self.sb(f"posb{i}", [128, 512], BF16) for i in range(3)]
        gsb = self.sb("pgsb", [128, NT, 16], F32)
        b_stg = [Buf(), Buf()]
        b_wbf = [Buf(), Buf()]
        b_osb = [Buf(), Buf(), Buf()]
        b_gsb = Buf()
        d_stg = [self.ds(), self.ds()]
        d_osb = [self.ds(), self.ds(), self.ds()]
        d_g = self.ds()
        nblk = 4608 // CW
        cnt = 0
        ocnt = 0
        def load_blk(cb):
            s = cb % 2
            c0 = cb * CW
            cw = CW if cb < nblk else 16
            self.dma("sp", stg[s][:, :, 0:cw], wv[:, :, c0:c0 + cw], w=[b_stg[s]], dsem=d_stg[s])
        load_blk(0)
        for cb in range(nblk + 1):
            s = cb % 2
            c0 = cb * CW
            cw = CW if cb < nblk else 16
            if cb + 1 <= nblk:
                load_blk(cb + 1)
            self.op("pool", lambda e, s=s, cw=cw: e.tensor_copy(wbf[s][:, :, 0:cw], stg[s][:, :, 0:cw]),
                    r=[b_stg[s]], w=[b_wbf[s]])
            fm_head = None
            if 1536 <= c0 < 2560:
                fm_head = (c0 - 1536) // 128
            for t in range(NT):
                bk = cnt % 4
                cnt += 1
                pt = self.ps[bk]
                for k in range(16):
                    self.op("pe", lambda e, k=k, t=t, s=s, cw=cw, pt=pt: e.matmul(pt[:, 0:cw], lhsT=hT[:, k, t * 128:(t + 1) * 128],
                                                                                   rhs=wbf[s][:, k, 0:cw], start=(k == 0), stop=(k == 15)),
                            r=[b_hT, b_wbf[s]], w=[self.b_ps[bk]])
                if cb < nblk:
                    o = ocnt % 3
                    ocnt += 1
                    scale = (128.0 ** -0.5) if 2048 <= c0 < 2560 else 1.0
                    if t % 2 == 0:
                        self.op("act", lambda e, o=o, pt=pt, scale=scale: e.activation(out=osb[o][:, 0:CW], in_=pt[:, 0:CW],
                                                                                        func=AF.Copy, scale=scale),
                                r=[self.b_ps[bk]], w=[b_osb[o]])
                    else:
                        self.op("dve", lambda e, o=o, pt=pt, scale=scale: e.tensor_scalar(osb[o][:, 0:CW], pt[:, 0:CW], scale, None, ALU.mult),
                                r=[self.b_ps[bk]], w=[b_osb[o]])
                    self.dma("sp", self.PX[t * 128:(t + 1) * 128, c0:c0 + CW], osb[o][:, 0:CW], r=[b_osb[o]], w=[self.b_px[t]],
                             dsem=d_osb[o])
                else:
                    self.op("dve", lambda e, t=t, pt=pt: e.tensor_copy(gsb[:, t, :], pt[:, 0:16]), r=[self.b_ps[bk]], w=[b_gsb])
            if fm_head is not None:
                for hh in range(CW // 128):
                    head = fm_head + hh
                    dst = self.MQT if head < 4 else self.MKT
                    bdst = self.b_mqt if head < 4 else self.b_mkt
                    scale = 1.0 if head < 4 else (128.0 ** -0.5)
                    for tb in range(6):
                        bk = 4 + (cnt % 4)
                        cnt += 1
                        pt = self.ps[bk]
                        for k in range(16):
                            self.op("pe", lambda e, k=k, tb=tb, s=s, hh=hh, pt=pt: e.matmul(
                                pt[:, 0:384], lhsT=wbf[s][:, k, hh * 128:(hh + 1) * 128], rhs=hT[:, k, tb * 384:(tb + 1) * 384],
                                start=(k == 0), stop=(k == 15)), r=[b_hT, b_wbf[s]], w=[self.b_ps[bk]])
                        o = ocnt % 3
                        ocnt += 1
                        self.op("act", lambda e, o=o, pt=pt, scale=scale: e.activation(out=osb[o][:, 0:384], in_=pt[:, 0:384],
                                                                                        func=AF.Copy, scale=scale),
                                r=[self.b_ps[bk]], w=[b_osb[o]])
                        self.dma("sp", dst[head % 4, :, tb * 384:(tb + 1) * 384], osb[o][:, 0:384], r=[b_osb[o]], w=[bdst],
                                 dsem=d_osb[o])
        self.dma("sp", self.GD.rearrange("(t p) g -> p t g", p=128), gsb[:], r=[b_gsb], w=self.b_gd, dsem=d_g)
        self.dbg(f"PX{l}", self.PX, [TT, 4608], BF16, r=self.b_px)
        self.dbg(f"GD{l}", self.GD, [TT, 16], F32, r=self.b_gd)
        self.dbg(f"MQT{l}", self.MQT, [4, 128, TT], BF16, r=[self.b_mqt])
        self.dbg(f"MKT{l}", self.MKT, [4, 128, TT], BF16, r=[self.b_mkt])
        self.barrier()


def build_program(upto="all", debug=(), n_exp=NE, dev=False, dev_layer=0):
    nc = bass.Bass("TRN2", target_bir_lowering=False)
    stack = ExitStack()
    B = Builder(nc, stack, upto=upto, debug=debug, n_exp=n_exp, dev=dev)
    B.setup()
    B.last_layer = 1
    for l in ([0] if dev else range(2)):
        B.cur_layer = dev_layer if dev else l
        B.phase_A(l)
        if upto == f"A{l}":
            break
        B.phase_BC(l)
        if upto in (f"B{l}", f"C{l}"):
            break
        B.phase_D(l)
        if upto in (f"D{l}", f"D1_{l}"):
            break
        B.phase_E(l)
        if upto in (f"E{l}", f"E1_{l}"):
            break
        B.phase_F(l)
        if upto == f"F{l}":
            break
        B.phase_G(l)
        if upto == f"G{l}":
            break
    B.op("sp", lambda e: None, r=B.final_reads)
    B.P.finalize()
    with nc.Block() as block:
        B.P.emit(block, B.esems)
    stack.close()
    return nc, B


def make_in_maps(inputs, n_exp=NE):
    cst = _consts()
    maps = []
    x = np.asarray(inputs["x"], np.float32)
    ctx = np.asarray(inputs["ctx"], np.float32)
    c = np.asarray(inputs["c"], np.float32)
    c_ctx = np.asarray(inputs["c_ctx"], np.float32)
    shared = {k: np.ascontiguousarray(np.asarray(inputs[k], np.float32)) for k in
              ("w_ada", "b_ada", "norm1_w", "norm2_w", "w_in", "b_gates", "q_norm_w", "k_norm_w", "attn_sink",
               "mlstm_norm_w", "w_out", "w_router", "b_router", "w_gate_up", "b_gate_up", "w_down", "b_down")}
    for b in range(8):
        m = dict(shared)
        m["x_in"] = np.concatenate([ctx[b], x[b]], axis=0)
        m["c2"] = np.concatenate([c[b].reshape(16, 128), c_ctx.reshape(16, 128)], axis=0)
        for k, v in cst.items():
            m["cst_" + k] = v
        maps.append(m)
    return maps


def kernel(**inputs):
    nc, B = build_program()
    maps = make_in_maps(inputs)
    maps = [{k: m[k] for k in B.in_names} for m in maps]
    res = run_bass_kernel_spmd(nc, maps, core_ids=list(range(8)))
    return np.stack([np.asarray(r["y"], np.float32) for r in res.results], axis=0)


def phase_D(self, l):
    sl = self.cur_layer
    self.phase_reset()
    qT = self.sb("qT", [128, 8, TT], BF16)
    kT2 = self.sb("kT2", [128, 4, 2, TT], BF16)
    vaug = self.sb("vaug", [128, NT, 4, 65], BF16)
    cos = self.sb("cos", [128, 16, 64], F32)
    sin = self.sb("sin", [128, 16, 64], F32)
    qw = self.sb("qw", [128, 64], F32)
    kw = self.sb("kw", [128, 64], F32)
    sinke = self.sb("sinke", [128, 16], F32)
    maskp = self.sb("maskp", [128, 128], BF16)
    maskn = self.sb("maskn", [128, 128], BF16)
    b_qT, b_kT2, b_vaug, b_c = Buf(), Buf(), Buf(), Buf()
    d0 = self.ds()
    self.dma("sp", cos[:], self.cst["rope_cos"], w=[b_c], dsem=d0)
    self.dma("sp", sin[:], self.cst["rope_sin"], w=[b_c], dsem=d0)
    self.dma("sp", qw[:], self.q_norm_w[l:l + 1, :].partition_broadcast(128), w=[b_c], dsem=d0)
    self.dma("sp", kw[:], self.k_norm_w[l:l + 1, :].partition_broadcast(128), w=[b_c], dsem=d0)
    self.dma("sp", sinke[:], self.attn_sink[l:l + 1, :].partition_broadcast(128), w=[b_c], dsem=d0)
    self.dma("sp", maskp[:], self.cst["mask_prev"], w=[b_c], dsem=d0)
    self.dma("sp", maskn[:], self.cst["mask_next"], w=[b_c], dsem=d0)
    b_c2 = Buf()
    self.op("act", lambda e: e.activation(out=qw[:], in_=qw[:], func=AF.Copy, scale=0.125), r=[b_c], w=[b_c2])
    self.op("act", lambda e: e.activation(out=sinke[:], in_=sinke[:], func=AF.Exp), r=[b_c], w=[b_c2])
    self.op("pool", lambda e: e.memset(vaug[:].rearrange("p t j d -> p (t j) d")[:, :, 64:65], 1.0), w=[b_vaug])
    slab = [self.sb(f"slab{i}", [128, 1536], BF16) for i in range(2)]
    t1 = [self.sb(f"dt1_{i}", [128, 20, 64], F32) for i in range(2)]
    t2 = [self.sb(f"dt2_{i}", [128, 20, 64], F32) for i in range(2)]
    t3 = [self.sb(f"dt3_{i}", [128, 20, 64], F32) for i in range(2)]
    qkr = [self.sb(f"qkr{i}", [128, 20, 64], BF16) for i in range(2)]
    k2 = [self.sb(f"k2_{i}", [128, 4, 2, 2, 64], BF16) for i in range(2)]
    st = [self.sb(f"dst_{i}", [128, 64], F32) for i in range(2)]
    b_slab, b_t1, b_t2, b_t3, b_qkr, b_k2, b_st = ([Buf(), Buf()] for _ in range(7))
    d_slab = [self.ds(), self.ds()]
    t_first = 0
    for i in range(2):
        self.op("pool", lambda e, i=i: e.memset(k2[i][:].rearrange("p j u h d -> p (j u h d)"), 0.0), w=[b_k2[i]])
    for t in range(t_first, NT):
        i = t % 2
        self.dma("sp", slab[i][:], self.PX[t * 128:(t + 1) * 128, 0:1536], r=[self.b_px[t]], w=[b_slab[i]], dsem=d_slab[i])
        qk = slab[i][:, 0:1280].rearrange("p (h d) -> p h d", d=64)
        self.op("dve", lambda e, i=i, qk=qk: e.tensor_tensor(t1[i][:], qk, qk, ALU.mult), r=[b_slab[i]], w=[b_t1[i]])
        self.op("dve", lambda e, i=i: e.tensor_reduce(st[i][:, 0:20], t1[i][:], AX.X, ALU.add), r=[b_t1[i]], w=[b_st[i]])
        self.op("dve", lambda e, i=i: e.tensor_scalar(st[i][:, 0:20], st[i][:, 0:20], 1.0 / 64, EPS, ALU.mult, ALU.add),
                r=[b_st[i]], w=[b_st[i]])
        self.op("act", lambda e, i=i: e.activation(out=st[i][:, 20:40], in_=st[i][:, 0:20], func=AF.Sqrt), r=[b_st[i]], w=[b_st[i]])
        self.op("dve", lambda e, i=i: e.reciprocal(st[i][:, 40:60], st[i][:, 20:40]), r=[b_st[i]], w=[b_st[i]])
        self.op("dve", lambda e, i=i, qk=qk: e.tensor_tensor(t1[i][:], qk, st[i][:, 40:60].unsqueeze(2).to_broadcast([128, 20, 64]),
                                                             ALU.mult), r=[b_slab[i], b_st[i]], w=[b_t1[i]])
        self.op("dve", lambda e, i=i: e.tensor_tensor(t1[i][:, 0:16, :], t1[i][:, 0:16, :],
                                                      qw[:].unsqueeze(1).to_broadcast([128, 16, 64]), ALU.mult),
                r=[b_c2], w=[b_t1[i]])
        self.op("dve", lambda e, i=i: e.tensor_tensor(t1[i][:, 16:20, :], t1[i][:, 16:20, :],
                                                      kw[:].unsqueeze(1).to_broadcast([128, 4, 64]), ALU.mult),
                r=[b_c], w=[b_t1[i]])
        if t >= 2:
            lt = t - 2
            self.op("pool", lambda e, i=i, lt=lt: e.tensor_tensor(t2[i][:], t1[i][:],
                                                                  cos[:, lt, :].unsqueeze(1).to_broadcast([128, 20, 64]), ALU.mult),
                    r=[b_t1[i], b_c], w=[b_t2[i]])
            v1 = t1[i][:].rearrange("p h (a s j) -> p h a s j", a=2, s=2)
            v3 = t3[i][:].rearrange("p h (a s j) -> p h a s j", a=2, s=2)
            sv = sin[:, lt, :].rearrange("p (a s j) -> p a s j", a=2, s=2)
            for s in range(2):
                self.op("dve", lambda e, s=s, v1=v1, v3=v3, sv=sv: e.tensor_tensor(
                    v3[:, :, :, s, :], v1[:, :, :, 1 - s, :], sv[:, :, s, :].unsqueeze(1).to_broadcast([128, 20, 2, 16]), ALU.mult),
                    r=[b_t1[i], b_c], w=[b_t3[i]])
            self.op("pool", lambda e, i=i: e.tensor_tensor(qkr[i][:], t2[i][:], t3[i][:], ALU.add),
                    r=[b_t2[i], b_t3[i]], w=[b_qkr[i]])
        else:
            self.op("pool", lambda e, i=i: e.tensor_copy(qkr[i][:], t1[i][:]), r=[b_t1[i]], w=[b_qkr[i]])
        for dup in range(2):
            self.op("pool", lambda e, i=i, dup=dup: e.tensor_copy(k2[i][:, :, dup, dup, :], qkr[i][:, 16:20, :]),
                    r=[b_qkr[i]], w=[b_k2[i]])
        self.op("pool", lambda e, i=i, t=t: e.tensor_copy(vaug[:, t, :, 0:64], slab[i][:, 1280:1536].rearrange("p (j d) -> p j d", d=64)),
                r=[b_slab[i]], w=[b_vaug])
        pq = self.ps[6][:].bitcast(BF16)
        pk = self.ps[7][:].bitcast(BF16)
        qflat = qkr[i][:].rearrange("p h d -> p (h d)")
        for pr in range(8):
            self.op("pe", lambda e, pr=pr, pq=pq, qflat=qflat: e.transpose(pq[:, pr * 128:(pr + 1) * 128], qflat[:, pr * 128:(pr + 1) * 128],
                                                                           self.identb[:]),
                    r=[b_qkr[i], self.b_const], w=[self.b_ps[6]])
        self.op("act", lambda e, t=t, pq=pq: e.activation(out=qT[:, :, t * 128:(t + 1) * 128], in_=pq.rearrange("p (a b) -> p a b", b=128),
                                                          func=AF.Copy), r=[self.b_ps[6]], w=[b_qT])
        kflat = k2[i][:].rearrange("p j u h d -> p (j u h d)")
        for j in range(8):
            self.op("pe", lambda e, j=j, pk=pk, kflat=kflat: e.transpose(pk[:, j * 128:(j + 1) * 128], kflat[:, j * 128:(j + 1) * 128],
                                                                         self.identb[:]),
                    r=[b_k2[i], self.b_const], w=[self.b_ps[7]])
        self.op("dve", lambda e, t=t, pk=pk: e.tensor_copy(kT2[:, :, :, t * 128:(t + 1) * 128], pk[:, 0:1024].rearrange("p (a u b) -> p a u b", u=2, b=128)),
                r=[self.b_ps[7]], w=[b_kT2])
    self.dbg(f"qT{l}", qT[:], [128, 8, TT], BF16, r=[b_qT])
    self.dbg(f"kT2{l}", kT2[:], [128, 4, 2, TT], BF16, r=[b_kT2])
    if self.upto == f"D1_{l}":
        return
    E = [self.sb(f"E{i}", [128, 5, 4, 128], BF16) for i in range(2)]
    att = [self.sb(f"att{i}", [128, 1024], BF16) for i in range(2)]
    den = [self.sb(f"den{i}", [128, 8], F32) for i in range(2)]
    b_E, b_att, b_den = [Buf(), Buf()], [Buf(), Buf()], [Buf(), Buf()]
    d_att = [self.ds(), self.ds()]
    sink_v = sinke[:].rearrange("q (j i p) -> q j p i", j=4, i=2, p=2)
    u = 0
    sc = 0
    tq0 = 0 if sl == 0 else 2
    for tq in range(tq0, NT):
        ai = tq % 2
        for j in range(4):
            ei = u % 2
            bo = 4 + (u % 2)
            u += 1
            if tq < 2:
                keys = [0, 1]
            else:
                keys = [0, 1] + [tk for tk in (tq - 1, tq, tq + 1) if 2 <= tk < NT]
            for ki, tk in enumerate(keys):
                bk = sc % 4
                sc += 1
                for p in range(2):
                    self.op("pe", lambda e, p=p, bk=bk, tk=tk, tq=tq, j=j: e.matmul(
                        self.ps[bk][:, p * 256:(p + 1) * 256], lhsT=kT2[:, j, p, tk * 128:(tk + 1) * 128],
                        rhs=qT[:, 2 * j:2 * j + 2, tq * 128:(tq + 1) * 128], start=True, stop=True),
                        r=[b_qT, b_kT2], w=[self.b_ps[bk]])
                self.op("act", lambda e, ei=ei, ki=ki, bk=bk: e.activation(out=E[ei][:, ki, :, :].rearrange("p a b -> p (a b)"),
                                                                            in_=self.ps[bk][:], func=AF.Exp),
                        r=[self.b_ps[bk]], w=[b_E[ei]])
                if tq >= 2 and tk >= 2 and tk != tq:
                    mk = maskp if tk == tq - 1 else maskn
                    self.op("pool", lambda e, ei=ei, ki=ki, mk=mk: e.tensor_tensor(
                        E[ei][:, ki, :, :], E[ei][:, ki, :, :], mk[:].unsqueeze(1).to_broadcast([128, 4, 128]), ALU.mult),
                        r=[b_c], w=[b_E[ei]])
            nk = len(keys)
            for slot in range(4):
                for ki, tk in enumerate(keys):
                    self.op("pe", lambda e, slot=slot, ki=ki, tk=tk, ei=ei, bo=bo, j=j, nk=nk: e.matmul(
                        self.ps[bo][:, slot * 128:slot * 128 + 65], lhsT=E[ei][:, ki, slot, :], rhs=vaug[:, tk, j, :],
                        start=(ki == 0), stop=(ki == nk - 1)), r=[b_E[ei], b_vaug], w=[self.b_ps[bo]])
            ov = self.ps[bo][:].rearrange("q (s d) -> q s d", d=128)
            self.op("dve", lambda e, ai=ai, ov=ov, j=j: e.tensor_tensor(
                den[ai][:, 0:4].rearrange("q (p i) -> q p i", p=2), ov[:, :, 64:65].rearrange("q (p i) o -> q p (i o)", p=2),
                sink_v[:, j], ALU.add), r=[self.b_ps[bo], b_c2], w=[b_den[ai]])
            self.op("dve", lambda e, ai=ai: e.reciprocal(den[ai][:, 4:8], den[ai][:, 0:4]), r=[b_den[ai]], w=[b_den[ai]])
            self.op("dve", lambda e, ai=ai, ov=ov, j=j: e.tensor_tensor(
                att[ai][:, j * 256:(j + 1) * 256].rearrange("q (i p d) -> q p i d", i=2, p=2),
                ov[:, :, 0:64].rearrange("q (p i) d -> q p i d", p=2),
                den[ai][:, 4:8].rearrange("q (p i) -> q p i", p=2).unsqueeze(3).to_broadcast([128, 2, 2, 64]), ALU.mult),
                r=[self.b_ps[bo], b_den[ai]], w=[b_att[ai]])
        self.dma("sp", self.CATD[tq * 128:(tq + 1) * 128, 0:1024], att[ai][:], r=[b_att[ai]], w=[self.b_catd[tq]], dsem=d_att[ai])
    self.dbg(f"ATT{l}", self.CATD, [TT, D], BF16, r=self.b_catd)
    self.barrier()


Builder.phase_D = phase_D


def phase_E(self, l):
    sl = self.cur_layer
    self.phase_reset()
    HS = self.sb("HS", [128, NT, 1024], F32)
    after_hs = self.sb_off
    mqT = self.sb("mqT", [128, 4, TT], BF16)
    mkT = self.sb("mkT", [128, 4, TT], BF16)
    mk = self.sb("mk", [128, NT, 512], BF16)
    vaug = self.sb("mvaug", [128, NT, 4, 257], BF16)
    G = self.sb("G", [128, NT, 16], F32)
    GI = self.sb("GI", [128, NT, 16], F32)
    E1 = self.sb("E1", [128, NT, 16], F32)
    LF = self.sb("LF", [128, NT, 16], F32)
    A = self.sb("Acol", [128, NT, 8], F32)
    bg = self.sb("bg", [128, 16], F32)
    tri = [self.sb("trif", [128, 128], F32), self.sb("trib", [128, 128], F32)]
    CT = self.sb("CT", [128, 8, 257], F32)
    CTb = self.sb("CTb", [128, 8, 257], BF16)
    LFB = [self.sb(f"LFB{i}", [128, 128], F32) for i in range(2)]
    DT = [self.sb(f"DT{i}", [128, 128], F32) for i in range(2)]
    EB = [self.sb(f"EB{i}", [128, 128], F32) for i in range(2)]
    DTm = [self.sb(f"DTm{i}", [128, 128], F32) for i in range(2)]
    WT = [self.sb(f"WT{i}", [128, 128], BF16) for i in range(2)]
    qTs = [self.sb(f"qTs{i}", [128, 128], BF16) for i in range(2)]
    kws = [self.sb(f"kws{i}", [128, 128], BF16) for i in range(2)]
    dd = [self.sb(f"dd{i}", [128, 2], F32) for i in range(2)]
    b_in, b_g, b_A, b_v = Buf(), Buf(), Buf(), Buf()
    b_HS = [[Buf() for h in range(4)] for c in range(NT)]
    b_CT = [Buf() for _ in range(8)]
    b_CTb = [Buf() for _ in range(8)]
    b_LFB, b_DT, b_EB, b_DTm, b_WT, b_qTs, b_kws, b_dd = ([Buf(), Buf()] for _ in range(8))
    d0 = self.ds()
    d1 = self.ds()
    self.dma("sp", mqT[:], self.MQT.rearrange("h p t -> p h t"), r=[self.b_mqt], w=[b_in], dsem=d0)
    self.dma("sp", mkT[:], self.MKT.rearrange("h p t -> p h t"), r=[self.b_mkt], w=[b_in], dsem=d0)
    self.dma("sp", tri[0][:], self.cst["tri_f"], w=[b_in], dsem=d0)
    self.dma("sp", tri[1][:], self.cst["tri_b"], w=[b_in], dsem=d0)
    self.dma("sp", bg[:], self.b_gates[l:l + 1, :].partition_broadcast(128), w=[b_g], dsem=d0)
    self.dma("sp", G[:], self.GD.rearrange("(t p) g -> p t g", p=128), r=self.b_gd, w=[b_g], dsem=d0)
    self.op("pool", lambda e: e.memset(vaug[:].rearrange("p t h v -> p (t h) v")[:, :, 256:257], 1.0), w=[b_v])
    for t in range(NT):
        self.dma("sp", mk[:, t, :], self.PX[t * 128:(t + 1) * 128, 2048:2560], r=[self.b_px[t]], w=[b_in], dsem=d1)
        self.dma("sp", vaug[:, t, :, 0:256], self.PX[t * 128:(t + 1) * 128, 2560:3584].rearrange("p (h v) -> p h v", v=256),
                 r=[self.b_px[t]], w=[b_v], dsem=d1)
    self.op("pool", lambda e: e.memset(CT[:].rearrange("p a b -> p (a b)"), 0.0), w=b_CT)
    self.op("pool", lambda e: e.memset(CTb[:].rearrange("p a b -> p (a b)"), 0.0), w=b_CTb)
    Gf = G[:].rearrange("p t g -> p (t g)")
    GIf = GI[:].rearrange("p t g -> p (t g)")
    E1f = E1[:].rearrange("p t g -> p (t g)")
    LFf = LF[:].rearrange("p t g -> p (t g)")
    self.op("dve", lambda e: e.tensor_tensor(G[:], G[:], bg[:].unsqueeze(1).to_broadcast([128, NT, 16]), ALU.add), r=[b_g], w=[b_g])
    self.op("act", lambda e: e.activation(out=Gf, in_=Gf, func=AF.Tanh, scale=1.0 / 15.0), r=[b_g], w=[b_g])
    self.op("dve", lambda e: e.tensor_scalar(GIf, Gf, 15.0, None, ALU.mult), r=[b_g], w=[b_g])
    P1 = self.sb("P1", [128, NT * 16], F32)
    Y2 = self.sb("Y2", [128, NT * 16], F32)
    self.op("dve", lambda e: e.tensor_scalar(E1f, GIf, -1.0, None, ALU.mult), r=[b_g], w=[b_g])
    self.op("dve", lambda e: e.tensor_tensor(E1f, E1f, GIf, ALU.max), r=[b_g], w=[b_g])
    self.op("act", lambda e: e.activation(out=E1f, in_=E1f, func=AF.Exp, scale=-1.0), r=[b_g], w=[b_g])
    self.op("dve", lambda e: e.tensor_scalar(P1[:], E1f, 2.0, None, ALU.add), r=[b_g], w=[b_g])
    self.op("dve", lambda e: e.reciprocal(P1[:], P1[:]), r=[b_g], w=[b_g])
    self.op("dve", lambda e: e.tensor_tensor(E1f, E1f, P1[:], ALU.mult), r=[b_g], w=[b_g])
    self.op("dve", lambda e: e.tensor_tensor(Y2[:], E1f, E1f, ALU.mult), r=[b_g], w=[b_g])
    self.op("dve", lambda e: e.tensor_scalar(P1[:], Y2[:], 1.0 / 13.0, 1.0 / 11.0, ALU.mult, ALU.add), r=[b_g], w=[b_g])
    for cc in (1.0 / 9.0, 1.0 / 7.0, 1.0 / 5.0, 1.0 / 3.0, 1.0):
        self.op("dve", lambda e: e.tensor_tensor(P1[:], P1[:], Y2[:], ALU.mult), r=[b_g], w=[b_g])
        self.op("dve", lambda e, cc=cc: e.tensor_scalar(P1[:], P1[:], cc, None, ALU.add), r=[b_g], w=[b_g])
    self.op("dve", lambda e: e.tensor_tensor(P1[:], P1[:], E1f, ALU.mult), r=[b_g], w=[b_g])
    self.op("dve", lambda e: e.tensor_scalar(E1f, GIf, 0.0, None, ALU.min), r=[b_g], w=[b_g])
    self.op("dve", lambda e: e.scalar_tensor_tensor(LFf, P1[:], -2.0, E1f, ALU.mult, ALU.add), r=[b_g], w=[b_g])
    for d in range(2):
        self.op("pe", lambda e, d=d: e.matmul(self.ps[d][:, 0:NT * 4], lhsT=tri[d][:], rhs=LF[:, :, 4 + 8 * d:8 + 8 * d],
                                              start=True, stop=True), r=[b_g, b_in], w=[self.b_ps[d]])
        self.op("dve", lambda e, d=d: e.tensor_tensor(A[:, :, 4 * d:4 * d + 4], GI[:, :, 8 * d:8 * d + 4],
                                                      self.ps[d][:, 0:NT * 4].rearrange("p (c h) -> p c h", h=4), ALU.subtract),
                r=[b_g, self.b_ps[d]], w=[b_A])
    order = [list(range(NT)), [1, 0] + list(range(NT - 1, 1, -1))]
    u = 0
    hs_written = set()
    for step in range(NT):
        for d in range(2):
            c = order[d][step]
            tl = 127 if d == 0 else 0
            tsl = slice(c * 128, (c + 1) * 128)
            need_h = not (sl == 1 and c < 2)
            for h in range(4):
                par = u % 2
                u += 1
                gf = 4 + 8 * d + h
                ch = d * 4 + h
                pa, pb, pc, pd = (4 * par + i for i in range(4))
                self.op("pool", lambda e, par=par, c=c, gf=gf: e.tensor_copy(LFB[par][:], LF[:, c, gf:gf + 1].to_broadcast([128, 128])),
                        r=[b_g], w=[b_LFB[par]])
                self.op("pe", lambda e, par=par, d=d, pb=pb: e.matmul(self.ps[pb][:, 0:128], lhsT=LFB[par][:], rhs=tri[d][:],
                                                                     start=True, stop=True),
                        r=[b_LFB[par], b_in], w=[self.b_ps[pb]])
                self.op("pe", lambda e, h=h, tsl=tsl, pa=pa: e.matmul(self.ps[pa][:, 0:128], lhsT=mkT[:, h, tsl], rhs=mqT[:, h, tsl],
                                                                     start=True, stop=True),
                        r=[b_in], w=[self.b_ps[pa]])
                self.op("act", lambda e, par=par, pb=pb, c=c, ch=ch: e.activation(out=DT[par][:], in_=self.ps[pb][:, 0:128], func=AF.Exp,
                                                                                 bias=A[:, c, ch:ch + 1]),
                        r=[self.b_ps[pb], b_A], w=[b_DT[par]])
                self.op("act", lambda e, par=par, pb=pb: e.activation(out=EB[par][:], in_=self.ps[pb][:, 0:128], func=AF.Exp),
                        r=[self.b_ps[pb]], w=[b_EB[par]])
                self.op("pool", lambda e, par=par, d=d: e.tensor_tensor(DTm[par][:], DT[par][:], tri[d][:], ALU.mult),
                        r=[b_DT[par], b_in], w=[b_DTm[par]])
                self.op("dve", lambda e, par=par, pa=pa: e.tensor_tensor(WT[par][:], self.ps[pa][:, 0:128], DTm[par][:], ALU.mult),
                        r=[self.b_ps[pa], b_DTm[par]], w=[b_WT[par]])
                self.op("dve", lambda e, par=par, h=h, tsl=tsl: e.tensor_tensor(qTs[par][:], mqT[:, h, tsl], EB[par][:], ALU.mult),
                        r=[b_in, b_EB[par]], w=[b_qTs[par]])
                self.op("pool", lambda e, par=par, c=c, h=h, tl=tl: e.tensor_scalar(kws[par][:], mk[:, c, h * 128:(h + 1) * 128],
                                                                                   DTm[par][:, tl:tl + 1], None, ALU.mult),
                        r=[b_in, b_DTm[par]], w=[b_kws[par]])
                if need_h:
                    self.op("pe", lambda e, par=par, c=c, h=h, pc=pc: e.matmul(self.ps[pc][:, 0:257], lhsT=WT[par][:], rhs=vaug[:, c, h, :],
                                                                              start=True, stop=False),
                            r=[b_WT[par], b_v], w=[self.b_ps[pc]])
                    self.op("pe", lambda e, par=par, ch=ch, pc=pc: e.matmul(self.ps[pc][:, 0:257], lhsT=qTs[par][:], rhs=CTb[:, ch, :],
                                                                           start=False, stop=True),
                            r=[b_qTs[par], b_CTb[ch]], w=[self.b_ps[pc]])
                    self.op("dve", lambda e, par=par, pc=pc: e.tensor_scalar(dd[par][:, 0:1], self.ps[pc][:, 256:257], -1.0, None, ALU.mult),
                            r=[self.b_ps[pc]], w=[b_dd[par]])
                    self.op("dve", lambda e, par=par, pc=pc: e.scalar_tensor_tensor(dd[par][:, 0:1], self.ps[pc][:, 256:257], 1.0, dd[par][:, 0:1],
                                                                                   ALU.max, ALU.max),
                            r=[self.b_ps[pc], b_dd[par]], w=[b_dd[par]])
                    self.op("dve", lambda e, par=par: e.reciprocal(dd[par][:, 1:2], dd[par][:, 0:1]), r=[b_dd[par]], w=[b_dd[par]])
                    hs = HS[:, c, h * 256:(h + 1) * 256]
                    first = (c, h) not in hs_written
                    hs_written.add((c, h))
                    if first:
                        self.op("act", lambda e, par=par, pc=pc, hs=hs: e.activation(out=hs, in_=self.ps[pc][:, 0:256], func=AF.Identity,
                                                                                    scale=dd[par][:, 1:2]),
                                r=[self.b_ps[pc], b_dd[par]], w=[b_HS[c][h]])
                    else:
                        self.op("dve", lambda e, par=par, pc=pc, hs=hs: e.scalar_tensor_tensor(hs, self.ps[pc][:, 0:256], dd[par][:, 1:2], hs,
                                                                                              ALU.mult, ALU.add),
                                r=[self.b_ps[pc], b_dd[par]], w=[b_HS[c][h]])
                if step < NT - 1:
                    self.op("pe", lambda e, par=par, c=c, h=h, pd=pd: e.matmul(self.ps[pd][:, 0:257], lhsT=kws[par][:], rhs=vaug[:, c, h, :],
                                                                              start=True, stop=True),
                            r=[b_kws[par], b_v], w=[self.b_ps[pd]])
                    self.op("dve", lambda e, par=par, ch=ch, pd=pd, tl=tl: e.scalar_tensor_tensor(CT[:, ch, :], CT[:, ch, :], EB[par][:, tl:tl + 1],
                                                                                                 self.ps[pd][:, 0:257], ALU.mult, ALU.add),
                            r=[self.b_ps[pd], b_EB[par]], w=[b_CT[ch]])
                    self.op("act", lambda e, ch=ch: e.activation(out=CTb[:, ch, :], in_=CT[:, ch, :], func=AF.Copy),
                            r=[b_CT[ch]], w=[b_CTb[ch]])
    t0 = 0 if sl == 0 else 2
    self.dbg(f"HS{l}", HS[:], [128, NT, 1024], F32, r=[b for c in range(NT) for b in b_HS[c]])
    if self.upto == f"E1_{l}":
        return
    self.barrier()
    self.sb_off = after_hs
    nw = self.sb("nw", [128, 1024], F32)
    mo = [self.sb(f"mo{i}", [128, 1024], BF16) for i in range(2)]
    t1 = [self.sb(f"et1_{i}", [128, 1024], F32) for i in range(2)]
    sg = [self.sb(f"esg{i}", [128, 1024], F32) for i in range(2)]
    ob = [self.sb(f"eob{i}", [128, 1024], BF16) for i in range(2)]
    junk = self.sb("ejunk", [128, 256], BF16)
    ss = [self.sb(f"ess{i}", [128, 16], F32) for i in range(2)]
    b_nw, b_junk = Buf(), Buf()
    b_mo, b_t1, b_sg, b_ob, b_ss = ([Buf(), Buf()] for _ in range(5))
    dn = self.ds()
    d_mo = [self.ds(), self.ds()]
    d_ob = [self.ds(), self.ds()]
    self.dma("sp", nw[:], self.mlstm_norm_w[l:l + 1, :].partition_broadcast(128), w=[b_nw], dsem=dn)
    for c in range(t0, NT):
        i = c % 2
        rhs_all = b_HS[c]
        self.dma("sp", mo[i][:], self.PX[c * 128:(c + 1) * 128, 3584:4608], r=[self.b_px[c]], w=[b_mo[i]], dsem=d_mo[i])
        for h in range(4):
            self.op("act", lambda e, i=i, c=c, h=h: e.activation(out=junk[:], in_=HS[:, c, h * 256:(h + 1) * 256], func=AF.Square,
                                                                 accum_out=ss[i][:, h:h + 1]),
                    r=[b_HS[c][h]], w=[b_ss[i], b_junk])
        self.op("dve", lambda e, i=i: e.tensor_scalar(ss[i][:, 4:8], ss[i][:, 0:4], 1.0 / 256, EPS, ALU.mult, ALU.add), r=[b_ss[i]], w=[b_ss[i]])
        self.op("act", lambda e, i=i: e.activation(out=ss[i][:, 8:12], in_=ss[i][:, 4:8], func=AF.Sqrt), r=[b_ss[i]], w=[b_ss[i]])
        self.op("dve", lambda e, i=i: e.reciprocal(ss[i][:, 12:16], ss[i][:, 8:12]), r=[b_ss[i]], w=[b_ss[i]])
        self.op("dve", lambda e, i=i, c=c: e.tensor_tensor(t1[i][:].rearrange("p (h v) -> p h v", v=256),
                                                           HS[:, c, :].rearrange("p (h v) -> p h v", v=256),
                                                           ss[i][:, 12:16].unsqueeze(2).to_broadcast([128, 4, 256]), ALU.mult),
                r=rhs_all + [b_ss[i]], w=[b_t1[i]])
        self.op("pool", lambda e, i=i: e.tensor_tensor(t1[i][:], t1[i][:], nw[:], ALU.mult), r=[b_nw], w=[b_t1[i]])
        self.op("act", lambda e, i=i: e.activation(out=sg[i][:], in_=mo[i][:], func=AF.Sigmoid), r=[b_mo[i]], w=[b_sg[i]])
        self.op("dve", lambda e, i=i: e.tensor_tensor(ob[i][:], t1[i][:], sg[i][:], ALU.mult), r=[b_t1[i], b_sg[i]], w=[b_ob[i]])
        self.dma("sp", self.CATD[c * 128:(c + 1) * 128, 1024:2048], ob[i][:], r=[b_ob[i]], w=[self.b_catd[c]], dsem=d_ob[i])
    self.dbg(f"CAT{l}", self.CATD, [TT, D], BF16, r=self.b_catd)
    self.barrier()


Builder.phase_E = phase_E


def phase_F(self, l):
    sl = self.cur_layer
    self.phase_reset()
    src = self.x_in if l == 0 else self.XRES
    wout = self.sb("wout", [128, 16, D], BF16)
    G1 = self.sb("G1", [128, 2, D], F32)
    cat = [self.sb(f"fcat{i}", [128, D], BF16) for i in range(2)]
    xt = [self.sb(f"fxt{i}", [128, D], F32) for i in range(2)]
    catT = [self.sb(f"fcatT{i}", [128, 16, 128], BF16) for i in range(2)]
    xo = [self.sb(f"fxo{i}", [128, D], F32) for i in range(2)]
    b_wout, b_G1 = Buf(), Buf()
    b_cat, b_xt, b_catT, b_xo = ([Buf(), Buf()] for _ in range(4))
    dw, dg = self.ds(), self.ds()
    d_cat, d_xt, d_xo = ([self.ds(), self.ds()] for _ in range(3))
    wv = self.w_out[l].rearrange("(k p) n -> p k n", p=128)
    for k0 in range(0, 16, 4):
        self.dma("pool", wout[:, k0:k0 + 4, :], wv[:, k0:k0 + 4, :], w=[b_wout], dsem=dw)
    for w_ in range(2):
        self.dma("sp", G1[:, w_, :], self.MODD[w_:w_ + 1, 2 * D:3 * D].partition_broadcast(128), r=[self.b_modd], w=[b_G1], dsem=dg)
    t0 = 0 if sl == 0 else 2
    for t in range(t0, NT):
        i = t % 2
        which = 1 if t < 2 else 0
        rows = slice(t * 128, (t + 1) * 128)
        self.dma("sp", cat[i][:], self.CATD[rows, :], r=[self.b_catd[t]], w=[b_cat[i]], dsem=d_cat[i])
        self.dma("sp", xt[i][:], src[rows, :], r=([self.b_xres[t]] if l > 0 else []), w=[b_xt[i]], dsem=d_xt[i])
        bA, bB = 4 + 2 * i, 5 + 2 * i
        pA = self.ps[bA][:].bitcast(BF16)
        pB = self.ps[bB][:].bitcast(BF16)
        for k in range(16):
            pX, bX = (pA, bA) if k < 8 else (pB, bB)
            self.op("pe", lambda e, k=k, pX=pX, i=i: e.transpose(pX[:, (k % 8) * 128:(k % 8 + 1) * 128], cat[i][:, k * 128:(k + 1) * 128],
                                                                 self.identb[:]),
                    r=[b_cat[i], self.b_const], w=[self.b_ps[bX]])
        self.op("act", lambda e, i=i, pA=pA: e.activation(out=catT[i][:, 0:8, :], in_=pA.rearrange("p (a b) -> p a b", b=128), func=AF.Copy),
                r=[self.b_ps[bA]], w=[b_catT[i]])
        self.op("dve", lambda e, i=i, pB=pB: e.tensor_copy(catT[i][:, 8:16, :], pB.rearrange("p (a b) -> p a b", b=128)),
                r=[self.b_ps[bB]], w=[b_catT[i]])
        for nb in range(4):
            cols = slice(nb * 512, (nb + 1) * 512)
            for k in range(16):
                self.op("pe", lambda e, k=k, nb=nb, i=i, cols=cols: e.matmul(self.ps[nb][:, 0:512], lhsT=catT[i][:, k, :], rhs=wout[:, k, cols],
                                                                            start=(k == 0), stop=(k == 15)),
                        r=[b_catT[i], b_wout], w=[self.b_ps[nb]])
            self.op("dve", lambda e, nb=nb, i=i, cols=cols, which=which: e.tensor_tensor(xo[i][:, cols], self.ps[nb][:, 0:512], G1[:, which, cols], ALU.mult),
                    r=[self.b_ps[nb], b_G1], w=[b_xo[i]])
            self.op("pool", lambda e, i=i, cols=cols: e.tensor_tensor(xo[i][:, cols], xo[i][:, cols], xt[i][:, cols], ALU.add),
                    r=[b_xt[i]], w=[b_xo[i]])
        self.dma("sp", self.XRES[rows, :], xo[i][:], r=[b_xo[i]], w=[self.b_xres[t]], dsem=d_xo[i])
    self.dbg(f"XMID{l}", self.XRES, [TT, D], F32, r=self.b_xres)
    self.barrier()


Builder.phase_F = phase_F


def phase_G(self, l):
    sl = self.cur_layer
    last = (sl == self.last_layer)
    self.phase_reset()
    NX = self.n_exp
    wr = self.sb("wr", [128, 16, NE], F32)
    br = self.sb("br", [128, NE], F32)
    bguT = self.sb("bguT", [128, 16, NE], F32)
    bd = self.sb("bd", [NE, D], F32)
    G2 = self.sb("G2", [128, 2, D], F32)
    selb = [self.sb(f"selb{i}", [NE, 128], F32) for i in range(2)]
    mark = self.sb_off
    braw = self.sb("braw", [NE, D], F32)
    b_c, b_braw = Buf(), Buf()
    dc = self.ds()
    self.dma("sp", wr[:], self.w_router[l].rearrange("(k p) e -> p k e", p=128), w=[b_c], dsem=dc)
    self.dma("sp", br[:], self.b_router[l:l + 1, :].partition_broadcast(128), w=[b_c], dsem=dc)
    self.dma("sp", bd[0:NX, :], self.b_down[l], w=[b_c], dsem=dc)
    self.dma("sp", braw[0:NX, :], self.b_gate_up[l], w=[b_braw], dsem=dc)
    for w_ in range(2):
        self.dma("sp", G2[:, w_, :], self.MODD[w_:w_ + 1, 5 * D:6 * D].partition_broadcast(128), r=[self.b_modd], w=[b_c], dsem=dc)
    for j in range(16):
        self.op("pe", lambda e, j=j: e.transpose(self.ps[0][:, j * NE:j * NE + NX], braw[0:NX, j * 128:(j + 1) * 128],
                                                 self.identf[0:NX, 0:NX]),
                r=[b_braw, self.b_const], w=[self.b_ps[0]])
    self.op("act", lambda e: e.activation(out=bguT[:, :, 0:NX], in_=self.ps[0][:, 0:16 * NE].rearrange("p (j e) -> p j e", e=NE)[:, :, 0:NX],
                                          func=AF.Copy), r=[self.b_ps[0]], w=[b_c])
    self.barrier()
    self.sb_off = mark
    fxT = self.sb("fxT", [128, 16, 512], BF16)
    acc = self.sb("acc", [128, 4, D], F32)
    actT = self.sb("actT", [128, 8, 512], BF16)
    wdb = self.sb("wdb", [128, 8, D], BF16)
    wgb = [self.sb(f"wgb{i}", [128, 16, 2, 128], BF16) for i in range(3)]
    CB = [self.sb(f"CB{i}", [128, 512], F32) for i in range(2)]
    gS = [self.sb(f"gS{i}", [128, 512], F32) for i in range(2)]
    sg = [self.sb(f"sg{i}", [128, 512], F32) for i in range(2)]
    uS = [self.sb(f"uS{i}", [128, 512], F32) for i in range(2)]
    xt = self.sb("gxt", [128, D], F32)
    xn = self.sb("gxn", [128, D], F32)
    f32T = self.sb("gf32T", [128, 16, 128], F32)
    junk = self.sb("gjunk", [128, D], BF16)
    ss = self.sb("gss", [128, 4], F32)
    combT = self.sb("combT", [NE, 512], F32)
    lg = self.sb("lg", [128, NE], F32)
    ex = self.sb("ex", [128, NE], F32)
    msk = self.sb("msk", [128, NE], F32)
    mx8 = self.sb("mx8", [128, 8], F32)
    sm = self.sb("sm", [128, 4], F32)
    b_fxT, b_actT, b_wdb, b_xt, b_f32T, b_combT, b_r = (Buf() for _ in range(7))
    b_acc = [Buf() for _ in range(4)]
    b_wgb = [Buf() for _ in range(3)]
    b_CB, b_gS, b_sg, b_uS, b_sel = ([Buf(), Buf()] for _ in range(5))
    b_tmp = (Buf(), Buf())
    d_xt = self.ds()
    d_wd = self.ds()
    d_wg = [self.ds() for _ in range(3)]
    d_out = self.ds()
    tiles_all = list(range(0 if sl == 0 else 2, NT))
    groups = [tiles_all[i:i + 4] for i in range(0, len(tiles_all), 4)]
    cnt = 0
    dcnt = 0
    for tiles in groups:
        ntok = 128 * len(tiles)
        for j, t in enumerate(tiles):
            which = 1 if t < 2 else 0
            rows = slice(t * 128, (t + 1) * 128)
            self.dma("sp", xt[:], self.XRES[rows, :], r=[self.b_xres[t]], w=[b_xt], dsem=d_xt)
            self.norm_tile(xt, b_xt, which, self.S2, 48, f32T, b_f32T, fxT, b_fxT, j * 128, [4, 5, 6, 7], junk, ss, xn, b_tmp)
            for k in range(16):
                self.op("pe", lambda e, k=k: e.matmul(self.ps[0][:, 0:NE], lhsT=f32T[:, k, :], rhs=wr[:, k, :], start=(k == 0), stop=(k == 15)),
                        r=[b_f32T, b_c], w=[self.b_ps[0]])
            self.op("dve", lambda e: e.tensor_tensor(lg[:], self.ps[0][:, 0:NE], br[:], ALU.add), r=[self.b_ps[0], b_c], w=[b_r])
            self.op("dve", lambda e: e.max(out=mx8[:], in_=lg[:]), r=[b_r], w=[b_r])
            self.op("dve", lambda e: e.tensor_scalar(msk[:], lg[:], mx8[:, 3:4], None, ALU.is_ge), r=[b_r], w=[b_r])
            self.op("dve", lambda e: e.tensor_scalar(sm[:, 0:1], mx8[:, 0:1], -1.0, None, ALU.mult), r=[b_r], w=[b_r])
            self.op("act", lambda e: e.activation(out=ex[:], in_=lg[:], func=AF.Exp, bias=sm[:, 0:1]), r=[b_r], w=[b_r])
            self.op("dve", lambda e: e.tensor_tensor(ex[:], ex[:], msk[:], ALU.mult), r=[b_r], w=[b_r])
            self.op("dve", lambda e: e.tensor_reduce(sm[:, 1:2], ex[:], AX.X, ALU.add), r=[b_r], w=[b_r])
            self.op("dve", lambda e: e.reciprocal(sm[:, 2:3], sm[:, 1:2]), r=[b_r], w=[b_r])
            self.op("dve", lambda e: e.tensor_scalar(ex[:], ex[:], sm[:, 2:3], None, ALU.mult), r=[b_r], w=[b_r])
            self.op("pe", lambda e: e.transpose(self.ps[1][0:NE, 0:128], ex[:], self.identf[:]), r=[b_r, self.b_const], w=[self.b_ps[1]])
            self.op("act", lambda e, j=j: e.activation(out=combT[:, j * 128:(j + 1) * 128], in_=self.ps[1][0:NE, 0:128], func=AF.Copy),
                    r=[self.b_ps[1]], w=[b_combT])
        if f"COMB{l}" in self.debug and tiles is groups[0]:
            self.dbg(f"COMB{l}", combT[:], [NE, 512], F32, r=[b_combT])
        for j in range(len(tiles)):
            for nb in range(4):
                bk = 2 + (nb % 2)
                cols = slice(nb * 512, (nb + 1) * 512)
                self.op("pe", lambda e, j=j, bk=bk, cols=cols: e.matmul(self.ps[bk][:, 0:512], lhsT=combT[0:NX, j * 128:(j + 1) * 128],
                                                                        rhs=bd[0:NX, cols], start=True, stop=True),
                        r=[b_combT, b_c], w=[self.b_ps[bk]])
                self.op("act", lambda e, j=j, bk=bk, cols=cols: e.activation(out=acc[:, j, cols], in_=self.ps[bk][:, 0:512], func=AF.Copy),
                        r=[self.b_ps[bk]], w=[b_acc[j]])
        for ex_i in range(NX):
            si = ex_i % 2
            self.op("dve", lambda e, si=si, ex_i=ex_i: e.tensor_copy(selb[si][:], self.identf[0:NE, ex_i:ex_i + 1].to_broadcast([NE, 128])),
                    r=[self.b_const], w=[b_sel[si]])
            self.op("pe", lambda e, si=si, ntok=ntok: e.matmul(self.ps[7][:, 0:ntok], lhsT=selb[si][:], rhs=combT[:, 0:ntok], start=True, stop=True),
                    r=[b_sel[si], b_combT], w=[self.b_ps[7]])
            self.op("act", lambda e, si=si, ntok=ntok: e.activation(out=CB[si][:, 0:ntok], in_=self.ps[7][:, 0:ntok], func=AF.Copy),
                    r=[self.b_ps[7]], w=[b_CB[si]])
            self.dma("pool", wdb[:], self.w_down[l, ex_i].rearrange("(c p) n -> p c n", p=128), w=[b_wdb], dsem=d_wd)
            wgv = self.w_gate_up[l, ex_i].rearrange("(k p) n -> p k n", p=128)
            for fc in range(8):
                s = cnt % 3
                par = cnt % 2
                cnt += 1
                self.dma("pool", wgb[s][:, :, 0, :], wgv[:, :, fc * 128:(fc + 1) * 128], w=[b_wgb[s]], dsem=d_wg[s])
                self.dma("pool", wgb[s][:, :, 1, :], wgv[:, :, DFF + fc * 128:DFF + (fc + 1) * 128], w=[b_wgb[s]], dsem=d_wg[s])
                pg, pu = 2 * par, 2 * par + 1
                for gu, pb in ((0, pg), (1, pu)):
                    for k in range(16):
                        self.op("pe", lambda e, k=k, s=s, gu=gu, pb=pb, ntok=ntok: e.matmul(self.ps[pb][:, 0:ntok], lhsT=wgb[s][:, k, gu, :],
                                                                                         rhs=fxT[:, k, 0:ntok], start=(k == 0), stop=(k == 15)),
                                r=[b_wgb[s], b_fxT], w=[self.b_ps[pb]])
                self.op("dve", lambda e, par=par, pg=pg, fc=fc, ex_i=ex_i, ntok=ntok: e.tensor_scalar(
                    gS[par][:, 0:ntok], self.ps[pg][:, 0:ntok], bguT[:, fc, ex_i:ex_i + 1], 7.0, ALU.add, ALU.min),
                    r=[self.b_ps[pg], b_c], w=[b_gS[par]])
                self.op("act", lambda e, par=par, ntok=ntok: e.activation(out=sg[par][:, 0:ntok], in_=gS[par][:, 0:ntok], func=AF.Sigmoid, scale=1.702),
                        r=[b_gS[par]], w=[b_sg[par]])
                self.op("dve", lambda e, par=par, pu=pu, fc=fc, ex_i=ex_i, ntok=ntok: e.tensor_scalar(
                    uS[par][:, 0:ntok], self.ps[pu][:, 0:ntok], bguT[:, 8 + fc, ex_i:ex_i + 1], 7.0, ALU.add, ALU.min),
                    r=[self.b_ps[pu], b_c], w=[b_uS[par]])
                self.op("dve", lambda e, par=par, ntok=ntok: e.tensor_scalar(uS[par][:, 0:ntok], uS[par][:, 0:ntok], -7.0, 1.0, ALU.max, ALU.add),
                        r=[b_uS[par]], w=[b_uS[par]])
                self.op("dve", lambda e, par=par, ntok=ntok: e.tensor_tensor(gS[par][:, 0:ntok], gS[par][:, 0:ntok], sg[par][:, 0:ntok], ALU.mult),
                        r=[b_sg[par]], w=[b_gS[par]])
                self.op("dve", lambda e, par=par, si=si, ntok=ntok: e.tensor_tensor(gS[par][:, 0:ntok], gS[par][:, 0:ntok], CB[si][:, 0:ntok], ALU.mult),
                        r=[b_CB[si]], w=[b_gS[par]])
                self.op("dve", lambda e, par=par, fc=fc, ntok=ntok: e.tensor_tensor(actT[:, fc, 0:ntok], uS[par][:, 0:ntok], gS[par][:, 0:ntok], ALU.mult),
                        r=[b_uS[par], b_gS[par]], w=[b_actT])
            for j in range(len(tiles)):
                for nb in range(4):
                    bk = 4 + (dcnt % 3)
                    dcnt += 1
                    cols = slice(nb * 512, (nb + 1) * 512)
                    for fc in range(8):
                        self.op("pe", lambda e, j=j, fc=fc, bk=bk, cols=cols: e.matmul(self.ps[bk][:, 0:512], lhsT=actT[:, fc, j * 128:(j + 1) * 128],
                                                                                     rhs=wdb[:, fc, cols], start=(fc == 0), stop=(fc == 7)),
                                r=[b_actT, b_wdb], w=[self.b_ps[bk]])
                    self.op("dve", lambda e, j=j, bk=bk, cols=cols: e.tensor_tensor(acc[:, j, cols], self.ps[bk][:, 0:512], acc[:, j, cols], ALU.add),
                            r=[self.b_ps[bk]], w=[b_acc[j]])
        for j, t in enumerate(tiles):
            which = 1 if t < 2 else 0
            rows = slice(t * 128, (t + 1) * 128)
            self.dma("sp", xt[:], self.XRES[rows, :], r=[self.b_xres[t]], w=[b_xt], dsem=d_xt)
            self.op("dve", lambda e, j=j, which=which: e.tensor_tensor(acc[:, j, :], acc[:, j, :], G2[:, which, :], ALU.mult), r=[b_c], w=[b_acc[j]])
            self.op("pool", lambda e, j=j: e.tensor_tensor(acc[:, j, :], acc[:, j, :], xt[:], ALU.add), r=[b_xt], w=[b_acc[j]])
            if last:
                by = Buf()
                self.dma("sp", self.y_out[(t - 2) * 128:(t - 1) * 128, :], acc[:, j, :], r=[b_acc[j]], w=[by], dsem=d_out)
                self.final_reads.append(by)
            else:
                self.dma("sp", self.XRES[rows, :], acc[:, j, :], r=[b_acc[j]], w=[self.b_xres[t]], dsem=d_out)
    if not last:
        self.dbg(f"XOUT{l}", self.XRES, [TT, D], F32, r=self.b_xres)
    else:
        self.dbg(f"YOUT{l}", self.y_out, [2048, D], F32, r=self.final_reads)
    self.barrier()


Builder.phase_G = phase_G
```

```python
import numpy as np
import ml_dtypes
import concourse.bass as bass
import concourse.mybir as mybir
from concourse.bass_utils import run_bass_kernel_spmd

F32 = mybir.dt.float32
BF16 = mybir.dt.bfloat16
AF = mybir.ActivationFunctionType
ALU = mybir.AluOpType
AX = mybir.AxisListType

D = 2048
NT = 18
TT = NT * 128
DIN = 4624
EPS = 1e-6
NE = 32
DFF = 1024
CAP = 768
KBIG = 40000.0
I32 = mybir.dt.int32


class Buf:
    __slots__ = ("name", "last_w", "readers")

    def __init__(self, name=""):
        self.name = name
        self.last_w = None
        self.readers = []


class DSem:
    __slots__ = ("sem", "count")

    def __init__(self, sem):
        self.sem = sem
        self.count = 0


class Op:
    __slots__ = ("eng", "emit", "deps", "signal", "signum", "dsem", "dval", "waits", "idx")


ENGS = ("pe", "act", "dve", "pool", "sp")


class Prog:
    def __init__(self):
        self.streams = {e: [] for e in ENGS}
        self.nops = 0
        self.dsems = []

    def new_dsem(self, sem):
        d = DSem(sem)
        self.dsems.append(d)
        return d

    def op(self, eng, emit, reads=(), writes=(), dsem=None, extra_dsem_waits=()):
        o = Op()
        o.eng = eng
        o.emit = emit
        o.signal = False
        o.signum = None
        o.dsem = dsem
        o.dval = None
        o.idx = self.nops
        self.nops += 1
        deps = []
        for b in reads:
            if b.last_w is not None:
                deps.append(b.last_w)
        for b in writes:
            if b.last_w is not None:
                deps.append(b.last_w)
            deps.extend(b.readers)
        seen = set()
        o.deps = []
        for d in deps:
            if id(d) in seen or d is o:
                continue
            seen.add(id(d))
            if d.dsem is not None:
                o.deps.append(("d", d.dsem, d.dsem.count))
            else:
                if d.eng == "pe" and eng == "pe" and dsem is None:
                    continue
                d.signal = True
                o.deps.append(("e", d))
        for ds in extra_dsem_waits:
            if ds.count > 0:
                o.deps.append(("d", ds, ds.count))
        if dsem is not None:
            dsem.count += 16
            o.dval = dsem.count
        for b in reads:
            b.readers.append(o)
        for b in writes:
            b.last_w = o
            b.readers = []
        self.streams[eng].append(o)
        return o

    def finalize(self):
        for e in ENGS:
            n = 0
            for o in self.streams[e]:
                if o.dsem is None and o.signal:
                    n += 1
                    o.signum = n
        for e in ENGS:
            known = {}
            for o in self.streams[e]:
                w = {}
                for d in o.deps:
                    if d[0] == "d":
                        key = ("d", id(d[1]))
                        val = d[2]
                        sem = d[1].sem
                    else:
                        key = ("e", d[1].eng)
                        val = d[1].signum
                        sem = d[1].eng
                    if known.get(key, 0) >= val:
                        continue
                    if key not in w or w[key][1] < val:
                        w[key] = (sem, val)
                for key, (sem, val) in w.items():
                    known[key] = val
                o.waits = list(w.values())

    def emit(self, block, esems):
        def run(engname):
            def f(eng):
                for o in self.streams[engname]:
                    for sem, val in o.waits:
                        s = esems[sem] if isinstance(sem, str) else sem
                        eng.wait_ge(s, val)
                    ins = o.emit(eng)
                    if ins is None:
                        continue
                    if o.dsem is not None:
                        ins.then_inc(o.dsem.sem, 16)
                    elif o.signal:
                        ins.then_inc(esems[engname], 1)
            return f
        block.tensor(run("pe"))
        block.scalar(run("act"))
        block.vector(run("dve"))
        block.gpsimd(run("pool"))
        block.sync(run("sp"))


def _consts():
    c = {}
    c["ident_f"] = np.eye(128, dtype=np.float32)
    c["ident_b"] = np.eye(128, dtype=np.float32).astype(ml_dtypes.bfloat16)
    a = np.arange(128)
    c["mask_prev"] = (a[None, :] <= a[:, None]).astype(np.float32).astype(ml_dtypes.bfloat16)
    c["mask_next"] = (a[:, None] <= a[None, :]).astype(np.float32).astype(ml_dtypes.bfloat16)
    c["ones_f"] = np.ones((128, 128), np.float32)
    sel = np.zeros((32, 32, 128), np.float32)
    for e in range(32):
        sel[e, e, :] = 1.0
    c["sel32"] = sel.transpose(1, 0, 2).copy()
    e0 = np.zeros((2, 2, 128), np.float32)
    e0[0, 0, :] = 1.0
    e0[1, 1, :] = 1.0
    c["sel2"] = e0
    rows = 2048 // 64
    row = np.repeat(np.arange(rows, dtype=np.float32), 64)
    col = np.tile(np.arange(64, dtype=np.float32), rows)
    half = 32
    inv = (10000.0 ** (-np.arange(0, half, 2, dtype=np.float32) / half)).astype(np.float32)

    def tab(p):
        ang = p[:, None] * inv[None, :]
        ang = np.concatenate([ang, ang], -1)
        return np.cos(ang).astype(np.float32), np.sin(ang).astype(np.float32)
    cr, sr = tab(row)
    cc, sc = tab(col)
    cos = np.concatenate([cr, cc], -1)
    sgn = np.concatenate([-np.ones(16), np.ones(16)]).astype(np.float32)
    sins = np.concatenate([sr * sgn, sc * sgn], -1)
    c["rope_cos"] = cos.reshape(16, 128, 64).transpose(1, 0, 2).copy()
    c["rope_sin"] = sins.reshape(16, 128, 64).transpose(1, 0, 2).copy()
    c["ml_mask_f"] = (a[:, None] <= a[None, :]).astype(np.float32).astype(ml_dtypes.bfloat16)
    c["ml_mask_b"] = (a[:, None] >= a[None, :]).astype(np.float32).astype(ml_dtypes.bfloat16)
    c["tri_s"] = (a[:, None] < a[None, :]).astype(np.float32)
    c["trash"] = (NE * CAP + np.arange(128, dtype=np.float32)).reshape(128, 1)
    c["ecst"] = np.tile((np.arange(32, dtype=np.float32) * CAP)[None, :], (128, 1))
    c["tri_f"] = (a[:, None] <= a[None, :]).astype(np.float32)
    c["tri_b"] = (a[:, None] >= a[None, :]).astype(np.float32)
    sl = np.zeros((128, 128), np.float32)
    sl[127, :] = 1.0
    c["sel_last"] = sl
    sf = np.zeros((128, 128), np.float32)
    sf[0, :] = 1.0
    c["sel_first"] = sf
    return c


from contextlib import ExitStack


class Builder:
    def __init__(self, nc, stack, upto="all", debug=(), n_exp=NE, layers=2, dev=False):
        self.nc = nc
        self.stack = stack
        self.P = Prog()
        self.upto = upto
        self.debug = set(debug)
        self.n_exp = n_exp
        self.layers = layers
        self.dev = dev
        self.L = 1 if dev else 2
        self.sb_off = 16512
        self.nsem = 0
        self.dpool = []
        self.dnext = 0
        self.dbg_names = []
        self.need_moe = upto == "all" or upto.startswith("G")

    def sb(self, name, shape, dtype):
        esz = 2 if dtype == BF16 else 4
        n = 1
        for s in shape[1:]:
            n *= s
        nbytes = (n * esz + 31) // 32 * 32
        t = self.nc.alloc_sbuf_tensor_at(f"{name}_{self.sb_off}", list(shape), dtype, offset=self.sb_off)
        self.sb_off += nbytes
        assert self.sb_off <= 16512 + 212000, (name, self.sb_off)
        return t

    def sem(self, name):
        self.nsem += 1
        return self.stack.enter_context(self.nc.semaphore(name))

    def ds(self):
        if self.dnext >= len(self.dpool):
            self.dpool.append(self.P.new_dsem(self.sem(f"d{len(self.dpool)}")))
        d = self.dpool[self.dnext]
        self.dnext += 1
        return d

    def dram(self, name, shape, dtype, kind="Internal"):
        return self.nc.dram_tensor(name, list(shape), dtype, kind=kind).ap()

    def op(self, eng, fn, r=(), w=(), dsem=None):
        return self.P.op(eng, fn, reads=r, writes=w, dsem=dsem)

    def dma(self, q, out, in_, r=(), w=(), dsem=None, **kw):
        assert dsem is not None
        return self.P.op(q, lambda e: e.dma_start(out=out, in_=in_, **kw), reads=r, writes=w, dsem=dsem)

    def barrier(self):
        P = self.P
        toks = []
        for e in ("act", "dve", "pool"):
            b = Buf("tok_" + e)
            scr = self.scr[e]
            if e == "act":
                P.op(e, lambda en, s=scr: en.activation(out=s[0:1, 0:2], in_=s[0:1, 0:2], func=AF.Identity), writes=[b])
            else:
                P.op(e, lambda en, s=scr: en.memset(s[0:1, 0:2], 0.0), writes=[b])
            toks.append(b)
        for e in ENGS:
            P.op(e, lambda en: None, reads=toks, extra_dsem_waits=list(P.dsems))
        self.dnext = 0

    def dbg(self, name, ap, shape, dtype, r=()):
        if name not in self.debug:
            return
        o = self.dram("dbg_" + name, shape, dtype, kind="ExternalOutput")
        self.dbg_names.append(name)
        d = self.P.new_dsem(self.sem("dbg_" + name))
        bo = Buf("dbg_" + name)
        self.dma("sp", o, ap, r=r, w=[bo], dsem=d)
        self.final_reads.append(bo)

    def setup(self):
        nc = self.nc
        self.esems = {e: self.sem("s_" + e) for e in ("pe", "act", "dve", "pool")}
        self.final_reads = []
        self.in_names = []

        def di(n, s, dt=F32):
            self.in_names.append(n)
            return self.dram(n, s, dt, kind="ExternalInput")
        self.x_in = di("x_in", [TT, D])
        self.c2 = di("c2", [32, 128])
        if not self.dev:
            self.w_ada = di("w_ada", [self.L, D, 6 * D])
        else:
            self.dev_mod = di("dev_mod", [2, 6 * D])
        self.b_ada = di("b_ada", [self.L, 6 * D])
        self.norm1_w = di("norm1_w", [self.L, D])
        self.norm2_w = di("norm2_w", [self.L, D])
        self.w_in = di("w_in", [self.L, D, DIN])
        self.b_gates = di("b_gates", [self.L, 16])
        self.q_norm_w = di("q_norm_w", [self.L, 64])
        self.k_norm_w = di("k_norm_w", [self.L, 64])
        self.attn_sink = di("attn_sink", [self.L, 16])
        self.mlstm_norm_w = di("mlstm_norm_w", [self.L, 1024])
        self.w_out = di("w_out", [self.L, D, D])
        self.w_router = di("w_router", [self.L, D, NE])
        self.b_router = di("b_router", [self.L, NE])
        if self.need_moe:
            self.w_gate_up = di("w_gate_up", [self.L, self.n_exp, D, 2 * DFF])
            self.b_gate_up = di("b_gate_up", [self.L, self.n_exp, 2 * DFF])
            self.w_down = di("w_down", [self.L, self.n_exp, DFF, D])
            self.b_down = di("b_down", [self.L, self.n_exp, D])
        self.cst = {}
        for k, v in _consts().items():
            self.cst[k] = di("cst_" + k, list(v.shape), BF16 if v.dtype != np.float32 else F32)
        self.y_out = self.dram("y", [2048, D], F32, kind="ExternalOutput")
        self.XRES = self.dram("xres", [TT, D], F32)
        self.PX = self.dram("px", [TT, 4608], BF16)
        self.GD = self.dram("gd", [TT, 16], F32)
        self.MQT = self.dram("mqt", [4, 128, TT], BF16)
        self.MKT = self.dram("mkt", [4, 128, TT], BF16)
        self.MODD = self.dram("modd", [2, 6 * D], F32)
        self.CATD = self.dram("catd", [TT, D], BF16)
        self.FXE = self.dram("fxe", [NE * CAP + 128, D], BF16)
        self.YE = self.dram("ye", [NE * CAP + 128, D], F32)
        self.b_fxe = Buf("fxe")
        self.b_ye = Buf("ye")
        self.b_xres = [Buf(f"xres{t}") for t in range(NT)]
        self.b_px = [Buf(f"px{t}") for t in range(NT)]
        self.b_gd = [Buf(f"gd{t}") for t in range(NT)]
        self.b_mqt = Buf("mqt")
        self.b_mkt = Buf("mkt")
        self.b_modd = Buf("modd")
        self.b_catd = [Buf(f"catd{t}") for t in range(NT)]
        self.ps = [nc.alloc_psum_tensor(f"psb{i}", [128, 512], F32) for i in range(8)]
        self.b_ps = [Buf(f"ps{i}") for i in range(8)]
        self.identf = self.sb("identf", [128, 128], F32)
        self.identb = self.sb("identb", [128, 128], BF16)
        self.onesf = self.sb("onesf", [128, 128], F32)
        self.scr = {e: self.sb("scr_" + e, [128, 8], F32) for e in ("act", "dve", "pool")}
        self.modT = self.sb("modT", [128, 96, 2], F32)
        self.S1 = self.sb("S1", [128, 16, 2], F32)
        self.S2 = self.sb("S2", [128, 16, 2], F32)
        self.b_modT = Buf("modT")
        self.b_S = Buf("S12")
        self.b_const = Buf("const")
        dc = self.P.new_dsem(self.sem("dconst"))
        self.dconst = dc
        self.dma("sp", self.identf[:], self.cst["ident_f"], w=[self.b_const], dsem=dc)
        self.dma("sp", self.identb[:], self.cst["ident_b"], w=[self.b_const], dsem=dc)
        self.dma("sp", self.onesf[:], self.cst["ones_f"], w=[self.b_const], dsem=dc)
        self.persist_off = self.sb_off

    def phase_reset(self):
        self.sb_off = self.persist_off

    def phase_A(self, l):
        nc, P = self.nc, self.P
        self.phase_reset()
        vec = self.sb("vec", [64, 128], F32)
        silu2 = self.sb("silu2", [128, 16, 2], F32)
        nwT = self.sb("nwT", [128, 2, 16], F32)
        bada = self.sb("bada", [2, 6 * D], F32)
        modsb = self.sb("modsb", [2, 6 * D], F32)
        sel2 = self.sb("sel2", [2, 2, 128], F32)
        stage = [self.sb(f"wst{i}", [128, 16, 512], F32) for i in range(2)]
        b_vec, b_silu, b_nwT, b_bada, b_modsb = Buf(), Buf(), Buf(), Buf(), Buf()
        b_stage = [Buf(), Buf()]
        d_stage = [self.ds(), self.ds()]
        d0 = self.ds()
        d1 = self.ds()
        self.dma("sp", vec[0:32, :], self.c2, w=[b_vec], dsem=d0)
        self.dma("sp", vec[32:48, :], self.norm1_w[l].rearrange("(k p) -> k p", p=128), w=[b_vec], dsem=d0)
        self.dma("sp", vec[48:64, :], self.norm2_w[l].rearrange("(k p) -> k p", p=128), w=[b_vec], dsem=d0)
        self.dma("sp", bada[0:1, :], self.b_ada[l:l + 1, :], w=[b_bada], dsem=d0)
        self.dma("sp", bada[1:2, :], self.b_ada[l:l + 1, :], w=[b_bada], dsem=d0)
        self.dma("sp", sel2[:], self.cst["sel2"], w=[b_bada], dsem=d0)
        pv = self.ps[7]
        bpv = self.b_ps[7]
        self.op("pe", lambda e: e.transpose(pv[:, 0:64], vec[0:64, :], self.identf[0:64, 0:64]),
                r=[b_vec, self.b_const], w=[bpv])
        self.op("act", lambda e: e.activation(out=silu2[:, :, 0], in_=pv[:, 0:16], func=AF.Silu), r=[bpv], w=[b_silu])
        self.op("act", lambda e: e.activation(out=silu2[:, :, 1], in_=pv[:, 16:32], func=AF.Silu), r=[bpv], w=[b_silu])
        self.op("dve", lambda e: e.tensor_copy(nwT[:].rearrange("p a b -> p (a b)"), pv[:, 32:64]), r=[bpv], w=[b_nwT])
        if self.dev:
            self.dma("sp", modsb[0:2, :], self.dev_mod, w=[b_modsb], dsem=d0)
        wv = None if self.dev else self.w_ada[l].rearrange("(k p) n -> p k n", p=128)
        for n in range(0 if self.dev else 24):
            s = n % 2
            self.dma("sp", stage[s][:], wv[:, :, n * 512:(n + 1) * 512], w=[b_stage[s]], dsem=d_stage[s])
            pm = self.ps[n % 2]
            bpm = self.b_ps[n % 2]
            for k in range(16):
                self.op("pe", lambda e, k=k, s=s, pm=pm: e.matmul(pm[0:2, :], lhsT=silu2[:, k, :], rhs=stage[s][:, k, :],
                                                                  start=(k == 0), stop=(k == 15)),
                        r=[b_silu, b_stage[s]], w=[bpm])
            self.op("dve", lambda e, n=n, pm=pm: e.tensor_tensor(modsb[0:2, n * 512:(n + 1) * 512], pm[0:2, :],
                                                                 bada[0:2, n * 512:(n + 1) * 512], ALU.add),
                    r=[bpm, b_bada], w=[b_modsb])
        self.dma("sp", self.MODD, modsb[0:2, :], r=[b_modsb], w=[self.b_modd], dsem=d1)
        pt = self.ps[2]
        for j in range(96):
            self.op("pe", lambda e, j=j: e.transpose(pt[:, 2 * j:2 * j + 2], modsb[0:2, j * 128:(j + 1) * 128],
                                                      self.identf[0:2, 0:2]),
                    r=[b_modsb, self.b_const], w=[self.b_ps[2]])
        self.op("act", lambda e: e.activation(out=self.modT[:].rearrange("p a b -> p (a b)"), in_=pt[:, 0:192],
                                              func=AF.Identity), r=[self.b_ps[2]], w=[self.b_modT])
        for (S, sc0, wi) in ((self.S1, 16, 0), (self.S2, 64, 1)):
            self.op("dve", lambda e, S=S, sc0=sc0: e.tensor_scalar(S[:], self.modT[:, sc0:sc0 + 16, :], 1.0, None, ALU.add),
                    r=[self.b_modT], w=[self.b_S])
            self.op("dve", lambda e, S=S, wi=wi: e.tensor_tensor(S[:], S[:], nwT[:, wi, :].unsqueeze(2).to_broadcast([128, 16, 2]),
                                                                 ALU.mult),
                    r=[b_nwT], w=[self.b_S])
        self.dbg(f"modT{l}", self.modT[:], [128, 96, 2], F32, r=[self.b_modT])
        self.dbg(f"S1_{l}", self.S1[:], [128, 16, 2], F32, r=[self.b_S])
        self.barrier()

    def norm_tile(self, xt, b_xt, which, S, sh0, f32T, b_f32T, dstT, b_dst, col0, banks, junk, ss, xn, b_tmp):
        P = self
        b_ss, b_xn = b_tmp
        self.op("act", lambda e: e.activation(out=junk[:], in_=xt[:], func=AF.Square, accum_out=ss[:, 0:1]),
                r=[b_xt], w=[b_ss])
        self.op("dve", lambda e: e.tensor_scalar(ss[:, 1:2], ss[:, 0:1], 1.0 / D, EPS, ALU.mult, ALU.add), r=[b_ss], w=[b_ss])
        self.op("act", lambda e: e.activation(out=ss[:, 3:4], in_=ss[:, 1:2], func=AF.Sqrt), r=[b_ss], w=[b_ss])
        self.op("dve", lambda e: e.reciprocal(ss[:, 2:3], ss[:, 3:4]), r=[b_ss], w=[b_ss])
        self.op("dve", lambda e: e.tensor_scalar(xn[:], xt[:], ss[:, 2:3], None, ALU.mult), r=[b_xt, b_ss], w=[b_xn])
        for c in range(16):
            bk = banks[c // 4]
            self.op("pe", lambda e, c=c, bk=bk: e.transpose(self.ps[bk][:, (c % 4) * 128:(c % 4 + 1) * 128],
                                                            xn[:, c * 128:(c + 1) * 128], self.identf[:]),
                    r=[b_xn, self.b_const], w=[self.b_ps[bk]])
        for c in range(16):
            bk = banks[c // 4]
            src = self.ps[bk][:, (c % 4) * 128:(c % 4 + 1) * 128]
            if c % 2 == 0:
                self.op("act", lambda e, c=c, src=src: e.activation(out=f32T[:, c, :], in_=src, func=AF.Identity,
                                                                    scale=S[:, c, which:which + 1],
                                                                    bias=self.modT[:, sh0 + c, which:which + 1]),
                        r=[self.b_ps[bk], self.b_S, self.b_modT], w=[b_f32T])
            else:
                self.op("dve", lambda e, c=c, src=src: e.tensor_scalar(f32T[:, c, :], src, S[:, c, which:which + 1],
                                                                       self.modT[:, sh0 + c, which:which + 1],
                                                                       ALU.mult, ALU.add),
                        r=[self.b_ps[bk], self.b_S, self.b_modT], w=[b_f32T])
        self.op("pool", lambda e: e.tensor_copy(dstT[:, :, col0:col0 + 128], f32T[:]), r=[b_f32T], w=[b_dst])

    def phase_BC(self, l):
        self.phase_reset()
        src = self.x_in if l == 0 else self.XRES
        hT = self.sb("hT", [128, 16, TT], BF16)
        b_hT = Buf("hT")
        xt = [self.sb(f"xt{i}", [128, D], F32) for i in range(2)]
        xn = [self.sb(f"xn{i}", [128, D], F32) for i in range(2)]
        f32T = [self.sb(f"f32T{i}", [128, 16, 128], F32) for i in range(2)]
        junk = self.sb("junk", [128, D], BF16)
        ss = [self.sb(f"ss{i}", [128, 4], F32) for i in range(2)]
        b_xt = [Buf(), Buf()]
        b_f = [Buf(), Buf()]
        b_tmp = [(Buf(), Buf()), (Buf(), Buf())]
        d_xt = [self.ds(), self.ds()]
        mark = self.sb_off
        for t in range(NT):
            i = t % 2
            which = 1 if t < 2 else 0
            rd = [self.b_xres[t]] if l > 0 else []
            self.dma("sp", xt[i][:], src[t * 128:(t + 1) * 128, :], r=rd, w=[b_xt[i]], dsem=d_xt[i])
            self.norm_tile(xt[i], b_xt[i], which, self.S1, 0, f32T[i], b_f[i], hT, b_hT, t * 128,
                           [4 * i + j for j in range(4)], junk, ss[i], xn[i], b_tmp[i])
        self.dbg(f"hT{l}", hT[:], [128, 16, TT], BF16, r=[b_hT])
        if self.upto == f"B{l}":
            return
        wv = self.w_in[l].rearrange("(k p) n -> p k n", p=128)
        CW = 256
        stg = [self.sb(f"pst{i}", [128, 16, CW], F32) for i in range(2)]
        wbf = [self.sb(f"pwb{i}", [128, 16, CW], BF16) for i in range(2)]
        osb = [self.sb(f"posb{i}", [128, 512], BF16) for i in range(3)]
        gsb = self.sb("pgsb", [128, NT, 16], F32)
        b_stg = [Buf(), Buf()]
        b_wbf = [Buf(), Buf()]
        b_osb = [Buf(), Buf(), Buf()]
        b_gsb = Buf()
        d_stg = [self.ds(), self.ds()]
        d_osb = [self.ds(), self.ds(), self.ds()]
        d_g = self.ds()
        nblk = 4608 // CW
        cnt = 0
        ocnt = 0
        def load_blk(cb):
            s = cb % 2
            c0 = cb * CW
            cw = CW if cb < nblk else 16
            self.dma("sp", stg[s][:, :, 0:cw], wv[:, :, c0:c0 + cw], w=[b_stg[s]], dsem=d_stg[s])
        load_blk(0)
        for cb in range(nblk + 1):
            s = cb % 2
            c0 = cb * CW
            cw = CW if cb < nblk else 16
            if cb + 1 <= nblk:
                load_blk(cb + 1)
            self.op("pool", lambda e, s=s, cw=cw: e.tensor_copy(wbf[s][:, :, 0:cw], stg[s][:, :, 0:cw]),
                    r=[b_stg[s]], w=[b_wbf[s]])
            fm_head = None
            if 1536 <= c0 < 2560:
                fm_head = (c0 - 1536) // 128
            for t in range(NT):
                bk = cnt % 4
                cnt += 1
                pt = self.ps[bk]
                for k in range(16):
                    self.op("pe", lambda e, k=k, t=t, s=s, cw=cw, pt=pt: e.matmul(pt[:, 0:cw], lhsT=hT[:, k, t * 128:(t + 1) * 128],
                                                                                   rhs=wbf[s][:, k, 0:cw], start=(k == 0), stop=(k == 15)),
                            r=[b_hT, b_wbf[s]], w=[self.b_ps[bk]])
                if cb < nblk:
                    o = ocnt % 3
                    ocnt += 1
                    scale = (128.0 ** -0.5) if 2048 <= c0 < 2560 else 1.0
                    if t % 2 == 0:
                        self.op("act", lambda e, o=o, pt=pt, scale=scale: e.activation(out=osb[o][:, 0:CW], in_=pt[:, 0:CW],
                                                                                        func=AF.Copy, scale=scale),
                                r=[self.b_ps[bk]], w=[b_osb[o]])
                    else:
                        self.op("dve", lambda e, o=o, pt=pt, scale=scale: e.tensor_scalar(osb[o][:, 0:CW], pt[:, 0:CW], scale, None, ALU.mult),
                                r=[self.b_ps[bk]], w=[b_osb[o]])
                    self.dma("sp", self.PX[t * 128:(t + 1) * 128, c0:c0 + CW], osb[o][:, 0:CW], r=[b_osb[o]], w=[self.b_px[t]],
                             dsem=d_osb[o])
                else:
                    self.op("dve", lambda e, t=t, pt=pt: e.tensor_copy(gsb[:, t, :], pt[:, 0:16]), r=[self.b_ps[bk]], w=[b_gsb])
            if fm_head is not None:
                for hh in range(CW // 128):
                    head = fm_head + hh
                    dst = self.MQT if head < 4 else self.MKT
                    bdst = self.b_mqt if head < 4 else self.b_mkt
                    scale = 1.0 if head < 4 else (128.0 ** -0.5)
                    for tb in range(6):
                        bk = 4 + (cnt % 4)
                        cnt += 1
                        pt = self.ps[bk]
                        for k in range(16):
                            self.op("pe", lambda e, k=k, tb=tb, s=s, hh=hh, pt=pt: e.matmul(
                                pt[:, 0:384], lhsT=wbf[s][:, k, hh * 128:(hh + 1) * 128], rhs=hT[:, k, tb * 384:(tb + 1) * 384],
                                start=(k == 0), stop=(k == 15)), r=[b_hT, b_wbf[s]], w=[self.b_ps[bk]])
                        o = ocnt % 3
                        ocnt += 1
                        self.op("act", lambda e, o=o, pt=pt, scale=scale: e.activation(out=osb[o][:, 0:384], in_=pt[:, 0:384],
                                                                                        func=AF.Copy, scale=scale),
                                r=[self.b_ps[bk]], w=[b_osb[o]])
                        self.dma("sp", dst[head % 4, :, tb * 384:(tb + 1) * 384], osb[o][:, 0:384], r=[b_osb[o]], w=[bdst],
                                 dsem=d_osb[o])
        self.dma("sp", self.GD.rearrange("(t p) g -> p t g", p=128), gsb[:], r=[b_gsb], w=self.b_gd, dsem=d_g)
        self.dbg(f"PX{l}", self.PX, [TT, 4608], BF16, r=self.b_px)
        self.dbg(f"GD{l}", self.GD, [TT, 16], F32, r=self.b_gd)
        self.dbg(f"MQT{l}", self.MQT, [4, 128, TT], BF16, r=[self.b_mqt])
        self.dbg(f"MKT{l}", self.MKT, [4, 128, TT], BF16, r=[self.b_mkt])
        self.barrier()


def build_program(upto="all", debug=(), n_exp=NE, dev=False, dev_layer=0):
    nc = bass.Bass("TRN2", target_bir_lowering=False)
    stack = ExitStack()
    B = Builder(nc, stack, upto=upto, debug=debug, n_exp=n_exp, dev=dev)
    B.setup()
    B.last_layer = 1
    for l in ([0] if dev else range(2)):
        B.cur_layer = dev_layer if dev else l
        B.phase_A(l)
        if upto == f"A{l}":
            break
        B.phase_BC(l)
        if upto in (f"B{l}", f"C{l}"):
            break
        B.phase_D(l)
        if upto in (f"D{l}", f"D1_{l}"):
            break
        B.phase_E(l)
        if upto in (f"E{l}", f"E1_{l}"):
            break
        B.phase_F(l)
        if upto == f"F{l}":
            break
        B.phase_G2(l)
        if upto == f"G{l}":
            break
    B.op("sp", lambda e: None, r=B.final_reads)
    B.P.finalize()
    with nc.Block() as block:
        B.P.emit(block, B.esems)
    stack.close()
    return nc, B


def make_in_maps(inputs, n_exp=NE):
    cst = _consts()
    maps = []
    x = np.asarray(inputs["x"], np.float32)
    ctx = np.asarray(inputs["ctx"], np.float32)
    c = np.asarray(inputs["c"], np.float32)
    c_ctx = np.asarray(inputs["c_ctx"], np.float32)
    shared = {k: np.ascontiguousarray(np.asarray(inputs[k], np.float32)) for k in
              ("w_ada", "b_ada", "norm1_w", "norm2_w", "w_in", "b_gates", "q_norm_w", "k_norm_w", "attn_sink",
               "mlstm_norm_w", "w_out", "w_router", "b_router", "w_gate_up", "b_gate_up", "w_down", "b_down")}
    for b in range(8):
        m = dict(shared)
        m["x_in"] = np.concatenate([ctx[b], x[b]], axis=0)
        m["c2"] = np.concatenate([c[b].reshape(16, 128), c_ctx.reshape(16, 128)], axis=0)
        for k, v in cst.items():
            m["cst_" + k] = v
        maps.append(m)
    return maps


def kernel(**inputs):
    nc, B = build_program()
    maps = make_in_maps(inputs)
    maps = [{k: m[k] for k in B.in_names} for m in maps]
    res = run_bass_kernel_spmd(nc, maps, core_ids=list(range(8)))
    return np.stack([np.asarray(r["y"], np.float32) for r in res.results], axis=0)


def phase_D(self, l):
    sl = self.cur_layer
    self.phase_reset()
    qT = self.sb("qT", [128, 8, TT], BF16)
    kT2 = self.sb("kT2", [128, 4, 2, TT], BF16)
    vaug = self.sb("vaug", [128, NT, 4, 65], BF16)
    cos = self.sb("cos", [128, 16, 64], F32)
    sin = self.sb("sin", [128, 16, 64], F32)
    qw = self.sb("qw", [128, 64], F32)
    kw = self.sb("kw", [128, 64], F32)
    sinke = self.sb("sinke", [128, 16], F32)
    maskp = self.sb("maskp", [128, 128], BF16)
    maskn = self.sb("maskn", [128, 128], BF16)
    b_qT, b_kT2, b_vaug, b_c = Buf(), Buf(), Buf(), Buf()
    d0 = self.ds()
    self.dma("sp", cos[:], self.cst["rope_cos"], w=[b_c], dsem=d0)
    self.dma("sp", sin[:], self.cst["rope_sin"], w=[b_c], dsem=d0)
    self.dma("sp", qw[:], self.q_norm_w[l:l + 1, :].partition_broadcast(128), w=[b_c], dsem=d0)
    self.dma("sp", kw[:], self.k_norm_w[l:l + 1, :].partition_broadcast(128), w=[b_c], dsem=d0)
    self.dma("sp", sinke[:], self.attn_sink[l:l + 1, :].partition_broadcast(128), w=[b_c], dsem=d0)
    self.dma("sp", maskp[:], self.cst["mask_prev"], w=[b_c], dsem=d0)
    self.dma("sp", maskn[:], self.cst["mask_next"], w=[b_c], dsem=d0)
    b_c2 = Buf()
    self.op("act", lambda e: e.activation(out=qw[:], in_=qw[:], func=AF.Copy, scale=0.125), r=[b_c], w=[b_c2])
    self.op("act", lambda e: e.activation(out=sinke[:], in_=sinke[:], func=AF.Exp), r=[b_c], w=[b_c2])
    self.op("pool", lambda e: e.memset(vaug[:].rearrange("p t j d -> p (t j) d")[:, :, 64:65], 1.0), w=[b_vaug])
    slab = [self.sb(f"slab{i}", [128, 1536], BF16) for i in range(2)]
    t1 = [self.sb(f"dt1_{i}", [128, 20, 64], F32) for i in range(2)]
    t2 = [self.sb(f"dt2_{i}", [128, 20, 64], F32) for i in range(2)]
    t3 = [self.sb(f"dt3_{i}", [128, 20, 64], F32) for i in range(2)]
    qkr = [self.sb(f"qkr{i}", [128, 20, 64], BF16) for i in range(2)]
    k2 = [self.sb(f"k2_{i}", [128, 4, 2, 2, 64], BF16) for i in range(2)]
    st = [self.sb(f"dst_{i}", [128, 64], F32) for i in range(2)]
    b_slab, b_t1, b_t2, b_t3, b_qkr, b_k2, b_st = ([Buf(), Buf()] for _ in range(7))
    d_slab = [self.ds(), self.ds()]
    t_first = 0
    for i in range(2):
        self.op("pool", lambda e, i=i: e.memset(k2[i][:].rearrange("p j u h d -> p (j u h d)"), 0.0), w=[b_k2[i]])
    for t in range(t_first, NT):
        i = t % 2
        self.dma("sp", slab[i][:], self.PX[t * 128:(t + 1) * 128, 0:1536], r=[self.b_px[t]], w=[b_slab[i]], dsem=d_slab[i])
        qk = slab[i][:, 0:1280].rearrange("p (h d) -> p h d", d=64)
        self.op("dve", lambda e, i=i, qk=qk: e.tensor_tensor(t1[i][:], qk, qk, ALU.mult), r=[b_slab[i]], w=[b_t1[i]])
        self.op("dve", lambda e, i=i: e.tensor_reduce(st[i][:, 0:20], t1[i][:], AX.X, ALU.add), r=[b_t1[i]], w=[b_st[i]])
        self.op("dve", lambda e, i=i: e.tensor_scalar(st[i][:, 0:20], st[i][:, 0:20], 1.0 / 64, EPS, ALU.mult, ALU.add),
                r=[b_st[i]], w=[b_st[i]])
        self.op("act", lambda e, i=i: e.activation(out=st[i][:, 20:40], in_=st[i][:, 0:20], func=AF.Sqrt), r=[b_st[i]], w=[b_st[i]])
        self.op("dve", lambda e, i=i: e.reciprocal(st[i][:, 40:60], st[i][:, 20:40]), r=[b_st[i]], w=[b_st[i]])
        self.op("dve", lambda e, i=i, qk=qk: e.tensor_tensor(t1[i][:], qk, st[i][:, 40:60].unsqueeze(2).to_broadcast([128, 20, 64]),
                                                             ALU.mult), r=[b_slab[i], b_st[i]], w=[b_t1[i]])
        self.op("dve", lambda e, i=i: e.tensor_tensor(t1[i][:, 0:16, :], t1[i][:, 0:16, :],
                                                      qw[:].unsqueeze(1).to_broadcast([128, 16, 64]), ALU.mult),
                r=[b_c2], w=[b_t1[i]])
        self.op("dve", lambda e, i=i: e.tensor_tensor(t1[i][:, 16:20, :], t1[i][:, 16:20, :],
                                                      kw[:].unsqueeze(1).to_broadcast([128, 4, 64]), ALU.mult),
                r=[b_c], w=[b_t1[i]])
        if t >= 2:
            lt = t - 2
            self.op("pool", lambda e, i=i, lt=lt: e.tensor_tensor(t2[i][:], t1[i][:],
                                                                  cos[:, lt, :].unsqueeze(1).to_broadcast([128, 20, 64]), ALU.mult),
                    r=[b_t1[i], b_c], w=[b_t2[i]])
            v1 = t1[i][:].rearrange("p h (a s j) -> p h a s j", a=2, s=2)
            v3 = t3[i][:].rearrange("p h (a s j) -> p h a s j", a=2, s=2)
            sv = sin[:, lt, :].rearrange("p (a s j) -> p a s j", a=2, s=2)
            for s in range(2):
                self.op("dve", lambda e, s=s, v1=v1, v3=v3, sv=sv: e.tensor_tensor(
                    v3[:, :, :, s, :], v1[:, :, :, 1 - s, :], sv[:, :, s, :].unsqueeze(1).to_broadcast([128, 20, 2, 16]), ALU.mult),
                    r=[b_t1[i], b_c], w=[b_t3[i]])
            self.op("pool", lambda e, i=i: e.tensor_tensor(qkr[i][:], t2[i][:], t3[i][:], ALU.add),
                    r=[b_t2[i], b_t3[i]], w=[b_qkr[i]])
        else:
            self.op("pool", lambda e, i=i: e.tensor_copy(qkr[i][:], t1[i][:]), r=[b_t1[i]], w=[b_qkr[i]])
        for dup in range(2):
            self.op("pool", lambda e, i=i, dup=dup: e.tensor_copy(k2[i][:, :, dup, dup, :], qkr[i][:, 16:20, :]),
                    r=[b_qkr[i]], w=[b_k2[i]])
        self.op("pool", lambda e, i=i, t=t: e.tensor_copy(vaug[:, t, :, 0:64], slab[i][:, 1280:1536].rearrange("p (j d) -> p j d", d=64)),
                r=[b_slab[i]], w=[b_vaug])
        pq = self.ps[6][:].bitcast(BF16)
        pk = self.ps[7][:].bitcast(BF16)
        qflat = qkr[i][:].rearrange("p h d -> p (h d)")
        for pr in range(8):
            self.op("pe", lambda e, pr=pr, pq=pq, qflat=qflat: e.transpose(pq[:, pr * 128:(pr + 1) * 128], qflat[:, pr * 128:(pr + 1) * 128],
                                                                           self.identb[:]),
                    r=[b_qkr[i], self.b_const], w=[self.b_ps[6]])
        self.op("act", lambda e, t=t, pq=pq: e.activation(out=qT[:, :, t * 128:(t + 1) * 128], in_=pq.rearrange("p (a b) -> p a b", b=128),
                                                          func=AF.Copy), r=[self.b_ps[6]], w=[b_qT])
        kflat = k2[i][:].rearrange("p j u h d -> p (j u h d)")
        for j in range(8):
            self.op("pe", lambda e, j=j, pk=pk, kflat=kflat: e.transpose(pk[:, j * 128:(j + 1) * 128], kflat[:, j * 128:(j + 1) * 128],
                                                                         self.identb[:]),
                    r=[b_k2[i], self.b_const], w=[self.b_ps[7]])
        self.op("dve", lambda e, t=t, pk=pk: e.tensor_copy(kT2[:, :, :, t * 128:(t + 1) * 128], pk[:, 0:1024].rearrange("p (a u b) -> p a u b", u=2, b=128)),
                r=[self.b_ps[7]], w=[b_kT2])
    self.dbg(f"qT{l}", qT[:], [128, 8, TT], BF16, r=[b_qT])
    self.dbg(f"kT2{l}", kT2[:], [128, 4, 2, TT], BF16, r=[b_kT2])
    if self.upto == f"D1_{l}":
        return
    E = [self.sb(f"E{i}", [128, 5, 4, 128], BF16) for i in range(2)]
    att = [self.sb(f"att{i}", [128, 1024], BF16) for i in range(2)]
    den = [self.sb(f"den{i}", [128, 8], F32) for i in range(2)]
    b_E, b_att, b_den = [Buf(), Buf()], [Buf(), Buf()], [Buf(), Buf()]
    d_att = [self.ds(), self.ds()]
    sink_v = sinke[:].rearrange("q (j i p) -> q j p i", j=4, i=2, p=2)
    u = 0
    sc = 0
    tq0 = 0 if sl == 0 else 2
    for tq in range(tq0, NT):
        ai = tq % 2
        for j in range(4):
            ei = u % 2
            bo = 4 + (u % 2)
            u += 1
            if tq < 2:
                keys = [0, 1]
            else:
                keys = [0, 1] + [tk for tk in (tq - 1, tq, tq + 1) if 2 <= tk < NT]
            for ki, tk in enumerate(keys):
                bk = sc % 4
                sc += 1
                for p in range(2):
                    self.op("pe", lambda e, p=p, bk=bk, tk=tk, tq=tq, j=j: e.matmul(
                        self.ps[bk][:, p * 256:(p + 1) * 256], lhsT=kT2[:, j, p, tk * 128:(tk + 1) * 128],
                        rhs=qT[:, 2 * j:2 * j + 2, tq * 128:(tq + 1) * 128], start=True, stop=True),
                        r=[b_qT, b_kT2], w=[self.b_ps[bk]])
                self.op("act", lambda e, ei=ei, ki=ki, bk=bk: e.activation(out=E[ei][:, ki, :, :].rearrange("p a b -> p (a b)"),
                                                                            in_=self.ps[bk][:], func=AF.Exp),
                        r=[self.b_ps[bk]], w=[b_E[ei]])
                if tq >= 2 and tk >= 2 and tk != tq:
                    mk = maskp if tk == tq - 1 else maskn
                    self.op("pool", lambda e, ei=ei, ki=ki, mk=mk: e.tensor_tensor(
                        E[ei][:, ki, :, :], E[ei][:, ki, :, :], mk[:].unsqueeze(1).to_broadcast([128, 4, 128]), ALU.mult),
                        r=[b_c], w=[b_E[ei]])
            nk = len(keys)
            for slot in range(4):
                for ki, tk in enumerate(keys):
                    self.op("pe", lambda e, slot=slot, ki=ki, tk=tk, ei=ei, bo=bo, j=j, nk=nk: e.matmul(
                        self.ps[bo][:, slot * 128:slot * 128 + 65], lhsT=E[ei][:, ki, slot, :], rhs=vaug[:, tk, j, :],
                        start=(ki == 0), stop=(ki == nk - 1)), r=[b_E[ei], b_vaug], w=[self.b_ps[bo]])
            ov = self.ps[bo][:].rearrange("q (s d) -> q s d", d=128)
            self.op("dve", lambda e, ai=ai, ov=ov, j=j: e.tensor_tensor(
                den[ai][:, 0:4].rearrange("q (p i) -> q p i", p=2), ov[:, :, 64:65].rearrange("q (p i) o -> q p (i o)", p=2),
                sink_v[:, j], ALU.add), r=[self.b_ps[bo], b_c2], w=[b_den[ai]])
            self.op("dve", lambda e, ai=ai: e.reciprocal(den[ai][:, 4:8], den[ai][:, 0:4]), r=[b_den[ai]], w=[b_den[ai]])
            self.op("dve", lambda e, ai=ai, ov=ov, j=j: e.tensor_tensor(
                att[ai][:, j * 256:(j + 1) * 256].rearrange("q (i p d) -> q p i d", i=2, p=2),
                ov[:, :, 0:64].rearrange("q (p i) d -> q p i d", p=2),
                den[ai][:, 4:8].rearrange("q (p i) -> q p i", p=2).unsqueeze(3).to_broadcast([128, 2, 2, 64]), ALU.mult),
                r=[self.b_ps[bo], b_den[ai]], w=[b_att[ai]])
        self.dma("sp", self.CATD[tq * 128:(tq + 1) * 128, 0:1024], att[ai][:], r=[b_att[ai]], w=[self.b_catd[tq]], dsem=d_att[ai])
    self.dbg(f"ATT{l}", self.CATD, [TT, D], BF16, r=self.b_catd)
    self.barrier()


Builder.phase_D = phase_D


def phase_E(self, l):
    sl = self.cur_layer
    self.phase_reset()
    HS = self.sb("HS", [128, NT, 1024], F32)
    after_hs = self.sb_off
    mqT = self.sb("mqT", [128, 4, TT], BF16)
    mkT = self.sb("mkT", [128, 4, TT], BF16)
    mk = self.sb("mk", [128, NT, 512], BF16)
    vaug = self.sb("mvaug", [128, NT, 4, 257], BF16)
    G = self.sb("G", [128, NT, 16], F32)
    GI = self.sb("GI", [128, NT, 16], F32)
    E1 = self.sb("E1", [128, NT, 16], F32)
    LF = self.sb("LF", [128, NT, 16], F32)
    A = self.sb("Acol", [128, NT, 8], F32)
    bg = self.sb("bg", [128, 16], F32)
    tri = [self.sb("trif", [128, 128], F32), self.sb("trib", [128, 128], F32)]
    CT = self.sb("CT", [128, 8, 257], F32)
    CTb = self.sb("CTb", [128, 8, 257], BF16)
    LFB = [self.sb(f"LFB{i}", [128, 128], F32) for i in range(2)]
    DT = [self.sb(f"DT{i}", [128, 128], F32) for i in range(2)]
    EB = [self.sb(f"EB{i}", [128, 128], F32) for i in range(2)]
    DTm = [self.sb(f"DTm{i}", [128, 128], F32) for i in range(2)]
    WT = [self.sb(f"WT{i}", [128, 128], BF16) for i in range(2)]
    qTs = [self.sb(f"qTs{i}", [128, 128], BF16) for i in range(2)]
    kws = [self.sb(f"kws{i}", [128, 128], BF16) for i in range(2)]
    dd = [self.sb(f"dd{i}", [128, 2], F32) for i in range(2)]
    b_in, b_g, b_A, b_v = Buf(), Buf(), Buf(), Buf()
    b_HS = [[Buf() for h in range(4)] for c in range(NT)]
    b_CT = [Buf() for _ in range(8)]
    b_CTb = [Buf() for _ in range(8)]
    b_LFB, b_DT, b_EB, b_DTm, b_WT, b_qTs, b_kws, b_dd = ([Buf(), Buf()] for _ in range(8))
    d0 = self.ds()
    d1 = self.ds()
    self.dma("sp", mqT[:], self.MQT.rearrange("h p t -> p h t"), r=[self.b_mqt], w=[b_in], dsem=d0)
    self.dma("sp", mkT[:], self.MKT.rearrange("h p t -> p h t"), r=[self.b_mkt], w=[b_in], dsem=d0)
    self.dma("sp", tri[0][:], self.cst["tri_f"], w=[b_in], dsem=d0)
    self.dma("sp", tri[1][:], self.cst["tri_b"], w=[b_in], dsem=d0)
    self.dma("sp", bg[:], self.b_gates[l:l + 1, :].partition_broadcast(128), w=[b_g], dsem=d0)
    self.dma("sp", G[:], self.GD.rearrange("(t p) g -> p t g", p=128), r=self.b_gd, w=[b_g], dsem=d0)
    self.op("pool", lambda e: e.memset(vaug[:].rearrange("p t h v -> p (t h) v")[:, :, 256:257], 1.0), w=[b_v])
    for t in range(NT):
        self.dma("sp", mk[:, t, :], self.PX[t * 128:(t + 1) * 128, 2048:2560], r=[self.b_px[t]], w=[b_in], dsem=d1)
        self.dma("sp", vaug[:, t, :, 0:256], self.PX[t * 128:(t + 1) * 128, 2560:3584].rearrange("p (h v) -> p h v", v=256),
                 r=[self.b_px[t]], w=[b_v], dsem=d1)
    self.op("pool", lambda e: e.memset(CT[:].rearrange("p a b -> p (a b)"), 0.0), w=b_CT)
    self.op("pool", lambda e: e.memset(CTb[:].rearrange("p a b -> p (a b)"), 0.0), w=b_CTb)
    Gf = G[:].rearrange("p t g -> p (t g)")
    GIf = GI[:].rearrange("p t g -> p (t g)")
    E1f = E1[:].rearrange("p t g -> p (t g)")
    LFf = LF[:].rearrange("p t g -> p (t g)")
    self.op("dve", lambda e: e.tensor_tensor(G[:], G[:], bg[:].unsqueeze(1).to_broadcast([128, NT, 16]), ALU.add), r=[b_g], w=[b_g])
    self.op("act", lambda e: e.activation(out=Gf, in_=Gf, func=AF.Tanh, scale=1.0 / 15.0), r=[b_g], w=[b_g])
    self.op("dve", lambda e: e.tensor_scalar(GIf, Gf, 15.0, None, ALU.mult), r=[b_g], w=[b_g])
    P1 = self.sb("P1", [128, NT * 16], F32)
    Y2 = self.sb("Y2", [128, NT * 16], F32)
    self.op("dve", lambda e: e.tensor_scalar(E1f, GIf, -1.0, None, ALU.mult), r=[b_g], w=[b_g])
    self.op("dve", lambda e: e.tensor_tensor(E1f, E1f, GIf, ALU.max), r=[b_g], w=[b_g])
    self.op("act", lambda e: e.activation(out=E1f, in_=E1f, func=AF.Exp, scale=-1.0), r=[b_g], w=[b_g])
    self.op("dve", lambda e: e.tensor_scalar(P1[:], E1f, 2.0, None, ALU.add), r=[b_g], w=[b_g])
    self.op("dve", lambda e: e.reciprocal(P1[:], P1[:]), r=[b_g], w=[b_g])
    self.op("dve", lambda e: e.tensor_tensor(E1f, E1f, P1[:], ALU.mult), r=[b_g], w=[b_g])
    self.op("dve", lambda e: e.tensor_tensor(Y2[:], E1f, E1f, ALU.mult), r=[b_g], w=[b_g])
    self.op("dve", lambda e: e.tensor_scalar(P1[:], Y2[:], 1.0 / 13.0, 1.0 / 11.0, ALU.mult, ALU.add), r=[b_g], w=[b_g])
    for cc in (1.0 / 9.0, 1.0 / 7.0, 1.0 / 5.0, 1.0 / 3.0, 1.0):
        self.op("dve", lambda e: e.tensor_tensor(P1[:], P1[:], Y2[:], ALU.mult), r=[b_g], w=[b_g])
        self.op("dve", lambda e, cc=cc: e.tensor_scalar(P1[:], P1[:], cc, None, ALU.add), r=[b_g], w=[b_g])
    self.op("dve", lambda e: e.tensor_tensor(P1[:], P1[:], E1f, ALU.mult), r=[b_g], w=[b_g])
    self.op("dve", lambda e: e.tensor_scalar(E1f, GIf, 0.0, None, ALU.min), r=[b_g], w=[b_g])
    self.op("dve", lambda e: e.scalar_tensor_tensor(LFf, P1[:], -2.0, E1f, ALU.mult, ALU.add), r=[b_g], w=[b_g])
    for d in range(2):
        self.op("pe", lambda e, d=d: e.matmul(self.ps[d][:, 0:NT * 4], lhsT=tri[d][:], rhs=LF[:, :, 4 + 8 * d:8 + 8 * d],
                                              start=True, stop=True), r=[b_g, b_in], w=[self.b_ps[d]])
        self.op("dve", lambda e, d=d: e.tensor_tensor(A[:, :, 4 * d:4 * d + 4], GI[:, :, 8 * d:8 * d + 4],
                                                      self.ps[d][:, 0:NT * 4].rearrange("p (c h) -> p c h", h=4), ALU.subtract),
                r=[b_g, self.b_ps[d]], w=[b_A])
    order = [list(range(NT)), [1, 0] + list(range(NT - 1, 1, -1))]
    u = 0
    hs_written = set()
    for step in range(NT):
        for d in range(2):
            c = order[d][step]
            tl = 127 if d == 0 else 0
            tsl = slice(c * 128, (c + 1) * 128)
            need_h = not (sl == 1 and c < 2)
            for h in range(4):
                par = u % 2
                u += 1
                gf = 4 + 8 * d + h
                ch = d * 4 + h
                pa, pb, pc, pd = (4 * par + i for i in range(4))
                self.op("pool", lambda e, par=par, c=c, gf=gf: e.tensor_copy(LFB[par][:], LF[:, c, gf:gf + 1].to_broadcast([128, 128])),
                        r=[b_g], w=[b_LFB[par]])
                self.op("pe", lambda e, par=par, d=d, pb=pb: e.matmul(self.ps[pb][:, 0:128], lhsT=LFB[par][:], rhs=tri[d][:],
                                                                     start=True, stop=True),
                        r=[b_LFB[par], b_in], w=[self.b_ps[pb]])
                self.op("pe", lambda e, h=h, tsl=tsl, pa=pa: e.matmul(self.ps[pa][:, 0:128], lhsT=mkT[:, h, tsl], rhs=mqT[:, h, tsl],
                                                                     start=True, stop=True),
                        r=[b_in], w=[self.b_ps[pa]])
                self.op("act", lambda e, par=par, pb=pb, c=c, ch=ch: e.activation(out=DT[par][:], in_=self.ps[pb][:, 0:128], func=AF.Exp,
                                                                                 bias=A[:, c, ch:ch + 1]),
                        r=[self.b_ps[pb], b_A], w=[b_DT[par]])
                self.op("act", lambda e, par=par, pb=pb: e.activation(out=EB[par][:], in_=self.ps[pb][:, 0:128], func=AF.Exp),
                        r=[self.b_ps[pb]], w=[b_EB[par]])
                self.op("pool", lambda e, par=par, d=d: e.tensor_tensor(DTm[par][:], DT[par][:], tri[d][:], ALU.mult),
                        r=[b_DT[par], b_in], w=[b_DTm[par]])
                self.op("dve", lambda e, par=par, pa=pa: e.tensor_tensor(WT[par][:], self.ps[pa][:, 0:128], DTm[par][:], ALU.mult),
                        r=[self.b_ps[pa], b_DTm[par]], w=[b_WT[par]])
                self.op("dve", lambda e, par=par, h=h, tsl=tsl: e.tensor_tensor(qTs[par][:], mqT[:, h, tsl], EB[par][:], ALU.mult),
                        r=[b_in, b_EB[par]], w=[b_qTs[par]])
                self.op("pool", lambda e, par=par, c=c, h=h, tl=tl: e.tensor_scalar(kws[par][:], mk[:, c, h * 128:(h + 1) * 128],
                                                                                   DTm[par][:, tl:tl + 1], None, ALU.mult),
                        r=[b_in, b_DTm[par]], w=[b_kws[par]])
                if need_h:
                    self.op("pe", lambda e, par=par, c=c, h=h, pc=pc: e.matmul(self.ps[pc][:, 0:257], lhsT=WT[par][:], rhs=vaug[:, c, h, :],
                                                                              start=True, stop=False),
                            r=[b_WT[par], b_v], w=[self.b_ps[pc]])
                    self.op("pe", lambda e, par=par, ch=ch, pc=pc: e.matmul(self.ps[pc][:, 0:257], lhsT=qTs[par][:], rhs=CTb[:, ch, :],
                                                                           start=False, stop=True),
                            r=[b_qTs[par], b_CTb[ch]], w=[self.b_ps[pc]])
                    self.op("dve", lambda e, par=par, pc=pc: e.tensor_scalar(dd[par][:, 0:1], self.ps[pc][:, 256:257], -1.0, None, ALU.mult),
                            r=[self.b_ps[pc]], w=[b_dd[par]])
                    self.op("dve", lambda e, par=par, pc=pc: e.scalar_tensor_tensor(dd[par][:, 0:1], self.ps[pc][:, 256:257], 1.0, dd[par][:, 0:1],
                                                                                   ALU.max, ALU.max),
                            r=[self.b_ps[pc], b_dd[par]], w=[b_dd[par]])
                    self.op("dve", lambda e, par=par: e.reciprocal(dd[par][:, 1:2], dd[par][:, 0:1]), r=[b_dd[par]], w=[b_dd[par]])
                    hs = HS[:, c, h * 256:(h + 1) * 256]
                    first = (c, h) not in hs_written
                    hs_written.add((c, h))
                    if first:
                        self.op("act", lambda e, par=par, pc=pc, hs=hs: e.activation(out=hs, in_=self.ps[pc][:, 0:256], func=AF.Identity,
                                                                                    scale=dd[par][:, 1:2]),
                                r=[self.b_ps[pc], b_dd[par]], w=[b_HS[c][h]])
                    else:
                        self.op("dve", lambda e, par=par, pc=pc, hs=hs: e.scalar_tensor_tensor(hs, self.ps[pc][:, 0:256], dd[par][:, 1:2], hs,
                                                                                              ALU.mult, ALU.add),
                                r=[self.b_ps[pc], b_dd[par]], w=[b_HS[c][h]])
                if step < NT - 1:
                    self.op("pe", lambda e, par=par, c=c, h=h, pd=pd: e.matmul(self.ps[pd][:, 0:257], lhsT=kws[par][:], rhs=vaug[:, c, h, :],
                                                                              start=True, stop=True),
                            r=[b_kws[par], b_v], w=[self.b_ps[pd]])
                    self.op("dve", lambda e, par=par, ch=ch, pd=pd, tl=tl: e.scalar_tensor_tensor(CT[:, ch, :], CT[:, ch, :], EB[par][:, tl:tl + 1],
                                                                                                 self.ps[pd][:, 0:257], ALU.mult, ALU.add),
                            r=[self.b_ps[pd], b_EB[par]], w=[b_CT[ch]])
                    self.op("act", lambda e, ch=ch: e.activation(out=CTb[:, ch, :], in_=CT[:, ch, :], func=AF.Copy),
                            r=[b_CT[ch]], w=[b_CTb[ch]])
    t0 = 0 if sl == 0 else 2
    self.dbg(f"HS{l}", HS[:], [128, NT, 1024], F32, r=[b for c in range(NT) for b in b_HS[c]])
    if self.upto == f"E1_{l}":
        return
    self.barrier()
    self.sb_off = after_hs
    nw = self.sb("nw", [128, 1024], F32)
    mo = [self.sb(f"mo{i}", [128, 1024], BF16) for i in range(2)]
    t1 = [self.sb(f"et1_{i}", [128, 1024], F32) for i in range(2)]
    sg = [self.sb(f"esg{i}", [128, 1024], F32) for i in range(2)]
    ob = [self.sb(f"eob{i}", [128, 1024], BF16) for i in range(2)]
    junk = self.sb("ejunk", [128, 256], BF16)
    ss = [self.sb(f"ess{i}", [128, 16], F32) for i in range(2)]
    b_nw, b_junk = Buf(), Buf()
    b_mo, b_t1, b_sg, b_ob, b_ss = ([Buf(), Buf()] for _ in range(5))
    dn = self.ds()
    d_mo = [self.ds(), self.ds()]
    d_ob = [self.ds(), self.ds()]
    self.dma("sp", nw[:], self.mlstm_norm_w[l:l + 1, :].partition_broadcast(128), w=[b_nw], dsem=dn)
    for c in range(t0, NT):
        i = c % 2
        rhs_all = b_HS[c]
        self.dma("sp", mo[i][:], self.PX[c * 128:(c + 1) * 128, 3584:4608], r=[self.b_px[c]], w=[b_mo[i]], dsem=d_mo[i])
        for h in range(4):
            self.op("act", lambda e, i=i, c=c, h=h: e.activation(out=junk[:], in_=HS[:, c, h * 256:(h + 1) * 256], func=AF.Square,
                                                                 accum_out=ss[i][:, h:h + 1]),
                    r=[b_HS[c][h]], w=[b_ss[i], b_junk])
        self.op("dve", lambda e, i=i: e.tensor_scalar(ss[i][:, 4:8], ss[i][:, 0:4], 1.0 / 256, EPS, ALU.mult, ALU.add), r=[b_ss[i]], w=[b_ss[i]])
        self.op("act", lambda e, i=i: e.activation(out=ss[i][:, 8:12], in_=ss[i][:, 4:8], func=AF.Sqrt), r=[b_ss[i]], w=[b_ss[i]])
        self.op("dve", lambda e, i=i: e.reciprocal(ss[i][:, 12:16], ss[i][:, 8:12]), r=[b_ss[i]], w=[b_ss[i]])
        self.op("dve", lambda e, i=i, c=c: e.tensor_tensor(t1[i][:].rearrange("p (h v) -> p h v", v=256),
                                                           HS[:, c, :].rearrange("p (h v) -> p h v", v=256),
                                                           ss[i][:, 12:16].unsqueeze(2).to_broadcast([128, 4, 256]), ALU.mult),
                r=rhs_all + [b_ss[i]], w=[b_t1[i]])
        self.op("pool", lambda e, i=i: e.tensor_tensor(t1[i][:], t1[i][:], nw[:], ALU.mult), r=[b_nw], w=[b_t1[i]])
        self.op("act", lambda e, i=i: e.activation(out=sg[i][:], in_=mo[i][:], func=AF.Sigmoid), r=[b_mo[i]], w=[b_sg[i]])
        self.op("dve", lambda e, i=i: e.tensor_tensor(ob[i][:], t1[i][:], sg[i][:], ALU.mult), r=[b_t1[i], b_sg[i]], w=[b_ob[i]])
        self.dma("sp", self.CATD[c * 128:(c + 1) * 128, 1024:2048], ob[i][:], r=[b_ob[i]], w=[self.b_catd[c]], dsem=d_ob[i])
    self.dbg(f"CAT{l}", self.CATD, [TT, D], BF16, r=self.b_catd)
    self.barrier()


Builder.phase_E = phase_E


def phase_F(self, l):
    sl = self.cur_layer
    self.phase_reset()
    src = self.x_in if l == 0 else self.XRES
    wout = self.sb("wout", [128, 16, D], BF16)
    G1 = self.sb("G1", [128, 2, D], F32)
    cat = [self.sb(f"fcat{i}", [128, D], BF16) for i in range(2)]
    xt = [self.sb(f"fxt{i}", [128, D], F32) for i in range(2)]
    catT = [self.sb(f"fcatT{i}", [128, 16, 128], BF16) for i in range(2)]
    xo = [self.sb(f"fxo{i}", [128, D], F32) for i in range(2)]
    b_wout, b_G1 = Buf(), Buf()
    b_cat, b_xt, b_catT, b_xo = ([Buf(), Buf()] for _ in range(4))
    dw, dg = self.ds(), self.ds()
    d_cat, d_xt, d_xo = ([self.ds(), self.ds()] for _ in range(3))
    wv = self.w_out[l].rearrange("(k p) n -> p k n", p=128)
    for k0 in range(0, 16, 4):
        self.dma("pool", wout[:, k0:k0 + 4, :], wv[:, k0:k0 + 4, :], w=[b_wout], dsem=dw)
    for w_ in range(2):
        self.dma("sp", G1[:, w_, :], self.MODD[w_:w_ + 1, 2 * D:3 * D].partition_broadcast(128), r=[self.b_modd], w=[b_G1], dsem=dg)
    t0 = 0 if sl == 0 else 2
    for t in range(t0, NT):
        i = t % 2
        which = 1 if t < 2 else 0
        rows = slice(t * 128, (t + 1) * 128)
        self.dma("sp", cat[i][:], self.CATD[rows, :], r=[self.b_catd[t]], w=[b_cat[i]], dsem=d_cat[i])
        self.dma("sp", xt[i][:], src[rows, :], r=([self.b_xres[t]] if l > 0 else []), w=[b_xt[i]], dsem=d_xt[i])
        bA, bB = 4 + 2 * i, 5 + 2 * i
        pA = self.ps[bA][:].bitcast(BF16)
        pB = self.ps[bB][:].bitcast(BF16)
        for k in range(16):
            pX, bX = (pA, bA) if k < 8 else (pB, bB)
            self.op("pe", lambda e, k=k, pX=pX, i=i: e.transpose(pX[:, (k % 8) * 128:(k % 8 + 1) * 128], cat[i][:, k * 128:(k + 1) * 128],
                                                                 self.identb[:]),
                    r=[b_cat[i], self.b_const], w=[self.b_ps[bX]])
        self.op("act", lambda e, i=i, pA=pA: e.activation(out=catT[i][:, 0:8, :], in_=pA.rearrange("p (a b) -> p a b", b=128), func=AF.Copy),
                r=[self.b_ps[bA]], w=[b_catT[i]])
        self.op("dve", lambda e, i=i, pB=pB: e.tensor_copy(catT[i][:, 8:16, :], pB.rearrange("p (a b) -> p a b", b=128)),
                r=[self.b_ps[bB]], w=[b_catT[i]])
        for nb in range(4):
            cols = slice(nb * 512, (nb + 1) * 512)
            for k in range(16):
                self.op("pe", lambda e, k=k, nb=nb, i=i, cols=cols: e.matmul(self.ps[nb][:, 0:512], lhsT=catT[i][:, k, :], rhs=wout[:, k, cols],
                                                                            start=(k == 0), stop=(k == 15)),
                        r=[b_catT[i], b_wout], w=[self.b_ps[nb]])
            self.op("dve", lambda e, nb=nb, i=i, cols=cols, which=which: e.tensor_tensor(xo[i][:, cols], self.ps[nb][:, 0:512], G1[:, which, cols], ALU.mult),
                    r=[self.b_ps[nb], b_G1], w=[b_xo[i]])
            self.op("pool", lambda e, i=i, cols=cols: e.tensor_tensor(xo[i][:, cols], xo[i][:, cols], xt[i][:, cols], ALU.add),
                    r=[b_xt[i]], w=[b_xo[i]])
        self.dma("sp", self.XRES[rows, :], xo[i][:], r=[b_xo[i]], w=[self.b_xres[t]], dsem=d_xo[i])
    self.dbg(f"XMID{l}", self.XRES, [TT, D], F32, r=self.b_xres)
    self.barrier()


Builder.phase_F = phase_F


def phase_G(self, l):
    sl = self.cur_layer
    last = (sl == self.last_layer)
    self.phase_reset()
    NX = self.n_exp
    wr = self.sb("wr", [128, 16, NE], F32)
    br = self.sb("br", [128, NE], F32)
    bguT = self.sb("bguT", [128, 16, NE], F32)
    bd = self.sb("bd", [NE, D], F32)
    G2 = self.sb("G2", [128, 2, D], F32)
    selb = [self.sb(f"selb{i}", [NE, 128], F32) for i in range(2)]
    mark = self.sb_off
    braw = self.sb("braw", [NE, D], F32)
    b_c, b_braw = Buf(), Buf()
    dc = self.ds()
    self.dma("sp", wr[:], self.w_router[l].rearrange("(k p) e -> p k e", p=128), w=[b_c], dsem=dc)
    self.dma("sp", br[:], self.b_router[l:l + 1, :].partition_broadcast(128), w=[b_c], dsem=dc)
    self.dma("sp", bd[0:NX, :], self.b_down[l], w=[b_c], dsem=dc)
    self.dma("sp", braw[0:NX, :], self.b_gate_up[l], w=[b_braw], dsem=dc)
    for w_ in range(2):
        self.dma("sp", G2[:, w_, :], self.MODD[w_:w_ + 1, 5 * D:6 * D].partition_broadcast(128), r=[self.b_modd], w=[b_c], dsem=dc)
    for j in range(16):
        self.op("pe", lambda e, j=j: e.transpose(self.ps[0][:, j * NE:j * NE + NX], braw[0:NX, j * 128:(j + 1) * 128],
                                                 self.identf[0:NX, 0:NX]),
                r=[b_braw, self.b_const], w=[self.b_ps[0]])
    self.op("act", lambda e: e.activation(out=bguT[:, :, 0:NX], in_=self.ps[0][:, 0:16 * NE].rearrange("p (j e) -> p j e", e=NE)[:, :, 0:NX],
                                          func=AF.Copy), r=[self.b_ps[0]], w=[b_c])
    self.barrier()
    self.sb_off = mark
    fxT = self.sb("fxT", [128, 16, 512], BF16)
    acc = self.sb("acc", [128, 4, D], F32)
    actT = self.sb("actT", [128, 8, 512], BF16)
    wdb = self.sb("wdb", [128, 8, D], BF16)
    wgb = [self.sb(f"wgb{i}", [128, 16, 2, 128], BF16) for i in range(3)]
    CB = [self.sb(f"CB{i}", [128, 512], F32) for i in range(2)]
    gS = [self.sb(f"gS{i}", [128, 512], F32) for i in range(2)]
    sg = [self.sb(f"sg{i}", [128, 512], F32) for i in range(2)]
    uS = [self.sb(f"uS{i}", [128, 512], F32) for i in range(2)]
    xt = self.sb("gxt", [128, D], F32)
    xn = self.sb("gxn", [128, D], F32)
    f32T = self.sb("gf32T", [128, 16, 128], F32)
    junk = self.sb("gjunk", [128, D], BF16)
    ss = self.sb("gss", [128, 4], F32)
    combT = self.sb("combT", [NE, 512], F32)
    lg = self.sb("lg", [128, NE], F32)
    ex = self.sb("ex", [128, NE], F32)
    msk = self.sb("msk", [128, NE], F32)
    mx8 = self.sb("mx8", [128, 8], F32)
    sm = self.sb("sm", [128, 4], F32)
    b_fxT, b_actT, b_wdb, b_xt, b_f32T, b_combT, b_r = (Buf() for _ in range(7))
    b_acc = [Buf() for _ in range(4)]
    b_wgb = [Buf() for _ in range(3)]
    b_CB, b_gS, b_sg, b_uS, b_sel = ([Buf(), Buf()] for _ in range(5))
    b_tmp = (Buf(), Buf())
    d_xt = self.ds()
    d_wd = self.ds()
    d_wg = [self.ds() for _ in range(3)]
    d_out = self.ds()
    tiles_all = list(range(0 if sl == 0 else 2, NT))
    groups = [tiles_all[i:i + 4] for i in range(0, len(tiles_all), 4)]
    cnt = 0
    dcnt = 0
    for tiles in groups:
        ntok = 128 * len(tiles)
        for j, t in enumerate(tiles):
            which = 1 if t < 2 else 0
            rows = slice(t * 128, (t + 1) * 128)
            self.dma("sp", xt[:], self.XRES[rows, :], r=[self.b_xres[t]], w=[b_xt], dsem=d_xt)
            self.norm_tile(xt, b_xt, which, self.S2, 48, f32T, b_f32T, fxT, b_fxT, j * 128, [4, 5, 6, 7], junk, ss, xn, b_tmp)
            for k in range(16):
                self.op("pe", lambda e, k=k: e.matmul(self.ps[0][:, 0:NE], lhsT=f32T[:, k, :], rhs=wr[:, k, :], start=(k == 0), stop=(k == 15)),
                        r=[b_f32T, b_c], w=[self.b_ps[0]])
            self.op("dve", lambda e: e.tensor_tensor(lg[:], self.ps[0][:, 0:NE], br[:], ALU.add), r=[self.b_ps[0], b_c], w=[b_r])
            self.op("dve", lambda e: e.max(out=mx8[:], in_=lg[:]), r=[b_r], w=[b_r])
            self.op("dve", lambda e: e.tensor_scalar(msk[:], lg[:], mx8[:, 3:4], None, ALU.is_ge), r=[b_r], w=[b_r])
            self.op("dve", lambda e: e.tensor_scalar(sm[:, 0:1], mx8[:, 0:1], -1.0, None, ALU.mult), r=[b_r], w=[b_r])
            self.op("act", lambda e: e.activation(out=ex[:], in_=lg[:], func=AF.Exp, bias=sm[:, 0:1]), r=[b_r], w=[b_r])
            self.op("dve", lambda e: e.tensor_tensor(ex[:], ex[:], msk[:], ALU.mult), r=[b_r], w=[b_r])
            self.op("dve", lambda e: e.tensor_reduce(sm[:, 1:2], ex[:], AX.X, ALU.add), r=[b_r], w=[b_r])
            self.op("dve", lambda e: e.reciprocal(sm[:, 2:3], sm[:, 1:2]), r=[b_r], w=[b_r])
            self.op("dve", lambda e: e.tensor_scalar(ex[:], ex[:], sm[:, 2:3], None, ALU.mult), r=[b_r], w=[b_r])
            self.op("pe", lambda e: e.transpose(self.ps[1][0:NE, 0:128], ex[:], self.identf[:]), r=[b_r, self.b_const], w=[self.b_ps[1]])
            self.op("act", lambda e, j=j: e.activation(out=combT[:, j * 128:(j + 1) * 128], in_=self.ps[1][0:NE, 0:128], func=AF.Copy),
                    r=[self.b_ps[1]], w=[b_combT])
        if f"COMB{l}" in self.debug and tiles is groups[0]:
            self.dbg(f"COMB{l}", combT[:], [NE, 512], F32, r=[b_combT])
        for j in range(len(tiles)):
            for nb in range(4):
                bk = 2 + (nb % 2)
                cols = slice(nb * 512, (nb + 1) * 512)
                self.op("pe", lambda e, j=j, bk=bk, cols=cols: e.matmul(self.ps[bk][:, 0:512], lhsT=combT[0:NX, j * 128:(j + 1) * 128],
                                                                        rhs=bd[0:NX, cols], start=True, stop=True),
                        r=[b_combT, b_c], w=[self.b_ps[bk]])
                self.op("act", lambda e, j=j, bk=bk, cols=cols: e.activation(out=acc[:, j, cols], in_=self.ps[bk][:, 0:512], func=AF.Copy),
                        r=[self.b_ps[bk]], w=[b_acc[j]])
        for ex_i in range(NX):
            si = ex_i % 2
            self.op("dve", lambda e, si=si, ex_i=ex_i: e.tensor_copy(selb[si][:], self.identf[0:NE, ex_i:ex_i + 1].to_broadcast([NE, 128])),
                    r=[self.b_const], w=[b_sel[si]])
            self.op("pe", lambda e, si=si, ntok=ntok: e.matmul(self.ps[7][:, 0:ntok], lhsT=selb[si][:], rhs=combT[:, 0:ntok], start=True, stop=True),
                    r=[b_sel[si], b_combT], w=[self.b_ps[7]])
            self.op("act", lambda e, si=si, ntok=ntok: e.activation(out=CB[si][:, 0:ntok], in_=self.ps[7][:, 0:ntok], func=AF.Copy),
                    r=[self.b_ps[7]], w=[b_CB[si]])
            self.dma("pool", wdb[:], self.w_down[l, ex_i].rearrange("(c p) n -> p c n", p=128), w=[b_wdb], dsem=d_wd)
            wgv = self.w_gate_up[l, ex_i].rearrange("(k p) n -> p k n", p=128)
            for fc in range(8):
                s = cnt % 3
                par = cnt % 2
                cnt += 1
                self.dma("pool", wgb[s][:, :, 0, :], wgv[:, :, fc * 128:(fc + 1) * 128], w=[b_wgb[s]], dsem=d_wg[s])
                self.dma("pool", wgb[s][:, :, 1, :], wgv[:, :, DFF + fc * 128:DFF + (fc + 1) * 128], w=[b_wgb[s]], dsem=d_wg[s])
                pg, pu = 2 * par, 2 * par + 1
                for gu, pb in ((0, pg), (1, pu)):
                    for k in range(16):
                        self.op("pe", lambda e, k=k, s=s, gu=gu, pb=pb, ntok=ntok: e.matmul(self.ps[pb][:, 0:ntok], lhsT=wgb[s][:, k, gu, :],
                                                                                         rhs=fxT[:, k, 0:ntok], start=(k == 0), stop=(k == 15)),
                                r=[b_wgb[s], b_fxT], w=[self.b_ps[pb]])
                self.op("dve", lambda e, par=par, pg=pg, fc=fc, ex_i=ex_i, ntok=ntok: e.tensor_scalar(
                    gS[par][:, 0:ntok], self.ps[pg][:, 0:ntok], bguT[:, fc, ex_i:ex_i + 1], 7.0, ALU.add, ALU.min),
                    r=[self.b_ps[pg], b_c], w=[b_gS[par]])
                self.op("act", lambda e, par=par, ntok=ntok: e.activation(out=sg[par][:, 0:ntok], in_=gS[par][:, 0:ntok], func=AF.Sigmoid, scale=1.702),
                        r=[b_gS[par]], w=[b_sg[par]])
                self.op("dve", lambda e, par=par, pu=pu, fc=fc, ex_i=ex_i, ntok=ntok: e.tensor_scalar(
                    uS[par][:, 0:ntok], self.ps[pu][:, 0:ntok], bguT[:, 8 + fc, ex_i:ex_i + 1], 7.0, ALU.add, ALU.min),
                    r=[self.b_ps[pu], b_c], w=[b_uS[par]])
                self.op("dve", lambda e, par=par, ntok=ntok: e.tensor_scalar(uS[par][:, 0:ntok], uS[par][:, 0:ntok], -7.0, 1.0, ALU.max, ALU.add),
                        r=[b_uS[par]], w=[b_uS[par]])
                self.op("dve", lambda e, par=par, ntok=ntok: e.tensor_tensor(gS[par][:, 0:ntok], gS[par][:, 0:ntok], sg[par][:, 0:ntok], ALU.mult),
                        r=[b_sg[par]], w=[b_gS[par]])
                self.op("dve", lambda e, par=par, si=si, ntok=ntok: e.tensor_tensor(gS[par][:, 0:ntok], gS[par][:, 0:ntok], CB[si][:, 0:ntok], ALU.mult),
                        r=[b_CB[si]], w=[b_gS[par]])
                self.op("dve", lambda e, par=par, fc=fc, ntok=ntok: e.tensor_tensor(actT[:, fc, 0:ntok], uS[par][:, 0:ntok], gS[par][:, 0:ntok], ALU.mult),
                        r=[b_uS[par], b_gS[par]], w=[b_actT])
            for j in range(len(tiles)):
                for nb in range(4):
                    bk = 4 + (dcnt % 3)
                    dcnt += 1
                    cols = slice(nb * 512, (nb + 1) * 512)
                    for fc in range(8):
                        self.op("pe", lambda e, j=j, fc=fc, bk=bk, cols=cols: e.matmul(self.ps[bk][:, 0:512], lhsT=actT[:, fc, j * 128:(j + 1) * 128],
                                                                                     rhs=wdb[:, fc, cols], start=(fc == 0), stop=(fc == 7)),
                                r=[b_actT, b_wdb], w=[self.b_ps[bk]])
                    self.op("dve", lambda e, j=j, bk=bk, cols=cols: e.tensor_tensor(acc[:, j, cols], self.ps[bk][:, 0:512], acc[:, j, cols], ALU.add),
                            r=[self.b_ps[bk]], w=[b_acc[j]])
        for j, t in enumerate(tiles):
            which = 1 if t < 2 else 0
            rows = slice(t * 128, (t + 1) * 128)
            self.dma("sp", xt[:], self.XRES[rows, :], r=[self.b_xres[t]], w=[b_xt], dsem=d_xt)
            self.op("dve", lambda e, j=j, which=which: e.tensor_tensor(acc[:, j, :], acc[:, j, :], G2[:, which, :], ALU.mult), r=[b_c], w=[b_acc[j]])
            self.op("pool", lambda e, j=j: e.tensor_tensor(acc[:, j, :], acc[:, j, :], xt[:], ALU.add), r=[b_xt], w=[b_acc[j]])
            if last:
                by = Buf()
                self.dma("sp", self.y_out[(t - 2) * 128:(t - 1) * 128, :], acc[:, j, :], r=[b_acc[j]], w=[by], dsem=d_out)
                self.final_reads.append(by)
            else:
                self.dma("sp", self.XRES[rows, :], acc[:, j, :], r=[b_acc[j]], w=[self.b_xres[t]], dsem=d_out)
    if not last:
        self.dbg(f"XOUT{l}", self.XRES, [TT, D], F32, r=self.b_xres)
    else:
        self.dbg(f"YOUT{l}", self.y_out, [2048, D], F32, r=self.final_reads)
    self.barrier()


Builder.phase_G = phase_G


def phase_G2(self, l):
    sl = self.cur_layer
    last = (sl == self.last_layer)
    self.phase_reset()
    NX = self.n_exp
    NR = NE * CAP
    wr = self.sb("wr", [128, 16, NE], F32)
    br = self.sb("br", [128, NE], F32)
    bguT = self.sb("bguT", [128, 16, NE], F32)
    bd = self.sb("bd", [NE, D], F32)
    G2 = self.sb("G2", [128, 2, D], F32)
    IDX = self.sb("IDX", [128, NT, 4], I32)
    WJ = self.sb("WJ", [128, NT, 4], F32)
    combT = self.sb("combT", [NE, TT], F32)
    OFF = self.sb("OFF", [128, NE], F32)
    ecst = self.sb("ecst", [128, NE], F32)
    tris = self.sb("tris", [128, 128], F32)
    trash = self.sb("trash", [128, 1], F32)
    mark = self.sb_off
    braw = self.sb("braw", [NE, D], F32)
    b_c, b_braw, b_idx, b_combT, b_off = Buf(), Buf(), Buf(), Buf(), Buf()
    dc = self.ds()
    self.dma("sp", wr[:], self.w_router[l].rearrange("(k p) e -> p k e", p=128), w=[b_c], dsem=dc)
    self.dma("sp", br[:], self.b_router[l:l + 1, :].partition_broadcast(128), w=[b_c], dsem=dc)
    self.dma("sp", bd[0:NX, :], self.b_down[l], w=[b_c], dsem=dc)
    self.dma("sp", braw[0:NX, :], self.b_gate_up[l], w=[b_braw], dsem=dc)
    self.dma("sp", ecst[:], self.cst["ecst"], w=[b_c], dsem=dc)
    self.dma("sp", tris[:], self.cst["tri_s"], w=[b_c], dsem=dc)
    self.dma("sp", trash[:], self.cst["trash"], w=[b_c], dsem=dc)
    for w_ in range(2):
        self.dma("sp", G2[:, w_, :], self.MODD[w_:w_ + 1, 5 * D:6 * D].partition_broadcast(128), r=[self.b_modd], w=[b_c], dsem=dc)
    for j in range(16):
        self.op("pe", lambda e, j=j: e.transpose(self.ps[0][:, j * NE:j * NE + NX], braw[0:NX, j * 128:(j + 1) * 128],
                                                 self.identf[0:NX, 0:NX]),
                r=[b_braw, self.b_const], w=[self.b_ps[0]])
    self.op("act", lambda e: e.activation(out=bguT[:, :, 0:NX], in_=self.ps[0][:, 0:16 * NE].rearrange("p (j e) -> p j e", e=NE)[:, :, 0:NX],
                                          func=AF.Copy), r=[self.b_ps[0]], w=[b_c])
    self.op("dve", lambda e: e.memset(OFF[:], 0.0), w=[b_off])
    if l == 0:
        zt = self.sb("zt", [128, D], BF16)
        b_zt = Buf()
        dzf = self.ds()
        self.op("dve", lambda e: e.memset(zt[:], 0.0), w=[b_zt])
        for r_ in range(0, NR + 128, 128):
            self.dma("sp", self.FXE[r_:r_ + 128, :], zt[:], r=[b_zt], w=[self.b_fxe], dsem=dzf)
    self.barrier()
    self.sb_off = mark
    tiles_all = list(range(0 if sl == 0 else 2, NT))
    xt = self.sb("gxt", [128, D], F32)
    xn = self.sb("gxn", [128, D], F32)
    f32T = self.sb("gf32T", [128, 16, 128], F32)
    junk = self.sb("gjunk", [128, D], BF16)
    dummy = self.sb("gdummy", [128, 16, 128], BF16)
    ss = self.sb("gss", [128, 4], F32)
    S2r = self.sb("S2r", [128, 2, D], F32)
    SHr = self.sb("SHr", [128, 2, D], F32)
    nwr = self.sb("nwr", [128, D], F32)
    ftok = [self.sb(f"ftok{i}", [128, D], BF16) for i in range(2)]
    lg = self.sb("lg", [128, NE], F32)
    ex = self.sb("ex", [128, NE], F32)
    msk = self.sb("msk", [128, NE], F32)
    rk = self.sb("rk", [128, NE], F32)
    key = self.sb("key", [128, NE], F32)
    tmpk = self.sb("tmpk", [128, NE], F32)
    mx8 = self.sb("mx8", [128, 8], F32)
    k8 = self.sb("k8", [128, 8], F32)
    sm = self.sb("sm", [128, 4], F32)
    b_xt, b_f32T, b_r, b_rows, b_dummy = (Buf() for _ in range(5))
    b_ftok = [Buf(), Buf()]
    b_tmp = (Buf(), Buf())
    d_xt, d_rows = self.ds(), self.ds()
    d_sc = [self.ds(), self.ds()]
    for w_ in range(2):
        self.dma("sp", S2r[:, w_, :], self.MODD[w_:w_ + 1, 4 * D:5 * D].partition_broadcast(128), r=[self.b_modd], w=[b_rows], dsem=d_rows)
        self.dma("sp", SHr[:, w_, :], self.MODD[w_:w_ + 1, 3 * D:4 * D].partition_broadcast(128), r=[self.b_modd], w=[b_rows], dsem=d_rows)
    self.dma("sp", nwr[:], self.norm2_w[l:l + 1, :].partition_broadcast(128), w=[b_rows], dsem=d_rows)
    for w_ in range(2):
        self.op("dve", lambda e, w_=w_: e.scalar_tensor_tensor(S2r[:, w_, :], S2r[:, w_, :], 1.0, nwr[:], ALU.add, ALU.mult), r=[b_rows], w=[b_rows])
    for t in tiles_all:
        which = 1 if t < 2 else 0
        fi = t % 2
        rows = slice(t * 128, (t + 1) * 128)
        self.dma("sp", xt[:], self.XRES[rows, :], r=[self.b_xres[t]], w=[b_xt], dsem=d_xt)
        self.norm_tile(xt, b_xt, which, self.S2, 48, f32T, b_f32T, dummy, b_dummy, 0, [4, 5, 6, 7], junk, ss, xn, b_tmp)
        self.op("pool", lambda e, which=which: e.tensor_tensor(xn[:], xn[:], S2r[:, which, :], ALU.mult), r=[b_rows], w=[b_tmp[1]])
        self.op("pool", lambda e, which=which, fi=fi: e.tensor_tensor(ftok[fi][:], xn[:], SHr[:, which, :], ALU.add), r=[b_rows, b_tmp[1]], w=[b_ftok[fi]])
        for k in range(16):
            self.op("pe", lambda e, k=k: e.matmul(self.ps[0][:, 0:NE], lhsT=f32T[:, k, :], rhs=wr[:, k, :], start=(k == 0), stop=(k == 15)),
                    r=[b_f32T, b_c], w=[self.b_ps[0]])
        self.op("dve", lambda e: e.tensor_tensor(lg[:], self.ps[0][:, 0:NE], br[:], ALU.add), r=[self.b_ps[0], b_c], w=[b_r])
        self.op("dve", lambda e: e.max(out=mx8[:], in_=lg[:]), r=[b_r], w=[b_r])
        self.op("dve", lambda e: e.tensor_scalar(msk[:], lg[:], mx8[:, 3:4], None, ALU.is_ge), r=[b_r], w=[b_r])
        self.op("dve", lambda e: e.tensor_scalar(sm[:, 0:1], mx8[:, 0:1], -1.0, None, ALU.mult), r=[b_r], w=[b_r])
        self.op("act", lambda e: e.activation(out=ex[:], in_=lg[:], func=AF.Exp, bias=sm[:, 0:1]), r=[b_r], w=[b_r])
        self.op("dve", lambda e: e.tensor_tensor(ex[:], ex[:], msk[:], ALU.mult), r=[b_r], w=[b_r])
        self.op("dve", lambda e: e.tensor_reduce(sm[:, 1:2], ex[:], AX.X, ALU.add), r=[b_r], w=[b_r])
        self.op("dve", lambda e: e.reciprocal(sm[:, 2:3], sm[:, 1:2]), r=[b_r], w=[b_r])
        self.op("dve", lambda e: e.tensor_scalar(ex[:], ex[:], sm[:, 2:3], None, ALU.mult), r=[b_r], w=[b_r])
        self.op("pe", lambda e: e.transpose(self.ps[1][0:NE, 0:128], ex[:], self.identf[:]), r=[b_r, self.b_const], w=[self.b_ps[1]])
        self.op("act", lambda e, t=t: e.activation(out=combT[:, t * 128:(t + 1) * 128], in_=self.ps[1][0:NE, 0:128], func=AF.Copy),
                r=[self.b_ps[1]], w=[b_combT])
        self.op("pe", lambda e: e.matmul(self.ps[2][:, 0:NE], lhsT=tris[:], rhs=msk[:], start=True, stop=True), r=[b_r, b_c], w=[self.b_ps[2]])
        self.op("pe", lambda e: e.matmul(self.ps[3][:, 0:NE], lhsT=self.onesf[:], rhs=msk[:], start=True, stop=True),
                r=[b_r, self.b_const], w=[self.b_ps[3]])
        self.op("dve", lambda e: e.tensor_tensor(rk[:], self.ps[2][:, 0:NE], OFF[:], ALU.add), r=[self.b_ps[2], b_off], w=[b_r])
        self.op("dve", lambda e: e.tensor_tensor(OFF[:], OFF[:], self.ps[3][:, 0:NE], ALU.add), r=[self.b_ps[3]], w=[b_off])
        self.op("dve", lambda e: e.tensor_scalar(tmpk[:], rk[:], float(CAP), None, ALU.is_lt), r=[b_r], w=[b_r])
        self.op("dve", lambda e: e.tensor_tensor(tmpk[:], tmpk[:], msk[:], ALU.mult), r=[b_r], w=[b_r])
        self.op("dve", lambda e: e.tensor_tensor(rk[:], rk[:], ecst[:], ALU.add), r=[b_r, b_c], w=[b_r])
        self.op("dve", lambda e: e.tensor_scalar(rk[:], rk[:], -1.0, KBIG, ALU.mult, ALU.add), r=[b_r], w=[b_r])
        self.op("dve", lambda e: e.tensor_tensor(key[:], rk[:], tmpk[:], ALU.mult), r=[b_r], w=[b_r])
        self.op("dve", lambda e: e.max(out=k8[:], in_=key[:]), r=[b_r], w=[b_r])
        self.op("dve", lambda e: e.tensor_scalar(sm[:, 0:4], k8[:, 0:4], -1.0, KBIG, ALU.mult, ALU.add), r=[b_r], w=[b_r])
        self.op("dve", lambda e: e.tensor_scalar(sm[:, 0:4], sm[:, 0:4], trash[:, 0:1], None, ALU.subtract), r=[b_r, b_c], w=[b_r])
        self.op("dve", lambda e: e.tensor_scalar(k8[:, 4:8], k8[:, 0:4], 0.0, None, ALU.is_gt), r=[b_r], w=[b_r])
        self.op("dve", lambda e: e.tensor_tensor(sm[:, 0:4], sm[:, 0:4], k8[:, 4:8], ALU.mult), r=[b_r], w=[b_r])
        self.op("dve", lambda e: e.tensor_scalar(sm[:, 0:4], sm[:, 0:4], trash[:, 0:1], None, ALU.add), r=[b_r, b_c], w=[b_r])
        self.op("dve", lambda e, t=t: e.tensor_copy(IDX[:, t, :], sm[:, 0:4]), r=[b_r], w=[b_idx])
        for j in range(4):
            self.op("dve", lambda e, j=j: e.tensor_scalar(tmpk[:], key[:], k8[:, j:j + 1], None, ALU.is_equal), r=[b_r], w=[b_r])
            self.op("dve", lambda e: e.tensor_tensor(tmpk[:], tmpk[:], ex[:], ALU.mult), r=[b_r], w=[b_r])
            self.op("dve", lambda e, j=j, t=t: e.tensor_reduce(WJ[:, t, j:j + 1], tmpk[:], AX.X, ALU.add), r=[b_r], w=[b_idx])
        for j in range(4):
            self.P.op("pool", lambda e, t=t, j=j, fi=fi: e.indirect_dma_start(
                out=self.FXE[:, :], out_offset=bass.IndirectOffsetOnAxis(ap=IDX[:, t, j:j + 1], axis=0),
                in_=ftok[fi][:, :], in_offset=None),
                reads=[b_idx, b_ftok[fi]], writes=[self.b_fxe], dsem=d_sc[fi])
    self.dbg(f"IDX{l}", IDX[:], [128, NT, 4], I32, r=[b_idx])
    self.dbg(f"WJ{l}", WJ[:], [128, NT, 4], F32, r=[b_idx])
    self.barrier()
    self.sb_off = mark
    fxT = self.sb("fxT", [128, 16, CAP], BF16)
    actT = self.sb("actT", [128, 8, CAP], BF16)
    wdb = self.sb("wdb", [128, 8, D], BF16)
    wgb = [self.sb(f"wgb{i}", [128, 16, 2, 128], BF16) for i in range(3)]
    gS = [self.sb(f"gS{i}", [128, 512], F32) for i in range(2)]
    sg = [self.sb(f"sg{i}", [128, 512], F32) for i in range(2)]
    uS = [self.sb(f"uS{i}", [128, 512], F32) for i in range(2)]
    xe = [self.sb(f"xe{i}", [128, D], BF16) for i in range(2)]
    yrow = [self.sb(f"yrow{i}", [128, D], F32) for i in range(2)]
    b_fxT, b_actT, b_wdb = Buf(), Buf(), Buf()
    b_wgb = [Buf() for _ in range(3)]
    b_gS, b_sg, b_uS, b_xe, b_yrow = ([Buf(), Buf()] for _ in range(5))
    d_wd = self.ds()
    d_wg = [self.ds() for _ in range(3)]
    d_xe = [self.ds(), self.ds()]
    d_y = [self.ds(), self.ds()]
    cnt = 0
    dcnt = 0
    xcnt = 0
    hcnt = 0
    ntok = CAP
    nst = CAP // 128
    for ex_i in range(NX):
        r0 = ex_i * CAP
        for s_ in range(nst):
            xi = xcnt % 2
            xcnt += 1
            self.dma("sp", xe[xi][:], self.FXE[r0 + s_ * 128:r0 + (s_ + 1) * 128, :], r=[self.b_fxe], w=[b_xe[xi]], dsem=d_xe[xi])
            bA, bB = 4 + 2 * xi, 5 + 2 * xi
            pA = self.ps[bA][:].bitcast(BF16)
            pB = self.ps[bB][:].bitcast(BF16)
            for k in range(16):
                pX, bX = (pA, bA) if k < 8 else (pB, bB)
                self.op("pe", lambda e, k=k, pX=pX, xi=xi: e.transpose(pX[:, (k % 8) * 128:(k % 8 + 1) * 128], xe[xi][:, k * 128:(k + 1) * 128],
                                                                      self.identb[:]),
                        r=[b_xe[xi], self.b_const], w=[self.b_ps[bX]])
            self.op("act", lambda e, s_=s_, pA=pA: e.activation(out=fxT[:, 0:8, s_ * 128:(s_ + 1) * 128], in_=pA.rearrange("p (a b) -> p a b", b=128),
                                                               func=AF.Copy), r=[self.b_ps[bA]], w=[b_fxT])
            self.op("dve", lambda e, s_=s_, pB=pB: e.tensor_copy(fxT[:, 8:16, s_ * 128:(s_ + 1) * 128], pB.rearrange("p (a b) -> p a b", b=128)),
                    r=[self.b_ps[bB]], w=[b_fxT])
        self.dma("pool", wdb[:], self.w_down[l, ex_i].rearrange("(c p) n -> p c n", p=128), w=[b_wdb], dsem=d_wd)
        wgv = self.w_gate_up[l, ex_i].rearrange("(k p) n -> p k n", p=128)
        for fc in range(8):
            s = cnt % 3
            par = cnt % 2
            cnt += 1
            self.dma("pool", wgb[s][:, :, 0, :], wgv[:, :, fc * 128:(fc + 1) * 128], w=[b_wgb[s]], dsem=d_wg[s])
            self.dma("pool", wgb[s][:, :, 1, :], wgv[:, :, DFF + fc * 128:DFF + (fc + 1) * 128], w=[b_wgb[s]], dsem=d_wg[s])
            for (h0, hn) in [(c0, min(512, CAP - c0)) for c0 in range(0, CAP, 512)]:
                par = hcnt % 2
                hcnt += 1
                hs = slice(h0, h0 + hn)
                pg, pu = 2 * par, 2 * par + 1
                for gu, pb in ((0, pg), (1, pu)):
                    for k in range(16):
                        self.op("pe", lambda e, k=k, s=s, gu=gu, pb=pb, hs=hs, hn=hn: e.matmul(self.ps[pb][:, 0:hn], lhsT=wgb[s][:, k, gu, :],
                                                                                       rhs=fxT[:, k, hs], start=(k == 0), stop=(k == 15)),
                                r=[b_wgb[s], b_fxT], w=[self.b_ps[pb]])
                self.op("dve", lambda e, par=par, pg=pg, fc=fc, ex_i=ex_i, hn=hn: e.tensor_scalar(
                    gS[par][:, 0:hn], self.ps[pg][:, 0:hn], bguT[:, fc, ex_i:ex_i + 1], 7.0, ALU.add, ALU.min),
                    r=[self.b_ps[pg], b_c], w=[b_gS[par]])
                self.op("act", lambda e, par=par, hn=hn: e.activation(out=sg[par][:, 0:hn], in_=gS[par][:, 0:hn], func=AF.Sigmoid, scale=1.702),
                        r=[b_gS[par]], w=[b_sg[par]])
                self.op("dve", lambda e, par=par, pu=pu, fc=fc, ex_i=ex_i, hn=hn: e.tensor_scalar(
                    uS[par][:, 0:hn], self.ps[pu][:, 0:hn], bguT[:, 8 + fc, ex_i:ex_i + 1], 7.0, ALU.add, ALU.min),
                    r=[self.b_ps[pu], b_c], w=[b_uS[par]])
                self.op("pool", lambda e, par=par, hn=hn: e.tensor_scalar(uS[par][:, 0:hn], uS[par][:, 0:hn], -7.0, 1.0, ALU.max, ALU.add),
                        r=[b_uS[par]], w=[b_uS[par]])
                self.op("dve", lambda e, par=par, hn=hn: e.tensor_tensor(gS[par][:, 0:hn], gS[par][:, 0:hn], sg[par][:, 0:hn], ALU.mult),
                        r=[b_sg[par]], w=[b_gS[par]])
                self.op("dve", lambda e, par=par, fc=fc, hs=hs, hn=hn: e.tensor_tensor(actT[:, fc, hs], uS[par][:, 0:hn], gS[par][:, 0:hn], ALU.mult),
                        r=[b_uS[par], b_gS[par]], w=[b_actT])
        for s_ in range(nst):
            yi = dcnt % 2
            dcnt += 1
            for nb in range(4):
                bk = 4 + nb
                cols = slice(nb * 512, (nb + 1) * 512)
                for fc in range(8):
                    self.op("pe", lambda e, s_=s_, fc=fc, bk=bk, cols=cols: e.matmul(self.ps[bk][:, 0:512], lhsT=actT[:, fc, s_ * 128:(s_ + 1) * 128],
                                                                                   rhs=wdb[:, fc, cols], start=(fc == 0), stop=(fc == 7)),
                            r=[b_actT, b_wdb], w=[self.b_ps[bk]])
                if nb % 2 == 0:
                    self.op("act", lambda e, yi=yi, bk=bk, cols=cols: e.activation(out=yrow[yi][:, cols], in_=self.ps[bk][:, 0:512], func=AF.Copy),
                            r=[self.b_ps[bk]], w=[b_yrow[yi]])
                else:
                    self.op("dve", lambda e, yi=yi, bk=bk, cols=cols: e.tensor_copy(yrow[yi][:, cols], self.ps[bk][:, 0:512]),
                            r=[self.b_ps[bk]], w=[b_yrow[yi]])
            self.dma("sp", self.YE[r0 + s_ * 128:r0 + (s_ + 1) * 128, :], yrow[yi][:], r=[b_yrow[yi]], w=[self.b_ye], dsem=d_y[yi])
    self.barrier()
    self.sb_off = mark
    acc = [self.sb(f"acc{i}", [128, D], F32) for i in range(2)]
    gb = [[self.sb(f"gb{i}_{j}", [128, D], F32) for j in range(4)] for i in range(2)]
    xt2 = [self.sb(f"gxt2_{i}", [128, D], F32) for i in range(2)]
    b_acc, b_xt2 = [Buf(), Buf()], [Buf(), Buf()]
    b_gb = [[Buf() for j in range(4)] for i in range(2)]
    d_g = [self.ds(), self.ds()]
    d_x2 = [self.ds(), self.ds()]
    d_out = [self.ds(), self.ds()]
    for i in range(2):
        for j in range(4):
            self.op("dve" if j % 2 else "act", (lambda e, i=i, j=j: e.memset(gb[i][j][:], 0.0)) if j % 2 else
                    (lambda e, i=i, j=j: e.activation(out=gb[i][j][:], in_=G2[:, 0, :], func=AF.Copy, scale=0.0)),
                    r=[b_c], w=[b_gb[i][j]])
    dz = self.ds()
    self.dma("sp", self.YE[NR:NR + 128, :], gb[0][1][:], r=[b_gb[0][1]], w=[self.b_ye], dsem=dz)
    for t in tiles_all:
        i = t % 2
        which = 1 if t < 2 else 0
        rows = slice(t * 128, (t + 1) * 128)
        self.dma("sp", xt2[i][:], self.XRES[rows, :], r=[self.b_xres[t]], w=[b_xt2[i]], dsem=d_x2[i])
        for j in range(4):
            self.P.op("pool", lambda e, t=t, j=j, i=i: e.indirect_dma_start(
                out=gb[i][j][:, :], out_offset=None, in_=self.YE[:, :],
                in_offset=bass.IndirectOffsetOnAxis(ap=IDX[:, t, j:j + 1], axis=0)),
                reads=[b_idx, self.b_ye], writes=[b_gb[i][j]], dsem=d_g[i])
        for nb in range(4):
            bk = nb
            cols = slice(nb * 512, (nb + 1) * 512)
            self.op("pe", lambda e, t=t, bk=bk, cols=cols: e.matmul(self.ps[bk][:, 0:512], lhsT=combT[0:NX, t * 128:(t + 1) * 128],
                                                                    rhs=bd[0:NX, cols], start=True, stop=True),
                    r=[b_combT, b_c], w=[self.b_ps[bk]])
            self.op("act", lambda e, i=i, bk=bk, cols=cols: e.activation(out=acc[i][:, cols], in_=self.ps[bk][:, 0:512], func=AF.Copy),
                    r=[self.b_ps[bk]], w=[b_acc[i]])
        for j in range(4):
            self.op("dve", lambda e, i=i, j=j, t=t: e.scalar_tensor_tensor(acc[i][:], gb[i][j][:], WJ[:, t, j:j + 1], acc[i][:], ALU.mult, ALU.add),
                    r=[b_gb[i][j], b_idx], w=[b_acc[i]])
        self.op("pool", lambda e, i=i, which=which: e.tensor_tensor(acc[i][:], acc[i][:], G2[:, which, :], ALU.mult), r=[b_c], w=[b_acc[i]])
        self.op("pool", lambda e, i=i: e.tensor_tensor(acc[i][:], acc[i][:], xt2[i][:], ALU.add), r=[b_xt2[i]], w=[b_acc[i]])
        if last:
            by = Buf()
            self.dma("sp", self.y_out[(t - 2) * 128:(t - 1) * 128, :], acc[i][:], r=[b_acc[i]], w=[by], dsem=d_out[i])
            self.final_reads.append(by)
        else:
            self.dma("sp", self.XRES[rows, :], acc[i][:], r=[b_acc[i]], w=[self.b_xres[t]], dsem=d_out[i])
    if not last:
        self.dbg(f"XOUT{l}", self.XRES, [TT, D], F32, r=self.b_xres)
    else:
        self.dbg(f"YOUT{l}", self.y_out, [2048, D], F32, r=self.final_reads)
    self.barrier()


Builder.phase_G2 = phase_G2
```

```python
import numpy as np
import ml_dtypes
import concourse.bass as bass
import concourse.mybir as mybir
from concourse.bass_utils import run_bass_kernel_spmd

F32 = mybir.dt.float32
BF16 = mybir.dt.bfloat16
AF = mybir.ActivationFunctionType
ALU = mybir.AluOpType
AX = mybir.AxisListType

D = 2048
NT = 18
TT = NT * 128
DIN = 4624
EPS = 1e-6
NE = 32
DFF = 1024
CAP = 768
KBIG = 40000.0
I32 = mybir.dt.int32


class Buf:
    __slots__ = ("name", "last_w", "readers")

    def __init__(self, name=""):
        self.name = name
        self.last_w = None
        self.readers = []


class DSem:
    __slots__ = ("sem", "count")

    def __init__(self, sem):
        self.sem = sem
        self.count = 0


class Op:
    __slots__ = ("eng", "emit", "deps", "signal", "signum", "dsem", "dval", "waits", "idx")


ENGS = ("pe", "act", "dve", "pool", "sp")


class Prog:
    def __init__(self):
        self.streams = {e: [] for e in ENGS}
        self.nops = 0
        self.dsems = []

    def new_dsem(self, sem):
        d = DSem(sem)
        self.dsems.append(d)
        return d

    def op(self, eng, emit, reads=(), writes=(), dsem=None, extra_dsem_waits=()):
        o = Op()
        o.eng = eng
        o.emit = emit
        o.signal = False
        o.signum = None
        o.dsem = dsem
        o.dval = None
        o.idx = self.nops
        self.nops += 1
        deps = []
        for b in reads:
            if b.last_w is not None:
                deps.append(b.last_w)
        for b in writes:
            if b.last_w is not None:
                deps.append(b.last_w)
            deps.extend(b.readers)
        seen = set()
        o.deps = []
        for d in deps:
            if id(d) in seen or d is o:
                continue
            seen.add(id(d))
            if d.dsem is not None:
                o.deps.append(("d", d.dsem, d.dsem.count))
            else:
                if d.eng == "pe" and eng == "pe" and dsem is None:
                    continue
                d.signal = True
                o.deps.append(("e", d))
        for ds in extra_dsem_waits:
            if ds.count > 0:
                o.deps.append(("d", ds, ds.count))
        if dsem is not None:
            dsem.count += 16
            o.dval = dsem.count
        for b in reads:
            b.readers.append(o)
        for b in writes:
            b.last_w = o
            b.readers = []
        self.streams[eng].append(o)
        return o

    def finalize(self):
        for e in ENGS:
            n = 0
            for o in self.streams[e]:
                if o.dsem is None and o.signal:
                    n += 1
                    o.signum = n
        for e in ENGS:
            known = {}
            for o in self.streams[e]:
                w = {}
                for d in o.deps:
                    if d[0] == "d":
                        key = ("d", id(d[1]))
                        val = d[2]
                        sem = d[1].sem
                    else:
                        key = ("e", d[1].eng)
                        val = d[1].signum
                        sem = d[1].eng
                    if known.get(key, 0) >= val:
                        continue
                    if key not in w or w[key][1] < val:
                        w[key] = (sem, val)
                for key, (sem, val) in w.items():
                    known[key] = val
                o.waits = list(w.values())

    def emit(self, block, esems):
        def run(engname):
            def f(eng):
                for o in self.streams[engname]:
                    for sem, val in o.waits:
                        s = esems[sem] if isinstance(sem, str) else sem
                        eng.wait_ge(s, val)
                    ins = o.emit(eng)
                    if ins is None:
                        continue
                    if o.dsem is not None:
                        ins.then_inc(o.dsem.sem, 16)
                    elif o.signal:
                        ins.then_inc(esems[engname], 1)
            return f
        block.tensor(run("pe"))
        block.scalar(run("act"))
        block.vector(run("dve"))
        block.gpsimd(run("pool"))
        block.sync(run("sp"))


def _consts():
    c = {}
    c["ident_f"] = np.eye(128, dtype=np.float32)
    c["ident_b"] = np.eye(128, dtype=np.float32).astype(ml_dtypes.bfloat16)
    a = np.arange(128)
    c["mask_prev"] = (a[None, :] <= a[:, None]).astype(np.float32).astype(ml_dtypes.bfloat16)
    c["mask_next"] = (a[:, None] <= a[None, :]).astype(np.float32).astype(ml_dtypes.bfloat16)
    c["ones_f"] = np.ones((128, 128), np.float32)
    sel = np.zeros((32, 32, 128), np.float32)
    for e in range(32):
        sel[e, e, :] = 1.0
    c["sel32"] = sel.transpose(1, 0, 2).copy()
    e0 = np.zeros((2, 2, 128), np.float32)
    e0[0, 0, :] = 1.0
    e0[1, 1, :] = 1.0
    c["sel2"] = e0
    rows = 2048 // 64
    row = np.repeat(np.arange(rows, dtype=np.float32), 64)
    col = np.tile(np.arange(64, dtype=np.float32), rows)
    half = 32
    inv = (10000.0 ** (-np.arange(0, half, 2, dtype=np.float32) / half)).astype(np.float32)

    def tab(p):
        ang = p[:, None] * inv[None, :]
        ang = np.concatenate([ang, ang], -1)
        return np.cos(ang).astype(np.float32), np.sin(ang).astype(np.float32)
    cr, sr = tab(row)
    cc, sc = tab(col)
    cos = np.concatenate([cr, cc], -1)
    sgn = np.concatenate([-np.ones(16), np.ones(16)]).astype(np.float32)
    sins = np.concatenate([sr * sgn, sc * sgn], -1)
    c["rope_cos"] = cos.reshape(16, 128, 64).transpose(1, 0, 2).copy()
    c["rope_sin"] = sins.reshape(16, 128, 64).transpose(1, 0, 2).copy()
    c["ml_mask_f"] = (a[:, None] <= a[None, :]).astype(np.float32).astype(ml_dtypes.bfloat16)
    c["ml_mask_b"] = (a[:, None] >= a[None, :]).astype(np.float32).astype(ml_dtypes.bfloat16)
    c["tri_s"] = (a[:, None] < a[None, :]).astype(np.float32)
    c["trash"] = (NE * CAP + np.arange(128, dtype=np.float32)).reshape(128, 1)
    c["ecst"] = np.tile((np.arange(32, dtype=np.float32) * CAP)[None, :], (128, 1))
    c["tri_f"] = (a[:, None] <= a[None, :]).astype(np.float32)
    c["tri_b"] = (a[:, None] >= a[None, :]).astype(np.float32)
    sl = np.zeros((128, 128), np.float32)
    sl[127, :] = 1.0
    c["sel_last"] = sl
    sf = np.zeros((128, 128), np.float32)
    sf[0, :] = 1.0
    c["sel_first"] = sf
    return c


from contextlib import ExitStack


class Builder:
    def __init__(self, nc, stack, upto="all", debug=(), n_exp=NE, layers=2, dev=False):
        self.nc = nc
        self.stack = stack
        self.P = Prog()
        self.upto = upto
        self.debug = set(debug)
        self.n_exp = n_exp
        self.layers = layers
        self.dev = dev
        self.L = 1 if dev else 2
        self.sb_off = 16512
        self.nsem = 0
        self.dpool = []
        self.dnext = 0
        self.dbg_names = []
        self.need_moe = upto == "all" or upto.startswith("G")

    def sb(self, name, shape, dtype):
        esz = 2 if dtype == BF16 else 4
        n = 1
        for s in shape[1:]:
            n *= s
        nbytes = (n * esz + 31) // 32 * 32
        t = self.nc.alloc_sbuf_tensor_at(f"{name}_{self.sb_off}", list(shape), dtype, offset=self.sb_off)
        self.sb_off += nbytes
        assert self.sb_off <= 16512 + 212000, (name, self.sb_off)
        return t

    def sem(self, name):
        self.nsem += 1
        return self.stack.enter_context(self.nc.semaphore(name))

    def ds(self):
        if self.dnext >= len(self.dpool):
            self.dpool.append(self.P.new_dsem(self.sem(f"d{len(self.dpool)}")))
        d = self.dpool[self.dnext]
        self.dnext += 1
        return d

    def dram(self, name, shape, dtype, kind="Internal"):
        return self.nc.dram_tensor(name, list(shape), dtype, kind=kind).ap()

    def op(self, eng, fn, r=(), w=(), dsem=None):
        return self.P.op(eng, fn, reads=r, writes=w, dsem=dsem)

    def dma(self, q, out, in_, r=(), w=(), dsem=None, **kw):
        assert dsem is not None
        return self.P.op(q, lambda e: e.dma_start(out=out, in_=in_, **kw), reads=r, writes=w, dsem=dsem)

    def barrier(self):
        P = self.P
        toks = []
        for e in ("act", "dve", "pool"):
            b = Buf("tok_" + e)
            scr = self.scr[e]
            if e == "act":
                P.op(e, lambda en, s=scr: en.activation(out=s[0:1, 0:2], in_=s[0:1, 0:2], func=AF.Identity), writes=[b])
            else:
                P.op(e, lambda en, s=scr: en.memset(s[0:1, 0:2], 0.0), writes=[b])
            toks.append(b)
        for e in ENGS:
            P.op(e, lambda en: None, reads=toks, extra_dsem_waits=list(P.dsems))
        self.dnext = 0

    def dbg(self, name, ap, shape, dtype, r=()):
        if name not in self.debug:
            return
        o = self.dram("dbg_" + name, shape, dtype, kind="ExternalOutput")
        self.dbg_names.append(name)
        d = self.P.new_dsem(self.sem("dbg_" + name))
        bo = Buf("dbg_" + name)
        self.dma("sp", o, ap, r=r, w=[bo], dsem=d)
        self.final_reads.append(bo)

    def setup(self):
        nc = self.nc
        self.esems = {e: self.sem("s_" + e) for e in ("pe", "act", "dve", "pool")}
        self.final_reads = []
        self.in_names = []

        def di(n, s, dt=F32):
            self.in_names.append(n)
            return self.dram(n, s, dt, kind="ExternalInput")
        self.x_in = di("x_in", [TT, D])
        self.c2 = di("c2", [32, 128])
        if not self.dev:
            self.w_ada = di("w_ada", [self.L, D, 6 * D])
        else:
            self.dev_mod = di("dev_mod", [2, 6 * D])
        self.b_ada = di("b_ada", [self.L, 6 * D])
        self.norm1_w = di("norm1_w", [self.L, D])
        self.norm2_w = di("norm2_w", [self.L, D])
        self.w_in = di("w_in", [self.L, D, DIN])
        self.b_gates = di("b_gates", [self.L, 16])
        self.q_norm_w = di("q_norm_w", [self.L, 64])
        self.k_norm_w = di("k_norm_w", [self.L, 64])
        self.attn_sink = di("attn_sink", [self.L, 16])
        self.mlstm_norm_w = di("mlstm_norm_w", [self.L, 1024])
        self.w_out = di("w_out", [self.L, D, D])
        self.w_router = di("w_router", [self.L, D, NE])
        self.b_router = di("b_router", [self.L, NE])
        if self.need_moe:
            self.w_gate_up = di("w_gate_up", [self.L, self.n_exp, D, 2 * DFF])
            self.b_gate_up = di("b_gate_up", [self.L, self.n_exp, 2 * DFF])
            self.w_down = di("w_down", [self.L, self.n_exp, DFF, D])
            self.b_down = di("b_down", [self.L, self.n_exp, D])
        self.cst = {}
        for k, v in _consts().items():
            self.cst[k] = di("cst_" + k, list(v.shape), BF16 if v.dtype != np.float32 else F32)
        self.y_out = self.dram("y", [2048, D], F32, kind="ExternalOutput")
        self.XRES = self.dram("xres", [TT, D], F32)
        self.PX = self.dram("px", [TT, 4608], BF16)
        self.GD = self.dram("gd", [TT, 16], F32)
        self.MQT = self.dram("mqt", [4, 128, TT], BF16)
        self.MKT = self.dram("mkt", [4, 128, TT], BF16)
        self.MODD = self.dram("modd", [2, 6 * D], F32)
        self.CATD = self.dram("catd", [TT, D], BF16)
        self.FXE = self.dram("fxe", [NE * CAP + 128, D], BF16)
        self.YE = self.dram("ye", [NE * CAP + 128, D], F32)
        self.b_fxe = Buf("fxe")
        self.b_ye = Buf("ye")
        self.b_xres = [Buf(f"xres{t}") for t in range(NT)]
        self.b_px = [Buf(f"px{t}") for t in range(NT)]
        self.b_gd = [Buf(f"gd{t}") for t in range(NT)]
        self.b_mqt = Buf("mqt")
        self.b_mkt = Buf("mkt")
        self.b_modd = Buf("modd")
        self.b_catd = [Buf(f"catd{t}") for t in range(NT)]
        self.ps = [nc.alloc_psum_tensor(f"psb{i}", [128, 512], F32) for i in range(8)]
        self.b_ps = [Buf(f"ps{i}") for i in range(8)]
        self.identf = self.sb("identf", [128, 128], F32)
        self.identb = self.sb("identb", [128, 128], BF16)
        self.onesf = self.sb("onesf", [128, 128], F32)
        self.scr = {e: self.sb("scr_" + e, [128, 8], F32) for e in ("act", "dve", "pool")}
        self.modT = self.sb("modT", [128, 96, 2], F32)
        self.S1 = self.sb("S1", [128, 16, 2], F32)
        self.S2 = self.sb("S2", [128, 16, 2], F32)
        self.b_modT = Buf("modT")
        self.b_S = Buf("S12")
        self.b_const = Buf("const")
        dc = self.P.new_dsem(self.sem("dconst"))
        self.dconst = dc
        self.dma("sp", self.identf[:], self.cst["ident_f"], w=[self.b_const], dsem=dc)
        self.dma("sp", self.identb[:], self.cst["ident_b"], w=[self.b_const], dsem=dc)
        self.dma("sp", self.onesf[:], self.cst["ones_f"], w=[self.b_const], dsem=dc)
        self.zt = self.sb("zt", [128, D], BF16)
        self.b_zt = Buf("zt")
        self.op("pool", lambda e: e.memset(self.zt[:], 0.0), w=[self.b_zt])
        self.persist_off = self.sb_off

    def phase_reset(self):
        self.sb_off = self.persist_off

    def phase_A(self, l):
        nc, P = self.nc, self.P
        self.phase_reset()
        vec = self.sb("vec", [64, 128], F32)
        silu2 = self.sb("silu2", [128, 16, 2], F32)
        nwT = self.sb("nwT", [128, 2, 16], F32)
        bada = self.sb("bada", [2, 6 * D], F32)
        modsb = self.sb("modsb", [2, 6 * D], F32)
        sel2 = self.sb("sel2", [2, 2, 128], F32)
        stage = [self.sb(f"wst{i}", [128, 16, 512], F32) for i in range(2)]
        b_vec, b_silu, b_nwT, b_bada, b_modsb = Buf(), Buf(), Buf(), Buf(), Buf()
        b_stage = [Buf(), Buf()]
        d_stage = [self.ds(), self.ds()]
        d0 = self.ds()
        d1 = self.ds()
        self.dma("sp", vec[0:32, :], self.c2, w=[b_vec], dsem=d0)
        self.dma("sp", vec[32:48, :], self.norm1_w[l].rearrange("(k p) -> k p", p=128), w=[b_vec], dsem=d0)
        self.dma("sp", vec[48:64, :], self.norm2_w[l].rearrange("(k p) -> k p", p=128), w=[b_vec], dsem=d0)
        self.dma("sp", bada[0:1, :], self.b_ada[l:l + 1, :], w=[b_bada], dsem=d0)
        self.dma("sp", bada[1:2, :], self.b_ada[l:l + 1, :], w=[b_bada], dsem=d0)
        self.dma("sp", sel2[:], self.cst["sel2"], w=[b_bada], dsem=d0)
        pv = self.ps[7]
        bpv = self.b_ps[7]
        self.op("pe", lambda e: e.transpose(pv[:, 0:64], vec[0:64, :], self.identf[0:64, 0:64]),
                r=[b_vec, self.b_const], w=[bpv])
        self.op("act", lambda e: e.activation(out=silu2[:, :, 0], in_=pv[:, 0:16], func=AF.Silu), r=[bpv], w=[b_silu])
        self.op("act", lambda e: e.activation(out=silu2[:, :, 1], in_=pv[:, 16:32], func=AF.Silu), r=[bpv], w=[b_silu])
        self.op("dve", lambda e: e.tensor_copy(nwT[:].rearrange("p a b -> p (a b)"), pv[:, 32:64]), r=[bpv], w=[b_nwT])
        if self.dev:
            self.dma("sp", modsb[0:2, :], self.dev_mod, w=[b_modsb], dsem=d0)
        wv = None if self.dev else self.w_ada[l].rearrange("(k p) n -> p k n", p=128)
        for n in range(0 if self.dev else 24):
            s = n % 2
            self.dma("sp", stage[s][:], wv[:, :, n * 512:(n + 1) * 512], w=[b_stage[s]], dsem=d_stage[s])
            pm = self.ps[n % 2]
            bpm = self.b_ps[n % 2]
            for k in range(16):
                self.op("pe", lambda e, k=k, s=s, pm=pm: e.matmul(pm[0:2, :], lhsT=silu2[:, k, :], rhs=stage[s][:, k, :],
                                                                  start=(k == 0), stop=(k == 15)),
                        r=[b_silu, b_stage[s]], w=[bpm])
            self.op("dve", lambda e, n=n, pm=pm: e.tensor_tensor(modsb[0:2, n * 512:(n + 1) * 512], pm[0:2, :],
                                                                 bada[0:2, n * 512:(n + 1) * 512], ALU.add),
                    r=[bpm, b_bada], w=[b_modsb])
        self.dma("sp", self.MODD, modsb[0:2, :], r=[b_modsb], w=[self.b_modd], dsem=d1)
        pt = self.ps[2]
        for j in range(96):
            self.op("pe", lambda e, j=j: e.transpose(pt[:, 2 * j:2 * j + 2], modsb[0:2, j * 128:(j + 1) * 128],
                                                      self.identf[0:2, 0:2]),
                    r=[b_modsb, self.b_const], w=[self.b_ps[2]])
        self.op("act", lambda e: e.activation(out=self.modT[:].rearrange("p a b -> p (a b)"), in_=pt[:, 0:192],
                                              func=AF.Identity), r=[self.b_ps[2]], w=[self.b_modT])
        for (S, sc0, wi) in ((self.S1, 16, 0), (self.S2, 64, 1)):
            self.op("dve", lambda e, S=S, sc0=sc0: e.tensor_scalar(S[:], self.modT[:, sc0:sc0 + 16, :], 1.0, None, ALU.add),
                    r=[self.b_modT], w=[self.b_S])
            self.op("dve", lambda e, S=S, wi=wi: e.tensor_tensor(S[:], S[:], nwT[:, wi, :].unsqueeze(2).to_broadcast([128, 16, 2]),
                                                                 ALU.mult),
                    r=[b_nwT], w=[self.b_S])
        self.dbg(f"modT{l}", self.modT[:], [128, 96, 2], F32, r=[self.b_modT])
        self.dbg(f"S1_{l}", self.S1[:], [128, 16, 2], F32, r=[self.b_S])
        self.barrier()

    def norm_tile(self, xt, b_xt, which, S, sh0, f32T, b_f32T, dstT, b_dst, col0, banks, junk, ss, xn, b_tmp):
        P = self
        b_ss, b_xn = b_tmp
        self.op("act", lambda e: e.activation(out=junk[:], in_=xt[:], func=AF.Square, accum_out=ss[:, 0:1]),
                r=[b_xt], w=[b_ss])
        self.op("dve", lambda e: e.tensor_scalar(ss[:, 1:2], ss[:, 0:1], 1.0 / D, EPS, ALU.mult, ALU.add), r=[b_ss], w=[b_ss])
        self.op("act", lambda e: e.activation(out=ss[:, 3:4], in_=ss[:, 1:2], func=AF.Sqrt), r=[b_ss], w=[b_ss])
        self.op("dve", lambda e: e.reciprocal(ss[:, 2:3], ss[:, 3:4]), r=[b_ss], w=[b_ss])
        self.op("dve", lambda e: e.tensor_scalar(xn[:], xt[:], ss[:, 2:3], None, ALU.mult), r=[b_xt, b_ss], w=[b_xn])
        for c in range(16):
            bk = banks[c // 4]
            self.op("pe", lambda e, c=c, bk=bk: e.transpose(self.ps[bk][:, (c % 4) * 128:(c % 4 + 1) * 128],
                                                            xn[:, c * 128:(c + 1) * 128], self.identf[:]),
                    r=[b_xn, self.b_const], w=[self.b_ps[bk]])
        for c in range(16):
            bk = banks[c // 4]
            src = self.ps[bk][:, (c % 4) * 128:(c % 4 + 1) * 128]
            if c % 2 == 0:
                self.op("act", lambda e, c=c, src=src: e.activation(out=f32T[:, c, :], in_=src, func=AF.Identity,
                                                                    scale=S[:, c, which:which + 1],
                                                                    bias=self.modT[:, sh0 + c, which:which + 1]),
                        r=[self.b_ps[bk], self.b_S, self.b_modT], w=[b_f32T])
            else:
                self.op("dve", lambda e, c=c, src=src: e.tensor_scalar(f32T[:, c, :], src, S[:, c, which:which + 1],
                                                                       self.modT[:, sh0 + c, which:which + 1],
                                                                       ALU.mult, ALU.add),
                        r=[self.b_ps[bk], self.b_S, self.b_modT], w=[b_f32T])
        if dstT is not None:
            self.op("pool", lambda e: e.tensor_copy(dstT[:, :, col0:col0 + 128], f32T[:]), r=[b_f32T], w=[b_dst])

    def phase_BC(self, l):
        self.phase_reset()
        src = self.x_in if l == 0 else self.XRES
        hT = self.sb("hT", [128, 16, TT], BF16)
        b_hT = Buf("hT")
        xt = [self.sb(f"xt{i}", [128, D], F32) for i in range(2)]
        xn = [self.sb(f"xn{i}", [128, D], F32) for i in range(2)]
        f32T = [self.sb(f"f32T{i}", [128, 16, 128], F32) for i in range(2)]
        junk = self.sb("junk", [128, D], BF16)
        ss = [self.sb(f"ss{i}", [128, 4], F32) for i in range(2)]
        b_xt = [Buf(), Buf()]
        b_f = [Buf(), Buf()]
        b_tmp = [(Buf(), Buf()), (Buf(), Buf())]
        d_xt = [self.ds(), self.ds()]
        mark = self.sb_off
        for t in range(NT):
            i = t % 2
            which = 1 if t < 2 else 0
            rd = [self.b_xres[t]] if l > 0 else []
            self.dma("sp", xt[i][:], src[t * 128:(t + 1) * 128, :], r=rd, w=[b_xt[i]], dsem=d_xt[i])
            self.norm_tile(xt[i], b_xt[i], which, self.S1, 0, f32T[i], b_f[i], hT, b_hT, t * 128,
                           [4 * i + j for j in range(4)], junk, ss[i], xn[i], b_tmp[i])
        self.dbg(f"hT{l}", hT[:], [128, 16, TT], BF16, r=[b_hT])
        if self.upto == f"B{l}":
            return
        wv = self.w_in[l].rearrange("(k p) n -> p k n", p=128)
        CW = 256
        stg = [self.sb(f"pst{i}", [128, 16, CW], F32) for i in range(2)]
        wbf = [self.sb(f"pwb{i}", [128, 16, CW], BF16) for i in range(2)]
        osb = [self.sb(f"posb{i}", [128, 512], BF16) for i in range(3)]
        gsb = self.sb("pgsb", [128, NT, 16], F32)
        b_stg = [Buf(), Buf()]
        b_wbf = [Buf(), Buf()]
        b_osb = [Buf(), Buf(), Buf()]
        b_gsb = Buf()
        d_stg = [self.ds(), self.ds()]
        d_osb = [self.ds(), self.ds(), self.ds()]
        d_g = self.ds()
        nblk = 4608 // CW
        cnt = 0
        ocnt = 0
        def load_blk(cb):
            s = cb % 2
            c0 = cb * CW
            cw = CW if cb < nblk else 16
            self.dma("sp", stg[s][:, :, 0:cw], wv[:, :, c0:c0 + cw], w=[b_stg[s]], dsem=d_stg[s])
        load_blk(0)
        for cb in range(nblk + 1):
            s = cb % 2
            c0 = cb * CW
            cw = CW if cb < nblk else 16
            if cb + 1 <= nblk:
                load_blk(cb + 1)
            self.op("pool", lambda e, s=s, cw=cw: e.tensor_copy(wbf[s][:, :, 0:cw], stg[s][:, :, 0:cw]),
                    r=[b_stg[s]], w=[b_wbf[s]])
            fm_head = None
            if 1536 <= c0 < 2560:
                fm_head = (c0 - 1536) // 128
            for t in range(NT):
                bk = cnt % 4
                cnt += 1
                pt = self.ps[bk]
                for k in range(16):
                    self.op("pe", lambda e, k=k, t=t, s=s, cw=cw, pt=pt: e.matmul(pt[:, 0:cw], lhsT=hT[:, k, t * 128:(t + 1) * 128],
                                                                                   rhs=wbf[s][:, k, 0:cw], start=(k == 0), stop=(k == 15)),
                            r=[b_hT, b_wbf[s]], w=[self.b_ps[bk]])
                if cb < nblk:
                    o = ocnt % 3
                    ocnt += 1
                    scale = (128.0 ** -0.5) if 2048 <= c0 < 2560 else 1.0
                    if t % 2 == 0:
                        self.op("act", lambda e, o=o, pt=pt, scale=scale: e.activation(out=osb[o][:, 0:CW], in_=pt[:, 0:CW],
                                                                                        func=AF.Copy, scale=scale),
                                r=[self.b_ps[bk]], w=[b_osb[o]])
                    else:
                        self.op("dve", lambda e, o=o, pt=pt, scale=scale: e.tensor_scalar(osb[o][:, 0:CW], pt[:, 0:CW], scale, None, ALU.mult),
                                r=[self.b_ps[bk]], w=[b_osb[o]])
                    self.dma("sp", self.PX[t * 128:(t + 1) * 128, c0:c0 + CW], osb[o][:, 0:CW], r=[b_osb[o]], w=[self.b_px[t]],
                             dsem=d_osb[o])
                else:
                    self.op("dve", lambda e, t=t, pt=pt: e.tensor_copy(gsb[:, t, :], pt[:, 0:16]), r=[self.b_ps[bk]], w=[b_gsb])
            if fm_head is not None:
                for hh in range(CW // 128):
                    head = fm_head + hh
                    dst = self.MQT if head < 4 else self.MKT
                    bdst = self.b_mqt if head < 4 else self.b_mkt
                    scale = 1.0 if head < 4 else (128.0 ** -0.5)
                    for tb in range(6):
                        bk = 4 + (cnt % 4)
                        cnt += 1
                        pt = self.ps[bk]
                        for k in range(16):
                            self.op("pe", lambda e, k=k, tb=tb, s=s, hh=hh, pt=pt: e.matmul(
                                pt[:, 0:384], lhsT=wbf[s][:, k, hh * 128:(hh + 1) * 128], rhs=hT[:, k, tb * 384:(tb + 1) * 384],
                                start=(k == 0), stop=(k == 15)), r=[b_hT, b_wbf[s]], w=[self.b_ps[bk]])
                        o = ocnt % 3
                        ocnt += 1
                        self.op("act", lambda e, o=o, pt=pt, scale=scale: e.activation(out=osb[o][:, 0:384], in_=pt[:, 0:384],
                                                                                        func=AF.Copy, scale=scale),
                                r=[self.b_ps[bk]], w=[b_osb[o]])
                        self.dma("sp", dst[head % 4, :, tb * 384:(tb + 1) * 384], osb[o][:, 0:384], r=[b_osb[o]], w=[bdst],
                                 dsem=d_osb[o])
        self.dma("sp", self.GD.rearrange("(t p) g -> p t g", p=128), gsb[:], r=[b_gsb], w=self.b_gd, dsem=d_g)
        self.dbg(f"PX{l}", self.PX, [TT, 4608], BF16, r=self.b_px)
        self.dbg(f"GD{l}", self.GD, [TT, 16], F32, r=self.b_gd)
        self.dbg(f"MQT{l}", self.MQT, [4, 128, TT], BF16, r=[self.b_mqt])
        self.dbg(f"MKT{l}", self.MKT, [4, 128, TT], BF16, r=[self.b_mkt])
        self.barrier()


def build_program(upto="all", debug=(), n_exp=NE, dev=False, dev_layer=0):
    nc = bass.Bass("TRN2", target_bir_lowering=False)
    stack = ExitStack()
    B = Builder(nc, stack, upto=upto, debug=debug, n_exp=n_exp, dev=dev)
    B.setup()
    B.last_layer = 1
    for l in ([0] if dev else range(2)):
        B.cur_layer = dev_layer if dev else l
        B.phase_A(l)
        if upto == f"A{l}":
            break
        B.phase_BC(l)
        if upto in (f"B{l}", f"C{l}"):
            break
        B.phase_D(l)
        if upto in (f"D{l}", f"D1_{l}"):
            break
        B.phase_E(l)
        if upto in (f"E{l}", f"E1_{l}"):
            break
        B.phase_F(l)
        if upto == f"F{l}":
            break
        B.phase_G2(l)
        if upto == f"G{l}":
            break
    B.op("sp", lambda e: None, r=B.final_reads)
    B.P.finalize()
    with nc.Block() as block:
        B.P.emit(block, B.esems)
    stack.close()
    return nc, B


def make_in_maps(inputs, n_exp=NE):
    cst = _consts()
    maps = []
    x = np.asarray(inputs["x"], np.float32)
    ctx = np.asarray(inputs["ctx"], np.float32)
    c = np.asarray(inputs["c"], np.float32)
    c_ctx = np.asarray(inputs["c_ctx"], np.float32)
    shared = {k: np.ascontiguousarray(np.asarray(inputs[k], np.float32)) for k in
              ("w_ada", "b_ada", "norm1_w", "norm2_w", "w_in", "b_gates", "q_norm_w", "k_norm_w", "attn_sink",
               "mlstm_norm_w", "w_out", "w_router", "b_router", "w_gate_up", "b_gate_up", "w_down", "b_down")}
    for b in range(8):
        m = dict(shared)
        m["x_in"] = np.concatenate([ctx[b], x[b]], axis=0)
        m["c2"] = np.concatenate([c[b].reshape(16, 128), c_ctx.reshape(16, 128)], axis=0)
        for k, v in cst.items():
            m["cst_" + k] = v
        maps.append(m)
    return maps


def kernel(**inputs):
    nc, B = build_program()
    maps = make_in_maps(inputs)
    maps = [{k: m[k] for k in B.in_names} for m in maps]
    res = run_bass_kernel_spmd(nc, maps, core_ids=list(range(8)))
    return np.stack([np.asarray(r["y"], np.float32) for r in res.results], axis=0)


def phase_D(self, l):
    sl = self.cur_layer
    self.phase_reset()
    qT = self.sb("qT", [128, 8, TT], BF16)
    kT2 = self.sb("kT2", [128, 4, 2, TT], BF16)
    vaug = self.sb("vaug", [128, NT, 4, 65], BF16)
    cos = self.sb("cos", [128, 16, 64], F32)
    sin = self.sb("sin", [128, 16, 64], F32)
    qw = self.sb("qw", [128, 64], F32)
    kw = self.sb("kw", [128, 64], F32)
    sinke = self.sb("sinke", [128, 16], F32)
    maskp = self.sb("maskp", [128, 128], BF16)
    maskn = self.sb("maskn", [128, 128], BF16)
    b_qT, b_kT2, b_vaug, b_c = Buf(), Buf(), Buf(), Buf()
    d0 = self.ds()
    self.dma("sp", cos[:], self.cst["rope_cos"], w=[b_c], dsem=d0)
    self.dma("sp", sin[:], self.cst["rope_sin"], w=[b_c], dsem=d0)
    self.dma("sp", qw[:], self.q_norm_w[l:l + 1, :].partition_broadcast(128), w=[b_c], dsem=d0)
    self.dma("sp", kw[:], self.k_norm_w[l:l + 1, :].partition_broadcast(128), w=[b_c], dsem=d0)
    self.dma("sp", sinke[:], self.attn_sink[l:l + 1, :].partition_broadcast(128), w=[b_c], dsem=d0)
    self.dma("sp", maskp[:], self.cst["mask_prev"], w=[b_c], dsem=d0)
    self.dma("sp", maskn[:], self.cst["mask_next"], w=[b_c], dsem=d0)
    b_c2 = Buf()
    self.op("act", lambda e: e.activation(out=qw[:], in_=qw[:], func=AF.Copy, scale=0.125), r=[b_c], w=[b_c2])
    self.op("act", lambda e: e.activation(out=sinke[:], in_=sinke[:], func=AF.Exp), r=[b_c], w=[b_c2])
    self.op("pool", lambda e: e.memset(vaug[:].rearrange("p t j d -> p (t j) d")[:, :, 64:65], 1.0), w=[b_vaug])
    slab = [self.sb(f"slab{i}", [128, 1536], BF16) for i in range(2)]
    t1 = [self.sb(f"dt1_{i}", [128, 20, 64], F32) for i in range(2)]
    t2 = [self.sb(f"dt2_{i}", [128, 20, 64], F32) for i in range(2)]
    t3 = [self.sb(f"dt3_{i}", [128, 20, 64], F32) for i in range(2)]
    qkr = [self.sb(f"qkr{i}", [128, 20, 64], BF16) for i in range(2)]
    k2 = [self.sb(f"k2_{i}", [128, 4, 2, 2, 64], BF16) for i in range(2)]
    st = [self.sb(f"dst_{i}", [128, 64], F32) for i in range(2)]
    b_slab, b_t1, b_t2, b_t3, b_qkr, b_k2, b_st = ([Buf(), Buf()] for _ in range(7))
    d_slab = [self.ds(), self.ds()]
    t_first = 0
    for i in range(2):
        self.op("pool", lambda e, i=i: e.memset(k2[i][:].rearrange("p j u h d -> p (j u h d)"), 0.0), w=[b_k2[i]])
    for t in range(t_first, NT):
        i = t % 2
        self.dma("sp", slab[i][:], self.PX[t * 128:(t + 1) * 128, 0:1536], r=[self.b_px[t]], w=[b_slab[i]], dsem=d_slab[i])
        qk = slab[i][:, 0:1280].rearrange("p (h d) -> p h d", d=64)
        self.op("dve", lambda e, i=i, qk=qk: e.tensor_tensor(t1[i][:], qk, qk, ALU.mult), r=[b_slab[i]], w=[b_t1[i]])
        self.op("dve", lambda e, i=i: e.tensor_reduce(st[i][:, 0:20], t1[i][:], AX.X, ALU.add), r=[b_t1[i]], w=[b_st[i]])
        self.op("dve", lambda e, i=i: e.tensor_scalar(st[i][:, 0:20], st[i][:, 0:20], 1.0 / 64, EPS, ALU.mult, ALU.add),
                r=[b_st[i]], w=[b_st[i]])
        self.op("act", lambda e, i=i: e.activation(out=st[i][:, 20:40], in_=st[i][:, 0:20], func=AF.Sqrt), r=[b_st[i]], w=[b_st[i]])
        self.op("dve", lambda e, i=i: e.reciprocal(st[i][:, 40:60], st[i][:, 20:40]), r=[b_st[i]], w=[b_st[i]])
        self.op("dve", lambda e, i=i, qk=qk: e.tensor_tensor(t1[i][:], qk, st[i][:, 40:60].unsqueeze(2).to_broadcast([128, 20, 64]),
                                                             ALU.mult), r=[b_slab[i], b_st[i]], w=[b_t1[i]])
        self.op("dve", lambda e, i=i: e.tensor_tensor(t1[i][:, 0:16, :], t1[i][:, 0:16, :],
                                                      qw[:].unsqueeze(1).to_broadcast([128, 16, 64]), ALU.mult),
                r=[b_c2], w=[b_t1[i]])
        self.op("dve", lambda e, i=i: e.tensor_tensor(t1[i][:, 16:20, :], t1[i][:, 16:20, :],
                                                      kw[:].unsqueeze(1).to_broadcast([128, 4, 64]), ALU.mult),
                r=[b_c], w=[b_t1[i]])
        if t >= 2:
            lt = t - 2
            self.op("pool", lambda e, i=i, lt=lt: e.tensor_tensor(t2[i][:], t1[i][:],
                                                                  cos[:, lt, :].unsqueeze(1).to_broadcast([128, 20, 64]), ALU.mult),
                    r=[b_t1[i], b_c], w=[b_t2[i]])
            v1 = t1[i][:].rearrange("p h (a s j) -> p h a s j", a=2, s=2)
            v3 = t3[i][:].rearrange("p h (a s j) -> p h a s j", a=2, s=2)
            sv = sin[:, lt, :].rearrange("p (a s j) -> p a s j", a=2, s=2)
            for s in range(2):
                self.op("dve", lambda e, s=s, v1=v1, v3=v3, sv=sv: e.tensor_tensor(
                    v3[:, :, :, s, :], v1[:, :, :, 1 - s, :], sv[:, :, s, :].unsqueeze(1).to_broadcast([128, 20, 2, 16]), ALU.mult),
                    r=[b_t1[i], b_c], w=[b_t3[i]])
            self.op("pool", lambda e, i=i: e.tensor_tensor(qkr[i][:], t2[i][:], t3[i][:], ALU.add),
                    r=[b_t2[i], b_t3[i]], w=[b_qkr[i]])
        else:
            self.op("pool", lambda e, i=i: e.tensor_copy(qkr[i][:], t1[i][:]), r=[b_t1[i]], w=[b_qkr[i]])
        for dup in range(2):
            self.op("pool", lambda e, i=i, dup=dup: e.tensor_copy(k2[i][:, :, dup, dup, :], qkr[i][:, 16:20, :]),
                    r=[b_qkr[i]], w=[b_k2[i]])
        self.op("pool", lambda e, i=i, t=t: e.tensor_copy(vaug[:, t, :, 0:64], slab[i][:, 1280:1536].rearrange("p (j d) -> p j d", d=64)),
                r=[b_slab[i]], w=[b_vaug])
        pq = self.ps[6][:].bitcast(BF16)
        pk = self.ps[7][:].bitcast(BF16)
        qflat = qkr[i][:].rearrange("p h d -> p (h d)")
        for pr in range(8):
            self.op("pe", lambda e, pr=pr, pq=pq, qflat=qflat: e.transpose(pq[:, pr * 128:(pr + 1) * 128], qflat[:, pr * 128:(pr + 1) * 128],
                                                                           self.identb[:]),
                    r=[b_qkr[i], self.b_const], w=[self.b_ps[6]])
        self.op("act", lambda e, t=t, pq=pq: e.activation(out=qT[:, :, t * 128:(t + 1) * 128], in_=pq.rearrange("p (a b) -> p a b", b=128),
                                                          func=AF.Copy), r=[self.b_ps[6]], w=[b_qT])
        kflat = k2[i][:].rearrange("p j u h d -> p (j u h d)")
        for j in range(8):
            self.op("pe", lambda e, j=j, pk=pk, kflat=kflat: e.transpose(pk[:, j * 128:(j + 1) * 128], kflat[:, j * 128:(j + 1) * 128],
                                                                         self.identb[:]),
                    r=[b_k2[i], self.b_const], w=[self.b_ps[7]])
        self.op("dve", lambda e, t=t, pk=pk: e.tensor_copy(kT2[:, :, :, t * 128:(t + 1) * 128], pk[:, 0:1024].rearrange("p (a u b) -> p a u b", u=2, b=128)),
                r=[self.b_ps[7]], w=[b_kT2])
    self.dbg(f"qT{l}", qT[:], [128, 8, TT], BF16, r=[b_qT])
    self.dbg(f"kT2{l}", kT2[:], [128, 4, 2, TT], BF16, r=[b_kT2])
    if self.upto == f"D1_{l}":
        return
    E = [self.sb(f"E{i}", [128, 5, 4, 128], BF16) for i in range(2)]
    att = [self.sb(f"att{i}", [128, 1024], BF16) for i in range(2)]
    den = [self.sb(f"den{i}", [128, 8], F32) for i in range(2)]
    b_E, b_att, b_den = [Buf(), Buf()], [Buf(), Buf()], [Buf(), Buf()]
    d_att = [self.ds(), self.ds()]
    sink_v = sinke[:].rearrange("q (j i p) -> q j p i", j=4, i=2, p=2)
    u = 0
    sc = 0
    tq0 = 0 if sl == 0 else 2
    for tq in range(tq0, NT):
        ai = tq % 2
        for j in range(4):
            ei = u % 2
            bo = 4 + (u % 2)
            u += 1
            if tq < 2:
                keys = [0, 1]
            else:
                keys = [0, 1] + [tk for tk in (tq - 1, tq, tq + 1) if 2 <= tk < NT]
            for ki, tk in enumerate(keys):
                bk = sc % 4
                sc += 1
                for p in range(2):
                    self.op("pe", lambda e, p=p, bk=bk, tk=tk, tq=tq, j=j: e.matmul(
                        self.ps[bk][:, p * 256:(p + 1) * 256], lhsT=kT2[:, j, p, tk * 128:(tk + 1) * 128],
                        rhs=qT[:, 2 * j:2 * j + 2, tq * 128:(tq + 1) * 128], start=True, stop=True),
                        r=[b_qT, b_kT2], w=[self.b_ps[bk]])
                self.op("act", lambda e, ei=ei, ki=ki, bk=bk: e.activation(out=E[ei][:, ki, :, :].rearrange("p a b -> p (a b)"),
                                                                            in_=self.ps[bk][:], func=AF.Exp),
                        r=[self.b_ps[bk]], w=[b_E[ei]])
                if tq >= 2 and tk >= 2 and tk != tq:
                    mk = maskp if tk == tq - 1 else maskn
                    self.op("pool", lambda e, ei=ei, ki=ki, mk=mk: e.tensor_tensor(
                        E[ei][:, ki, :, :], E[ei][:, ki, :, :], mk[:].unsqueeze(1).to_broadcast([128, 4, 128]), ALU.mult),
                        r=[b_c], w=[b_E[ei]])
            nk = len(keys)
            for slot in range(4):
                for ki, tk in enumerate(keys):
                    self.op("pe", lambda e, slot=slot, ki=ki, tk=tk, ei=ei, bo=bo, j=j, nk=nk: e.matmul(
                        self.ps[bo][:, slot * 128:slot * 128 + 65], lhsT=E[ei][:, ki, slot, :], rhs=vaug[:, tk, j, :],
                        start=(ki == 0), stop=(ki == nk - 1)), r=[b_E[ei], b_vaug], w=[self.b_ps[bo]])
            ov = self.ps[bo][:].rearrange("q (s d) -> q s d", d=128)
            self.op("dve", lambda e, ai=ai, ov=ov, j=j: e.tensor_tensor(
                den[ai][:, 0:4].rearrange("q (p i) -> q p i", p=2), ov[:, :, 64:65].rearrange("q (p i) o -> q p (i o)", p=2),
                sink_v[:, j], ALU.add), r=[self.b_ps[bo], b_c2], w=[b_den[ai]])
            self.op("dve", lambda e, ai=ai: e.reciprocal(den[ai][:, 4:8], den[ai][:, 0:4]), r=[b_den[ai]], w=[b_den[ai]])
            self.op("dve", lambda e, ai=ai, ov=ov, j=j: e.tensor_tensor(
                att[ai][:, j * 256:(j + 1) * 256].rearrange("q (i p d) -> q p i d", i=2, p=2),
                ov[:, :, 0:64].rearrange("q (p i) d -> q p i d", p=2),
                den[ai][:, 4:8].rearrange("q (p i) -> q p i", p=2).unsqueeze(3).to_broadcast([128, 2, 2, 64]), ALU.mult),
                r=[self.b_ps[bo], b_den[ai]], w=[b_att[ai]])
        self.dma("sp", self.CATD[tq * 128:(tq + 1) * 128, 0:1024], att[ai][:], r=[b_att[ai]], w=[self.b_catd[tq]], dsem=d_att[ai])
    self.dbg(f"ATT{l}", self.CATD, [TT, D], BF16, r=self.b_catd)
    self.barrier()


Builder.phase_D = phase_D


def phase_E(self, l):
    sl = self.cur_layer
    self.phase_reset()
    HS = self.sb("HS", [128, NT, 1024], F32)
    after_hs = self.sb_off
    mqT = self.sb("mqT", [128, 4, TT], BF16)
    mkT = self.sb("mkT", [128, 4, TT], BF16)
    mk = self.sb("mk", [128, NT, 512], BF16)
    vaug = self.sb("mvaug", [128, NT, 4, 257], BF16)
    G = self.sb("G", [128, NT, 16], F32)
    GI = self.sb("GI", [128, NT, 16], F32)
    E1 = self.sb("E1", [128, NT, 16], F32)
    LF = self.sb("LF", [128, NT, 16], F32)
    A = self.sb("Acol", [128, NT, 8], F32)
    bg = self.sb("bg", [128, 16], F32)
    tri = [self.sb("trif", [128, 128], F32), self.sb("trib", [128, 128], F32)]
    CT = self.sb("CT", [128, 8, 257], F32)
    CTb = self.sb("CTb", [128, 8, 257], BF16)
    LFB = [self.sb(f"LFB{i}", [128, 128], F32) for i in range(2)]
    DT = [self.sb(f"DT{i}", [128, 128], F32) for i in range(2)]
    EB = [self.sb(f"EB{i}", [128, 128], F32) for i in range(2)]
    DTm = [self.sb(f"DTm{i}", [128, 128], F32) for i in range(2)]
    WT = [self.sb(f"WT{i}", [128, 128], BF16) for i in range(2)]
    qTs = [self.sb(f"qTs{i}", [128, 128], BF16) for i in range(2)]
    kws = [self.sb(f"kws{i}", [128, 128], BF16) for i in range(2)]
    dd = [self.sb(f"dd{i}", [128, 2], F32) for i in range(2)]
    b_in, b_g, b_A, b_v = Buf(), Buf(), Buf(), Buf()
    b_HS = [[Buf() for h in range(4)] for c in range(NT)]
    b_CT = [Buf() for _ in range(8)]
    b_CTb = [Buf() for _ in range(8)]
    b_LFB, b_DT, b_EB, b_DTm, b_WT, b_qTs, b_kws, b_dd = ([Buf(), Buf()] for _ in range(8))
    d0 = self.ds()
    d1 = self.ds()
    self.dma("sp", mqT[:], self.MQT.rearrange("h p t -> p h t"), r=[self.b_mqt], w=[b_in], dsem=d0)
    self.dma("sp", mkT[:], self.MKT.rearrange("h p t -> p h t"), r=[self.b_mkt], w=[b_in], dsem=d0)
    self.dma("sp", tri[0][:], self.cst["tri_f"], w=[b_in], dsem=d0)
    self.dma("sp", tri[1][:], self.cst["tri_b"], w=[b_in], dsem=d0)
    self.dma("sp", bg[:], self.b_gates[l:l + 1, :].partition_broadcast(128), w=[b_g], dsem=d0)
    self.dma("sp", G[:], self.GD.rearrange("(t p) g -> p t g", p=128), r=self.b_gd, w=[b_g], dsem=d0)
    self.op("pool", lambda e: e.memset(vaug[:].rearrange("p t h v -> p (t h) v")[:, :, 256:257], 1.0), w=[b_v])
    for t in range(NT):
        self.dma("sp", mk[:, t, :], self.PX[t * 128:(t + 1) * 128, 2048:2560], r=[self.b_px[t]], w=[b_in], dsem=d1)
        self.dma("sp", vaug[:, t, :, 0:256], self.PX[t * 128:(t + 1) * 128, 2560:3584].rearrange("p (h v) -> p h v", v=256),
                 r=[self.b_px[t]], w=[b_v], dsem=d1)
    self.op("pool", lambda e: e.memset(CT[:].rearrange("p a b -> p (a b)"), 0.0), w=b_CT)
    self.op("pool", lambda e: e.memset(CTb[:].rearrange("p a b -> p (a b)"), 0.0), w=b_CTb)
    Gf = G[:].rearrange("p t g -> p (t g)")
    GIf = GI[:].rearrange("p t g -> p (t g)")
    E1f = E1[:].rearrange("p t g -> p (t g)")
    LFf = LF[:].rearrange("p t g -> p (t g)")
    self.op("dve", lambda e: e.tensor_tensor(G[:], G[:], bg[:].unsqueeze(1).to_broadcast([128, NT, 16]), ALU.add), r=[b_g], w=[b_g])
    self.op("act", lambda e: e.activation(out=Gf, in_=Gf, func=AF.Tanh, scale=1.0 / 15.0), r=[b_g], w=[b_g])
    self.op("dve", lambda e: e.tensor_scalar(GIf, Gf, 15.0, None, ALU.mult), r=[b_g], w=[b_g])
    P1 = self.sb("P1", [128, NT * 16], F32)
    Y2 = self.sb("Y2", [128, NT * 16], F32)
    self.op("dve", lambda e: e.tensor_scalar(E1f, GIf, -1.0, None, ALU.mult), r=[b_g], w=[b_g])
    self.op("dve", lambda e: e.tensor_tensor(E1f, E1f, GIf, ALU.max), r=[b_g], w=[b_g])
    self.op("act", lambda e: e.activation(out=E1f, in_=E1f, func=AF.Exp, scale=-1.0), r=[b_g], w=[b_g])
    self.op("dve", lambda e: e.tensor_scalar(P1[:], E1f, 2.0, None, ALU.add), r=[b_g], w=[b_g])
    self.op("dve", lambda e: e.reciprocal(P1[:], P1[:]), r=[b_g], w=[b_g])
    self.op("dve", lambda e: e.tensor_tensor(E1f, E1f, P1[:], ALU.mult), r=[b_g], w=[b_g])
    self.op("dve", lambda e: e.tensor_tensor(Y2[:], E1f, E1f, ALU.mult), r=[b_g], w=[b_g])
    self.op("dve", lambda e: e.tensor_scalar(P1[:], Y2[:], 1.0 / 13.0, 1.0 / 11.0, ALU.mult, ALU.add), r=[b_g], w=[b_g])
    for cc in (1.0 / 9.0, 1.0 / 7.0, 1.0 / 5.0, 1.0 / 3.0, 1.0):
        self.op("dve", lambda e: e.tensor_tensor(P1[:], P1[:], Y2[:], ALU.mult), r=[b_g], w=[b_g])
        self.op("dve", lambda e, cc=cc: e.tensor_scalar(P1[:], P1[:], cc, None, ALU.add), r=[b_g], w=[b_g])
    self.op("dve", lambda e: e.tensor_tensor(P1[:], P1[:], E1f, ALU.mult), r=[b_g], w=[b_g])
    self.op("dve", lambda e: e.tensor_scalar(E1f, GIf, 0.0, None, ALU.min), r=[b_g], w=[b_g])
    self.op("dve", lambda e: e.scalar_tensor_tensor(LFf, P1[:], -2.0, E1f, ALU.mult, ALU.add), r=[b_g], w=[b_g])
    for d in range(2):
        self.op("pe", lambda e, d=d: e.matmul(self.ps[d][:, 0:NT * 4], lhsT=tri[d][:], rhs=LF[:, :, 4 + 8 * d:8 + 8 * d],
                                              start=True, stop=True), r=[b_g, b_in], w=[self.b_ps[d]])
        self.op("dve", lambda e, d=d: e.tensor_tensor(A[:, :, 4 * d:4 * d + 4], GI[:, :, 8 * d:8 * d + 4],
                                                      self.ps[d][:, 0:NT * 4].rearrange("p (c h) -> p c h", h=4), ALU.subtract),
                r=[b_g, self.b_ps[d]], w=[b_A])
    order = [list(range(NT)), [1, 0] + list(range(NT - 1, 1, -1))]
    u = 0
    hs_written = set()
    for step in range(NT):
        for d in range(2):
            c = order[d][step]
            tl = 127 if d == 0 else 0
            tsl = slice(c * 128, (c + 1) * 128)
            need_h = not (sl == 1 and c < 2)
            for h in range(4):
                par = u % 2
                u += 1
                gf = 4 + 8 * d + h
                ch = d * 4 + h
                pa, pb, pc, pd = (4 * par + i for i in range(4))
                self.op("pool", lambda e, par=par, c=c, gf=gf: e.tensor_copy(LFB[par][:], LF[:, c, gf:gf + 1].to_broadcast([128, 128])),
                        r=[b_g], w=[b_LFB[par]])
                self.op("pe", lambda e, par=par, d=d, pb=pb: e.matmul(self.ps[pb][:, 0:128], lhsT=LFB[par][:], rhs=tri[d][:],
                                                                     start=True, stop=True),
                        r=[b_LFB[par], b_in], w=[self.b_ps[pb]])
                self.op("pe", lambda e, h=h, tsl=tsl, pa=pa: e.matmul(self.ps[pa][:, 0:128], lhsT=mkT[:, h, tsl], rhs=mqT[:, h, tsl],
                                                                     start=True, stop=True),
                        r=[b_in], w=[self.b_ps[pa]])
                self.op("act", lambda e, par=par, pb=pb, c=c, ch=ch: e.activation(out=DT[par][:], in_=self.ps[pb][:, 0:128], func=AF.Exp,
                                                                                 bias=A[:, c, ch:ch + 1]),
                        r=[self.b_ps[pb], b_A], w=[b_DT[par]])
                self.op("act", lambda e, par=par, pb=pb: e.activation(out=EB[par][:], in_=self.ps[pb][:, 0:128], func=AF.Exp),
                        r=[self.b_ps[pb]], w=[b_EB[par]])
                self.op("pool", lambda e, par=par, d=d: e.tensor_tensor(DTm[par][:], DT[par][:], tri[d][:], ALU.mult),
                        r=[b_DT[par], b_in], w=[b_DTm[par]])
                self.op("dve", lambda e, par=par, pa=pa: e.tensor_tensor(WT[par][:], self.ps[pa][:, 0:128], DTm[par][:], ALU.mult),
                        r=[self.b_ps[pa], b_DTm[par]], w=[b_WT[par]])
                self.op("dve", lambda e, par=par, h=h, tsl=tsl: e.tensor_tensor(qTs[par][:], mqT[:, h, tsl], EB[par][:], ALU.mult),
                        r=[b_in, b_EB[par]], w=[b_qTs[par]])
                self.op("pool", lambda e, par=par, c=c, h=h, tl=tl: e.tensor_scalar(kws[par][:], mk[:, c, h * 128:(h + 1) * 128],
                                                                                   DTm[par][:, tl:tl + 1], None, ALU.mult),
                        r=[b_in, b_DTm[par]], w=[b_kws[par]])
                if need_h:
                    self.op("pe", lambda e, par=par, c=c, h=h, pc=pc: e.matmul(self.ps[pc][:, 0:257], lhsT=WT[par][:], rhs=vaug[:, c, h, :],
                                                                              start=True, stop=False),
                            r=[b_WT[par], b_v], w=[self.b_ps[pc]])
                    self.op("pe", lambda e, par=par, ch=ch, pc=pc: e.matmul(self.ps[pc][:, 0:257], lhsT=qTs[par][:], rhs=CTb[:, ch, :],
                                                                           start=False, stop=True),
                            r=[b_qTs[par], b_CTb[ch]], w=[self.b_ps[pc]])
                    self.op("dve", lambda e, par=par, pc=pc: e.tensor_scalar(dd[par][:, 0:1], self.ps[pc][:, 256:257], -1.0, None, ALU.mult),
                            r=[self.b_ps[pc]], w=[b_dd[par]])
                    self.op("dve", lambda e, par=par, pc=pc: e.scalar_tensor_tensor(dd[par][:, 0:1], self.ps[pc][:, 256:257], 1.0, dd[par][:, 0:1],
                                                                                   ALU.max, ALU.max),
                            r=[self.b_ps[pc], b_dd[par]], w=[b_dd[par]])
                    self.op("dve", lambda e, par=par: e.reciprocal(dd[par][:, 1:2], dd[par][:, 0:1]), r=[b_dd[par]], w=[b_dd[par]])
                    hs = HS[:, c, h * 256:(h + 1) * 256]
                    first = (c, h) not in hs_written
                    hs_written.add((c, h))
                    if first:
                        self.op("act", lambda e, par=par, pc=pc, hs=hs: e.activation(out=hs, in_=self.ps[pc][:, 0:256], func=AF.Identity,
                                                                                    scale=dd[par][:, 1:2]),
                                r=[self.b_ps[pc], b_dd[par]], w=[b_HS[c][h]])
                    else:
                        self.op("dve", lambda e, par=par, pc=pc, hs=hs: e.scalar_tensor_tensor(hs, self.ps[pc][:, 0:256], dd[par][:, 1:2], hs,
                                                                                              ALU.mult, ALU.add),
                                r=[self.b_ps[pc], b_dd[par]], w=[b_HS[c][h]])
                if step < NT - 1:
                    self.op("pe", lambda e, par=par, c=c, h=h, pd=pd: e.matmul(self.ps[pd][:, 0:257], lhsT=kws[par][:], rhs=vaug[:, c, h, :],
                                                                              start=True, stop=True),
                            r=[b_kws[par], b_v], w=[self.b_ps[pd]])
                    self.op("dve", lambda e, par=par, ch=ch, pd=pd, tl=tl: e.scalar_tensor_tensor(CT[:, ch, :], CT[:, ch, :], EB[par][:, tl:tl + 1],
                                                                                                 self.ps[pd][:, 0:257], ALU.mult, ALU.add),
                            r=[self.b_ps[pd], b_EB[par]], w=[b_CT[ch]])
                    self.op("act", lambda e, ch=ch: e.activation(out=CTb[:, ch, :], in_=CT[:, ch, :], func=AF.Copy),
                            r=[b_CT[ch]], w=[b_CTb[ch]])
    t0 = 0 if sl == 0 else 2
    self.dbg(f"HS{l}", HS[:], [128, NT, 1024], F32, r=[b for c in range(NT) for b in b_HS[c]])
    if self.upto == f"E1_{l}":
        return
    self.barrier()
    self.sb_off = after_hs
    nw = self.sb("nw", [128, 1024], F32)
    mo = [self.sb(f"mo{i}", [128, 1024], BF16) for i in range(2)]
    t1 = [self.sb(f"et1_{i}", [128, 1024], F32) for i in range(2)]
    sg = [self.sb(f"esg{i}", [128, 1024], F32) for i in range(2)]
    ob = [self.sb(f"eob{i}", [128, 1024], BF16) for i in range(2)]
    junk = self.sb("ejunk", [128, 256], BF16)
    ss = [self.sb(f"ess{i}", [128, 16], F32) for i in range(2)]
    b_nw, b_junk = Buf(), Buf()
    b_mo, b_t1, b_sg, b_ob, b_ss = ([Buf(), Buf()] for _ in range(5))
    dn = self.ds()
    d_mo = [self.ds(), self.ds()]
    d_ob = [self.ds(), self.ds()]
    self.dma("sp", nw[:], self.mlstm_norm_w[l:l + 1, :].partition_broadcast(128), w=[b_nw], dsem=dn)
    for c in range(t0, NT):
        i = c % 2
        rhs_all = b_HS[c]
        self.dma("sp", mo[i][:], self.PX[c * 128:(c + 1) * 128, 3584:4608], r=[self.b_px[c]], w=[b_mo[i]], dsem=d_mo[i])
        for h in range(4):
            self.op("act", lambda e, i=i, c=c, h=h: e.activation(out=junk[:], in_=HS[:, c, h * 256:(h + 1) * 256], func=AF.Square,
                                                                 accum_out=ss[i][:, h:h + 1]),
                    r=[b_HS[c][h]], w=[b_ss[i], b_junk])
        self.op("dve", lambda e, i=i: e.tensor_scalar(ss[i][:, 4:8], ss[i][:, 0:4], 1.0 / 256, EPS, ALU.mult, ALU.add), r=[b_ss[i]], w=[b_ss[i]])
        self.op("act", lambda e, i=i: e.activation(out=ss[i][:, 8:12], in_=ss[i][:, 4:8], func=AF.Sqrt), r=[b_ss[i]], w=[b_ss[i]])
        self.op("dve", lambda e, i=i: e.reciprocal(ss[i][:, 12:16], ss[i][:, 8:12]), r=[b_ss[i]], w=[b_ss[i]])
        self.op("dve", lambda e, i=i, c=c: e.tensor_tensor(t1[i][:].rearrange("p (h v) -> p h v", v=256),
                                                           HS[:, c, :].rearrange("p (h v) -> p h v", v=256),
                                                           ss[i][:, 12:16].unsqueeze(2).to_broadcast([128, 4, 256]), ALU.mult),
                r=rhs_all + [b_ss[i]], w=[b_t1[i]])
        self.op("pool", lambda e, i=i: e.tensor_tensor(t1[i][:], t1[i][:], nw[:], ALU.mult), r=[b_nw], w=[b_t1[i]])
        self.op("act", lambda e, i=i: e.activation(out=sg[i][:], in_=mo[i][:], func=AF.Sigmoid), r=[b_mo[i]], w=[b_sg[i]])
        self.op("dve", lambda e, i=i: e.tensor_tensor(ob[i][:], t1[i][:], sg[i][:], ALU.mult), r=[b_t1[i], b_sg[i]], w=[b_ob[i]])
        self.dma("sp", self.CATD[c * 128:(c + 1) * 128, 1024:2048], ob[i][:], r=[b_ob[i]], w=[self.b_catd[c]], dsem=d_ob[i])
    self.dbg(f"CAT{l}", self.CATD, [TT, D], BF16, r=self.b_catd)
    self.barrier()


Builder.phase_E = phase_E


def phase_F(self, l):
    sl = self.cur_layer
    self.phase_reset()
    src = self.x_in if l == 0 else self.XRES
    wout = self.sb("wout", [128, 16, D], BF16)
    G1 = self.sb("G1", [128, 2, D], F32)
    cat = [self.sb(f"fcat{i}", [128, D], BF16) for i in range(2)]
    xt = [self.sb(f"fxt{i}", [128, D], F32) for i in range(2)]
    catT = [self.sb(f"fcatT{i}", [128, 16, 128], BF16) for i in range(2)]
    xo = [self.sb(f"fxo{i}", [128, D], F32) for i in range(2)]
    b_wout, b_G1 = Buf(), Buf()
    b_cat, b_xt, b_catT, b_xo = ([Buf(), Buf()] for _ in range(4))
    dw, dg = self.ds(), self.ds()
    d_cat, d_xt, d_xo = ([self.ds(), self.ds()] for _ in range(3))
    wv = self.w_out[l].rearrange("(k p) n -> p k n", p=128)
    for k0 in range(0, 16, 4):
        self.dma("pool", wout[:, k0:k0 + 4, :], wv[:, k0:k0 + 4, :], w=[b_wout], dsem=dw)
    for w_ in range(2):
        self.dma("sp", G1[:, w_, :], self.MODD[w_:w_ + 1, 2 * D:3 * D].partition_broadcast(128), r=[self.b_modd], w=[b_G1], dsem=dg)
    t0 = 0 if sl == 0 else 2
    for t in range(t0, NT):
        i = t % 2
        which = 1 if t < 2 else 0
        rows = slice(t * 128, (t + 1) * 128)
        self.dma("sp", cat[i][:], self.CATD[rows, :], r=[self.b_catd[t]], w=[b_cat[i]], dsem=d_cat[i])
        self.dma("sp", xt[i][:], src[rows, :], r=([self.b_xres[t]] if l > 0 else []), w=[b_xt[i]], dsem=d_xt[i])
        bA, bB = 4 + 2 * i, 5 + 2 * i
        pA = self.ps[bA][:].bitcast(BF16)
        pB = self.ps[bB][:].bitcast(BF16)
        for k in range(16):
            pX, bX = (pA, bA) if k < 8 else (pB, bB)
            self.op("pe", lambda e, k=k, pX=pX, i=i: e.transpose(pX[:, (k % 8) * 128:(k % 8 + 1) * 128], cat[i][:, k * 128:(k + 1) * 128],
                                                                 self.identb[:]),
                    r=[b_cat[i], self.b_const], w=[self.b_ps[bX]])
        self.op("act", lambda e, i=i, pA=pA: e.activation(out=catT[i][:, 0:8, :], in_=pA.rearrange("p (a b) -> p a b", b=128), func=AF.Copy),
                r=[self.b_ps[bA]], w=[b_catT[i]])
        self.op("dve", lambda e, i=i, pB=pB: e.tensor_copy(catT[i][:, 8:16, :], pB.rearrange("p (a b) -> p a b", b=128)),
                r=[self.b_ps[bB]], w=[b_catT[i]])
        for nb in range(4):
            cols = slice(nb * 512, (nb + 1) * 512)
            for k in range(16):
                self.op("pe", lambda e, k=k, nb=nb, i=i, cols=cols: e.matmul(self.ps[nb][:, 0:512], lhsT=catT[i][:, k, :], rhs=wout[:, k, cols],
                                                                            start=(k == 0), stop=(k == 15)),
                        r=[b_catT[i], b_wout], w=[self.b_ps[nb]])
            self.op("dve", lambda e, nb=nb, i=i, cols=cols, which=which: e.tensor_tensor(xo[i][:, cols], self.ps[nb][:, 0:512], G1[:, which, cols], ALU.mult),
                    r=[self.b_ps[nb], b_G1], w=[b_xo[i]])
            self.op("pool", lambda e, i=i, cols=cols: e.tensor_tensor(xo[i][:, cols], xo[i][:, cols], xt[i][:, cols], ALU.add),
                    r=[b_xt[i]], w=[b_xo[i]])
        self.dma("sp", self.XRES[rows, :], xo[i][:], r=[b_xo[i]], w=[self.b_xres[t]], dsem=d_xo[i])
    self.dbg(f"XMID{l}", self.XRES, [TT, D], F32, r=self.b_xres)
    self.barrier()


Builder.phase_F = phase_F


def phase_G(self, l):
    sl = self.cur_layer
    last = (sl == self.last_layer)
    self.phase_reset()
    NX = self.n_exp
    wr = self.sb("wr", [128, 16, NE], F32)
    br = self.sb("br", [128, NE], F32)
    bguT = self.sb("bguT", [128, 16, NE], F32)
    bd = self.sb("bd", [NE, D], F32)
    G2 = self.sb("G2", [128, 2, D], F32)
    selb = [self.sb(f"selb{i}", [NE, 128], F32) for i in range(2)]
    mark = self.sb_off
    braw = self.sb("braw", [NE, D], F32)
    b_c, b_braw = Buf(), Buf()
    dc = self.ds()
    self.dma("sp", wr[:], self.w_router[l].rearrange("(k p) e -> p k e", p=128), w=[b_c], dsem=dc)
    self.dma("sp", br[:], self.b_router[l:l + 1, :].partition_broadcast(128), w=[b_c], dsem=dc)
    self.dma("sp", bd[0:NX, :], self.b_down[l], w=[b_c], dsem=dc)
    self.dma("sp", braw[0:NX, :], self.b_gate_up[l], w=[b_braw], dsem=dc)
    for w_ in range(2):
        self.dma("sp", G2[:, w_, :], self.MODD[w_:w_ + 1, 5 * D:6 * D].partition_broadcast(128), r=[self.b_modd], w=[b_c], dsem=dc)
    for j in range(16):
        self.op("pe", lambda e, j=j: e.transpose(self.ps[0][:, j * NE:j * NE + NX], braw[0:NX, j * 128:(j + 1) * 128],
                                                 self.identf[0:NX, 0:NX]),
                r=[b_braw, self.b_const], w=[self.b_ps[0]])
    self.op("act", lambda e: e.activation(out=bguT[:, :, 0:NX], in_=self.ps[0][:, 0:16 * NE].rearrange("p (j e) -> p j e", e=NE)[:, :, 0:NX],
                                          func=AF.Copy), r=[self.b_ps[0]], w=[b_c])
    self.barrier()
    self.sb_off = mark
    fxT = self.sb("fxT", [128, 16, 512], BF16)
    acc = self.sb("acc", [128, 4, D], F32)
    actT = self.sb("actT", [128, 8, 512], BF16)
    wdb = self.sb("wdb", [128, 8, D], BF16)
    wgb = [self.sb(f"wgb{i}", [128, 16, 2, 128], BF16) for i in range(3)]
    CB = [self.sb(f"CB{i}", [128, 512], F32) for i in range(2)]
    gS = [self.sb(f"gS{i}", [128, 512], F32) for i in range(2)]
    sg = [self.sb(f"sg{i}", [128, 512], F32) for i in range(2)]
    uS = [self.sb(f"uS{i}", [128, 512], F32) for i in range(2)]
    xt = self.sb("gxt", [128, D], F32)
    xn = self.sb("gxn", [128, D], F32)
    f32T = self.sb("gf32T", [128, 16, 128], F32)
    junk = self.sb("gjunk", [128, D], BF16)
    ss = self.sb("gss", [128, 4], F32)
    combT = self.sb("combT", [NE, 512], F32)
    lg = self.sb("lg", [128, NE], F32)
    ex = self.sb("ex", [128, NE], F32)
    msk = self.sb("msk", [128, NE], F32)
    mx8 = self.sb("mx8", [128, 8], F32)
    sm = self.sb("sm", [128, 4], F32)
    b_fxT, b_actT, b_wdb, b_xt, b_f32T, b_combT, b_r = (Buf() for _ in range(7))
    b_acc = [Buf() for _ in range(4)]
    b_wgb = [Buf() for _ in range(3)]
    b_CB, b_gS, b_sg, b_uS, b_sel = ([Buf(), Buf()] for _ in range(5))
    b_tmp = (Buf(), Buf())
    d_xt = self.ds()
    d_wd = self.ds()
    d_wg = [self.ds() for _ in range(3)]
    d_out = self.ds()
    tiles_all = list(range(0 if sl == 0 else 2, NT))
    groups = [tiles_all[i:i + 4] for i in range(0, len(tiles_all), 4)]
    cnt = 0
    dcnt = 0
    for tiles in groups:
        ntok = 128 * len(tiles)
        for j, t in enumerate(tiles):
            which = 1 if t < 2 else 0
            rows = slice(t * 128, (t + 1) * 128)
            self.dma("sp", xt[:], self.XRES[rows, :], r=[self.b_xres[t]], w=[b_xt], dsem=d_xt)
            self.norm_tile(xt, b_xt, which, self.S2, 48, f32T, b_f32T, fxT, b_fxT, j * 128, [4, 5, 6, 7], junk, ss, xn, b_tmp)
            for k in range(16):
                self.op("pe", lambda e, k=k: e.matmul(self.ps[0][:, 0:NE], lhsT=f32T[:, k, :], rhs=wr[:, k, :], start=(k == 0), stop=(k == 15)),
                        r=[b_f32T, b_c], w=[self.b_ps[0]])
            self.op("dve", lambda e: e.tensor_tensor(lg[:], self.ps[0][:, 0:NE], br[:], ALU.add), r=[self.b_ps[0], b_c], w=[b_r])
            self.op("dve", lambda e: e.max(out=mx8[:], in_=lg[:]), r=[b_r], w=[b_r])
            self.op("dve", lambda e: e.tensor_scalar(msk[:], lg[:], mx8[:, 3:4], None, ALU.is_ge), r=[b_r], w=[b_r])
            self.op("dve", lambda e: e.tensor_scalar(sm[:, 0:1], mx8[:, 0:1], -1.0, None, ALU.mult), r=[b_r], w=[b_r])
            self.op("act", lambda e: e.activation(out=ex[:], in_=lg[:], func=AF.Exp, bias=sm[:, 0:1]), r=[b_r], w=[b_r])
            self.op("dve", lambda e: e.tensor_tensor(ex[:], ex[:], msk[:], ALU.mult), r=[b_r], w=[b_r])
            self.op("dve", lambda e: e.tensor_reduce(sm[:, 1:2], ex[:], AX.X, ALU.add), r=[b_r], w=[b_r])
            self.op("dve", lambda e: e.reciprocal(sm[:, 2:3], sm[:, 1:2]), r=[b_r], w=[b_r])
            self.op("dve", lambda e: e.tensor_scalar(ex[:], ex[:], sm[:, 2:3], None, ALU.mult), r=[b_r], w=[b_r])
            self.op("pe", lambda e: e.transpose(self.ps[1][0:NE, 0:128], ex[:], self.identf[:]), r=[b_r, self.b_const], w=[self.b_ps[1]])
            self.op("act", lambda e, j=j: e.activation(out=combT[:, j * 128:(j + 1) * 128], in_=self.ps[1][0:NE, 0:128], func=AF.Copy),
                    r=[self.b_ps[1]], w=[b_combT])
        if f"COMB{l}" in self.debug and tiles is groups[0]:
            self.dbg(f"COMB{l}", combT[:], [NE, 512], F32, r=[b_combT])
        for j in range(len(tiles)):
            for nb in range(4):
                bk = 2 + (nb % 2)
                cols = slice(nb * 512, (nb + 1) * 512)
                self.op("pe", lambda e, j=j, bk=bk, cols=cols: e.matmul(self.ps[bk][:, 0:512], lhsT=combT[0:NX, j * 128:(j + 1) * 128],
                                                                        rhs=bd[0:NX, cols], start=True, stop=True),
                        r=[b_combT, b_c], w=[self.b_ps[bk]])
                self.op("act", lambda e, j=j, bk=bk, cols=cols: e.activation(out=acc[:, j, cols], in_=self.ps[bk][:, 0:512], func=AF.Copy),
                        r=[self.b_ps[bk]], w=[b_acc[j]])
        for ex_i in range(NX):
            si = ex_i % 2
            self.op("dve", lambda e, si=si, ex_i=ex_i: e.tensor_copy(selb[si][:], self.identf[0:NE, ex_i:ex_i + 1].to_broadcast([NE, 128])),
                    r=[self.b_const], w=[b_sel[si]])
            self.op("pe", lambda e, si=si, ntok=ntok: e.matmul(self.ps[7][:, 0:ntok], lhsT=selb[si][:], rhs=combT[:, 0:ntok], start=True, stop=True),
                    r=[b_sel[si], b_combT], w=[self.b_ps[7]])
            self.op("act", lambda e, si=si, ntok=ntok: e.activation(out=CB[si][:, 0:ntok], in_=self.ps[7][:, 0:ntok], func=AF.Copy),
                    r=[self.b_ps[7]], w=[b_CB[si]])
            self.dma("pool", wdb[:], self.w_down[l, ex_i].rearrange("(c p) n -> p c n", p=128), w=[b_wdb], dsem=d_wd)
            wgv = self.w_gate_up[l, ex_i].rearrange("(k p) n -> p k n", p=128)
            for fc in range(8):
                s = cnt % 3
                par = cnt % 2
                cnt += 1
                self.dma("pool", wgb[s][:, :, 0, :], wgv[:, :, fc * 128:(fc + 1) * 128], w=[b_wgb[s]], dsem=d_wg[s])
                self.dma("pool", wgb[s][:, :, 1, :], wgv[:, :, DFF + fc * 128:DFF + (fc + 1) * 128], w=[b_wgb[s]], dsem=d_wg[s])
                pg, pu = 2 * par, 2 * par + 1
                for gu, pb in ((0, pg), (1, pu)):
                    for k in range(16):
                        self.op("pe", lambda e, k=k, s=s, gu=gu, pb=pb, ntok=ntok: e.matmul(self.ps[pb][:, 0:ntok], lhsT=wgb[s][:, k, gu, :],
                                                                                         rhs=fxT[:, k, 0:ntok], start=(k == 0), stop=(k == 15)),
                                r=[b_wgb[s], b_fxT], w=[self.b_ps[pb]])
                self.op("dve", lambda e, par=par, pg=pg, fc=fc, ex_i=ex_i, ntok=ntok: e.tensor_scalar(
                    gS[par][:, 0:ntok], self.ps[pg][:, 0:ntok], bguT[:, fc, ex_i:ex_i + 1], 7.0, ALU.add, ALU.min),
                    r=[self.b_ps[pg], b_c], w=[b_gS[par]])
                self.op("act", lambda e, par=par, ntok=ntok: e.activation(out=sg[par][:, 0:ntok], in_=gS[par][:, 0:ntok], func=AF.Sigmoid, scale=1.702),
                        r=[b_gS[par]], w=[b_sg[par]])
                self.op("dve", lambda e, par=par, pu=pu, fc=fc, ex_i=ex_i, ntok=ntok: e.tensor_scalar(
                    uS[par][:, 0:ntok], self.ps[pu][:, 0:ntok], bguT[:, 8 + fc, ex_i:ex_i + 1], 7.0, ALU.add, ALU.min),
                    r=[self.b_ps[pu], b_c], w=[b_uS[par]])
                self.op("dve", lambda e, par=par, ntok=ntok: e.tensor_scalar(uS[par][:, 0:ntok], uS[par][:, 0:ntok], -7.0, 1.0, ALU.max, ALU.add),
                        r=[b_uS[par]], w=[b_uS[par]])
                self.op("dve", lambda e, par=par, ntok=ntok: e.tensor_tensor(gS[par][:, 0:ntok], gS[par][:, 0:ntok], sg[par][:, 0:ntok], ALU.mult),
                        r=[b_sg[par]], w=[b_gS[par]])
                self.op("dve", lambda e, par=par, si=si, ntok=ntok: e.tensor_tensor(gS[par][:, 0:ntok], gS[par][:, 0:ntok], CB[si][:, 0:ntok], ALU.mult),
                        r=[b_CB[si]], w=[b_gS[par]])
                self.op("dve", lambda e, par=par, fc=fc, ntok=ntok: e.tensor_tensor(actT[:, fc, 0:ntok], uS[par][:, 0:ntok], gS[par][:, 0:ntok], ALU.mult),
                        r=[b_uS[par], b_gS[par]], w=[b_actT])
            for j in range(len(tiles)):
                for nb in range(4):
                    bk = 4 + (dcnt % 3)
                    dcnt += 1
                    cols = slice(nb * 512, (nb + 1) * 512)
                    for fc in range(8):
                        self.op("pe", lambda e, j=j, fc=fc, bk=bk, cols=cols: e.matmul(self.ps[bk][:, 0:512], lhsT=actT[:, fc, j * 128:(j + 1) * 128],
                                                                                     rhs=wdb[:, fc, cols], start=(fc == 0), stop=(fc == 7)),
                                r=[b_actT, b_wdb], w=[self.b_ps[bk]])
                    self.op("dve", lambda e, j=j, bk=bk, cols=cols: e.tensor_tensor(acc[:, j, cols], self.ps[bk][:, 0:512], acc[:, j, cols], ALU.add),
                            r=[self.b_ps[bk]], w=[b_acc[j]])
        for j, t in enumerate(tiles):
            which = 1 if t < 2 else 0
            rows = slice(t * 128, (t + 1) * 128)
            self.dma("sp", xt[:], self.XRES[rows, :], r=[self.b_xres[t]], w=[b_xt], dsem=d_xt)
            self.op("dve", lambda e, j=j, which=which: e.tensor_tensor(acc[:, j, :], acc[:, j, :], G2[:, which, :], ALU.mult), r=[b_c], w=[b_acc[j]])
            self.op("pool", lambda e, j=j: e.tensor_tensor(acc[:, j, :], acc[:, j, :], xt[:], ALU.add), r=[b_xt], w=[b_acc[j]])
            if last:
                by = Buf()
                self.dma("sp", self.y_out[(t - 2) * 128:(t - 1) * 128, :], acc[:, j, :], r=[b_acc[j]], w=[by], dsem=d_out)
                self.final_reads.append(by)
            else:
                self.dma("sp", self.XRES[rows, :], acc[:, j, :], r=[b_acc[j]], w=[self.b_xres[t]], dsem=d_out)
    if not last:
        self.dbg(f"XOUT{l}", self.XRES, [TT, D], F32, r=self.b_xres)
    else:
        self.dbg(f"YOUT{l}", self.y_out, [2048, D], F32, r=self.final_reads)
    self.barrier()


Builder.phase_G = phase_G


def phase_G2(self, l):
    sl = self.cur_layer
    last = (sl == self.last_layer)
    self.phase_reset()
    NX = self.n_exp
    NR = NE * CAP
    wr = self.sb("wr", [128, 16, NE], F32)
    br = self.sb("br", [128, NE], F32)
    bguT = self.sb("bguT", [128, 16, NE], F32)
    bd = self.sb("bd", [NE, D], F32)
    G2 = self.sb("G2", [128, 2, D], F32)
    IDX = self.sb("IDX", [128, NT, 4], I32)
    WJ = self.sb("WJ", [128, NT, 4], F32)
    combT = self.sb("combT", [NE, TT], F32)
    OFF = self.sb("OFF", [128, NE], F32)
    ecst = self.sb("ecst", [128, NE], F32)
    tris = self.sb("tris", [128, 128], F32)
    trash = self.sb("trash", [128, 1], F32)
    mark = self.sb_off
    braw = self.sb("braw", [NE, D], F32)
    b_c, b_braw, b_idx, b_combT, b_off = Buf(), Buf(), Buf(), Buf(), Buf()
    dc = self.ds()
    self.dma("sp", wr[:], self.w_router[l].rearrange("(k p) e -> p k e", p=128), w=[b_c], dsem=dc)
    self.dma("sp", br[:], self.b_router[l:l + 1, :].partition_broadcast(128), w=[b_c], dsem=dc)
    self.dma("sp", bd[0:NX, :], self.b_down[l], w=[b_c], dsem=dc)
    self.dma("sp", braw[0:NX, :], self.b_gate_up[l], w=[b_braw], dsem=dc)
    self.dma("sp", ecst[:], self.cst["ecst"], w=[b_c], dsem=dc)
    self.dma("sp", tris[:], self.cst["tri_s"], w=[b_c], dsem=dc)
    self.dma("sp", trash[:], self.cst["trash"], w=[b_c], dsem=dc)
    for w_ in range(2):
        self.dma("sp", G2[:, w_, :], self.MODD[w_:w_ + 1, 5 * D:6 * D].partition_broadcast(128), r=[self.b_modd], w=[b_c], dsem=dc)
    for j in range(16):
        self.op("pe", lambda e, j=j: e.transpose(self.ps[0][:, j * NE:j * NE + NX], braw[0:NX, j * 128:(j + 1) * 128],
                                                 self.identf[0:NX, 0:NX]),
                r=[b_braw, self.b_const], w=[self.b_ps[0]])
    self.op("act", lambda e: e.activation(out=bguT[:, :, 0:NX], in_=self.ps[0][:, 0:16 * NE].rearrange("p (j e) -> p j e", e=NE)[:, :, 0:NX],
                                          func=AF.Copy), r=[self.b_ps[0]], w=[b_c])
    self.op("dve", lambda e: e.memset(OFF[:], 0.0), w=[b_off])
    if l == 0:
        dzf = self.ds()
        for r_ in range(0, NR + 128, 128):
            self.dma("sp", self.FXE[r_:r_ + 128, :], self.zt[:], r=[self.b_zt], w=[self.b_fxe], dsem=dzf)
    self.barrier()
    self.sb_off = mark
    tiles_all = list(range(0 if sl == 0 else 2, NT))
    xt2_ = [self.sb(f"gxt{i}", [128, D], F32) for i in range(2)]
    xn2_ = [self.sb(f"gxn{i}", [128, D], F32) for i in range(2)]
    f32T2_ = [self.sb(f"gf32T{i}", [128, 16, 128], F32) for i in range(2)]
    junk = self.sb("gjunk", [128, D], BF16)
    ss2_ = [self.sb(f"gss{i}", [128, 4], F32) for i in range(2)]
    S2r = self.sb("S2r", [128, 2, D], F32)
    SHr = self.sb("SHr", [128, 2, D], F32)
    nwr = self.sb("nwr", [128, D], F32)
    ftok = [self.sb(f"ftok{i}", [128, D], BF16) for i in range(2)]
    lg = self.sb("lg", [128, NE], F32)
    ex = self.sb("ex", [128, NE], F32)
    msk = self.sb("msk", [128, NE], F32)
    rk = self.sb("rk", [128, NE], F32)
    key = self.sb("key", [128, NE], F32)
    tmpk = self.sb("tmpk", [128, NE], F32)
    mx8 = self.sb("mx8", [128, 8], F32)
    k8 = self.sb("k8", [128, 8], F32)
    sm = self.sb("sm", [128, 4], F32)
    b_r, b_rows = Buf(), Buf()
    b_xt2_, b_f32T2_, b_ftok = ([Buf(), Buf()] for _ in range(3))
    b_tmp2_ = [(Buf(), Buf()), (Buf(), Buf())]
    d_xt2_ = [self.ds(), self.ds()]
    d_rows = self.ds()
    d_sc = [self.ds(), self.ds()]
    for w_ in range(2):
        self.dma("sp", S2r[:, w_, :], self.MODD[w_:w_ + 1, 4 * D:5 * D].partition_broadcast(128), r=[self.b_modd], w=[b_rows], dsem=d_rows)
        self.dma("sp", SHr[:, w_, :], self.MODD[w_:w_ + 1, 3 * D:4 * D].partition_broadcast(128), r=[self.b_modd], w=[b_rows], dsem=d_rows)
    self.dma("sp", nwr[:], self.norm2_w[l:l + 1, :].partition_broadcast(128), w=[b_rows], dsem=d_rows)
    for w_ in range(2):
        self.op("dve", lambda e, w_=w_: e.scalar_tensor_tensor(S2r[:, w_, :], S2r[:, w_, :], 1.0, nwr[:], ALU.add, ALU.mult), r=[b_rows], w=[b_rows])
    for t in tiles_all:
        which = 1 if t < 2 else 0
        fi = t % 2
        rows = slice(t * 128, (t + 1) * 128)
        xt, xn, f32T, ss = xt2_[fi], xn2_[fi], f32T2_[fi], ss2_[fi]
        b_xt, b_f32T, b_tmp = b_xt2_[fi], b_f32T2_[fi], b_tmp2_[fi]
        self.dma("sp", xt[:], self.XRES[rows, :], r=[self.b_xres[t]], w=[b_xt], dsem=d_xt2_[fi])
        self.norm_tile(xt, b_xt, which, self.S2, 48, f32T, b_f32T, None, None, 0, [4, 5, 6, 7], junk, ss, xn, b_tmp)
        self.op("dve", lambda e, which=which, xn=xn: e.tensor_tensor(xn[:], xn[:], S2r[:, which, :], ALU.mult), r=[b_rows], w=[b_tmp[1]])
        self.op("dve", lambda e, which=which, fi=fi, xn=xn: e.tensor_tensor(ftok[fi][:], xn[:], SHr[:, which, :], ALU.add), r=[b_rows, b_tmp[1]], w=[b_ftok[fi]])
        for k in range(16):
            self.op("pe", lambda e, k=k, f32T=f32T: e.matmul(self.ps[0][:, 0:NE], lhsT=f32T[:, k, :], rhs=wr[:, k, :], start=(k == 0), stop=(k == 15)),
                    r=[b_f32T, b_c], w=[self.b_ps[0]])
        self.op("dve", lambda e: e.tensor_tensor(lg[:], self.ps[0][:, 0:NE], br[:], ALU.add), r=[self.b_ps[0], b_c], w=[b_r])
        self.op("dve", lambda e: e.max(out=mx8[:], in_=lg[:]), r=[b_r], w=[b_r])
        self.op("dve", lambda e: e.tensor_scalar(msk[:], lg[:], mx8[:, 3:4], None, ALU.is_ge), r=[b_r], w=[b_r])
        self.op("dve", lambda e: e.tensor_scalar(sm[:, 0:1], mx8[:, 0:1], -1.0, None, ALU.mult), r=[b_r], w=[b_r])
        self.op("act", lambda e: e.activation(out=ex[:], in_=lg[:], func=AF.Exp, bias=sm[:, 0:1]), r=[b_r], w=[b_r])
        self.op("dve", lambda e: e.tensor_tensor(ex[:], ex[:], msk[:], ALU.mult), r=[b_r], w=[b_r])
        self.op("dve", lambda e: e.tensor_reduce(sm[:, 1:2], ex[:], AX.X, ALU.add), r=[b_r], w=[b_r])
        self.op("dve", lambda e: e.reciprocal(sm[:, 2:3], sm[:, 1:2]), r=[b_r], w=[b_r])
        self.op("dve", lambda e: e.tensor_scalar(ex[:], ex[:], sm[:, 2:3], None, ALU.mult), r=[b_r], w=[b_r])
        self.op("pe", lambda e: e.transpose(self.ps[1][0:NE, 0:128], ex[:], self.identf[:]), r=[b_r, self.b_const], w=[self.b_ps[1]])
        self.op("act", lambda e, t=t: e.activation(out=combT[:, t * 128:(t + 1) * 128], in_=self.ps[1][0:NE, 0:128], func=AF.Copy),
                r=[self.b_ps[1]], w=[b_combT])
        self.op("pe", lambda e: e.matmul(self.ps[2][:, 0:NE], lhsT=tris[:], rhs=msk[:], start=True, stop=True), r=[b_r, b_c], w=[self.b_ps[2]])
        self.op("pe", lambda e: e.matmul(self.ps[3][:, 0:NE], lhsT=self.onesf[:], rhs=msk[:], start=True, stop=True),
                r=[b_r, self.b_const], w=[self.b_ps[3]])
        self.op("dve", lambda e: e.tensor_tensor(rk[:], self.ps[2][:, 0:NE], OFF[:], ALU.add), r=[self.b_ps[2], b_off], w=[b_r])
        self.op("dve", lambda e: e.tensor_tensor(OFF[:], OFF[:], self.ps[3][:, 0:NE], ALU.add), r=[self.b_ps[3]], w=[b_off])
        self.op("dve", lambda e: e.tensor_scalar(tmpk[:], rk[:], float(CAP), None, ALU.is_lt), r=[b_r], w=[b_r])
        self.op("dve", lambda e: e.tensor_tensor(tmpk[:], tmpk[:], msk[:], ALU.mult), r=[b_r], w=[b_r])
        self.op("dve", lambda e: e.tensor_tensor(rk[:], rk[:], ecst[:], ALU.add), r=[b_r, b_c], w=[b_r])
        self.op("dve", lambda e: e.tensor_scalar(rk[:], rk[:], -1.0, KBIG, ALU.mult, ALU.add), r=[b_r], w=[b_r])
        self.op("dve", lambda e: e.tensor_tensor(key[:], rk[:], tmpk[:], ALU.mult), r=[b_r], w=[b_r])
        self.op("dve", lambda e: e.max(out=k8[:], in_=key[:]), r=[b_r], w=[b_r])
        self.op("dve", lambda e: e.tensor_scalar(sm[:, 0:4], k8[:, 0:4], -1.0, KBIG, ALU.mult, ALU.add), r=[b_r], w=[b_r])
        self.op("dve", lambda e: e.tensor_scalar(sm[:, 0:4], sm[:, 0:4], trash[:, 0:1], None, ALU.subtract), r=[b_r, b_c], w=[b_r])
        self.op("dve", lambda e: e.tensor_scalar(k8[:, 4:8], k8[:, 0:4], 0.0, None, ALU.is_gt), r=[b_r], w=[b_r])
        self.op("dve", lambda e: e.tensor_tensor(sm[:, 0:4], sm[:, 0:4], k8[:, 4:8], ALU.mult), r=[b_r], w=[b_r])
        self.op("dve", lambda e: e.tensor_scalar(sm[:, 0:4], sm[:, 0:4], trash[:, 0:1], None, ALU.add), r=[b_r, b_c], w=[b_r])
        self.op("dve", lambda e, t=t: e.tensor_copy(IDX[:, t, :], sm[:, 0:4]), r=[b_r], w=[b_idx])
        for j in range(4):
            self.op("dve", lambda e, j=j: e.tensor_scalar(tmpk[:], key[:], k8[:, j:j + 1], None, ALU.is_equal), r=[b_r], w=[b_r])
            self.op("dve", lambda e: e.tensor_tensor(tmpk[:], tmpk[:], ex[:], ALU.mult), r=[b_r], w=[b_r])
            self.op("dve", lambda e, j=j, t=t: e.tensor_reduce(WJ[:, t, j:j + 1], tmpk[:], AX.X, ALU.add), r=[b_r], w=[b_idx])
        for j in range(4):
            self.P.op("pool", lambda e, t=t, j=j, fi=fi: e.indirect_dma_start(
                out=self.FXE[:, :], out_offset=bass.IndirectOffsetOnAxis(ap=IDX[:, t, j:j + 1], axis=0),
                in_=ftok[fi][:, :], in_offset=None),
                reads=[b_idx, b_ftok[fi]], writes=[self.b_fxe], dsem=d_sc[fi])
    self.dbg(f"IDX{l}", IDX[:], [128, NT, 4], I32, r=[b_idx])
    self.dbg(f"WJ{l}", WJ[:], [128, NT, 4], F32, r=[b_idx])
    self.barrier()
    self.sb_off = mark
    fxT = self.sb("fxT", [128, 16, CAP], BF16)
    actT = self.sb("actT", [128, 8, CAP], BF16)
    wdb = self.sb("wdb", [128, 8, D], BF16)
    wgb = [self.sb(f"wgb{i}", [128, 16, 2, 128], BF16) for i in range(3)]
    gS = [self.sb(f"gS{i}", [128, 512], F32) for i in range(2)]
    sg = [self.sb(f"sg{i}", [128, 512], F32) for i in range(2)]
    uS = [self.sb(f"uS{i}", [128, 512], F32) for i in range(2)]
    xe = [self.sb(f"xe{i}", [128, D], BF16) for i in range(2)]
    yrow = [self.sb(f"yrow{i}", [128, D], F32) for i in range(2)]
    b_fxT, b_actT, b_wdb = Buf(), Buf(), Buf()
    b_wgb = [Buf() for _ in range(3)]
    b_gS, b_sg, b_uS, b_xe, b_yrow = ([Buf(), Buf()] for _ in range(5))
    d_wd = self.ds()
    d_wg = [self.ds() for _ in range(3)]
    d_xe = [self.ds(), self.ds()]
    d_y = [self.ds(), self.ds()]
    cnt = 0
    dcnt = 0
    xcnt = 0
    hcnt = 0
    ntok = CAP
    nst = CAP // 128
    for ex_i in range(NX):
        r0 = ex_i * CAP
        for s_ in range(nst):
            xi = xcnt % 2
            xcnt += 1
            self.dma("sp", xe[xi][:], self.FXE[r0 + s_ * 128:r0 + (s_ + 1) * 128, :], r=[self.b_fxe], w=[b_xe[xi]], dsem=d_xe[xi])
            bA, bB = 4 + 2 * xi, 5 + 2 * xi
            pA = self.ps[bA][:].bitcast(BF16)
            pB = self.ps[bB][:].bitcast(BF16)
            for k in range(16):
                pX, bX = (pA, bA) if k < 8 else (pB, bB)
                self.op("pe", lambda e, k=k, pX=pX, xi=xi: e.transpose(pX[:, (k % 8) * 128:(k % 8 + 1) * 128], xe[xi][:, k * 128:(k + 1) * 128],
                                                                      self.identb[:]),
                        r=[b_xe[xi], self.b_const], w=[self.b_ps[bX]])
            self.op("act", lambda e, s_=s_, pA=pA: e.activation(out=fxT[:, 0:8, s_ * 128:(s_ + 1) * 128], in_=pA.rearrange("p (a b) -> p a b", b=128),
                                                               func=AF.Copy), r=[self.b_ps[bA]], w=[b_fxT])
            self.op("dve", lambda e, s_=s_, pB=pB: e.tensor_copy(fxT[:, 8:16, s_ * 128:(s_ + 1) * 128], pB.rearrange("p (a b) -> p a b", b=128)),
                    r=[self.b_ps[bB]], w=[b_fxT])
        self.dma("pool", wdb[:], self.w_down[l, ex_i].rearrange("(c p) n -> p c n", p=128), w=[b_wdb], dsem=d_wd)
        wgv = self.w_gate_up[l, ex_i].rearrange("(k p) n -> p k n", p=128)
        for fc in range(8):
            s = cnt % 3
            par = cnt % 2
            cnt += 1
            self.dma("pool", wgb[s][:, :, 0, :], wgv[:, :, fc * 128:(fc + 1) * 128], w=[b_wgb[s]], dsem=d_wg[s])
            self.dma("pool", wgb[s][:, :, 1, :], wgv[:, :, DFF + fc * 128:DFF + (fc + 1) * 128], w=[b_wgb[s]], dsem=d_wg[s])
            for (h0, hn) in [(c0, min(512, CAP - c0)) for c0 in range(0, CAP, 512)]:
                par = hcnt % 2
                hcnt += 1
                hs = slice(h0, h0 + hn)
                pg, pu = 2 * par, 2 * par + 1
                for gu, pb in ((0, pg), (1, pu)):
                    for k in range(16):
                        self.op("pe", lambda e, k=k, s=s, gu=gu, pb=pb, hs=hs, hn=hn: e.matmul(self.ps[pb][:, 0:hn], lhsT=wgb[s][:, k, gu, :],
                                                                                       rhs=fxT[:, k, hs], start=(k == 0), stop=(k == 15)),
                                r=[b_wgb[s], b_fxT], w=[self.b_ps[pb]])
                self.op("dve", lambda e, par=par, pg=pg, fc=fc, ex_i=ex_i, hn=hn: e.tensor_scalar(
                    gS[par][:, 0:hn], self.ps[pg][:, 0:hn], bguT[:, fc, ex_i:ex_i + 1], 7.0, ALU.add, ALU.min),
                    r=[self.b_ps[pg], b_c], w=[b_gS[par]])
                self.op("act", lambda e, par=par, hn=hn: e.activation(out=sg[par][:, 0:hn], in_=gS[par][:, 0:hn], func=AF.Sigmoid, scale=1.702),
                        r=[b_gS[par]], w=[b_sg[par]])
                self.op("dve", lambda e, par=par, pu=pu, fc=fc, ex_i=ex_i, hn=hn: e.tensor_scalar(
                    uS[par][:, 0:hn], self.ps[pu][:, 0:hn], bguT[:, 8 + fc, ex_i:ex_i + 1], 7.0, ALU.add, ALU.min),
                    r=[self.b_ps[pu], b_c], w=[b_uS[par]])
                self.op("pool", lambda e, par=par, hn=hn: e.tensor_scalar(uS[par][:, 0:hn], uS[par][:, 0:hn], -7.0, 1.0, ALU.max, ALU.add),
                        r=[b_uS[par]], w=[b_uS[par]])
                self.op("dve", lambda e, par=par, hn=hn: e.tensor_tensor(gS[par][:, 0:hn], gS[par][:, 0:hn], sg[par][:, 0:hn], ALU.mult),
                        r=[b_sg[par]], w=[b_gS[par]])
                self.op("dve", lambda e, par=par, fc=fc, hs=hs, hn=hn: e.tensor_tensor(actT[:, fc, hs], uS[par][:, 0:hn], gS[par][:, 0:hn], ALU.mult),
                        r=[b_uS[par], b_gS[par]], w=[b_actT])
        for s_ in range(nst):
            yi = dcnt % 2
            dcnt += 1
            for nb in range(4):
                bk = 4 + nb
                cols = slice(nb * 512, (nb + 1) * 512)
                for fc in range(8):
                    self.op("pe", lambda e, s_=s_, fc=fc, bk=bk, cols=cols: e.matmul(self.ps[bk][:, 0:512], lhsT=actT[:, fc, s_ * 128:(s_ + 1) * 128],
                                                                                   rhs=wdb[:, fc, cols], start=(fc == 0), stop=(fc == 7)),
                            r=[b_actT, b_wdb], w=[self.b_ps[bk]])
                if nb % 2 == 0:
                    self.op("act", lambda e, yi=yi, bk=bk, cols=cols: e.activation(out=yrow[yi][:, cols], in_=self.ps[bk][:, 0:512], func=AF.Copy),
                            r=[self.b_ps[bk]], w=[b_yrow[yi]])
                else:
                    self.op("dve", lambda e, yi=yi, bk=bk, cols=cols: e.tensor_copy(yrow[yi][:, cols], self.ps[bk][:, 0:512]),
                            r=[self.b_ps[bk]], w=[b_yrow[yi]])
            self.dma("sp", self.YE[r0 + s_ * 128:r0 + (s_ + 1) * 128, :], yrow[yi][:], r=[b_yrow[yi]], w=[self.b_ye], dsem=d_y[yi])
    self.barrier()
    self.sb_off = mark
    acc = [self.sb(f"acc{i}", [128, D], F32) for i in range(2)]
    gb = [[self.sb(f"gb{i}_{j}", [128, D], F32) for j in range(4)] for i in range(2)]
    xt2 = [self.sb(f"gxt2_{i}", [128, D], F32) for i in range(2)]
    b_acc, b_xt2 = [Buf(), Buf()], [Buf(), Buf()]
    b_gb = [[Buf() for j in range(4)] for i in range(2)]
    d_g = [self.ds(), self.ds()]
    d_x2 = [self.ds(), self.ds()]
    d_out = [self.ds(), self.ds()]
    for i in range(2):
        for j in range(4):
            self.op("dve" if j % 2 else "act", (lambda e, i=i, j=j: e.memset(gb[i][j][:], 0.0)) if j % 2 else
                    (lambda e, i=i, j=j: e.activation(out=gb[i][j][:], in_=G2[:, 0, :], func=AF.Copy, scale=0.0)),
                    r=[b_c], w=[b_gb[i][j]])
    dz = self.ds()
    self.dma("sp", self.YE[NR:NR + 128, :], gb[0][1][:], r=[b_gb[0][1]], w=[self.b_ye], dsem=dz)
    def issue_loads(t):
        i = t % 2
        self.dma("sp", xt2[i][:], self.XRES[t * 128:(t + 1) * 128, :], r=[self.b_xres[t]], w=[b_xt2[i]], dsem=d_x2[i])
        for j in range(4):
            self.P.op("pool", lambda e, t=t, j=j, i=i: e.indirect_dma_start(
                out=gb[i][j][:, :], out_offset=None, in_=self.YE[:, :],
                in_offset=bass.IndirectOffsetOnAxis(ap=IDX[:, t, j:j + 1], axis=0)),
                reads=[b_idx, self.b_ye], writes=[b_gb[i][j]], dsem=d_g[i])
    issue_loads(tiles_all[0])
    for n_, t in enumerate(tiles_all):
        i = t % 2
        which = 1 if t < 2 else 0
        rows = slice(t * 128, (t + 1) * 128)
        if n_ + 1 < len(tiles_all):
            issue_loads(tiles_all[n_ + 1])
        for nb in range(4):
            bk = nb
            cols = slice(nb * 512, (nb + 1) * 512)
            self.op("pe", lambda e, t=t, bk=bk, cols=cols: e.matmul(self.ps[bk][:, 0:512], lhsT=combT[0:NX, t * 128:(t + 1) * 128],
                                                                    rhs=bd[0:NX, cols], start=True, stop=True),
                    r=[b_combT, b_c], w=[self.b_ps[bk]])
            self.op("act", lambda e, i=i, bk=bk, cols=cols: e.activation(out=acc[i][:, cols], in_=self.ps[bk][:, 0:512], func=AF.Copy),
                    r=[self.b_ps[bk]], w=[b_acc[i]])
        for j in range(4):
            self.op("dve", lambda e, i=i, j=j, t=t: e.scalar_tensor_tensor(acc[i][:], gb[i][j][:], WJ[:, t, j:j + 1], acc[i][:], ALU.mult, ALU.add),
                    r=[b_gb[i][j], b_idx], w=[b_acc[i]])
        self.op("dve", lambda e, i=i, which=which: e.tensor_tensor(acc[i][:], acc[i][:], G2[:, which, :], ALU.mult), r=[b_c], w=[b_acc[i]])
        self.op("dve", lambda e, i=i: e.tensor_tensor(acc[i][:], acc[i][:], xt2[i][:], ALU.add), r=[b_xt2[i]], w=[b_acc[i]])
        if last:
            by = Buf()
            self.dma("sp", self.y_out[(t - 2) * 128:(t - 1) * 128, :], acc[i][:], r=[b_acc[i]], w=[by], dsem=d_out[i])
            self.final_reads.append(by)
        else:
            self.dma("sp", self.XRES[rows, :], acc[i][:], r=[b_acc[i]], w=[self.b_xres[t]], dsem=d_out[i])
    if not last:
        self.dbg(f"XOUT{l}", self.XRES, [TT, D], F32, r=self.b_xres)
    else:
        self.dbg(f"YOUT{l}", self.y_out, [2048, D], F32, r=self.final_reads)
    self.barrier()


Builder.phase_G2 = phase_G2
```

```python
import numpy as np
import ml_dtypes
import concourse.bass as bass
import concourse.mybir as mybir
from concourse.bass_utils import run_bass_kernel_spmd

F32 = mybir.dt.float32
BF16 = mybir.dt.bfloat16
AF = mybir.ActivationFunctionType
ALU = mybir.AluOpType
AX = mybir.AxisListType

D = 2048
NT = 18
TT = NT * 128
DIN = 4624
EPS = 1e-6
NE = 32
DFF = 1024
CAP = 768
KBIG = 40000.0
I32 = mybir.dt.int32


class Buf:
    __slots__ = ("name", "last_w", "readers")

    def __init__(self, name=""):
        self.name = name
        self.last_w = None
        self.readers = []


class DSem:
    __slots__ = ("sem", "count")

    def __init__(self, sem):
        self.sem = sem
        self.count = 0


class Op:
    __slots__ = ("eng", "emit", "deps", "signal", "signum", "dsem", "dval", "waits", "idx")


ENGS = ("pe", "act", "dve", "pool", "sp")


class Prog:
    def __init__(self):
        self.streams = {e: [] for e in ENGS}
        self.nops = 0
        self.dsems = []

    def new_dsem(self, sem):
        d = DSem(sem)
        self.dsems.append(d)
        return d

    def op(self, eng, emit, reads=(), writes=(), dsem=None, extra_dsem_waits=()):
        o = Op()
        o.eng = eng
        o.emit = emit
        o.signal = False
        o.signum = None
        o.dsem = dsem
        o.dval = None
        o.idx = self.nops
        self.nops += 1
        deps = []
        for b in reads:
            if b.last_w is not None:
                deps.append(b.last_w)
        for b in writes:
            if b.last_w is not None:
                deps.append(b.last_w)
            deps.extend(b.readers)
        seen = set()
        o.deps = []
        for d in deps:
            if id(d) in seen or d is o:
                continue
            seen.add(id(d))
            if d.dsem is not None:
                o.deps.append(("d", d.dsem, d.dsem.count))
            else:
                if d.eng == "pe" and eng == "pe" and dsem is None:
                    continue
                d.signal = True
                o.deps.append(("e", d))
        for ds in extra_dsem_waits:
            if ds.count > 0:
                o.deps.append(("d", ds, ds.count))
        if dsem is not None:
            dsem.count += 16
            o.dval = dsem.count
        for b in reads:
            b.readers.append(o)
        for b in writes:
            b.last_w = o
            b.readers = []
        self.streams[eng].append(o)
        return o

    def finalize(self):
        for e in ENGS:
            n = 0
            for o in self.streams[e]:
                if o.dsem is None and o.signal:
                    n += 1
                    o.signum = n
        for e in ENGS:
            known = {}
            for o in self.streams[e]:
                w = {}
                for d in o.deps:
                    if d[0] == "d":
                        key = ("d", id(d[1]))
                        val = d[2]
                        sem = d[1].sem
                    else:
                        key = ("e", d[1].eng)
                        val = d[1].signum
                        sem = d[1].eng
                    if known.get(key, 0) >= val:
                        continue
                    if key not in w or w[key][1] < val:
                        w[key] = (sem, val)
                for key, (sem, val) in w.items():
                    known[key] = val
                o.waits = list(w.values())

    def emit(self, block, esems):
        def run(engname):
            def f(eng):
                for o in self.streams[engname]:
                    for sem, val in o.waits:
                        s = esems[sem] if isinstance(sem, str) else sem
                        eng.wait_ge(s, val)
                    ins = o.emit(eng)
                    if ins is None:
                        continue
                    if o.dsem is not None:
                        ins.then_inc(o.dsem.sem, 16)
                    elif o.signal:
                        ins.then_inc(esems[engname], 1)
            return f
        block.tensor(run("pe"))
        block.scalar(run("act"))
        block.vector(run("dve"))
        block.gpsimd(run("pool"))
        block.sync(run("sp"))


def _consts():
    c = {}
    c["ident_f"] = np.eye(128, dtype=np.float32)
    c["ident_b"] = np.eye(128, dtype=np.float32).astype(ml_dtypes.bfloat16)
    a = np.arange(128)
    c["mask_prev"] = (a[None, :] <= a[:, None]).astype(np.float32).astype(ml_dtypes.bfloat16)
    c["mask_next"] = (a[:, None] <= a[None, :]).astype(np.float32).astype(ml_dtypes.bfloat16)
    c["ones_f"] = np.ones((128, 128), np.float32)
    sel = np.zeros((32, 32, 128), np.float32)
    for e in range(32):
        sel[e, e, :] = 1.0
    c["sel32"] = sel.transpose(1, 0, 2).copy()
    e0 = np.zeros((2, 2, 128), np.float32)
    e0[0, 0, :] = 1.0
    e0[1, 1, :] = 1.0
    c["sel2"] = e0
    rows = 2048 // 64
    row = np.repeat(np.arange(rows, dtype=np.float32), 64)
    col = np.tile(np.arange(64, dtype=np.float32), rows)
    half = 32
    inv = (10000.0 ** (-np.arange(0, half, 2, dtype=np.float32) / half)).astype(np.float32)

    def tab(p):
        ang = p[:, None] * inv[None, :]
        ang = np.concatenate([ang, ang], -1)
        return np.cos(ang).astype(np.float32), np.sin(ang).astype(np.float32)
    cr, sr = tab(row)
    cc, sc = tab(col)
    cos = np.concatenate([cr, cc], -1)
    sgn = np.concatenate([-np.ones(16), np.ones(16)]).astype(np.float32)
    sins = np.concatenate([sr * sgn, sc * sgn], -1)
    c["rope_cos"] = cos.reshape(16, 128, 64).transpose(1, 0, 2).copy()
    c["rope_sin"] = sins.reshape(16, 128, 64).transpose(1, 0, 2).copy()
    c["ml_mask_f"] = (a[:, None] <= a[None, :]).astype(np.float32).astype(ml_dtypes.bfloat16)
    c["ml_mask_b"] = (a[:, None] >= a[None, :]).astype(np.float32).astype(ml_dtypes.bfloat16)
    c["tri_s"] = (a[:, None] < a[None, :]).astype(np.float32)
    c["trash"] = (NE * CAP + np.arange(128, dtype=np.float32)).reshape(128, 1)
    c["ecst"] = np.tile((np.arange(32, dtype=np.float32) * CAP)[None, :], (128, 1))
    c["tri_f"] = (a[:, None] <= a[None, :]).astype(np.float32)
    c["tri_b"] = (a[:, None] >= a[None, :]).astype(np.float32)
    sl = np.zeros((128, 128), np.float32)
    sl[127, :] = 1.0
    c["sel_last"] = sl
    sf = np.zeros((128, 128), np.float32)
    sf[0, :] = 1.0
    c["sel_first"] = sf
    return c


from contextlib import ExitStack


class Builder:
    def __init__(self, nc, stack, upto="all", debug=(), n_exp=NE, layers=2, dev=False):
        self.nc = nc
        self.stack = stack
        self.P = Prog()
        self.upto = upto
        self.debug = set(debug)
        self.n_exp = n_exp
        self.layers = layers
        self.dev = dev
        self.L = 1 if dev else 2
        self.sb_off = 16512
        self.nsem = 0
        self.dpool = []
        self.dnext = 0
        self.dbg_names = []
        self.need_moe = upto == "all" or upto.startswith("G")

    def sb(self, name, shape, dtype):
        esz = 2 if dtype == BF16 else 4
        n = 1
        for s in shape[1:]:
            n *= s
        nbytes = (n * esz + 31) // 32 * 32
        t = self.nc.alloc_sbuf_tensor_at(f"{name}_{self.sb_off}", list(shape), dtype, offset=self.sb_off)
        self.sb_off += nbytes
        assert self.sb_off <= 16512 + 212000, (name, self.sb_off)
        return t

    def sem(self, name):
        self.nsem += 1
        return self.stack.enter_context(self.nc.semaphore(name))

    def ds(self):
        if self.dnext >= len(self.dpool):
            self.dpool.append(self.P.new_dsem(self.sem(f"d{len(self.dpool)}")))
        d = self.dpool[self.dnext]
        self.dnext += 1
        return d

    def dram(self, name, shape, dtype, kind="Internal"):
        return self.nc.dram_tensor(name, list(shape), dtype, kind=kind).ap()

    def op(self, eng, fn, r=(), w=(), dsem=None):
        return self.P.op(eng, fn, reads=r, writes=w, dsem=dsem)

    def dma(self, q, out, in_, r=(), w=(), dsem=None, **kw):
        assert dsem is not None
        return self.P.op(q, lambda e: e.dma_start(out=out, in_=in_, **kw), reads=r, writes=w, dsem=dsem)

    def barrier(self):
        P = self.P
        toks = []
        for e in ("act", "dve", "pool"):
            b = Buf("tok_" + e)
            scr = self.scr[e]
            if e == "act":
                P.op(e, lambda en, s=scr: en.activation(out=s[0:1, 0:2], in_=s[0:1, 0:2], func=AF.Identity), writes=[b])
            else:
                P.op(e, lambda en, s=scr: en.memset(s[0:1, 0:2], 0.0), writes=[b])
            toks.append(b)
        for e in ENGS:
            P.op(e, lambda en: None, reads=toks, extra_dsem_waits=list(P.dsems))
        self.dnext = 0

    def dbg(self, name, ap, shape, dtype, r=()):
        if name not in self.debug:
            return
        o = self.dram("dbg_" + name, shape, dtype, kind="ExternalOutput")
        self.dbg_names.append(name)
        d = self.P.new_dsem(self.sem("dbg_" + name))
        bo = Buf("dbg_" + name)
        self.dma("sp", o, ap, r=r, w=[bo], dsem=d)
        self.final_reads.append(bo)

    def setup(self):
        nc = self.nc
        self.esems = {e: self.sem("s_" + e) for e in ("pe", "act", "dve", "pool")}
        self.final_reads = []
        self.in_names = []

        def di(n, s, dt=F32):
            self.in_names.append(n)
            return self.dram(n, s, dt, kind="ExternalInput")
        self.x_in = di("x_in", [TT, D])
        self.c2 = di("c2", [32, 128])
        if not self.dev:
            self.w_ada = di("w_ada", [self.L, D, 6 * D])
        else:
            self.dev_mod = di("dev_mod", [2, 6 * D])
        self.b_ada = di("b_ada", [self.L, 6 * D])
        self.norm1_w = di("norm1_w", [self.L, D])
        self.norm2_w = di("norm2_w", [self.L, D])
        self.w_in = di("w_in", [self.L, D, DIN])
        self.b_gates = di("b_gates", [self.L, 16])
        self.q_norm_w = di("q_norm_w", [self.L, 64])
        self.k_norm_w = di("k_norm_w", [self.L, 64])
        self.attn_sink = di("attn_sink", [self.L, 16])
        self.mlstm_norm_w = di("mlstm_norm_w", [self.L, 1024])
        self.w_out = di("w_out", [self.L, D, D])
        self.w_router = di("w_router", [self.L, D, NE])
        self.b_router = di("b_router", [self.L, NE])
        if self.need_moe:
            self.w_gate_up = di("w_gate_up", [self.L, self.n_exp, D, 2 * DFF])
            self.b_gate_up = di("b_gate_up", [self.L, self.n_exp, 2 * DFF])
            self.w_down = di("w_down", [self.L, self.n_exp, DFF, D])
            self.b_down = di("b_down", [self.L, self.n_exp, D])
        self.cst = {}
        for k, v in _consts().items():
            self.cst[k] = di("cst_" + k, list(v.shape), BF16 if v.dtype != np.float32 else F32)
        self.y_out = self.dram("y", [2048, D], F32, kind="ExternalOutput")
        self.XRES = self.dram("xres", [TT, D], F32)
        self.PX = self.dram("px", [TT, 4608], BF16)
        self.GD = self.dram("gd", [TT, 16], F32)
        self.MQT = self.dram("mqt", [4, 128, TT], BF16)
        self.MKT = self.dram("mkt", [4, 128, TT], BF16)
        self.MODD = self.dram("modd", [2, 6 * D], F32)
        self.CATD = self.dram("catd", [TT, D], BF16)
        self.FXE = self.dram("fxe", [NE * CAP + 128, D], BF16)
        self.YE = self.dram("ye", [NE * CAP + 128, D], F32)
        self.b_fxe = Buf("fxe")
        self.b_ye = Buf("ye")
        self.b_xres = [Buf(f"xres{t}") for t in range(NT)]
        self.b_px = [Buf(f"px{t}") for t in range(NT)]
        self.b_gd = [Buf(f"gd{t}") for t in range(NT)]
        self.b_mqt = Buf("mqt")
        self.b_mkt = Buf("mkt")
        self.b_modd = Buf("modd")
        self.b_catd = [Buf(f"catd{t}") for t in range(NT)]
        self.ps = [nc.alloc_psum_tensor(f"psb{i}", [128, 512], F32) for i in range(8)]
        self.b_ps = [Buf(f"ps{i}") for i in range(8)]
        self.identf = self.sb("identf", [128, 128], F32)
        self.identb = self.sb("identb", [128, 128], BF16)
        self.onesf = self.sb("onesf", [128, 128], F32)
        self.scr = {e: self.sb("scr_" + e, [128, 8], F32) for e in ("act", "dve", "pool")}
        self.modT = self.sb("modT", [128, 96, 2], F32)
        self.S1 = self.sb("S1", [128, 16, 2], F32)
        self.S2 = self.sb("S2", [128, 16, 2], F32)
        self.b_modT = Buf("modT")
        self.b_S = Buf("S12")
        self.b_const = Buf("const")
        dc = self.P.new_dsem(self.sem("dconst"))
        self.dconst = dc
        self.dma("sp", self.identf[:], self.cst["ident_f"], w=[self.b_const], dsem=dc)
        self.dma("sp", self.identb[:], self.cst["ident_b"], w=[self.b_const], dsem=dc)
        self.dma("sp", self.onesf[:], self.cst["ones_f"], w=[self.b_const], dsem=dc)
        self.zt = self.sb("zt", [128, D], BF16)
        self.b_zt = Buf("zt")
        self.op("pool", lambda e: e.memset(self.zt[:], 0.0), w=[self.b_zt])
        self.persist_off = self.sb_off

    def phase_reset(self):
        self.sb_off = self.persist_off

    def phase_A(self, l):
        nc, P = self.nc, self.P
        self.phase_reset()
        vec = self.sb("vec", [64, 128], F32)
        silu2 = self.sb("silu2", [128, 16, 2], F32)
        nwT = self.sb("nwT", [128, 2, 16], F32)
        bada = self.sb("bada", [2, 6 * D], F32)
        modsb = self.sb("modsb", [2, 6 * D], F32)
        sel2 = self.sb("sel2", [2, 2, 128], F32)
        stage = [self.sb(f"wst{i}", [128, 16, 512], F32) for i in range(2)]
        b_vec, b_silu, b_nwT, b_bada, b_modsb = Buf(), Buf(), Buf(), Buf(), Buf()
        b_stage = [Buf(), Buf()]
        d_stage = [self.ds(), self.ds()]
        d0 = self.ds()
        d1 = self.ds()
        self.dma("sp", vec[0:32, :], self.c2, w=[b_vec], dsem=d0)
        self.dma("sp", vec[32:48, :], self.norm1_w[l].rearrange("(k p) -> k p", p=128), w=[b_vec], dsem=d0)
        self.dma("sp", vec[48:64, :], self.norm2_w[l].rearrange("(k p) -> k p", p=128), w=[b_vec], dsem=d0)
        self.dma("sp", bada[0:1, :], self.b_ada[l:l + 1, :], w=[b_bada], dsem=d0)
        self.dma("sp", bada[1:2, :], self.b_ada[l:l + 1, :], w=[b_bada], dsem=d0)
        self.dma("sp", sel2[:], self.cst["sel2"], w=[b_bada], dsem=d0)
        pv = self.ps[7]
        bpv = self.b_ps[7]
        self.op("pe", lambda e: e.transpose(pv[:, 0:64], vec[0:64, :], self.identf[0:64, 0:64]),
                r=[b_vec, self.b_const], w=[bpv])
        self.op("act", lambda e: e.activation(out=silu2[:, :, 0], in_=pv[:, 0:16], func=AF.Silu), r=[bpv], w=[b_silu])
        self.op("act", lambda e: e.activation(out=silu2[:, :, 1], in_=pv[:, 16:32], func=AF.Silu), r=[bpv], w=[b_silu])
        self.op("dve", lambda e: e.tensor_copy(nwT[:].rearrange("p a b -> p (a b)"), pv[:, 32:64]), r=[bpv], w=[b_nwT])
        if self.dev:
            self.dma("sp", modsb[0:2, :], self.dev_mod, w=[b_modsb], dsem=d0)
        wv = None if self.dev else self.w_ada[l].rearrange("(k p) n -> p k n", p=128)
        for n in range(0 if self.dev else 24):
            s = n % 2
            self.dma("sp", stage[s][:], wv[:, :, n * 512:(n + 1) * 512], w=[b_stage[s]], dsem=d_stage[s])
            pm = self.ps[n % 2]
            bpm = self.b_ps[n % 2]
            for k in range(16):
                self.op("pe", lambda e, k=k, s=s, pm=pm: e.matmul(pm[0:2, :], lhsT=silu2[:, k, :], rhs=stage[s][:, k, :],
                                                                  start=(k == 0), stop=(k == 15)),
                        r=[b_silu, b_stage[s]], w=[bpm])
            self.op("dve", lambda e, n=n, pm=pm: e.tensor_tensor(modsb[0:2, n * 512:(n + 1) * 512], pm[0:2, :],
                                                                 bada[0:2, n * 512:(n + 1) * 512], ALU.add),
                    r=[bpm, b_bada], w=[b_modsb])
        self.dma("sp", self.MODD, modsb[0:2, :], r=[b_modsb], w=[self.b_modd], dsem=d1)
        pt = self.ps[2]
        for j in range(96):
            self.op("pe", lambda e, j=j: e.transpose(pt[:, 2 * j:2 * j + 2], modsb[0:2, j * 128:(j + 1) * 128],
                                                      self.identf[0:2, 0:2]),
                    r=[b_modsb, self.b_const], w=[self.b_ps[2]])
        self.op("act", lambda e: e.activation(out=self.modT[:].rearrange("p a b -> p (a b)"), in_=pt[:, 0:192],
                                              func=AF.Identity), r=[self.b_ps[2]], w=[self.b_modT])
        for (S, sc0, wi) in ((self.S1, 16, 0), (self.S2, 64, 1)):
            self.op("dve", lambda e, S=S, sc0=sc0: e.tensor_scalar(S[:], self.modT[:, sc0:sc0 + 16, :], 1.0, None, ALU.add),
                    r=[self.b_modT], w=[self.b_S])
            self.op("dve", lambda e, S=S, wi=wi: e.tensor_tensor(S[:], S[:], nwT[:, wi, :].unsqueeze(2).to_broadcast([128, 16, 2]),
                                                                 ALU.mult),
                    r=[b_nwT], w=[self.b_S])
        self.dbg(f"modT{l}", self.modT[:], [128, 96, 2], F32, r=[self.b_modT])
        self.dbg(f"S1_{l}", self.S1[:], [128, 16, 2], F32, r=[self.b_S])
        self.barrier()

    def norm_tile(self, xt, b_xt, which, S, sh0, f32T, b_f32T, dstT, b_dst, col0, banks, junk, ss, xn, b_tmp):
        P = self
        b_ss, b_xn = b_tmp
        self.op("act", lambda e: e.activation(out=junk[:], in_=xt[:], func=AF.Square, accum_out=ss[:, 0:1]),
                r=[b_xt], w=[b_ss])
        self.op("dve", lambda e: e.tensor_scalar(ss[:, 1:2], ss[:, 0:1], 1.0 / D, EPS, ALU.mult, ALU.add), r=[b_ss], w=[b_ss])
        self.op("act", lambda e: e.activation(out=ss[:, 3:4], in_=ss[:, 1:2], func=AF.Sqrt), r=[b_ss], w=[b_ss])
        self.op("dve", lambda e: e.reciprocal(ss[:, 2:3], ss[:, 3:4]), r=[b_ss], w=[b_ss])
        self.op("dve", lambda e: e.tensor_scalar(xn[:], xt[:], ss[:, 2:3], None, ALU.mult), r=[b_xt, b_ss], w=[b_xn])
        for c in range(16):
            bk = banks[c // 4]
            self.op("pe", lambda e, c=c, bk=bk: e.transpose(self.ps[bk][:, (c % 4) * 128:(c % 4 + 1) * 128],
                                                            xn[:, c * 128:(c + 1) * 128], self.identf[:]),
                    r=[b_xn, self.b_const], w=[self.b_ps[bk]])
        for c in range(16):
            bk = banks[c // 4]
            src = self.ps[bk][:, (c % 4) * 128:(c % 4 + 1) * 128]
            if c % 2 == 0:
                self.op("act", lambda e, c=c, src=src: e.activation(out=f32T[:, c, :], in_=src, func=AF.Identity,
                                                                    scale=S[:, c, which:which + 1],
                                                                    bias=self.modT[:, sh0 + c, which:which + 1]),
                        r=[self.b_ps[bk], self.b_S, self.b_modT], w=[b_f32T])
            else:
                self.op("dve", lambda e, c=c, src=src: e.tensor_scalar(f32T[:, c, :], src, S[:, c, which:which + 1],
                                                                       self.modT[:, sh0 + c, which:which + 1],
                                                                       ALU.mult, ALU.add),
                        r=[self.b_ps[bk], self.b_S, self.b_modT], w=[b_f32T])
        if dstT is not None:
            self.op("pool", lambda e: e.tensor_copy(dstT[:, :, col0:col0 + 128], f32T[:]), r=[b_f32T], w=[b_dst])

    def phase_BC(self, l):
        self.phase_reset()
        src = self.x_in if l == 0 else self.XRES
        hT = self.sb("hT", [128, 16, TT], BF16)
        b_hT = Buf("hT")
        xt = [self.sb(f"xt{i}", [128, D], F32) for i in range(2)]
        xn = [self.sb(f"xn{i}", [128, D], F32) for i in range(2)]
        f32T = [self.sb(f"f32T{i}", [128, 16, 128], F32) for i in range(2)]
        junk = self.sb("junk", [128, D], BF16)
        ss = [self.sb(f"ss{i}", [128, 4], F32) for i in range(2)]
        b_xt = [Buf(), Buf()]
        b_f = [Buf(), Buf()]
        b_tmp = [(Buf(), Buf()), (Buf(), Buf())]
        d_xt = [self.ds(), self.ds()]
        mark = self.sb_off
        for t in range(NT):
            i = t % 2
            which = 1 if t < 2 else 0
            rd = [self.b_xres[t]] if l > 0 else []
            self.dma("sp", xt[i][:], src[t * 128:(t + 1) * 128, :], r=rd, w=[b_xt[i]], dsem=d_xt[i])
            self.norm_tile(xt[i], b_xt[i], which, self.S1, 0, f32T[i], b_f[i], hT, b_hT, t * 128,
                           [4 * i + j for j in range(4)], junk, ss[i], xn[i], b_tmp[i])
        self.dbg(f"hT{l}", hT[:], [128, 16, TT], BF16, r=[b_hT])
        if self.upto == f"B{l}":
            return
        wv = self.w_in[l].rearrange("(k p) n -> p k n", p=128)
        CW = 256
        stg = [self.sb(f"pst{i}", [128, 16, CW], F32) for i in range(2)]
        wbf = [self.sb(f"pwb{i}", [128, 16, CW], BF16) for i in range(2)]
        osb = [self.sb(f"posb{i}", [128, 512], BF16) for i in range(3)]
        gsb = self.sb("pgsb", [128, NT, 16], F32)
        b_stg = [Buf(), Buf()]
        b_wbf = [Buf(), Buf()]
        b_osb = [Buf(), Buf(), Buf()]
        b_gsb = Buf()
        d_stg = [self.ds(), self.ds()]
        d_osb = [self.ds(), self.ds(), self.ds()]
        d_g = self.ds()
        nblk = 4608 // CW
        cnt = 0
        ocnt = 0
        def load_blk(cb):
            s = cb % 2
            c0 = cb * CW
            cw = CW if cb < nblk else 16
            self.dma("sp", stg[s][:, :, 0:cw], wv[:, :, c0:c0 + cw], w=[b_stg[s]], dsem=d_stg[s])
        load_blk(0)
        for cb in range(nblk + 1):
            s = cb % 2
            c0 = cb * CW
            cw = CW if cb < nblk else 16
            if cb + 1 <= nblk:
                load_blk(cb + 1)
            self.op("pool", lambda e, s=s, cw=cw: e.tensor_copy(wbf[s][:, :, 0:cw], stg[s][:, :, 0:cw]),
                    r=[b_stg[s]], w=[b_wbf[s]])
            fm_head = None
            if 1536 <= c0 < 2560:
                fm_head = (c0 - 1536) // 128
            for t in range(NT):
                bk = cnt % 4
                cnt += 1
                pt = self.ps[bk]
                for k in range(16):
                    self.op("pe", lambda e, k=k, t=t, s=s, cw=cw, pt=pt: e.matmul(pt[:, 0:cw], lhsT=hT[:, k, t * 128:(t + 1) * 128],
                                                                                   rhs=wbf[s][:, k, 0:cw], start=(k == 0), stop=(k == 15)),
                            r=[b_hT, b_wbf[s]], w=[self.b_ps[bk]])
                if cb < nblk:
                    o = ocnt % 3
                    ocnt += 1
                    scale = (128.0 ** -0.5) if 2048 <= c0 < 2560 else 1.0
                    if t % 2 == 0:
                        self.op("act", lambda e, o=o, pt=pt, scale=scale: e.activation(out=osb[o][:, 0:CW], in_=pt[:, 0:CW],
                                                                                        func=AF.Copy, scale=scale),
                                r=[self.b_ps[bk]], w=[b_osb[o]])
                    else:
                        self.op("dve", lambda e, o=o, pt=pt, scale=scale: e.tensor_scalar(osb[o][:, 0:CW], pt[:, 0:CW], scale, None, ALU.mult),
                                r=[self.b_ps[bk]], w=[b_osb[o]])
                    self.dma("sp", self.PX[t * 128:(t + 1) * 128, c0:c0 + CW], osb[o][:, 0:CW], r=[b_osb[o]], w=[self.b_px[t]],
                             dsem=d_osb[o])
                else:
                    self.op("dve", lambda e, t=t, pt=pt: e.tensor_copy(gsb[:, t, :], pt[:, 0:16]), r=[self.b_ps[bk]], w=[b_gsb])
            if fm_head is not None:
                for hh in range(CW // 128):
                    head = fm_head + hh
                    dst = self.MQT if head < 4 else self.MKT
                    bdst = self.b_mqt if head < 4 else self.b_mkt
                    scale = 1.0 if head < 4 else (128.0 ** -0.5)
                    for tb in range(6):
                        bk = 4 + (cnt % 4)
                        cnt += 1
                        pt = self.ps[bk]
                        for k in range(16):
                            self.op("pe", lambda e, k=k, tb=tb, s=s, hh=hh, pt=pt: e.matmul(
                                pt[:, 0:384], lhsT=wbf[s][:, k, hh * 128:(hh + 1) * 128], rhs=hT[:, k, tb * 384:(tb + 1) * 384],
                                start=(k == 0), stop=(k == 15)), r=[b_hT, b_wbf[s]], w=[self.b_ps[bk]])
                        o = ocnt % 3
                        ocnt += 1
                        self.op("act", lambda e, o=o, pt=pt, scale=scale: e.activation(out=osb[o][:, 0:384], in_=pt[:, 0:384],
                                                                                        func=AF.Copy, scale=scale),
                                r=[self.b_ps[bk]], w=[b_osb[o]])
                        self.dma("sp", dst[head % 4, :, tb * 384:(tb + 1) * 384], osb[o][:, 0:384], r=[b_osb[o]], w=[bdst],
                                 dsem=d_osb[o])
        self.dma("sp", self.GD.rearrange("(t p) g -> p t g", p=128), gsb[:], r=[b_gsb], w=self.b_gd, dsem=d_g)
        self.dbg(f"PX{l}", self.PX, [TT, 4608], BF16, r=self.b_px)
        self.dbg(f"GD{l}", self.GD, [TT, 16], F32, r=self.b_gd)
        self.dbg(f"MQT{l}", self.MQT, [4, 128, TT], BF16, r=[self.b_mqt])
        self.dbg(f"MKT{l}", self.MKT, [4, 128, TT], BF16, r=[self.b_mkt])
        self.barrier()


def build_program(upto="all", debug=(), n_exp=NE, dev=False, dev_layer=0):
    nc = bass.Bass("TRN2", target_bir_lowering=False)
    stack = ExitStack()
    B = Builder(nc, stack, upto=upto, debug=debug, n_exp=n_exp, dev=dev)
    B.setup()
    B.last_layer = 1
    for l in ([0] if dev else range(2)):
        B.cur_layer = dev_layer if dev else l
        B.phase_A(l)
        if upto == f"A{l}":
            break
        B.phase_BC(l)
        if upto in (f"B{l}", f"C{l}"):
            break
        B.phase_D(l)
        if upto in (f"D{l}", f"D1_{l}"):
            break
        B.phase_E(l)
        if upto in (f"E{l}", f"E1_{l}"):
            break
        B.phase_F(l)
        if upto == f"F{l}":
            break
        B.phase_G2(l)
        if upto == f"G{l}":
            break
    B.op("sp", lambda e: None, r=B.final_reads)
    B.P.finalize()
    with nc.Block() as block:
        B.P.emit(block, B.esems)
    stack.close()
    return nc, B


def make_in_maps(inputs, n_exp=NE):
    cst = _consts()
    maps = []
    x = np.asarray(inputs["x"], np.float32)
    ctx = np.asarray(inputs["ctx"], np.float32)
    c = np.asarray(inputs["c"], np.float32)
    c_ctx = np.asarray(inputs["c_ctx"], np.float32)
    shared = {k: np.ascontiguousarray(np.asarray(inputs[k], np.float32)) for k in
              ("w_ada", "b_ada", "norm1_w", "norm2_w", "w_in", "b_gates", "q_norm_w", "k_norm_w", "attn_sink",
               "mlstm_norm_w", "w_out", "w_router", "b_router", "w_gate_up", "b_gate_up", "w_down", "b_down")}
    for b in range(8):
        m = dict(shared)
        m["x_in"] = np.concatenate([ctx[b], x[b]], axis=0)
        m["c2"] = np.concatenate([c[b].reshape(16, 128), c_ctx.reshape(16, 128)], axis=0)
        for k, v in cst.items():
            m["cst_" + k] = v
        maps.append(m)
    return maps


def kernel(**inputs):
    nc, B = build_program()
    maps = make_in_maps(inputs)
    maps = [{k: m[k] for k in B.in_names} for m in maps]
    res = run_bass_kernel_spmd(nc, maps, core_ids=list(range(8)))
    return np.stack([np.asarray(r["y"], np.float32) for r in res.results], axis=0)


def phase_D(self, l):
    sl = self.cur_layer
    self.phase_reset()
    qT = self.sb("qT", [128, 8, TT], BF16)
    kT2 = self.sb("kT2", [128, 4, 2, TT], BF16)
    vaug = self.sb("vaug", [128, NT, 4, 65], BF16)
    cos = self.sb("cos", [128, 16, 64], F32)
    sin = self.sb("sin", [128, 16, 64], F32)
    qw = self.sb("qw", [128, 64], F32)
    kw = self.sb("kw", [128, 64], F32)
    sinke = self.sb("sinke", [128, 16], F32)
    maskp = self.sb("maskp", [128, 128], BF16)
    maskn = self.sb("maskn", [128, 128], BF16)
    b_qT, b_kT2, b_vaug, b_c = Buf(), Buf(), Buf(), Buf()
    d0 = self.ds()
    self.dma("sp", cos[:], self.cst["rope_cos"], w=[b_c], dsem=d0)
    self.dma("sp", sin[:], self.cst["rope_sin"], w=[b_c], dsem=d0)
    self.dma("sp", qw[:], self.q_norm_w[l:l + 1, :].partition_broadcast(128), w=[b_c], dsem=d0)
    self.dma("sp", kw[:], self.k_norm_w[l:l + 1, :].partition_broadcast(128), w=[b_c], dsem=d0)
    self.dma("sp", sinke[:], self.attn_sink[l:l + 1, :].partition_broadcast(128), w=[b_c], dsem=d0)
    self.dma("sp", maskp[:], self.cst["mask_prev"], w=[b_c], dsem=d0)
    self.dma("sp", maskn[:], self.cst["mask_next"], w=[b_c], dsem=d0)
    b_c2 = Buf()
    self.op("act", lambda e: e.activation(out=qw[:], in_=qw[:], func=AF.Copy, scale=0.125), r=[b_c], w=[b_c2])
    self.op("act", lambda e: e.activation(out=sinke[:], in_=sinke[:], func=AF.Exp), r=[b_c], w=[b_c2])
    self.op("pool", lambda e: e.memset(vaug[:].rearrange("p t j d -> p (t j) d")[:, :, 64:65], 1.0), w=[b_vaug])
    slab = [self.sb(f"slab{i}", [128, 1536], BF16) for i in range(2)]
    t1 = [self.sb(f"dt1_{i}", [128, 20, 64], F32) for i in range(2)]
    t2 = [self.sb(f"dt2_{i}", [128, 20, 64], F32) for i in range(2)]
    t3 = [self.sb(f"dt3_{i}", [128, 20, 64], F32) for i in range(2)]
    qkr = [self.sb(f"qkr{i}", [128, 20, 64], BF16) for i in range(2)]
    k2 = [self.sb(f"k2_{i}", [128, 4, 2, 2, 64], BF16) for i in range(2)]
    st = [self.sb(f"dst_{i}", [128, 64], F32) for i in range(2)]
    b_slab, b_t1, b_t2, b_t3, b_qkr, b_k2, b_st = ([Buf(), Buf()] for _ in range(7))
    d_slab = [self.ds(), self.ds()]
    t_first = 0
    for i in range(2):
        self.op("pool", lambda e, i=i: e.memset(k2[i][:].rearrange("p j u h d -> p (j u h d)"), 0.0), w=[b_k2[i]])
    for t in range(t_first, NT):
        i = t % 2
        self.dma("sp", slab[i][:], self.PX[t * 128:(t + 1) * 128, 0:1536], r=[self.b_px[t]], w=[b_slab[i]], dsem=d_slab[i])
        qk = slab[i][:, 0:1280].rearrange("p (h d) -> p h d", d=64)
        self.op("dve", lambda e, i=i, qk=qk: e.tensor_tensor(t1[i][:], qk, qk, ALU.mult), r=[b_slab[i]], w=[b_t1[i]])
        self.op("dve", lambda e, i=i: e.tensor_reduce(st[i][:, 0:20], t1[i][:], AX.X, ALU.add), r=[b_t1[i]], w=[b_st[i]])
        self.op("dve", lambda e, i=i: e.tensor_scalar(st[i][:, 0:20], st[i][:, 0:20], 1.0 / 64, EPS, ALU.mult, ALU.add),
                r=[b_st[i]], w=[b_st[i]])
        self.op("act", lambda e, i=i: e.activation(out=st[i][:, 20:40], in_=st[i][:, 0:20], func=AF.Sqrt), r=[b_st[i]], w=[b_st[i]])
        self.op("dve", lambda e, i=i: e.reciprocal(st[i][:, 40:60], st[i][:, 20:40]), r=[b_st[i]], w=[b_st[i]])
        self.op("dve", lambda e, i=i, qk=qk: e.tensor_tensor(t1[i][:], qk, st[i][:, 40:60].unsqueeze(2).to_broadcast([128, 20, 64]),
                                                             ALU.mult), r=[b_slab[i], b_st[i]], w=[b_t1[i]])
        self.op("dve", lambda e, i=i: e.tensor_tensor(t1[i][:, 0:16, :], t1[i][:, 0:16, :],
                                                      qw[:].unsqueeze(1).to_broadcast([128, 16, 64]), ALU.mult),
                r=[b_c2], w=[b_t1[i]])
        self.op("dve", lambda e, i=i: e.tensor_tensor(t1[i][:, 16:20, :], t1[i][:, 16:20, :],
                                                      kw[:].unsqueeze(1).to_broadcast([128, 4, 64]), ALU.mult),
                r=[b_c], w=[b_t1[i]])
        if t >= 2:
            lt = t - 2
            self.op("pool", lambda e, i=i, lt=lt: e.tensor_tensor(t2[i][:], t1[i][:],
                                                                  cos[:, lt, :].unsqueeze(1).to_broadcast([128, 20, 64]), ALU.mult),
                    r=[b_t1[i], b_c], w=[b_t2[i]])
            v1 = t1[i][:].rearrange("p h (a s j) -> p h a s j", a=2, s=2)
            v3 = t3[i][:].rearrange("p h (a s j) -> p h a s j", a=2, s=2)
            sv = sin[:, lt, :].rearrange("p (a s j) -> p a s j", a=2, s=2)
            for s in range(2):
                self.op("dve", lambda e, s=s, v1=v1, v3=v3, sv=sv: e.tensor_tensor(
                    v3[:, :, :, s, :], v1[:, :, :, 1 - s, :], sv[:, :, s, :].unsqueeze(1).to_broadcast([128, 20, 2, 16]), ALU.mult),
                    r=[b_t1[i], b_c], w=[b_t3[i]])
            self.op("pool", lambda e, i=i: e.tensor_tensor(qkr[i][:], t2[i][:], t3[i][:], ALU.add),
                    r=[b_t2[i], b_t3[i]], w=[b_qkr[i]])
        else:
            self.op("pool", lambda e, i=i: e.tensor_copy(qkr[i][:], t1[i][:]), r=[b_t1[i]], w=[b_qkr[i]])
        for dup in range(2):
            self.op("pool", lambda e, i=i, dup=dup: e.tensor_copy(k2[i][:, :, dup, dup, :], qkr[i][:, 16:20, :]),
                    r=[b_qkr[i]], w=[b_k2[i]])
        self.op("pool", lambda e, i=i, t=t: e.tensor_copy(vaug[:, t, :, 0:64], slab[i][:, 1280:1536].rearrange("p (j d) -> p j d", d=64)),
                r=[b_slab[i]], w=[b_vaug])
        pq = self.ps[6][:].bitcast(BF16)
        pk = self.ps[7][:].bitcast(BF16)
        qflat = qkr[i][:].rearrange("p h d -> p (h d)")
        for pr in range(8):
            self.op("pe", lambda e, pr=pr, pq=pq, qflat=qflat: e.transpose(pq[:, pr * 128:(pr + 1) * 128], qflat[:, pr * 128:(pr + 1) * 128],
                                                                           self.identb[:]),
                    r=[b_qkr[i], self.b_const], w=[self.b_ps[6]])
        self.op("act", lambda e, t=t, pq=pq: e.activation(out=qT[:, :, t * 128:(t + 1) * 128], in_=pq.rearrange("p (a b) -> p a b", b=128),
                                                          func=AF.Copy), r=[self.b_ps[6]], w=[b_qT])
        kflat = k2[i][:].rearrange("p j u h d -> p (j u h d)")
        for j in range(8):
            self.op("pe", lambda e, j=j, pk=pk, kflat=kflat: e.transpose(pk[:, j * 128:(j + 1) * 128], kflat[:, j * 128:(j + 1) * 128],
                                                                         self.identb[:]),
                    r=[b_k2[i], self.b_const], w=[self.b_ps[7]])
        self.op("dve", lambda e, t=t, pk=pk: e.tensor_copy(kT2[:, :, :, t * 128:(t + 1) * 128], pk[:, 0:1024].rearrange("p (a u b) -> p a u b", u=2, b=128)),
                r=[self.b_ps[7]], w=[b_kT2])
    self.dbg(f"qT{l}", qT[:], [128, 8, TT], BF16, r=[b_qT])
    self.dbg(f"kT2{l}", kT2[:], [128, 4, 2, TT], BF16, r=[b_kT2])
    if self.upto == f"D1_{l}":
        return
    E = [self.sb(f"E{i}", [128, 5, 4, 128], BF16) for i in range(2)]
    att = [self.sb(f"att{i}", [128, 1024], BF16) for i in range(2)]
    den = [self.sb(f"den{i}", [128, 8], F32) for i in range(2)]
    b_E, b_att, b_den = [Buf(), Buf()], [Buf(), Buf()], [Buf(), Buf()]
    d_att = [self.ds(), self.ds()]
    sink_v = sinke[:].rearrange("q (j i p) -> q j p i", j=4, i=2, p=2)
    u = 0
    sc = 0
    tq0 = 0 if sl == 0 else 2
    for tq in range(tq0, NT):
        ai = tq % 2
        for j in range(4):
            ei = u % 2
            bo = 4 + (u % 2)
            u += 1
            if tq < 2:
                keys = [0, 1]
            else:
                keys = [0, 1] + [tk for tk in (tq - 1, tq, tq + 1) if 2 <= tk < NT]
            for ki, tk in enumerate(keys):
                bk = sc % 4
                sc += 1
                for p in range(2):
                    self.op("pe", lambda e, p=p, bk=bk, tk=tk, tq=tq, j=j: e.matmul(
                        self.ps[bk][:, p * 256:(p + 1) * 256], lhsT=kT2[:, j, p, tk * 128:(tk + 1) * 128],
                        rhs=qT[:, 2 * j:2 * j + 2, tq * 128:(tq + 1) * 128], start=True, stop=True),
                        r=[b_qT, b_kT2], w=[self.b_ps[bk]])
                self.op("act", lambda e, ei=ei, ki=ki, bk=bk: e.activation(out=E[ei][:, ki, :, :].rearrange("p a b -> p (a b)"),
                                                                            in_=self.ps[bk][:], func=AF.Exp),
                        r=[self.b_ps[bk]], w=[b_E[ei]])
                if tq >= 2 and tk >= 2 and tk != tq:
                    mk = maskp if tk == tq - 1 else maskn
                    self.op("pool", lambda e, ei=ei, ki=ki, mk=mk: e.tensor_tensor(
                        E[ei][:, ki, :, :], E[ei][:, ki, :, :], mk[:].unsqueeze(1).to_broadcast([128, 4, 128]), ALU.mult),
                        r=[b_c], w=[b_E[ei]])
            nk = len(keys)
            for slot in range(4):
                for ki, tk in enumerate(keys):
                    self.op("pe", lambda e, slot=slot, ki=ki, tk=tk, ei=ei, bo=bo, j=j, nk=nk: e.matmul(
                        self.ps[bo][:, slot * 128:slot * 128 + 65], lhsT=E[ei][:, ki, slot, :], rhs=vaug[:, tk, j, :],
                        start=(ki == 0), stop=(ki == nk - 1)), r=[b_E[ei], b_vaug], w=[self.b_ps[bo]])
            ov = self.ps[bo][:].rearrange("q (s d) -> q s d", d=128)
            self.op("dve", lambda e, ai=ai, ov=ov, j=j: e.tensor_tensor(
                den[ai][:, 0:4].rearrange("q (p i) -> q p i", p=2), ov[:, :, 64:65].rearrange("q (p i) o -> q p (i o)", p=2),
                sink_v[:, j], ALU.add), r=[self.b_ps[bo], b_c2], w=[b_den[ai]])
            self.op("dve", lambda e, ai=ai: e.reciprocal(den[ai][:, 4:8], den[ai][:, 0:4]), r=[b_den[ai]], w=[b_den[ai]])
            self.op("dve", lambda e, ai=ai, ov=ov, j=j: e.tensor_tensor(
                att[ai][:, j * 256:(j + 1) * 256].rearrange("q (i p d) -> q p i d", i=2, p=2),
                ov[:, :, 0:64].rearrange("q (p i) d -> q p i d", p=2),
                den[ai][:, 4:8].rearrange("q (p i) -> q p i", p=2).unsqueeze(3).to_broadcast([128, 2, 2, 64]), ALU.mult),
                r=[self.b_ps[bo], b_den[ai]], w=[b_att[ai]])
        self.dma("sp", self.CATD[tq * 128:(tq + 1) * 128, 0:1024], att[ai][:], r=[b_att[ai]], w=[self.b_catd[tq]], dsem=d_att[ai])
    self.dbg(f"ATT{l}", self.CATD, [TT, D], BF16, r=self.b_catd)
    self.barrier()


Builder.phase_D = phase_D


def phase_E(self, l):
    sl = self.cur_layer
    self.phase_reset()
    HS = self.sb("HS", [128, NT, 1024], F32)
    after_hs = self.sb_off
    mqT = self.sb("mqT", [128, 4, TT], BF16)
    mkT = self.sb("mkT", [128, 4, TT], BF16)
    mk = self.sb("mk", [128, NT, 512], BF16)
    vaug = self.sb("mvaug", [128, NT, 4, 257], BF16)
    G = self.sb("G", [128, NT, 16], F32)
    GI = self.sb("GI", [128, NT, 16], F32)
    E1 = self.sb("E1", [128, NT, 16], F32)
    LF = self.sb("LF", [128, NT, 16], F32)
    A = self.sb("Acol", [128, NT, 8], F32)
    bg = self.sb("bg", [128, 16], F32)
    tri = [self.sb("trif", [128, 128], F32), self.sb("trib", [128, 128], F32)]
    CT = self.sb("CT", [128, 8, 257], F32)
    CTb = self.sb("CTb", [128, 8, 257], BF16)
    LFB = [self.sb(f"LFB{i}", [128, 128], F32) for i in range(2)]
    DT = [self.sb(f"DT{i}", [128, 128], F32) for i in range(2)]
    EB = [self.sb(f"EB{i}", [128, 128], F32) for i in range(2)]
    DTm = [self.sb(f"DTm{i}", [128, 128], F32) for i in range(2)]
    WT = [self.sb(f"WT{i}", [128, 128], BF16) for i in range(2)]
    qTs = [self.sb(f"qTs{i}", [128, 128], BF16) for i in range(2)]
    kws = [self.sb(f"kws{i}", [128, 128], BF16) for i in range(2)]
    dd = [self.sb(f"dd{i}", [128, 2], F32) for i in range(2)]
    b_in, b_g, b_A, b_v = Buf(), Buf(), Buf(), Buf()
    b_HS = [[Buf() for h in range(4)] for c in range(NT)]
    b_CT = [Buf() for _ in range(8)]
    b_CTb = [Buf() for _ in range(8)]
    b_LFB, b_DT, b_EB, b_DTm, b_WT, b_qTs, b_kws, b_dd = ([Buf(), Buf()] for _ in range(8))
    d0 = self.ds()
    d1 = self.ds()
    self.dma("sp", mqT[:], self.MQT.rearrange("h p t -> p h t"), r=[self.b_mqt], w=[b_in], dsem=d0)
    self.dma("sp", mkT[:], self.MKT.rearrange("h p t -> p h t"), r=[self.b_mkt], w=[b_in], dsem=d0)
    self.dma("sp", tri[0][:], self.cst["tri_f"], w=[b_in], dsem=d0)
    self.dma("sp", tri[1][:], self.cst["tri_b"], w=[b_in], dsem=d0)
    self.dma("sp", bg[:], self.b_gates[l:l + 1, :].partition_broadcast(128), w=[b_g], dsem=d0)
    self.dma("sp", G[:], self.GD.rearrange("(t p) g -> p t g", p=128), r=self.b_gd, w=[b_g], dsem=d0)
    self.op("pool", lambda e: e.memset(vaug[:].rearrange("p t h v -> p (t h) v")[:, :, 256:257], 1.0), w=[b_v])
    for t in range(NT):
        self.dma("sp", mk[:, t, :], self.PX[t * 128:(t + 1) * 128, 2048:2560], r=[self.b_px[t]], w=[b_in], dsem=d1)
        self.dma("sp", vaug[:, t, :, 0:256], self.PX[t * 128:(t + 1) * 128, 2560:3584].rearrange("p (h v) -> p h v", v=256),
                 r=[self.b_px[t]], w=[b_v], dsem=d1)
    if l == 0:
        dzf = self.ds()
        for r_ in range(0, NE * CAP + 128, 128):
            self.dma("sp", self.FXE[r_:r_ + 128, :], self.zt[:], r=[self.b_zt], w=[self.b_fxe], dsem=dzf)
    self.op("pool", lambda e: e.memset(CT[:].rearrange("p a b -> p (a b)"), 0.0), w=b_CT)
    self.op("pool", lambda e: e.memset(CTb[:].rearrange("p a b -> p (a b)"), 0.0), w=b_CTb)
    Gf = G[:].rearrange("p t g -> p (t g)")
    GIf = GI[:].rearrange("p t g -> p (t g)")
    E1f = E1[:].rearrange("p t g -> p (t g)")
    LFf = LF[:].rearrange("p t g -> p (t g)")
    self.op("dve", lambda e: e.tensor_tensor(G[:], G[:], bg[:].unsqueeze(1).to_broadcast([128, NT, 16]), ALU.add), r=[b_g], w=[b_g])
    self.op("act", lambda e: e.activation(out=Gf, in_=Gf, func=AF.Tanh, scale=1.0 / 15.0), r=[b_g], w=[b_g])
    self.op("dve", lambda e: e.tensor_scalar(GIf, Gf, 15.0, None, ALU.mult), r=[b_g], w=[b_g])
    P1 = self.sb("P1", [128, NT * 16], F32)
    Y2 = self.sb("Y2", [128, NT * 16], F32)
    self.op("dve", lambda e: e.tensor_scalar(E1f, GIf, -1.0, None, ALU.mult), r=[b_g], w=[b_g])
    self.op("dve", lambda e: e.tensor_tensor(E1f, E1f, GIf, ALU.max), r=[b_g], w=[b_g])
    self.op("act", lambda e: e.activation(out=E1f, in_=E1f, func=AF.Exp, scale=-1.0), r=[b_g], w=[b_g])
    self.op("dve", lambda e: e.tensor_scalar(P1[:], E1f, 2.0, None, ALU.add), r=[b_g], w=[b_g])
    self.op("dve", lambda e: e.reciprocal(P1[:], P1[:]), r=[b_g], w=[b_g])
    self.op("dve", lambda e: e.tensor_tensor(E1f, E1f, P1[:], ALU.mult), r=[b_g], w=[b_g])
    self.op("dve", lambda e: e.tensor_tensor(Y2[:], E1f, E1f, ALU.mult), r=[b_g], w=[b_g])
    self.op("dve", lambda e: e.tensor_scalar(P1[:], Y2[:], 1.0 / 13.0, 1.0 / 11.0, ALU.mult, ALU.add), r=[b_g], w=[b_g])
    for cc in (1.0 / 9.0, 1.0 / 7.0, 1.0 / 5.0, 1.0 / 3.0, 1.0):
        self.op("dve", lambda e: e.tensor_tensor(P1[:], P1[:], Y2[:], ALU.mult), r=[b_g], w=[b_g])
        self.op("dve", lambda e, cc=cc: e.tensor_scalar(P1[:], P1[:], cc, None, ALU.add), r=[b_g], w=[b_g])
    self.op("dve", lambda e: e.tensor_tensor(P1[:], P1[:], E1f, ALU.mult), r=[b_g], w=[b_g])
    self.op("dve", lambda e: e.tensor_scalar(E1f, GIf, 0.0, None, ALU.min), r=[b_g], w=[b_g])
    self.op("dve", lambda e: e.scalar_tensor_tensor(LFf, P1[:], -2.0, E1f, ALU.mult, ALU.add), r=[b_g], w=[b_g])
    for d in range(2):
        self.op("pe", lambda e, d=d: e.matmul(self.ps[d][:, 0:NT * 4], lhsT=tri[d][:], rhs=LF[:, :, 4 + 8 * d:8 + 8 * d],
                                              start=True, stop=True), r=[b_g, b_in], w=[self.b_ps[d]])
        self.op("dve", lambda e, d=d: e.tensor_tensor(A[:, :, 4 * d:4 * d + 4], GI[:, :, 8 * d:8 * d + 4],
                                                      self.ps[d][:, 0:NT * 4].rearrange("p (c h) -> p c h", h=4), ALU.subtract),
                r=[b_g, self.b_ps[d]], w=[b_A])
    order = [list(range(NT)), [1, 0] + list(range(NT - 1, 1, -1))]
    u = 0
    hs_written = set()
    for step in range(NT):
        for d in range(2):
            c = order[d][step]
            tl = 127 if d == 0 else 0
            tsl = slice(c * 128, (c + 1) * 128)
            need_h = not (sl == 1 and c < 2)
            for h in range(4):
                par = u % 2
                u += 1
                gf = 4 + 8 * d + h
                ch = d * 4 + h
                pa, pb, pc, pd = (4 * par + i for i in range(4))
                self.op("pool", lambda e, par=par, c=c, gf=gf: e.tensor_copy(LFB[par][:], LF[:, c, gf:gf + 1].to_broadcast([128, 128])),
                        r=[b_g], w=[b_LFB[par]])
                self.op("pe", lambda e, par=par, d=d, pb=pb: e.matmul(self.ps[pb][:, 0:128], lhsT=LFB[par][:], rhs=tri[d][:],
                                                                     start=True, stop=True),
                        r=[b_LFB[par], b_in], w=[self.b_ps[pb]])
                self.op("pe", lambda e, h=h, tsl=tsl, pa=pa: e.matmul(self.ps[pa][:, 0:128], lhsT=mkT[:, h, tsl], rhs=mqT[:, h, tsl],
                                                                     start=True, stop=True),
                        r=[b_in], w=[self.b_ps[pa]])
                self.op("act", lambda e, par=par, pb=pb, c=c, ch=ch: e.activation(out=DT[par][:], in_=self.ps[pb][:, 0:128], func=AF.Exp,
                                                                                 bias=A[:, c, ch:ch + 1]),
                        r=[self.b_ps[pb], b_A], w=[b_DT[par]])
                self.op("act", lambda e, par=par, pb=pb: e.activation(out=EB[par][:], in_=self.ps[pb][:, 0:128], func=AF.Exp),
                        r=[self.b_ps[pb]], w=[b_EB[par]])
                self.op("pool", lambda e, par=par, d=d: e.tensor_tensor(DTm[par][:], DT[par][:], tri[d][:], ALU.mult),
                        r=[b_DT[par], b_in], w=[b_DTm[par]])
                self.op("dve", lambda e, par=par, pa=pa: e.tensor_tensor(WT[par][:], self.ps[pa][:, 0:128], DTm[par][:], ALU.mult),
                        r=[self.b_ps[pa], b_DTm[par]], w=[b_WT[par]])
                self.op("dve", lambda e, par=par, h=h, tsl=tsl: e.tensor_tensor(qTs[par][:], mqT[:, h, tsl], EB[par][:], ALU.mult),
                        r=[b_in, b_EB[par]], w=[b_qTs[par]])
                self.op("pool", lambda e, par=par, c=c, h=h, tl=tl: e.tensor_scalar(kws[par][:], mk[:, c, h * 128:(h + 1) * 128],
                                                                                   DTm[par][:, tl:tl + 1], None, ALU.mult),
                        r=[b_in, b_DTm[par]], w=[b_kws[par]])
                if need_h:
                    self.op("pe", lambda e, par=par, c=c, h=h, pc=pc: e.matmul(self.ps[pc][:, 0:257], lhsT=WT[par][:], rhs=vaug[:, c, h, :],
                                                                              start=True, stop=False),
                            r=[b_WT[par], b_v], w=[self.b_ps[pc]])
                    self.op("pe", lambda e, par=par, ch=ch, pc=pc: e.matmul(self.ps[pc][:, 0:257], lhsT=qTs[par][:], rhs=CTb[:, ch, :],
                                                                           start=False, stop=True),
                            r=[b_qTs[par], b_CTb[ch]], w=[self.b_ps[pc]])
                    self.op("dve", lambda e, par=par, pc=pc: e.tensor_scalar(dd[par][:, 0:1], self.ps[pc][:, 256:257], -1.0, None, ALU.mult),
                            r=[self.b_ps[pc]], w=[b_dd[par]])
                    self.op("dve", lambda e, par=par, pc=pc: e.scalar_tensor_tensor(dd[par][:, 0:1], self.ps[pc][:, 256:257], 1.0, dd[par][:, 0:1],
                                                                                   ALU.max, ALU.max),
                            r=[self.b_ps[pc], b_dd[par]], w=[b_dd[par]])
                    self.op("dve", lambda e, par=par: e.reciprocal(dd[par][:, 1:2], dd[par][:, 0:1]), r=[b_dd[par]], w=[b_dd[par]])
                    hs = HS[:, c, h * 256:(h + 1) * 256]
                    first = (c, h) not in hs_written
                    hs_written.add((c, h))
                    if first:
                        self.op("act", lambda e, par=par, pc=pc, hs=hs: e.activation(out=hs, in_=self.ps[pc][:, 0:256], func=AF.Identity,
                                                                                    scale=dd[par][:, 1:2]),
                                r=[self.b_ps[pc], b_dd[par]], w=[b_HS[c][h]])
                    else:
                        self.op("dve", lambda e, par=par, pc=pc, hs=hs: e.scalar_tensor_tensor(hs, self.ps[pc][:, 0:256], dd[par][:, 1:2], hs,
                                                                                              ALU.mult, ALU.add),
                                r=[self.b_ps[pc], b_dd[par]], w=[b_HS[c][h]])
                if step < NT - 1:
                    self.op("pe", lambda e, par=par, c=c, h=h, pd=pd: e.matmul(self.ps[pd][:, 0:257], lhsT=kws[par][:], rhs=vaug[:, c, h, :],
                                                                              start=True, stop=True),
                            r=[b_kws[par], b_v], w=[self.b_ps[pd]])
                    self.op("dve", lambda e, par=par, ch=ch, pd=pd, tl=tl: e.scalar_tensor_tensor(CT[:, ch, :], CT[:, ch, :], EB[par][:, tl:tl + 1],
                                                                                                 self.ps[pd][:, 0:257], ALU.mult, ALU.add),
                            r=[self.b_ps[pd], b_EB[par]], w=[b_CT[ch]])
                    self.op("act", lambda e, ch=ch: e.activation(out=CTb[:, ch, :], in_=CT[:, ch, :], func=AF.Copy),
                            r=[b_CT[ch]], w=[b_CTb[ch]])
    t0 = 0 if sl == 0 else 2
    self.dbg(f"HS{l}", HS[:], [128, NT, 1024], F32, r=[b for c in range(NT) for b in b_HS[c]])
    if self.upto == f"E1_{l}":
        return
    self.barrier()
    self.sb_off = after_hs
    nw = self.sb("nw", [128, 1024], F32)
    mo = [self.sb(f"mo{i}", [128, 1024], BF16) for i in range(2)]
    t1 = [self.sb(f"et1_{i}", [128, 1024], F32) for i in range(2)]
    sg = [self.sb(f"esg{i}", [128, 1024], F32) for i in range(2)]
    ob = [self.sb(f"eob{i}", [128, 1024], BF16) for i in range(2)]
    junk = self.sb("ejunk", [128, 256], BF16)
    ss = [self.sb(f"ess{i}", [128, 16], F32) for i in range(2)]
    b_nw, b_junk = Buf(), Buf()
    b_mo, b_t1, b_sg, b_ob, b_ss = ([Buf(), Buf()] for _ in range(5))
    dn = self.ds()
    d_mo = [self.ds(), self.ds()]
    d_ob = [self.ds(), self.ds()]
    self.dma("sp", nw[:], self.mlstm_norm_w[l:l + 1, :].partition_broadcast(128), w=[b_nw], dsem=dn)
    for c in range(t0, NT):
        i = c % 2
        rhs_all = b_HS[c]
        self.dma("sp", mo[i][:], self.PX[c * 128:(c + 1) * 128, 3584:4608], r=[self.b_px[c]], w=[b_mo[i]], dsem=d_mo[i])
        for h in range(4):
            self.op("act", lambda e, i=i, c=c, h=h: e.activation(out=junk[:], in_=HS[:, c, h * 256:(h + 1) * 256], func=AF.Square,
                                                                 accum_out=ss[i][:, h:h + 1]),
                    r=[b_HS[c][h]], w=[b_ss[i], b_junk])
        self.op("dve", lambda e, i=i: e.tensor_scalar(ss[i][:, 4:8], ss[i][:, 0:4], 1.0 / 256, EPS, ALU.mult, ALU.add), r=[b_ss[i]], w=[b_ss[i]])
        self.op("act", lambda e, i=i: e.activation(out=ss[i][:, 8:12], in_=ss[i][:, 4:8], func=AF.Sqrt), r=[b_ss[i]], w=[b_ss[i]])
        self.op("dve", lambda e, i=i: e.reciprocal(ss[i][:, 12:16], ss[i][:, 8:12]), r=[b_ss[i]], w=[b_ss[i]])
        self.op("dve", lambda e, i=i, c=c: e.tensor_tensor(t1[i][:].rearrange("p (h v) -> p h v", v=256),
                                                           HS[:, c, :].rearrange("p (h v) -> p h v", v=256),
                                                           ss[i][:, 12:16].unsqueeze(2).to_broadcast([128, 4, 256]), ALU.mult),
                r=rhs_all + [b_ss[i]], w=[b_t1[i]])
        self.op("pool", lambda e, i=i: e.tensor_tensor(t1[i][:], t1[i][:], nw[:], ALU.mult), r=[b_nw], w=[b_t1[i]])
        self.op("act", lambda e, i=i: e.activation(out=sg[i][:], in_=mo[i][:], func=AF.Sigmoid), r=[b_mo[i]], w=[b_sg[i]])
        self.op("dve", lambda e, i=i: e.tensor_tensor(ob[i][:], t1[i][:], sg[i][:], ALU.mult), r=[b_t1[i], b_sg[i]], w=[b_ob[i]])
        self.dma("sp", self.CATD[c * 128:(c + 1) * 128, 1024:2048], ob[i][:], r=[b_ob[i]], w=[self.b_catd[c]], dsem=d_ob[i])
    self.dbg(f"CAT{l}", self.CATD, [TT, D], BF16, r=self.b_catd)
    self.barrier()


Builder.phase_E = phase_E


def phase_F(self, l):
    sl = self.cur_layer
    self.phase_reset()
    src = self.x_in if l == 0 else self.XRES
    wout = self.sb("wout", [128, 16, D], BF16)
    G1 = self.sb("G1", [128, 2, D], F32)
    cat = [self.sb(f"fcat{i}", [128, D], BF16) for i in range(2)]
    xt = [self.sb(f"fxt{i}", [128, D], F32) for i in range(2)]
    catT = [self.sb(f"fcatT{i}", [128, 16, 128], BF16) for i in range(2)]
    xo = [self.sb(f"fxo{i}", [128, D], F32) for i in range(2)]
    b_wout, b_G1 = Buf(), Buf()
    b_cat, b_xt, b_catT, b_xo = ([Buf(), Buf()] for _ in range(4))
    dw, dg = self.ds(), self.ds()
    d_cat, d_xt, d_xo = ([self.ds(), self.ds()] for _ in range(3))
    wv = self.w_out[l].rearrange("(k p) n -> p k n", p=128)
    for k0 in range(0, 16, 4):
        self.dma("pool", wout[:, k0:k0 + 4, :], wv[:, k0:k0 + 4, :], w=[b_wout], dsem=dw)
    for w_ in range(2):
        self.dma("sp", G1[:, w_, :], self.MODD[w_:w_ + 1, 2 * D:3 * D].partition_broadcast(128), r=[self.b_modd], w=[b_G1], dsem=dg)
    t0 = 0 if sl == 0 else 2
    for t in range(t0, NT):
        i = t % 2
        which = 1 if t < 2 else 0
        rows = slice(t * 128, (t + 1) * 128)
        self.dma("sp", cat[i][:], self.CATD[rows, :], r=[self.b_catd[t]], w=[b_cat[i]], dsem=d_cat[i])
        self.dma("sp", xt[i][:], src[rows, :], r=([self.b_xres[t]] if l > 0 else []), w=[b_xt[i]], dsem=d_xt[i])
        bA, bB = 4 + 2 * i, 5 + 2 * i
        pA = self.ps[bA][:].bitcast(BF16)
        pB = self.ps[bB][:].bitcast(BF16)
        for k in range(16):
            pX, bX = (pA, bA) if k < 8 else (pB, bB)
            self.op("pe", lambda e, k=k, pX=pX, i=i: e.transpose(pX[:, (k % 8) * 128:(k % 8 + 1) * 128], cat[i][:, k * 128:(k + 1) * 128],
                                                                 self.identb[:]),
                    r=[b_cat[i], self.b_const], w=[self.b_ps[bX]])
        self.op("act", lambda e, i=i, pA=pA: e.activation(out=catT[i][:, 0:8, :], in_=pA.rearrange("p (a b) -> p a b", b=128), func=AF.Copy),
                r=[self.b_ps[bA]], w=[b_catT[i]])
        self.op("dve", lambda e, i=i, pB=pB: e.tensor_copy(catT[i][:, 8:16, :], pB.rearrange("p (a b) -> p a b", b=128)),
                r=[self.b_ps[bB]], w=[b_catT[i]])
        for nb in range(4):
            cols = slice(nb * 512, (nb + 1) * 512)
            for k in range(16):
                self.op("pe", lambda e, k=k, nb=nb, i=i, cols=cols: e.matmul(self.ps[nb][:, 0:512], lhsT=catT[i][:, k, :], rhs=wout[:, k, cols],
                                                                            start=(k == 0), stop=(k == 15)),
                        r=[b_catT[i], b_wout], w=[self.b_ps[nb]])
            self.op("dve", lambda e, nb=nb, i=i, cols=cols, which=which: e.tensor_tensor(xo[i][:, cols], self.ps[nb][:, 0:512], G1[:, which, cols], ALU.mult),
                    r=[self.b_ps[nb], b_G1], w=[b_xo[i]])
            self.op("pool", lambda e, i=i, cols=cols: e.tensor_tensor(xo[i][:, cols], xo[i][:, cols], xt[i][:, cols], ALU.add),
                    r=[b_xt[i]], w=[b_xo[i]])
        self.dma("sp", self.XRES[rows, :], xo[i][:], r=[b_xo[i]], w=[self.b_xres[t]], dsem=d_xo[i])
    self.dbg(f"XMID{l}", self.XRES, [TT, D], F32, r=self.b_xres)
    self.barrier()


Builder.phase_F = phase_F


def phase_G(self, l):
    sl = self.cur_layer
    last = (sl == self.last_layer)
    self.phase_reset()
    NX = self.n_exp
    wr = self.sb("wr", [128, 16, NE], F32)
    br = self.sb("br", [128, NE], F32)
    bguT = self.sb("bguT", [128, 16, NE], F32)
    bd = self.sb("bd", [NE, D], F32)
    G2 = self.sb("G2", [128, 2, D], F32)
    selb = [self.sb(f"selb{i}", [NE, 128], F32) for i in range(2)]
    mark = self.sb_off
    braw = self.sb("braw", [NE, D], F32)
    b_c, b_braw = Buf(), Buf()
    dc = self.ds()
    self.dma("sp", wr[:], self.w_router[l].rearrange("(k p) e -> p k e", p=128), w=[b_c], dsem=dc)
    self.dma("sp", br[:], self.b_router[l:l + 1, :].partition_broadcast(128), w=[b_c], dsem=dc)
    self.dma("sp", bd[0:NX, :], self.b_down[l], w=[b_c], dsem=dc)
    self.dma("sp", braw[0:NX, :], self.b_gate_up[l], w=[b_braw], dsem=dc)
    for w_ in range(2):
        self.dma("sp", G2[:, w_, :], self.MODD[w_:w_ + 1, 5 * D:6 * D].partition_broadcast(128), r=[self.b_modd], w=[b_c], dsem=dc)
    for j in range(16):
        self.op("pe", lambda e, j=j: e.transpose(self.ps[0][:, j * NE:j * NE + NX], braw[0:NX, j * 128:(j + 1) * 128],
                                                 self.identf[0:NX, 0:NX]),
                r=[b_braw, self.b_const], w=[self.b_ps[0]])
    self.op("act", lambda e: e.activation(out=bguT[:, :, 0:NX], in_=self.ps[0][:, 0:16 * NE].rearrange("p (j e) -> p j e", e=NE)[:, :, 0:NX],
                                          func=AF.Copy), r=[self.b_ps[0]], w=[b_c])
    self.barrier()
    self.sb_off = mark
    fxT = self.sb("fxT", [128, 16, 512], BF16)
    acc = self.sb("acc", [128, 4, D], F32)
    actT = self.sb("actT", [128, 8, 512], BF16)
    wdb = self.sb("wdb", [128, 8, D], BF16)
    wgb = [self.sb(f"wgb{i}", [128, 16, 2, 128], BF16) for i in range(3)]
    CB = [self.sb(f"CB{i}", [128, 512], F32) for i in range(2)]
    gS = [self.sb(f"gS{i}", [128, 512], F32) for i in range(2)]
    sg = [self.sb(f"sg{i}", [128, 512], F32) for i in range(2)]
    uS = [self.sb(f"uS{i}", [128, 512], F32) for i in range(2)]
    xt = self.sb("gxt", [128, D], F32)
    xn = self.sb("gxn", [128, D], F32)
    f32T = self.sb("gf32T", [128, 16, 128], F32)
    junk = self.sb("gjunk", [128, D], BF16)
    ss = self.sb("gss", [128, 4], F32)
    combT = self.sb("combT", [NE, 512], F32)
    lg = self.sb("lg", [128, NE], F32)
    ex = self.sb("ex", [128, NE], F32)
    msk = self.sb("msk", [128, NE], F32)
    mx8 = self.sb("mx8", [128, 8], F32)
    sm = self.sb("sm", [128, 4], F32)
    b_fxT, b_actT, b_wdb, b_xt, b_f32T, b_combT, b_r = (Buf() for _ in range(7))
    b_acc = [Buf() for _ in range(4)]
    b_wgb = [Buf() for _ in range(3)]
    b_CB, b_gS, b_sg, b_uS, b_sel = ([Buf(), Buf()] for _ in range(5))
    b_tmp = (Buf(), Buf())
    d_xt = self.ds()
    d_wd = self.ds()
    d_wg = [self.ds() for _ in range(3)]
    d_out = self.ds()
    tiles_all = list(range(0 if sl == 0 else 2, NT))
    groups = [tiles_all[i:i + 4] for i in range(0, len(tiles_all), 4)]
    cnt = 0
    dcnt = 0
    for tiles in groups:
        ntok = 128 * len(tiles)
        for j, t in enumerate(tiles):
            which = 1 if t < 2 else 0
            rows = slice(t * 128, (t + 1) * 128)
            self.dma("sp", xt[:], self.XRES[rows, :], r=[self.b_xres[t]], w=[b_xt], dsem=d_xt)
            self.norm_tile(xt, b_xt, which, self.S2, 48, f32T, b_f32T, fxT, b_fxT, j * 128, [4, 5, 6, 7], junk, ss, xn, b_tmp)
            for k in range(16):
                self.op("pe", lambda e, k=k: e.matmul(self.ps[0][:, 0:NE], lhsT=f32T[:, k, :], rhs=wr[:, k, :], start=(k == 0), stop=(k == 15)),
                        r=[b_f32T, b_c], w=[self.b_ps[0]])
            self.op("dve", lambda e: e.tensor_tensor(lg[:], self.ps[0][:, 0:NE], br[:], ALU.add), r=[self.b_ps[0], b_c], w=[b_r])
            self.op("dve", lambda e: e.max(out=mx8[:], in_=lg[:]), r=[b_r], w=[b_r])
            self.op("dve", lambda e: e.tensor_scalar(msk[:], lg[:], mx8[:, 3:4], None, ALU.is_ge), r=[b_r], w=[b_r])
            self.op("dve", lambda e: e.tensor_scalar(sm[:, 0:1], mx8[:, 0:1], -1.0, None, ALU.mult), r=[b_r], w=[b_r])
            self.op("act", lambda e: e.activation(out=ex[:], in_=lg[:], func=AF.Exp, bias=sm[:, 0:1]), r=[b_r], w=[b_r])
            self.op("dve", lambda e: e.tensor_tensor(ex[:], ex[:], msk[:], ALU.mult), r=[b_r], w=[b_r])
            self.op("dve", lambda e: e.tensor_reduce(sm[:, 1:2], ex[:], AX.X, ALU.add), r=[b_r], w=[b_r])
            self.op("dve", lambda e: e.reciprocal(sm[:, 2:3], sm[:, 1:2]), r=[b_r], w=[b_r])
            self.op("dve", lambda e: e.tensor_scalar(ex[:], ex[:], sm[:, 2:3], None, ALU.mult), r=[b_r], w=[b_r])
            self.op("pe", lambda e: e.transpose(self.ps[1][0:NE, 0:128], ex[:], self.identf[:]), r=[b_r, self.b_const], w=[self.b_ps[1]])
            self.op("act", lambda e, j=j: e.activation(out=combT[:, j * 128:(j + 1) * 128], in_=self.ps[1][0:NE, 0:128], func=AF.Copy),
                    r=[self.b_ps[1]], w=[b_combT])
        if f"COMB{l}" in self.debug and tiles is groups[0]:
            self.dbg(f"COMB{l}", combT[:], [NE, 512], F32, r=[b_combT])
        for j in range(len(tiles)):
            for nb in range(4):
                bk = 2 + (nb % 2)
                cols = slice(nb * 512, (nb + 1) * 512)
                self.op("pe", lambda e, j=j, bk=bk, cols=cols: e.matmul(self.ps[bk][:, 0:512], lhsT=combT[0:NX, j * 128:(j + 1) * 128],
                                                                        rhs=bd[0:NX, cols], start=True, stop=True),
                        r=[b_combT, b_c], w=[self.b_ps[bk]])
                self.op("act", lambda e, j=j, bk=bk, cols=cols: e.activation(out=acc[:, j, cols], in_=self.ps[bk][:, 0:512], func=AF.Copy),
                        r=[self.b_ps[bk]], w=[b_acc[j]])
        for ex_i in range(NX):
            si = ex_i % 2
            self.op("dve", lambda e, si=si, ex_i=ex_i: e.tensor_copy(selb[si][:], self.identf[0:NE, ex_i:ex_i + 1].to_broadcast([NE, 128])),
                    r=[self.b_const], w=[b_sel[si]])
            self.op("pe", lambda e, si=si, ntok=ntok: e.matmul(self.ps[7][:, 0:ntok], lhsT=selb[si][:], rhs=combT[:, 0:ntok], start=True, stop=True),
                    r=[b_sel[si], b_combT], w=[self.b_ps[7]])
            self.op("act", lambda e, si=si, ntok=ntok: e.activation(out=CB[si][:, 0:ntok], in_=self.ps[7][:, 0:ntok], func=AF.Copy),
                    r=[self.b_ps[7]], w=[b_CB[si]])
            self.dma("pool", wdb[:], self.w_down[l, ex_i].rearrange("(c p) n -> p c n", p=128), w=[b_wdb], dsem=d_wd)
            wgv = self.w_gate_up[l, ex_i].rearrange("(k p) n -> p k n", p=128)
            for fc in range(8):
                s = cnt % 3
                par = cnt % 2
                cnt += 1
                self.dma("pool", wgb[s][:, :, 0, :], wgv[:, :, fc * 128:(fc + 1) * 128], w=[b_wgb[s]], dsem=d_wg[s])
                self.dma("pool", wgb[s][:, :, 1, :], wgv[:, :, DFF + fc * 128:DFF + (fc + 1) * 128], w=[b_wgb[s]], dsem=d_wg[s])
                pg, pu = 2 * par, 2 * par + 1
                for gu, pb in ((0, pg), (1, pu)):
                    for k in range(16):
                        self.op("pe", lambda e, k=k, s=s, gu=gu, pb=pb, ntok=ntok: e.matmul(self.ps[pb][:, 0:ntok], lhsT=wgb[s][:, k, gu, :],
                                                                                         rhs=fxT[:, k, 0:ntok], start=(k == 0), stop=(k == 15)),
                                r=[b_wgb[s], b_fxT], w=[self.b_ps[pb]])
                self.op("dve", lambda e, par=par, pg=pg, fc=fc, ex_i=ex_i, ntok=ntok: e.tensor_scalar(
                    gS[par][:, 0:ntok], self.ps[pg][:, 0:ntok], bguT[:, fc, ex_i:ex_i + 1], 7.0, ALU.add, ALU.min),
                    r=[self.b_ps[pg], b_c], w=[b_gS[par]])
                self.op("act", lambda e, par=par, ntok=ntok: e.activation(out=sg[par][:, 0:ntok], in_=gS[par][:, 0:ntok], func=AF.Sigmoid, scale=1.702),
                        r=[b_gS[par]], w=[b_sg[par]])
                self.op("dve", lambda e, par=par, pu=pu, fc=fc, ex_i=ex_i, ntok=ntok: e.tensor_scalar(
                    uS[par][:, 0:ntok], self.ps[pu][:, 0:ntok], bguT[:, 8 + fc, ex_i:ex_i + 1], 7.0, ALU.add, ALU.min),
                    r=[self.b_ps[pu], b_c], w=[b_uS[par]])
                self.op("dve", lambda e, par=par, ntok=ntok: e.tensor_scalar(uS[par][:, 0:ntok], uS[par][:, 0:ntok], -7.0, 1.0, ALU.max, ALU.add),
                        r=[b_uS[par]], w=[b_uS[par]])
                self.op("dve", lambda e, par=par, ntok=ntok: e.tensor_tensor(gS[par][:, 0:ntok], gS[par][:, 0:ntok], sg[par][:, 0:ntok], ALU.mult),
                        r=[b_sg[par]], w=[b_gS[par]])
                self.op("dve", lambda e, par=par, si=si, ntok=ntok: e.tensor_tensor(gS[par][:, 0:ntok], gS[par][:, 0:ntok], CB[si][:, 0:ntok], ALU.mult),
                        r=[b_CB[si]], w=[b_gS[par]])
                self.op("dve", lambda e, par=par, fc=fc, ntok=ntok: e.tensor_tensor(actT[:, fc, 0:ntok], uS[par][:, 0:ntok], gS[par][:, 0:ntok], ALU.mult),
                        r=[b_uS[par], b_gS[par]], w=[b_actT])
            for j in range(len(tiles)):
                for nb in range(4):
                    bk = 4 + (dcnt % 3)
                    dcnt += 1
                    cols = slice(nb * 512, (nb + 1) * 512)
                    for fc in range(8):
                        self.op("pe", lambda e, j=j, fc=fc, bk=bk, cols=cols: e.matmul(self.ps[bk][:, 0:512], lhsT=actT[:, fc, j * 128:(j + 1) * 128],
                                                                                     rhs=wdb[:, fc, cols], start=(fc == 0), stop=(fc == 7)),
                                r=[b_actT, b_wdb], w=[self.b_ps[bk]])
                    self.op("dve", lambda e, j=j, bk=bk, cols=cols: e.tensor_tensor(acc[:, j, cols], self.ps[bk][:, 0:512], acc[:, j, cols], ALU.add),
                            r=[self.b_ps[bk]], w=[b_acc[j]])
        for j, t in enumerate(tiles):
            which = 1 if t < 2 else 0
            rows = slice(t * 128, (t + 1) * 128)
            self.dma("sp", xt[:], self.XRES[rows, :], r=[self.b_xres[t]], w=[b_xt], dsem=d_xt)
            self.op("dve", lambda e, j=j, which=which: e.tensor_tensor(acc[:, j, :], acc[:, j, :], G2[:, which, :], ALU.mult), r=[b_c], w=[b_acc[j]])
            self.op("pool", lambda e, j=j: e.tensor_tensor(acc[:, j, :], acc[:, j, :], xt[:], ALU.add), r=[b_xt], w=[b_acc[j]])
            if last:
                by = Buf()
                self.dma("sp", self.y_out[(t - 2) * 128:(t - 1) * 128, :], acc[:, j, :], r=[b_acc[j]], w=[by], dsem=d_out)
                self.final_reads.append(by)
            else:
                self.dma("sp", self.XRES[rows, :], acc[:, j, :], r=[b_acc[j]], w=[self.b_xres[t]], dsem=d_out)
    if not last:
        self.dbg(f"XOUT{l}", self.XRES, [TT, D], F32, r=self.b_xres)
    else:
        self.dbg(f"YOUT{l}", self.y_out, [2048, D], F32, r=self.final_reads)
    self.barrier()


Builder.phase_G = phase_G


def phase_G2(self, l):
    sl = self.cur_layer
    last = (sl == self.last_layer)
    self.phase_reset()
    NX = self.n_exp
    NR = NE * CAP
    wr = self.sb("wr", [128, 16, NE], F32)
    br = self.sb("br", [128, NE], F32)
    bguT = self.sb("bguT", [128, 16, NE], F32)
    bd = self.sb("bd", [NE, D], F32)
    G2 = self.sb("G2", [128, 2, D], F32)
    IDX = self.sb("IDX", [128, NT, 4], I32)
    WJ = self.sb("WJ", [128, NT, 4], F32)
    combT = self.sb("combT", [NE, TT], F32)
    OFF = self.sb("OFF", [128, NE], F32)
    ecst = self.sb("ecst", [128, NE], F32)
    tris = self.sb("tris", [128, 128], F32)
    trash = self.sb("trash", [128, 1], F32)
    mark = self.sb_off
    braw = self.sb("braw", [NE, D], F32)
    b_c, b_braw, b_idx, b_combT, b_off = Buf(), Buf(), Buf(), Buf(), Buf()
    dc = self.ds()
    self.dma("sp", wr[:], self.w_router[l].rearrange("(k p) e -> p k e", p=128), w=[b_c], dsem=dc)
    self.dma("sp", br[:], self.b_router[l:l + 1, :].partition_broadcast(128), w=[b_c], dsem=dc)
    self.dma("sp", bd[0:NX, :], self.b_down[l], w=[b_c], dsem=dc)
    self.dma("sp", braw[0:NX, :], self.b_gate_up[l], w=[b_braw], dsem=dc)
    self.dma("sp", ecst[:], self.cst["ecst"], w=[b_c], dsem=dc)
    self.dma("sp", tris[:], self.cst["tri_s"], w=[b_c], dsem=dc)
    self.dma("sp", trash[:], self.cst["trash"], w=[b_c], dsem=dc)
    for w_ in range(2):
        self.dma("sp", G2[:, w_, :], self.MODD[w_:w_ + 1, 5 * D:6 * D].partition_broadcast(128), r=[self.b_modd], w=[b_c], dsem=dc)
    for j in range(16):
        self.op("pe", lambda e, j=j: e.transpose(self.ps[0][:, j * NE:j * NE + NX], braw[0:NX, j * 128:(j + 1) * 128],
                                                 self.identf[0:NX, 0:NX]),
                r=[b_braw, self.b_const], w=[self.b_ps[0]])
    self.op("act", lambda e: e.activation(out=bguT[:, :, 0:NX], in_=self.ps[0][:, 0:16 * NE].rearrange("p (j e) -> p j e", e=NE)[:, :, 0:NX],
                                          func=AF.Copy), r=[self.b_ps[0]], w=[b_c])
    self.op("dve", lambda e: e.memset(OFF[:], 0.0), w=[b_off])
    self.barrier()
    self.sb_off = mark
    tiles_all = list(range(0 if sl == 0 else 2, NT))
    xt2_ = [self.sb(f"gxt{i}", [128, D], F32) for i in range(2)]
    xn2_ = [self.sb(f"gxn{i}", [128, D], F32) for i in range(2)]
    f32T2_ = [self.sb(f"gf32T{i}", [128, 16, 128], F32) for i in range(2)]
    junk = self.sb("gjunk", [128, D], BF16)
    ss2_ = [self.sb(f"gss{i}", [128, 4], F32) for i in range(2)]
    S2r = self.sb("S2r", [128, 2, D], F32)
    SHr = self.sb("SHr", [128, 2, D], F32)
    nwr = self.sb("nwr", [128, D], F32)
    ftok = [self.sb(f"ftok{i}", [128, D], BF16) for i in range(2)]
    lg = self.sb("lg", [128, NE], F32)
    ex = self.sb("ex", [128, NE], F32)
    msk = self.sb("msk", [128, NE], F32)
    rk = self.sb("rk", [128, NE], F32)
    key = self.sb("key", [128, NE], F32)
    tmpk = self.sb("tmpk", [128, NE], F32)
    mx8 = self.sb("mx8", [128, 8], F32)
    k8 = self.sb("k8", [128, 8], F32)
    sm = self.sb("sm", [128, 4], F32)
    b_r, b_rows = Buf(), Buf()
    b_xt2_, b_f32T2_, b_ftok = ([Buf(), Buf()] for _ in range(3))
    b_tmp2_ = [(Buf(), Buf()), (Buf(), Buf())]
    d_xt2_ = [self.ds(), self.ds()]
    d_rows = self.ds()
    d_sc = [self.ds(), self.ds()]
    for w_ in range(2):
        self.dma("sp", S2r[:, w_, :], self.MODD[w_:w_ + 1, 4 * D:5 * D].partition_broadcast(128), r=[self.b_modd], w=[b_rows], dsem=d_rows)
        self.dma("sp", SHr[:, w_, :], self.MODD[w_:w_ + 1, 3 * D:4 * D].partition_broadcast(128), r=[self.b_modd], w=[b_rows], dsem=d_rows)
    self.dma("sp", nwr[:], self.norm2_w[l:l + 1, :].partition_broadcast(128), w=[b_rows], dsem=d_rows)
    for w_ in range(2):
        self.op("dve", lambda e, w_=w_: e.scalar_tensor_tensor(S2r[:, w_, :], S2r[:, w_, :], 1.0, nwr[:], ALU.add, ALU.mult), r=[b_rows], w=[b_rows])
    for t in tiles_all:
        which = 1 if t < 2 else 0
        fi = t % 2
        rows = slice(t * 128, (t + 1) * 128)
        xt, xn, f32T, ss = xt2_[fi], xn2_[fi], f32T2_[fi], ss2_[fi]
        b_xt, b_f32T, b_tmp = b_xt2_[fi], b_f32T2_[fi], b_tmp2_[fi]
        self.dma("sp", xt[:], self.XRES[rows, :], r=[self.b_xres[t]], w=[b_xt], dsem=d_xt2_[fi])
        self.norm_tile(xt, b_xt, which, self.S2, 48, f32T, b_f32T, None, None, 0, [4, 5, 6, 7], junk, ss, xn, b_tmp)
        self.op("dve", lambda e, which=which, xn=xn: e.tensor_tensor(xn[:], xn[:], S2r[:, which, :], ALU.mult), r=[b_rows], w=[b_tmp[1]])
        self.op("dve", lambda e, which=which, fi=fi, xn=xn: e.tensor_tensor(ftok[fi][:], xn[:], SHr[:, which, :], ALU.add), r=[b_rows, b_tmp[1]], w=[b_ftok[fi]])
        for k in range(16):
            self.op("pe", lambda e, k=k, f32T=f32T: e.matmul(self.ps[0][:, 0:NE], lhsT=f32T[:, k, :], rhs=wr[:, k, :], start=(k == 0), stop=(k == 15)),
                    r=[b_f32T, b_c], w=[self.b_ps[0]])
        self.op("dve", lambda e: e.tensor_tensor(lg[:], self.ps[0][:, 0:NE], br[:], ALU.add), r=[self.b_ps[0], b_c], w=[b_r])
        self.op("dve", lambda e: e.max(out=mx8[:], in_=lg[:]), r=[b_r], w=[b_r])
        self.op("dve", lambda e: e.tensor_scalar(msk[:], lg[:], mx8[:, 3:4], None, ALU.is_ge), r=[b_r], w=[b_r])
        self.op("dve", lambda e: e.tensor_scalar(sm[:, 0:1], mx8[:, 0:1], -1.0, None, ALU.mult), r=[b_r], w=[b_r])
        self.op("act", lambda e: e.activation(out=ex[:], in_=lg[:], func=AF.Exp, bias=sm[:, 0:1]), r=[b_r], w=[b_r])
        self.op("dve", lambda e: e.tensor_tensor(ex[:], ex[:], msk[:], ALU.mult), r=[b_r], w=[b_r])
        self.op("dve", lambda e: e.tensor_reduce(sm[:, 1:2], ex[:], AX.X, ALU.add), r=[b_r], w=[b_r])
        self.op("dve", lambda e: e.reciprocal(sm[:, 2:3], sm[:, 1:2]), r=[b_r], w=[b_r])
        self.op("dve", lambda e: e.tensor_scalar(ex[:], ex[:], sm[:, 2:3], None, ALU.mult), r=[b_r], w=[b_r])
        self.op("pe", lambda e: e.transpose(self.ps[1][0:NE, 0:128], ex[:], self.identf[:]), r=[b_r, self.b_const], w=[self.b_ps[1]])
        self.op("act", lambda e, t=t: e.activation(out=combT[:, t * 128:(t + 1) * 128], in_=self.ps[1][0:NE, 0:128], func=AF.Copy),
                r=[self.b_ps[1]], w=[b_combT])
        self.op("pe", lambda e: e.matmul(self.ps[2][:, 0:NE], lhsT=tris[:], rhs=msk[:], start=True, stop=True), r=[b_r, b_c], w=[self.b_ps[2]])
        self.op("pe", lambda e: e.matmul(self.ps[3][:, 0:NE], lhsT=self.onesf[:], rhs=msk[:], start=True, stop=True),
                r=[b_r, self.b_const], w=[self.b_ps[3]])
        self.op("dve", lambda e: e.tensor_tensor(rk[:], self.ps[2][:, 0:NE], OFF[:], ALU.add), r=[self.b_ps[2], b_off], w=[b_r])
        self.op("dve", lambda e: e.tensor_tensor(OFF[:], OFF[:], self.ps[3][:, 0:NE], ALU.add), r=[self.b_ps[3]], w=[b_off])
        self.op("dve", lambda e: e.tensor_scalar(tmpk[:], rk[:], float(CAP), None, ALU.is_lt), r=[b_r], w=[b_r])
        self.op("dve", lambda e: e.tensor_tensor(tmpk[:], tmpk[:], msk[:], ALU.mult), r=[b_r], w=[b_r])
        self.op("dve", lambda e: e.tensor_tensor(rk[:], rk[:], ecst[:], ALU.add), r=[b_r, b_c], w=[b_r])
        self.op("dve", lambda e: e.tensor_scalar(rk[:], rk[:], -1.0, KBIG, ALU.mult, ALU.add), r=[b_r], w=[b_r])
        self.op("dve", lambda e: e.tensor_tensor(key[:], rk[:], tmpk[:], ALU.mult), r=[b_r], w=[b_r])
        self.op("dve", lambda e: e.max(out=k8[:], in_=key[:]), r=[b_r], w=[b_r])
        self.op("dve", lambda e: e.tensor_scalar(sm[:, 0:4], k8[:, 0:4], -1.0, KBIG, ALU.mult, ALU.add), r=[b_r], w=[b_r])
        self.op("dve", lambda e: e.tensor_scalar(sm[:, 0:4], sm[:, 0:4], trash[:, 0:1], None, ALU.subtract), r=[b_r, b_c], w=[b_r])
        self.op("dve", lambda e: e.tensor_scalar(k8[:, 4:8], k8[:, 0:4], 0.0, None, ALU.is_gt), r=[b_r], w=[b_r])
        self.op("dve", lambda e: e.tensor_tensor(sm[:, 0:4], sm[:, 0:4], k8[:, 4:8], ALU.mult), r=[b_r], w=[b_r])
        self.op("dve", lambda e: e.tensor_scalar(sm[:, 0:4], sm[:, 0:4], trash[:, 0:1], None, ALU.add), r=[b_r, b_c], w=[b_r])
        self.op("dve", lambda e, t=t: e.tensor_copy(IDX[:, t, :], sm[:, 0:4]), r=[b_r], w=[b_idx])
        for j in range(4):
            self.op("dve", lambda e, j=j: e.tensor_scalar(tmpk[:], key[:], k8[:, j:j + 1], None, ALU.is_equal), r=[b_r], w=[b_r])
            self.op("dve", lambda e: e.tensor_tensor(tmpk[:], tmpk[:], ex[:], ALU.mult), r=[b_r], w=[b_r])
            self.op("dve", lambda e, j=j, t=t: e.tensor_reduce(WJ[:, t, j:j + 1], tmpk[:], AX.X, ALU.add), r=[b_r], w=[b_idx])
        for j in range(4):
            self.P.op("pool", lambda e, t=t, j=j, fi=fi: e.indirect_dma_start(
                out=self.FXE[:, :], out_offset=bass.IndirectOffsetOnAxis(ap=IDX[:, t, j:j + 1], axis=0),
                in_=ftok[fi][:, :], in_offset=None),
                reads=[b_idx, b_ftok[fi]], writes=[self.b_fxe], dsem=d_sc[fi])
    self.dbg(f"IDX{l}", IDX[:], [128, NT, 4], I32, r=[b_idx])
    self.dbg(f"WJ{l}", WJ[:], [128, NT, 4], F32, r=[b_idx])
    self.barrier()
    self.sb_off = mark
    fxT = self.sb("fxT", [128, 16, CAP], BF16)
    actT = self.sb("actT", [128, 8, CAP], BF16)
    wdb = self.sb("wdb", [128, 8, D], BF16)
    wgb = [self.sb(f"wgb{i}", [128, 16, 2, 128], BF16) for i in range(3)]
    gS = [self.sb(f"gS{i}", [128, 512], F32) for i in range(2)]
    sg = [self.sb(f"sg{i}", [128, 512], F32) for i in range(2)]
    uS = [self.sb(f"uS{i}", [128, 512], F32) for i in range(2)]
    xe = [self.sb(f"xe{i}", [128, D], BF16) for i in range(2)]
    yrow = [self.sb(f"yrow{i}", [128, D], F32) for i in range(2)]
    b_fxT, b_actT, b_wdb = Buf(), Buf(), Buf()
    b_wgb = [Buf() for _ in range(3)]
    b_gS, b_sg, b_uS, b_xe, b_yrow = ([Buf(), Buf()] for _ in range(5))
    d_wd = self.ds()
    d_wg = [self.ds() for _ in range(3)]
    d_xe = [self.ds(), self.ds()]
    d_y = [self.ds(), self.ds()]
    cnt = 0
    dcnt = 0
    xcnt = 0
    hcnt = 0
    ntok = CAP
    nst = CAP // 128
    for ex_i in range(NX):
        r0 = ex_i * CAP
        for s_ in range(nst):
            xi = xcnt % 2
            xcnt += 1
            self.dma("sp", xe[xi][:], self.FXE[r0 + s_ * 128:r0 + (s_ + 1) * 128, :], r=[self.b_fxe], w=[b_xe[xi]], dsem=d_xe[xi])
            bA, bB = 4 + 2 * xi, 5 + 2 * xi
            pA = self.ps[bA][:].bitcast(BF16)
            pB = self.ps[bB][:].bitcast(BF16)
            for k in range(16):
                pX, bX = (pA, bA) if k < 8 else (pB, bB)
                self.op("pe", lambda e, k=k, pX=pX, xi=xi: e.transpose(pX[:, (k % 8) * 128:(k % 8 + 1) * 128], xe[xi][:, k * 128:(k + 1) * 128],
                                                                      self.identb[:]),
                        r=[b_xe[xi], self.b_const], w=[self.b_ps[bX]])
            self.op("act", lambda e, s_=s_, pA=pA: e.activation(out=fxT[:, 0:8, s_ * 128:(s_ + 1) * 128], in_=pA.rearrange("p (a b) -> p a b", b=128),
                                                               func=AF.Copy), r=[self.b_ps[bA]], w=[b_fxT])
            self.op("dve", lambda e, s_=s_, pB=pB: e.tensor_copy(fxT[:, 8:16, s_ * 128:(s_ + 1) * 128], pB.rearrange("p (a b) -> p a b", b=128)),
                    r=[self.b_ps[bB]], w=[b_fxT])
        self.dma("pool", wdb[:], self.w_down[l, ex_i].rearrange("(c p) n -> p c n", p=128), w=[b_wdb], dsem=d_wd)
        wgv = self.w_gate_up[l, ex_i].rearrange("(k p) n -> p k n", p=128)
        for fc in range(8):
            s = cnt % 3
            par = cnt % 2
            cnt += 1
            self.dma("pool", wgb[s][:, :, 0, :], wgv[:, :, fc * 128:(fc + 1) * 128], w=[b_wgb[s]], dsem=d_wg[s])
            self.dma("pool", wgb[s][:, :, 1, :], wgv[:, :, DFF + fc * 128:DFF + (fc + 1) * 128], w=[b_wgb[s]], dsem=d_wg[s])
            for (h0, hn) in [(c0, min(512, CAP - c0)) for c0 in range(0, CAP, 512)]:
                par = hcnt % 2
                hcnt += 1
                hs = slice(h0, h0 + hn)
                pg, pu = 2 * par, 2 * par + 1
                for gu, pb in ((0, pg), (1, pu)):
                    for k in range(16):
                        self.op("pe", lambda e, k=k, s=s, gu=gu, pb=pb, hs=hs, hn=hn: e.matmul(self.ps[pb][:, 0:hn], lhsT=wgb[s][:, k, gu, :],
                                                                                       rhs=fxT[:, k, hs], start=(k == 0), stop=(k == 15)),
                                r=[b_wgb[s], b_fxT], w=[self.b_ps[pb]])
                self.op("dve", lambda e, par=par, pg=pg, fc=fc, ex_i=ex_i, hn=hn: e.tensor_scalar(
                    gS[par][:, 0:hn], self.ps[pg][:, 0:hn], bguT[:, fc, ex_i:ex_i + 1], 7.0, ALU.add, ALU.min),
                    r=[self.b_ps[pg], b_c], w=[b_gS[par]])
                self.op("act", lambda e, par=par, hn=hn: e.activation(out=sg[par][:, 0:hn], in_=gS[par][:, 0:hn], func=AF.Sigmoid, scale=1.702),
                        r=[b_gS[par]], w=[b_sg[par]])
                self.op("dve", lambda e, par=par, pu=pu, fc=fc, ex_i=ex_i, hn=hn: e.tensor_scalar(
                    uS[par][:, 0:hn], self.ps[pu][:, 0:hn], bguT[:, 8 + fc, ex_i:ex_i + 1], 7.0, ALU.add, ALU.min),
                    r=[self.b_ps[pu], b_c], w=[b_uS[par]])
                self.op("pool", lambda e, par=par, hn=hn: e.tensor_scalar(uS[par][:, 0:hn], uS[par][:, 0:hn], -7.0, 1.0, ALU.max, ALU.add),
                        r=[b_uS[par]], w=[b_uS[par]])
                self.op("dve", lambda e, par=par, hn=hn: e.tensor_tensor(gS[par][:, 0:hn], gS[par][:, 0:hn], sg[par][:, 0:hn], ALU.mult),
                        r=[b_sg[par]], w=[b_gS[par]])
                self.op("dve", lambda e, par=par, fc=fc, hs=hs, hn=hn: e.tensor_tensor(actT[:, fc, hs], uS[par][:, 0:hn], gS[par][:, 0:hn], ALU.mult),
                        r=[b_uS[par], b_gS[par]], w=[b_actT])
        for s_ in range(nst):
            yi = dcnt % 2
            dcnt += 1
            for nb in range(4):
                bk = 4 + nb
                cols = slice(nb * 512, (nb + 1) * 512)
                for fc in range(8):
                    self.op("pe", lambda e, s_=s_, fc=fc, bk=bk, cols=cols: e.matmul(self.ps[bk][:, 0:512], lhsT=actT[:, fc, s_ * 128:(s_ + 1) * 128],
                                                                                   rhs=wdb[:, fc, cols], start=(fc == 0), stop=(fc == 7)),
                            r=[b_actT, b_wdb], w=[self.b_ps[bk]])
                if nb % 2 == 0:
                    self.op("act", lambda e, yi=yi, bk=bk, cols=cols: e.activation(out=yrow[yi][:, cols], in_=self.ps[bk][:, 0:512], func=AF.Copy),
                            r=[self.b_ps[bk]], w=[b_yrow[yi]])
                else:
                    self.op("dve", lambda e, yi=yi, bk=bk, cols=cols: e.tensor_copy(yrow[yi][:, cols], self.ps[bk][:, 0:512]),
                            r=[self.b_ps[bk]], w=[b_yrow[yi]])
            self.dma("sp", self.YE[r0 + s_ * 128:r0 + (s_ + 1) * 128, :], yrow[yi][:], r=[b_yrow[yi]], w=[self.b_ye], dsem=d_y[yi])
    self.barrier()
    self.sb_off = mark
    acc = [self.sb(f"acc{i}", [128, D], F32) for i in range(2)]
    gb = [[self.sb(f"gb{i}_{j}", [128, D], F32) for j in range(4)] for i in range(2)]
    xt2 = [self.sb(f"gxt2_{i}", [128, D], F32) for i in range(2)]
    b_acc, b_xt2 = [Buf(), Buf()], [Buf(), Buf()]
    b_gb = [[Buf() for j in range(4)] for i in range(2)]
    d_g = [self.ds(), self.ds()]
    d_x2 = [self.ds(), self.ds()]
    d_out = [self.ds(), self.ds()]
    for i in range(2):
        for j in range(4):
            self.op("dve" if j % 2 else "act", (lambda e, i=i, j=j: e.memset(gb[i][j][:], 0.0)) if j % 2 else
                    (lambda e, i=i, j=j: e.activation(out=gb[i][j][:], in_=G2[:, 0, :], func=AF.Copy, scale=0.0)),
                    r=[b_c], w=[b_gb[i][j]])
    dz = self.ds()
    self.dma("sp", self.YE[NR:NR + 128, :], gb[0][1][:], r=[b_gb[0][1]], w=[self.b_ye], dsem=dz)
    def issue_loads(t):
        i = t % 2
        self.dma("sp", xt2[i][:], self.XRES[t * 128:(t + 1) * 128, :], r=[self.b_xres[t]], w=[b_xt2[i]], dsem=d_x2[i])
        for j in range(4):
            self.P.op("pool", lambda e, t=t, j=j, i=i: e.indirect_dma_start(
                out=gb[i][j][:, :], out_offset=None, in_=self.YE[:, :],
                in_offset=bass.IndirectOffsetOnAxis(ap=IDX[:, t, j:j + 1], axis=0)),
                reads=[b_idx, self.b_ye], writes=[b_gb[i][j]], dsem=d_g[i])
    issue_loads(tiles_all[0])
    for n_, t in enumerate(tiles_all):
        i = t % 2
        which = 1 if t < 2 else 0
        rows = slice(t * 128, (t + 1) * 128)
        if n_ + 1 < len(tiles_all):
            issue_loads(tiles_all[n_ + 1])
        for nb in range(4):
            bk = nb
            cols = slice(nb * 512, (nb + 1) * 512)
            self.op("pe", lambda e, t=t, bk=bk, cols=cols: e.matmul(self.ps[bk][:, 0:512], lhsT=combT[0:NX, t * 128:(t + 1) * 128],
                                                                    rhs=bd[0:NX, cols], start=True, stop=True),
                    r=[b_combT, b_c], w=[self.b_ps[bk]])
            self.op("act", lambda e, i=i, bk=bk, cols=cols: e.activation(out=acc[i][:, cols], in_=self.ps[bk][:, 0:512], func=AF.Copy),
                    r=[self.b_ps[bk]], w=[b_acc[i]])
        for j in range(4):
            self.op("dve", lambda e, i=i, j=j, t=t: e.scalar_tensor_tensor(acc[i][:], gb[i][j][:], WJ[:, t, j:j + 1], acc[i][:], ALU.mult, ALU.add),
                    r=[b_gb[i][j], b_idx], w=[b_acc[i]])
        self.op("dve", lambda e, i=i, which=which: e.tensor_tensor(acc[i][:], acc[i][:], G2[:, which, :], ALU.mult), r=[b_c], w=[b_acc[i]])
        self.op("dve", lambda e, i=i: e.tensor_tensor(acc[i][:], acc[i][:], xt2[i][:], ALU.add), r=[b_xt2[i]], w=[b_acc[i]])
        if last:
            by = Buf()
            self.dma("sp", self.y_out[(t - 2) * 128:(t - 1) * 128, :], acc[i][:], r=[b_acc[i]], w=[by], dsem=d_out[i])
            self.final_reads.append(by)
        else:
            self.dma("sp", self.XRES[rows, :], acc[i][:], r=[b_acc[i]], w=[self.b_xres[t]], dsem=d_out[i])
    if not last:
        self.dbg(f"XOUT{l}", self.XRES, [TT, D], F32, r=self.b_xres)
    else:
        self.dbg(f"YOUT{l}", self.y_out, [2048, D], F32, r=self.final_reads)
    self.barrier()


Builder.phase_G2 = phase_G2
```
